# Optimizing a Trainium2 kernel written in Bass

```python
import math
import jax, jax.numpy as jnp
from jax import lax
import numpy as np

D_MODEL = 1024
BATCH = 8
SEQ = 4096
DEPTH = 1

DA_HEADS = 4
DA_HEAD_DIM = 64
DA_V_DIM = 2 * DA_HEAD_DIM
DA_WIDTH = DA_HEADS * DA_V_DIM
DA_QK_COLS = DA_HEADS * 2 * DA_HEAD_DIM
RW_HEADS = 8
RW_HEAD_DIM = 64
RW_WIDTH = RW_HEADS * RW_HEAD_DIM
DECAY_LORA = 64
AAA_LORA = 64
GATE_LORA = 128
MIX_WIDTH = DA_WIDTH + RW_WIDTH
DA_SIZES = (DA_QK_COLS, DA_QK_COLS, DA_WIDTH)
RW_SIZES = (RW_WIDTH, RW_WIDTH, RW_WIDTH, DECAY_LORA, AAA_LORA, GATE_LORA)
DA_COLS = DA_QK_COLS * 2 + DA_WIDTH
RW_COLS = RW_WIDTH * 3 + DECAY_LORA + AAA_LORA + GATE_LORA
IN_COLS = DA_COLS + RW_COLS
ROPE_THETA = 500000.0
ROPE_DIM = DA_HEAD_DIM // 4
Q_BLOCK = 128
N_EXPERTS = 32
TOP_K = 4
D_EXPERT = D_MODEL
SWIGLU_ALPHA = 1.702
SWIGLU_LIMIT = 7.0
NORM_EPS = 1e-6
SUBLN_EPS = 1e-5
LN_X_EPS = 64e-5
N_MOD = 6

kernel_name = 'hybrid_diffattn_rwkv7_moe'


def rms_norm(x, w, eps=NORM_EPS):
    xf = x.astype(jnp.float32)
    y = xf * lax.rsqrt(jnp.mean(xf * xf, axis=-1, keepdims=True) + eps)
    return (y * w.astype(jnp.float32)).astype(x.dtype)


def lambda_init_fn(layer):
    return 0.8 - 0.6 * math.exp(-0.3 * layer)


def split_cols(a, sizes):
    idx, s = [], 0
    for n in sizes[:-1]:
        s += n
        idx.append(s)
    return jnp.split(a, idx, axis=-1)


def partial_rope(x, cos, sin):
    half = ROPE_DIM // 2
    x1 = x[..., :half]
    x2 = x[..., half:ROPE_DIM]
    out = jnp.concatenate([x1 * cos - x2 * sin, x2 * cos + x1 * sin, x[..., ROPE_DIM:]], axis=-1)
    return out.astype(x.dtype)


def token_shift(p, mu):
    prev = jnp.pad(p, ((0, 0), (1, 0), (0, 0)))[:, :-1]
    return p + (prev - p) * mu


def diff_attention(q, k, v, cos, sin, lam_q1, lam_k1, lam_q2, lam_k2, subln_w, lambda_init):
    b, t = q.shape[0], q.shape[1]
    f32 = jnp.float32
    q = partial_rope(q.reshape(b, t, 2 * DA_HEADS, DA_HEAD_DIM), cos, sin) * (DA_HEAD_DIM ** -0.5)
    k = partial_rope(k.reshape(b, t, 2 * DA_HEADS, DA_HEAD_DIM), cos, sin)
    q = q.transpose(0, 2, 1, 3)
    k = k.transpose(0, 2, 1, 3)
    v = v.reshape(b, t, DA_HEADS, DA_V_DIM).transpose(0, 2, 1, 3)
    lam = (jnp.exp(jnp.sum(lam_q1.astype(f32) * lam_k1.astype(f32)))
           - jnp.exp(jnp.sum(lam_q2.astype(f32) * lam_k2.astype(f32))) + lambda_init)
    kpos = jnp.arange(t)

    def one_block(i):
        q_blk = lax.dynamic_slice_in_dim(q, i * Q_BLOCK, Q_BLOCK, axis=2)
        s = jnp.einsum('bhqd,bhkd->bhqk', q_blk, k).astype(f32)
        qpos = i * Q_BLOCK + jnp.arange(Q_BLOCK)
        s = jnp.where(qpos[:, None] >= kpos[None, :], s, -jnp.inf)
        p = jax.nn.softmax(s, axis=-1).reshape(b, DA_HEADS, 2, Q_BLOCK, t)
        attn = p[:, :, 0] - lam * p[:, :, 1]
        return jnp.einsum('bhqk,bhkd->bhqd', attn.astype(v.dtype), v)

    o = lax.map(one_block, jnp.arange(t // Q_BLOCK))
    o = o.transpose(1, 0, 3, 2, 4).reshape(b, t, DA_HEADS, DA_V_DIM)
    o = rms_norm(o, subln_w, SUBLN_EPS) * (1.0 - lambda_init)
    return o.reshape(b, t, DA_WIDTH)


def rwkv7_time_mix(r, k, v, w_lora, a_lora, g_lora, w0, w2, a0, a2, g2, k_k, k_a, r_k, ln_w, ln_b):
    b, t = r.shape[0], r.shape[1]
    f32 = jnp.float32
    log_w = -jax.nn.softplus(-(w0 + jnp.tanh(w_lora) @ w2).astype(f32)) - 0.5
    decay = jnp.exp(-jnp.exp(log_w))
    a = jax.nn.sigmoid((a0 + a_lora @ a2).astype(f32))
    g = jax.nn.sigmoid(g_lora) @ g2

    def heads(z):
        return z.astype(f32).reshape(b, t, RW_HEADS, RW_HEAD_DIM)

    kk = heads(k * k_k)
    kk = kk / jnp.maximum(jnp.linalg.norm(kk, axis=-1, keepdims=True), 1e-12)
    k_mod = k.astype(f32) * (1.0 + (a - 1.0) * k_a.astype(f32))
    r_h, k_h, v_h, w_h, a_h = heads(r), heads(k_mod), heads(v), heads(decay), heads(a)
    delta_a = -kk
    delta_b = kk * a_h

    def step(state, inp):
        r_t, w_t, k_t, v_t, da_t, db_t = inp
        sa = jnp.einsum('bhij,bhj->bhi', state, da_t)
        state = (state * w_t[:, :, None, :] + sa[..., None] * db_t[:, :, None, :]
                 + v_t[..., None] * k_t[:, :, None, :])
        return state, jnp.einsum('bhij,bhj->bhi', state, r_t)

    def tm(z):
        return z.transpose(1, 0, 2, 3)

    state0 = jnp.zeros((b, RW_HEADS, RW_HEAD_DIM, RW_HEAD_DIM), f32)
    _, y = lax.scan(step, state0, (tm(r_h), tm(w_h), tm(k_h), tm(v_h), tm(delta_a), tm(delta_b)))
    y = tm(y)
    mean = jnp.mean(y, axis=-1, keepdims=True)
    var = jnp.mean(jnp.square(y - mean), axis=-1, keepdims=True)
    y = ((y - mean) * lax.rsqrt(var + LN_X_EPS) * ln_w.astype(f32).reshape(RW_HEADS, RW_HEAD_DIM)
         + ln_b.astype(f32).reshape(RW_HEADS, RW_HEAD_DIM))
    y = y + jnp.sum(r_h * k_h * r_k.astype(f32), axis=-1, keepdims=True) * v_h
    return (y.reshape(b, t, RW_WIDTH) * g.astype(f32)).astype(r.dtype)


def clamped_swiglu(hid):
    x_glu = jnp.minimum(hid[..., ::2], SWIGLU_LIMIT)
    x_lin = jnp.clip(hid[..., 1::2], -SWIGLU_LIMIT, SWIGLU_LIMIT)
    return x_glu * jax.nn.sigmoid(SWIGLU_ALPHA * x_glu) * (x_lin + 1.0)


def moe_ffn(h, router_w, router_b, w1, b1, w2, b2):
    b, t, d = h.shape
    tok = h.reshape(b * t, d)
    logits = (tok @ router_w + router_b).astype(jnp.float32)
    top_vals, top_idx = lax.top_k(logits, TOP_K)
    top_w = jax.nn.softmax(top_vals, axis=-1)
    gates = jnp.sum(jax.nn.one_hot(top_idx, N_EXPERTS, dtype=jnp.float32) * top_w[..., None], axis=1)

    def expert(acc, xs):
        w1_e, b1_e, w2_e, b2_e, g_e = xs
        out = clamped_swiglu(tok @ w1_e + b1_e) @ w2_e + b2_e
        return acc + g_e[:, None] * out, None

    acc0 = jnp.zeros((b * t, d), jnp.float32)
    acc, _ = lax.scan(expert, acc0, (w1, b1, w2, b2, gates.T))
    return acc.reshape(b, t, d).astype(h.dtype)


def hybrid_layer(x, c, cos, sin, lambda_init, ada_w, ada_b, pre_mix_norm, post_mix_norm,
                 pre_ffn_norm, post_ffn_norm, w_in, w_out, da_lambda_q1, da_lambda_k1,
                 da_lambda_q2, da_lambda_k2, da_subln, rw_mu, rw_w0, rw_w2, rw_a0, rw_a2,
                 rw_g2, rw_k_k, rw_k_a, rw_r_k, rw_ln_w, rw_ln_b, router_w, router_b,
                 moe_w1, moe_b1, moe_w2, moe_b2):
    mod = jax.nn.silu(c) @ ada_w + ada_b
    sh1, sc1, gt1, sh2, sc2, gt2 = [m[:, None, :] for m in jnp.split(mod, N_MOD, axis=-1)]

    h = rms_norm(x, pre_mix_norm) * (1.0 + sc1) + sh1
    proj = h @ w_in
    da_part, rw_part = proj[..., :DA_COLS], proj[..., DA_COLS:]
    da_q, da_k, da_v = split_cols(da_part, DA_SIZES)
    rw_part = token_shift(rw_part, rw_mu)
    rw_r, rw_k, rw_v, rw_wl, rw_al, rw_gl = split_cols(rw_part, RW_SIZES)
    y_da = diff_attention(da_q, da_k, da_v, cos, sin, da_lambda_q1, da_lambda_k1,
                          da_lambda_q2, da_lambda_k2, da_subln, lambda_init)
    y_rw = rwkv7_time_mix(rw_r, rw_k, rw_v, rw_wl, rw_al, rw_gl, rw_w0, rw_w2, rw_a0, rw_a2,
                          rw_g2, rw_k_k, rw_k_a, rw_r_k, rw_ln_w, rw_ln_b)
    y = jnp.concatenate([y_da, y_rw], axis=-1) @ w_out
    x = x + gt1 * rms_norm(y, post_mix_norm)

    h = rms_norm(x, pre_ffn_norm) * (1.0 + sc2) + sh2
    y = moe_ffn(h, router_w, router_b, moe_w1, moe_b1, moe_w2, moe_b2)
    return x + gt2 * rms_norm(y, post_ffn_norm)


def setup_inputs(seed: int = 0) -> dict:
    key = jax.random.key(seed)
    ks = iter(jax.random.split(key, 40))
    f32 = jnp.float32
    L, D, E, F = DEPTH, D_MODEL, N_EXPERTS, D_EXPERT

    def nrm(shape, scale):
        return jax.random.normal(next(ks), shape, f32) * scale

    def gain(shape):
        return 1.0 + nrm(shape, 0.02)

    x = nrm((BATCH, SEQ, D), 1.0)
    c = nrm((BATCH, D), 1.0)
    offset = jax.random.randint(next(ks), (BATCH, 1), 0, 2048, jnp.int32)
    positions = offset + jnp.arange(SEQ, dtype=jnp.int32)[None, :]
    return {
        'x': x,
        'c': c,
        'positions': positions,
        'ada_w': nrm((L, D, N_MOD * D), 0.5 * D ** -0.5),
        'ada_b': nrm((L, N_MOD * D), 0.02),
        'pre_mix_norm': gain((L, D)),
        'post_mix_norm': gain((L, D)),
        'pre_ffn_norm': gain((L, D)),
        'post_ffn_norm': gain((L, D)),
        'w_in': nrm((L, D, IN_COLS), D ** -0.5),
        'w_out': nrm((L, MIX_WIDTH, D), MIX_WIDTH ** -0.5),
        'da_lambda_q1': nrm((L, DA_HEAD_DIM), 0.1),
        'da_lambda_k1': nrm((L, DA_HEAD_DIM), 0.1),
        'da_lambda_q2': nrm((L, DA_HEAD_DIM), 0.1),
        'da_lambda_k2': nrm((L, DA_HEAD_DIM), 0.1),
        'da_subln': gain((L, DA_V_DIM)),
        'rw_mu': jax.random.uniform(next(ks), (L, RW_COLS), f32),
        'rw_w0': -4.0 + 4.0 * jax.random.uniform(next(ks), (L, RW_WIDTH), f32),
        'rw_w2': nrm((L, DECAY_LORA, RW_WIDTH), 0.1 * DECAY_LORA ** -0.5),
        'rw_a0': nrm((L, RW_WIDTH), 0.1),
        'rw_a2': nrm((L, AAA_LORA, RW_WIDTH), 0.1 * AAA_LORA ** -0.5),
        'rw_g2': nrm((L, GATE_LORA, RW_WIDTH), GATE_LORA ** -0.5),
        'rw_k_k': 0.85 + nrm((L, RW_WIDTH), 0.02),
        'rw_k_a': gain((L, RW_WIDTH)),
        'rw_r_k': nrm((L, RW_HEADS, RW_HEAD_DIM), 0.1),
        'rw_ln_w': gain((L, RW_WIDTH)),
        'rw_ln_b': nrm((L, RW_WIDTH), 0.02),
        'router_w': nrm((L, D, E), D ** -0.5),
        'router_b': nrm((L, E), 0.01),
        'moe_w1': nrm((L, E, D, 2 * F), D ** -0.5),
        'moe_b1': nrm((L, E, 2 * F), 0.01),
        'moe_w2': nrm((L, E, F, D), F ** -0.5),
        'moe_b2': nrm((L, E, D), 0.01),
    }


def reference(x, c, positions, ada_w, ada_b, pre_mix_norm, post_mix_norm, pre_ffn_norm,
              post_ffn_norm, w_in, w_out, da_lambda_q1, da_lambda_k1, da_lambda_q2,
              da_lambda_k2, da_subln, rw_mu, rw_w0, rw_w2, rw_a0, rw_a2, rw_g2, rw_k_k,
              rw_k_a, rw_r_k, rw_ln_w, rw_ln_b, router_w, router_b, moe_w1, moe_b1,
              moe_w2, moe_b2):
    inv_freq = ROPE_THETA ** (-jnp.arange(0, ROPE_DIM, 2, dtype=jnp.float32) / ROPE_DIM)
    ang = positions.astype(jnp.float32)[..., None] * inv_freq
    cos = jnp.cos(ang)[:, :, None, :]
    sin = jnp.sin(ang)[:, :, None, :]
    for l in range(DEPTH):
        x = hybrid_layer(x, c, cos, sin, lambda_init_fn(l), ada_w[l], ada_b[l],
                         pre_mix_norm[l], post_mix_norm[l], pre_ffn_norm[l], post_ffn_norm[l],
                         w_in[l], w_out[l], da_lambda_q1[l], da_lambda_k1[l], da_lambda_q2[l],
                         da_lambda_k2[l], da_subln[l], rw_mu[l], rw_w0[l], rw_w2[l], rw_a0[l],
                         rw_a2[l], rw_g2[l], rw_k_k[l], rw_k_a[l], rw_r_k[l], rw_ln_w[l],
                         rw_ln_b[l], router_w[l], router_b[l], moe_w1[l], moe_b1[l],
                         moe_w2[l], moe_b2[l])
    return x
```

```python
import contextlib
import math
import numpy as np
import concourse.bass as bass
import concourse.mybir as mybir
from concourse.bass_utils import run_bass_kernel_spmd

ALU = mybir.AluOpType
AF = mybir.ActivationFunctionType
F32 = mybir.dt.float32
BF16 = mybir.dt.bfloat16
I32 = mybir.dt.int32
AX = mybir.AxisListType

D = 1024
T = 4096
NT = 32
NE = 32
C0 = math.exp(-0.5)
LAMBDA_INIT = 0.8 - 0.6 * math.exp(0.0)

O_ID = 0
O_M5 = 128
O_UI = O_M5 + 384
O_SU = O_M5
O_SL = O_M5 + 256
O_ONES = 768
O_IND = 896
O_FREQ = 898
O_SIGN = 899
O_PM = 900
O_CM = 1028
NCONST = O_CM + 2048
DBG_STOP = 99
DBG_X = 0
DBG_NT = NT


class Buf:
    __slots__ = ("name", "w", "r", "dsem", "dcount")

    def __init__(self, name):
        self.name = name
        self.w = {}
        self.r = {}
        self.dsem = None
        self.dcount = 0


class Eng:
    def __init__(self, name, sem):
        self.name = name
        self.sem = sem
        self.count = 0
        self.waited = {}
        self.thunks = []


class Prog:
    def __init__(self, nc, stack):
        self.nc = nc
        self.gstack = stack
        self.stack = stack
        self.engs = {}
        self.sems = {}
        self.vals = {}
        for n in ("tensor", "vector", "scalar", "gpsimd", "sync"):
            sem = stack.enter_context(nc.semaphore("es_" + n))
            self.engs[n] = Eng(n, sem)
            self.sems[("e", n)] = sem
            self.vals[("e", n)] = 0
        self.nbuf = 0
        self.ninstr = 0
        self.allbufs = []
        self.gen = 0

    def buf(self, name=None):
        self.nbuf += 1
        b = Buf(f"{name or 'b'}{self.nbuf}")
        self.allbufs.append(b)
        return b

    def new_engine_sems(self):
        self.gen += 1
        for n, eng in self.engs.items():
            old = ("e", n)
            self.vals.pop(old, None)
            sem = self.gstack.enter_context(self.nc.semaphore(f"es{self.gen}_{n}"))
            eng.sem = sem
            eng.count = 0
            eng.waited = {}
            self.sems[old] = sem
            self.vals[old] = 0
        for b in self.allbufs:
            b.w = {}
            b.r = {}

    def sb(self, name, shape, dtype):
        self.nbuf += 1
        t = self.stack.enter_context(self.nc.sbuf_tensor(f"sb{self.nbuf}_{name}", list(shape), dtype))
        return t, self.buf(name)

    def ps(self, name, shape, dtype=F32):
        self.nbuf += 1
        t = self.stack.enter_context(self.nc.psum_tensor(f"ps{self.nbuf}_{name}", list(shape), dtype))
        return t, self.buf(name)

    def _dsem(self, b):
        if b.dsem is None:
            s = self.gstack.enter_context(self.nc.semaphore("ds_" + b.name))
            b.dsem = ("d", b.name)
            self.sems[b.dsem] = s
            self.vals[b.dsem] = 0
        return b.dsem

    def _collect(self, eng, reads, writes):
        need = {}
        for b in reads:
            for k, v in b.w.items():
                if need.get(k, 0) < v:
                    need[k] = v
        for b in writes:
            for k, v in b.w.items():
                if need.get(k, 0) < v:
                    need[k] = v
            for k, v in b.r.items():
                if need.get(k, 0) < v:
                    need[k] = v
        for k, v in need.items():
            if eng.waited.get(k, 0) < v:
                eng.waited[k] = v
                sem = self.sems[k]
                eng.thunks.append(lambda e, sem=sem, v=v: e.wait_ge(sem, v))

    def op(self, engname, fn, reads=(), writes=()):
        eng = self.engs[engname]
        self._collect(eng, reads, writes)
        eng.count += 1
        c = eng.count
        sem = eng.sem
        eng.thunks.append(lambda e, fn=fn, sem=sem: fn(e).then_inc(sem, 1))
        key = ("e", engname)
        self.vals[key] = c
        for b in reads:
            b.r[key] = c
        for b in writes:
            b.w = {key: c}
            b.r = {}
        self.ninstr += 1

    def dma(self, q, out_ap, in_ap, reads, writes, sbuf_buf, **kw):
        eng = self.engs[q]
        self._collect(eng, reads, writes)
        key = self._dsem(sbuf_buf)
        sbuf_buf.dcount += 16
        c = sbuf_buf.dcount
        self.vals[key] = c
        sem = self.sems[key]
        eng.thunks.append(
            lambda e, o=out_ap, i=in_ap, sem=sem, kw=kw: e.dma_start(out=o, in_=i, **kw).then_inc(sem, 16))
        for b in reads:
            b.r[key] = c
        for b in writes:
            if b is sbuf_buf:
                b.w = {key: c}
                b.r = {}
            else:
                b.w[key] = c
        self.ninstr += 1

    def barrier(self):
        for eng in self.engs.values():
            for k, v in self.vals.items():
                if v > 0 and eng.waited.get(k, 0) < v:
                    eng.waited[k] = v
                    sem = self.sems[k]
                    eng.thunks.append(lambda e, sem=sem, v=v: e.wait_ge(sem, v))

    def flush(self):
        nc = self.nc
        engs = self.engs
        with nc.Block() as block:
            @block.tensor
            def _(e):
                for t in engs["tensor"].thunks:
                    t(e)

            @block.vector
            def _(e):
                for t in engs["vector"].thunks:
                    t(e)

            @block.scalar
            def _(e):
                for t in engs["scalar"].thunks:
                    t(e)

            @block.gpsimd
            def _(e):
                for t in engs["gpsimd"].thunks:
                    t(e)

            @block.sync
            def _(e):
                for t in engs["sync"].thunks:
                    t(e)
        for e in engs.values():
            e.thunks = []

    @contextlib.contextmanager
    def phase(self):
        with contextlib.ExitStack() as ph:
            self.stack = ph
            yield
            self.barrier()
            self.flush()
        self.stack = self.gstack
        self.new_engine_sems()

    def mm(self, out, lhsT, rhs, R, W, start=True, stop=True):
        self.op("tensor", lambda e: e.matmul(out, lhsT=lhsT, rhs=rhs, start=start, stop=stop), R, W)

    def tr(self, out, in_, ident, R, W):
        self.op("tensor", lambda e: e.transpose(out, in_, ident), R, W)

    def tt(self, eng, out, in0, in1, op, R, W):
        self.op(eng, lambda e: e.tensor_tensor(out=out, in0=in0, in1=in1, op=op), R, W)

    def ts(self, eng, out, in0, s1, s2, op0, op1, R, W):
        if s2 is None:
            self.op(eng, lambda e: e.tensor_scalar(out=out, in0=in0, scalar1=s1, scalar2=None, op0=op0), R, W)
        else:
            self.op(eng, lambda e: e.tensor_scalar(out=out, in0=in0, scalar1=s1, scalar2=s2, op0=op0, op1=op1), R, W)

    def stt(self, eng, out, in0, scalar, in1, op0, op1, R, W):
        eng = "vector"
        self.op(eng, lambda e: e.scalar_tensor_tensor(out=out, in0=in0, scalar=scalar, in1=in1, op0=op0, op1=op1), R, W)

    def act(self, out, in_, func, R, W, bias=None, scale=None, accum_out=None):
        kw = {}
        if bias is not None:
            kw["bias"] = bias
        if scale is not None:
            kw["scale"] = scale
        if accum_out is not None:
            kw["accum_out"] = accum_out
        self.op("scalar", lambda e: e.activation(out=out, in_=in_, func=func, **kw), R, W)

    def cp(self, eng, out, in_, R, W):
        if eng == "scalar":
            self.op("scalar", lambda e: e.copy(out=out, in_=in_), R, W)
        else:
            self.op(eng, lambda e: e.tensor_copy(out=out, in_=in_), R, W)

    def ms(self, eng, ap, val, W):
        self.op(eng, lambda e: e.memset(ap, val), [], W)


def _rr(P):
    state = {"i": 0}

    def nxt():
        state["i"] += 1
        return "vector" if state["i"] % 3 else "gpsimd"
    return nxt


def build_program(debug=False, upto=3):
    nc = bass.Bass("TRN2", target_bir_lowering=False)
    skind = "ExternalOutput" if debug else "Internal"

    def din(name, shape, dt=F32):
        return nc.dram_tensor(name, list(shape), dt, kind="ExternalInput").ap()

    x_d = din("x", [T, D])
    cT_d = din("cT", [128, 8])
    pos_d = din("pos", [1, T], I32)
    adaw_d = din("ada_w", [D, 6 * D])
    adab_d = din("ada_b", [1, 6 * D])
    norms_d = din("norms", [4, D])
    win_d = din("w_in", [D, 3328])
    wout_d = din("w_out", [D, D])
    lamv_d = din("lamv", [4, 64])
    subln_d = din("subln", [1, 128])
    mu_d = din("mu", [1, 1792])
    rwv_d = din("rwv", [7, 512])
    w2_d = din("rw_w2", [64, 512])
    a2_d = din("rw_a2", [64, 512])
    g2_d = din("rw_g2", [128, 512])
    rtw_d = din("router_w", [D, NE])
    rtb_d = din("router_b", [1, NE])
    w1g_d = din("w1g", [NE, D, D]) if upto >= 3 else None
    w1l_d = din("w1l", [NE, D, D]) if upto >= 3 else None
    b1T_d = din("b1T", [128, NE * 16])
    w2e_d = din("w2e", [NE, D, D]) if upto >= 3 else None
    b2_d = din("b2", [NE, D])
    con_d = din("consts", [128, NCONST])
    out_d = nc.dram_tensor("out", [T, D], F32, kind="ExternalOutput").ap()

    modp_d = nc.dram_tensor("modp", [6, D], F32, kind=skind).ap()
    qk_d = nc.dram_tensor("qk_s", [D, T], BF16, kind=skind).ap()
    v_d = nc.dram_tensor("v_s", [T, 4 * 129], BF16, kind=skind).ap()
    yrw_d = nc.dram_tensor("yrw_s", [T, 512], BF16, kind=skind).ap()
    x1_d = nc.dram_tensor("x1_s", [T, D], F32, kind=skind).ap()
    h2T_d = nc.dram_tensor("h2T_s", [D, T], BF16, kind=skind).ap()
    gat_d = nc.dram_tensor("gat_s", [T, NE], F32, kind=skind).ap()

    with contextlib.ExitStack() as gst:
        P = Prog(nc, gst)
        b_modp = P.buf("modp")
        b_qk = P.buf("qkd")
        b_v = P.buf("vd")
        b_yrw = P.buf("yrwd")
        b_x1 = P.buf("x1d")
        b_h2T = P.buf("h2Td")
        b_gat = P.buf("gatd")
        b_out = P.buf("outd")

        with P.phase():
            cT, bcT = P.sb("cT", [128, 8], F32)
            sc, bsc = P.sb("sc", [128, 8], F32)
            P.dma("sync", cT[:], cT_d, [], [bcT], bcT)
            P.act(sc[:], cT[:], AF.Silu, [bcT], [bsc])
            aw = [P.sb(f"aw{i}", [128, 3072], F32) for i in range(2)]
            pm, bpm = P.ps("pmod", [128, 3072], F32)
            mrow, bmrow = P.sb("mrow", [1, 6 * D], F32)
            brow, bbrow = P.sb("brow", [1, 6 * D], F32)
            nrm, bnrm = P.sb("nrm", [1, 4 * D], F32)
            orow, borow = P.sb("orow", [1, 6 * D], F32)
            P.dma("sync", brow[:], adab_d, [], [bbrow], bbrow)
            P.dma("sync", nrm[:], norms_d.rearrange("(o a) d -> o (a d)", o=1), [], [bnrm], bnrm)
            i = 0
            for half in range(2):
                for k in range(8):
                    t_, b_ = aw[i % 2]
                    i += 1
                    P.dma("sync", t_[:], adaw_d[k * 128:(k + 1) * 128, half * 3072:(half + 1) * 3072], [], [b_], b_)
                    for j in range(6):
                        P.mm(pm[0:1, j * 512:(j + 1) * 512], sc[:, k:k + 1], t_[:, j * 512:(j + 1) * 512],
                             [bsc, b_], [bpm], start=(k == 0), stop=(k == 7))
                P.tt("vector", mrow[:, half * 3072:(half + 1) * 3072], pm[0:1, :], brow[:, half * 3072:(half + 1) * 3072],
                     ALU.add, [bpm, bbrow], [bmrow])

            def mseg(i_):
                return mrow[:, i_ * D:(i_ + 1) * D]

            def nseg(i_):
                return nrm[:, i_ * D:(i_ + 1) * D]
            P.stt("vector", orow[:, 0:D], mseg(1), 1.0, nseg(0), ALU.add, ALU.mult, [bmrow, bnrm], [borow])
            P.cp("vector", orow[:, D:2 * D], mseg(0), [bmrow], [borow])
            P.tt("vector", orow[:, 2 * D:3 * D], mseg(2), nseg(1), ALU.mult, [bmrow, bnrm], [borow])
            P.stt("vector", orow[:, 3 * D:4 * D], mseg(4), 1.0, nseg(2), ALU.add, ALU.mult, [bmrow, bnrm], [borow])
            P.cp("vector", orow[:, 4 * D:5 * D], mseg(3), [bmrow], [borow])
            P.tt("vector", orow[:, 5 * D:6 * D], mseg(5), nseg(3), ALU.mult, [bmrow, bnrm], [borow])
            P.dma("sync", modp_d.rearrange("(o a) d -> o (a d)", o=1), orow[:], [borow], [b_modp], borow)

        if upto >= 1:
          with P.phase():
            phase_front(P, nc, locals())

        if upto >= 2:
          with P.phase():
            phase_attn(P, nc, locals())

        if upto >= 3:
          with P.phase():
            phase_moe(P, nc, locals())
    return nc


def phase_front(P, nc, G):
    x_d, pos_d, win_d, mu_d, rwv_d = G["x_d"], G["pos_d"], G["win_d"], G["mu_d"], G["rwv_d"]
    w2_d, a2_d, g2_d, con_d, modp_d = G["w2_d"], G["a2_d"], G["g2_d"], G["con_d"], G["modp_d"]
    qk_d, v_d, yrw_d = G["qk_d"], G["v_d"], G["yrw_d"]
    b_modp, b_qk, b_v, b_yrw = G["b_modp"], G["b_qk"], G["b_v"], G["b_yrw"]
    rr = _rr(P)

    con, bcon = P.sb("con", [128, NCONST], F32)
    P.dma("sync", con[:], con_d, [], [bcon], bcon)
    identf = con[:, O_ID:O_ID + 128]
    idb, bidb = P.sb("idb", [128, 128], BF16)
    P.cp("vector", idb[:], identf, [bcon], [bidb])

    mcol, bmcol = P.sb("mcol", [128, 6, 8], F32)
    P.dma("sync", mcol[:], modp_d.rearrange("a (k p) -> p a k", p=128), [b_modp], [bmcol], bmcol,
          allow_slow_non_contiguous=True)

    wda, bwda = P.sb("wda", [128, 8, 1536], BF16)
    P.dma("gpsimd", wda[:], win_d[:, 0:1536].rearrange("(k p) c -> p k c", p=128), [], [bwda], bwda)
    w1, bw1 = P.sb("w1", [128, 8, 1792], BF16)
    w2m, bw2m = P.sb("w2m", [128, 8, 1792], BF16)
    prm, bprm = P.sb("prm", [128, 7, 512], F32)
    P.dma("sync", prm[:].rearrange("p a d -> p (a d)"),
          rwv_d.rearrange("(o a) d -> o (a d)", o=1).partition_broadcast(128), [], [bprm], bprm)
    lw2, blw2 = P.sb("lw2", [128, 512], F32)
    lg2, blg2 = P.sb("lg2", [128, 512], F32)
    P.dma("sync", lw2[0:64, :], w2_d, [], [blw2], blw2)
    P.dma("sync", lw2[64:128, :], a2_d, [], [blw2], blw2)
    P.dma("sync", lg2[:], g2_d, [], [blg2], blg2)

    ctab, bctab = P.sb("ctab", [128, T], BF16)
    stab, bstab = P.sb("stab", [128, T], BF16)
    with contextlib.ExitStack() as tmp:
        old = P.stack
        P.stack = tmp
        mub, bmub = P.sb("mub", [128, 1792], F32)
        omu, bomu = P.sb("omu", [128, 1792], F32)
        P.dma("sync", mub[:], mu_d.partition_broadcast(128), [], [bmub], bmub)
        P.ts("vector", omu[:], mub[:], -1.0, 1.0, ALU.mult, ALU.add, [bmub], [bomu])
        wst = [P.sb(f"wst{i}", [128, 1792], F32) for i in range(2)]
        for k in range(8):
            t_, b_ = wst[k % 2]
            P.dma("sync", t_[:], win_d[k * 128:(k + 1) * 128, 1536:3328], [], [b_], b_)
            P.tt("vector", w1[:, k, :], t_[:], omu[:], ALU.mult, [b_, bomu], [bw1])
            P.tt("gpsimd", w2m[:, k, :], t_[:], mub[:], ALU.mult, [b_, bmub], [bw2m])
        posi, bposi = P.sb("posi", [128, 1024], I32)
        ang, bang = P.sb("ang", [128, 1024], F32)
        y_, by_ = P.sb("ry", [128, 1024], F32)
        kf, bkf = P.sb("rkf", [128, 1024], F32)
        ki, bki = P.sb("rki", [128, 1024], I32)
        for q4 in range(4):
            sl = slice(q4 * 1024, (q4 + 1) * 1024)
            P.dma("sync", posi[:], pos_d[:, sl].partition_broadcast(128), [], [bposi], bposi)
            P.cp("vector", ang[:], posi[:], [bposi], [bang])
            P.ts("vector", ang[:], ang[:], con[:, O_FREQ:O_FREQ + 1], None, ALU.mult, None, [bang, bcon], [bang])
            for which, shift in ((0, math.pi * 1.5), (1, math.pi)):
                P.ts("vector", y_[:], ang[:], shift, None, ALU.add, None, [bang], [by_])
                P.ts("vector", kf[:], y_[:], 1.0 / (2 * math.pi), None, ALU.mult, None, [by_], [bkf])
                P.cp("vector", ki[:], kf[:], [bkf], [bki])
                P.cp("vector", kf[:], ki[:], [bki], [bkf])
                P.stt("vector", y_[:], kf[:], -2 * math.pi, y_[:], ALU.mult, ALU.add, [bkf, by_], [by_])
                P.ts("vector", kf[:], y_[:], 0.0, 2 * math.pi, ALU.is_lt, ALU.mult, [by_], [bkf])
                P.tt("vector", y_[:], y_[:], kf[:], ALU.add, [by_, bkf], [by_])
                P.ts("vector", y_[:], y_[:], -math.pi, None, ALU.add, None, [by_], [by_])
                P.ts("vector", y_[:], y_[:], -math.pi, math.pi, ALU.max, ALU.min, [by_], [by_])
                if which == 0:
                    P.act(ctab[:, sl], y_[:], AF.Sin, [by_], [bctab])
                else:
                    P.act(kf[:], y_[:], AF.Sin, [by_], [bkf])
                    P.ts("vector", stab[:, sl], kf[:], con[:, O_SIGN:O_SIGN + 1], None, ALU.mult, None, [bkf, bcon], [bstab])
        P.barrier()
        P.flush()
        P.stack = old

    def f32t(name, w=512):
        return P.sb(name, [128, w], F32)

    xt, bxt = P.sb("xt", [128, D], F32)
    xn, bxn = P.sb("xn", [128, D], BF16)
    junk, bjunk = xn, bxn
    st8, bst8 = P.sb("st8", [128, 8], F32)
    hT = [P.sb(f"hT{i}", [128, 8, 129], BF16) for i in range(2)]
    P.ms("vector", hT[1][0][:, :, 128:129], 0.0, [hT[1][1]])
    qf, bqf = P.sb("qf", [128, 128], F32)
    t1, bt1 = P.sb("t1", [128, 128], F32)
    t2, bt2 = P.sb("t2", [128, 128], F32)
    qko, bqko = P.sb("qko", [128, 8, 128], BF16)
    vo, bvo = P.sb("vo", [128, 4, 129], BF16)
    P.ms("vector", vo[:, :, 128:129], 1.0, [bvo])
    r_s, br = f32t("r_s")
    k_s, bk = f32t("k_s")
    v_s, bv = f32t("v_s")
    lo0, blo0 = P.sb("lo0", [128, 128], F32)
    lo1, blo1 = P.sb("lo1", [128, 128], F32)
    sig, bsig = f32t("sig")
    a_s, ba = f32t("a_s")
    g_s, bg = f32t("g_s")
    kk, bkk = f32t("kk")
    km, bkm = f32t("km")
    bb, bbb = f32t("bb")
    e1, be1 = f32t("e1")
    e2, be2 = f32t("e2")
    tm1, btm1 = f32t("tm1")
    At, bAt = f32t("At")
    Rt, bRt = f32t("Rt")
    Bt, bBt = f32t("Bt")
    Kt, bKt = f32t("Kt")
    Bh, bBh = f32t("Bh")
    Kh, bKh = f32t("Kh")
    FM, bFM_ = P.sb("FM", [128, 4, 4, 128], F32)
    bFM = [P.buf("FMp") for _ in range(4)]
    RP, bRP_ = P.sb("RP", [128, 4, 384], F32)
    bRP = [P.buf("RPp") for _ in range(4)]
    P.ms("vector", RP[:], 0.0, bRP)
    pc, bpc = P.sb("pc", [128, 4, 2], F32)
    ST, bST_ = P.sb("ST", [128, 4, 64], F32)
    bST = [P.buf("STh") for _ in range(8)]
    P.ms("vector", ST[:], 0.0, bST)
    Gs = [P.sb(f"Gs{i}", [128, 640], F32) for i in range(2)]
    Nb = [[P.sb(f"N{i}_{j}", [128, 128], F32) for j in range(2)] for i in range(2)]
    Lb = [[P.sb(f"L{i}_{j}", [128, 128], F32) for j in range(2)] for i in range(2)]
    Tb = [[P.sb(f"T{i}_{j}", [128, 128], F32) for j in range(2)] for i in range(2)]
    Zs = [P.sb(f"Zs{i}", [128, 64], F32) for i in range(2)]
    Us = [P.sb(f"Us{i}", [128, 64], F32) for i in range(2)]
    ysb, bysb = xt[:, 0:512], bxt
    yo, byo = P.sb("yo", [128, 512], BF16)

    pT, bpT = P.ps("pT", [128, 8, 128], BF16)
    pQ, bpQ = P.ps("pQ", [128, 512], F32)
    pV, bpV = P.ps("pV", [128, 512], F32)
    pF, bpF = P.ps("pF", [128, 512], F32)
    pF1, bpF1 = P.ps("pF1", [128, 512], F32)
    pG0, bpG0 = P.ps("pG0", [128, 512], F32)
    pG1, bpG1 = P.ps("pG1", [128, 512], F32)
    pY, bpY = P.ps("pY", [128, 512], F32)

    A1c = mcol[:, 0, :]
    B1c = mcol[:, 1, :]

    if DBG_STOP < 1:
        return
    for n in range(DBG_NT):
        tsl = slice(n * 128, (n + 1) * 128)
        hcur, bhcur = hT[n % 2]
        hprev, bhprev = hT[(n + 1) % 2]
        P.dma("sync", xt[:], x_d[tsl, :], [], [bxt], bxt)
        P.act(junk[:], xt[:], AF.Square, [bxt], [bjunk, bst8], accum_out=st8[:, 0:1])
        P.ts("vector", st8[:, 1:2], st8[:, 0:1], 1.0 / D, 1e-6, ALU.mult, ALU.add, [bst8], [bst8])
        P.act(st8[:, 2:3], st8[:, 1:2], AF.Ln, [bst8], [bst8])
        P.act(st8[:, 3:4], st8[:, 2:3], AF.Exp, [bst8], [bst8], scale=-0.5)
        P.ts("vector", xn[:], xt[:], st8[:, 3:4], None, ALU.mult, None, [bxt, bst8], [bxn])
        for k in range(8):
            P.tr(pT[:, k, :], xn[:, k * 128:(k + 1) * 128], idb[:], [bxn, bidb], [bpT])
        P.cp("vector", hcur[:, :, 0:1], hprev[:, :, 128:129], [bhprev], [bhcur])
        for k in range(8):
            P.act(hcur[:, k, 1:129], pT[:, k, :], AF.Identity, [bpT, bmcol], [bhcur],
                  bias=B1c[:, k:k + 1], scale=A1c[:, k:k + 1])
        hx = lambda k: hcur[:, k, 1:129]
        hs = lambda k: hcur[:, k, 0:128]
        for cq in range(8):
            for k in range(8):
                P.mm(pQ[:, 0:128], wda[:, k, cq * 128:(cq + 1) * 128], hx(k), [bwda, bhcur], [bpQ],
                     start=(k == 0), stop=(k == 7))
            P.cp("scalar", qf[:], pQ[:, 0:128], [bpQ], [bqf])
            P.mm(pQ[:, 128:256], con[:, O_PM:O_PM + 128], qf[:], [bcon, bqf], [bpQ])
            P.cp("scalar", t2[:], pQ[:, 128:256], [bpQ], [bt2])
            P.tt("vector", t1[:], qf[:], ctab[:, tsl], ALU.mult, [bqf, bctab], [bt1])
            P.tt("gpsimd", qf[:], t2[:], stab[:, tsl], ALU.mult, [bt2, bstab], [bqf])
            P.tt("vector", qko[:, cq, :], t1[:], qf[:], ALU.add, [bt1, bqf], [bqko])
        P.dma("sync", qk_d.rearrange("(c p) t -> p c t", p=128)[:, :, tsl], qko[:], [bqko], [b_qk], bqko)
        for k in range(8):
            P.mm(pV[:], hx(k), wda[:, k, 1024:1536], [bhcur, bwda], [bpV], start=(k == 0), stop=(k == 7))
        P.cp("scalar", vo[:, :, 0:128], pV[:].rearrange("p (h d) -> p h d", h=4), [bpV], [bvo])
        P.dma("sync", v_d[tsl, :], vo[:].rearrange("p h d -> p (h d)"), [bvo], [b_v], bvo)
        if DBG_STOP < 2:
            continue
        for cc, (dst, bdst) in enumerate(((r_s, br), (k_s, bk), (v_s, bv))):
            pp, bpp = pV, bpV
            for k in range(8):
                P.mm(pp[:], hx(k), w1[:, k, cc * 512:(cc + 1) * 512], [bhcur, bw1], [bpp], start=(k == 0), stop=False)
            for k in range(8):
                P.mm(pp[:], hs(k), w2m[:, k, cc * 512:(cc + 1) * 512], [bhcur, bw2m], [bpp], start=False, stop=(k == 7))
            P.cp("scalar", dst[:], pp[:], [bpp], [bdst])
        for lc in range(2):
            cs_ = slice(1536 + lc * 128, 1536 + (lc + 1) * 128)
            osl = pQ[:, 256:384]
            for k in range(8):
                P.mm(osl, w1[:, k, cs_], hx(k), [bw1, bhcur], [bpQ], start=(k == 0), stop=False)
            for k in range(8):
                P.mm(osl, w2m[:, k, cs_], hs(k), [bw2m, bhcur], [bpQ], start=False, stop=(k == 7))
            if lc == 0:
                P.act(lo0[0:64, :], pQ[0:64, 256:384], AF.Tanh, [bpQ], [blo0])
                P.cp("scalar", lo0[64:128, :], pQ[64:128, 256:384], [bpQ], [blo0])
            else:
                P.act(lo1[:], pQ[:, 256:384], AF.Sigmoid, [bpQ], [blo1])
        P.mm(pV[:], lo0[0:64, :], lw2[0:64, :], [blo0, blw2], [bpV])
        P.tt("vector", sig[:], pV[:], prm[:, 0, :], ALU.add, [bpV, bprm], [bsig])
        P.act(sig[:], sig[:], AF.Sigmoid, [bsig], [bsig])
        P.mm(pV[:], lo0[64:128, :], lw2[64:128, :], [blo0, blw2], [bpV])
        P.tt("vector", a_s[:], pV[:], prm[:, 1, :], ALU.add, [bpV, bprm], [ba])
        P.act(a_s[:], a_s[:], AF.Sigmoid, [ba], [ba])
        P.mm(pV[:], lo1[:], lg2[:], [blo1, blg2], [bpV])
        P.cp("scalar", g_s[:], pV[:], [bpV], [bg])
        P.tt(rr(), kk[:], k_s[:], prm[:, 2, :], ALU.mult, [bk, bprm], [bkk])
        P.tt(rr(), tm1[:], kk[:], kk[:], ALU.mult, [bkk], [btm1])
        P.op("vector", lambda e: e.tensor_reduce(out=st8[:, 0:8], in_=tm1[:].rearrange("p (h j) -> p h j", h=8),
                                                 axis=AX.X, op=ALU.add), [btm1], [bst8])
        P.ts("vector", st8[:, 0:8], st8[:, 0:8], 1e-24, None, ALU.max, None, [bst8], [bst8])
        P.act(st8[:, 0:8], st8[:, 0:8], AF.Ln, [bst8], [bst8])
        P.act(st8[:, 0:8], st8[:, 0:8], AF.Exp, [bst8], [bst8], scale=-0.5)
        P.tt("vector", kk[:].rearrange("p (h j) -> p h j", h=8), kk[:].rearrange("p (h j) -> p h j", h=8),
             st8[:, 0:8].unsqueeze(2).to_broadcast([128, 8, 64]), ALU.mult, [bkk, bst8], [bkk])
        P.stt(rr(), tm1[:], a_s[:], -1.0, prm[:, 3, :], ALU.add, ALU.mult, [ba, bprm], [btm1])
        P.stt(rr(), km[:], tm1[:], 1.0, k_s[:], ALU.add, ALU.mult, [btm1, bk], [bkm])
        P.tt(rr(), bb[:], kk[:], a_s[:], ALU.mult, [bkk, ba], [bbb])
        P.mm(pV[:], con[:, O_UI:O_UI + 128], sig[:], [bcon, bsig], [bpV])
        P.act(e1[:], pV[:], AF.Exp, [bpV], [be1], scale=-C0)
        P.act(e2[:], pV[:], AF.Exp, [bpV], [be2], scale=C0)
        P.tt(rr(), Rt[:], r_s[:], e1[:], ALU.mult, [br, be1], [bRt])
        P.tt(rr(), Bt[:], bb[:], e2[:], ALU.mult, [bbb, be2], [bBt])
        P.tt(rr(), Kt[:], km[:], e2[:], ALU.mult, [bkm, be2], [bKt])
        P.mm(pV[:], con[:, O_SU:O_SU + 128], sig[:], [bcon, bsig], [bpV])
        P.act(e1[:], pV[:], AF.Exp, [bpV], [be1], scale=-C0)
        P.stt(rr(), At[:], kk[:], -1.0, e1[:], ALU.mult, ALU.mult, [bkk, be1], [bAt])
        P.mm(pV[:], con[:, O_SL:O_SL + 128], sig[:], [bcon, bsig], [bpV])
        P.act(e2[:], pV[:], AF.Exp, [bpV], [be2], scale=-C0)
        P.tt(rr(), Bh[:], bb[:], e2[:], ALU.mult, [bbb, be2], [bBh])
        P.tt(rr(), Kh[:], km[:], e2[:], ALU.mult, [bkm, be2], [bKh])
        for pr in range(4):
            P.mm(pQ[:, 384 + pr * 2:384 + pr * 2 + 2], sig[:, pr * 128:(pr + 1) * 128], con[:, O_IND:O_IND + 2],
                 [bsig, bcon], [bpQ])
        P.act(pc[:].rearrange("p a c -> p (a c)"), pQ[:, 384:392], AF.Exp, [bpQ], [bpc], scale=-C0)
        P.tt(rr(), tm1[:], r_s[:], km[:], ALU.mult, [br, bkm], [btm1])
        P.tt(rr(), tm1[:], tm1[:], prm[:, 4, :], ALU.mult, [btm1, bprm], [btm1])
        P.op("vector", lambda e: e.tensor_reduce(out=st8[:, 0:8], in_=tm1[:].rearrange("p (h j) -> p h j", h=8),
                                                 axis=AX.X, op=ALU.add), [btm1], [bst8])
        if DBG_STOP < 3:
            continue
        for pr in range(4):
            psl = slice(pr * 128, (pr + 1) * 128)
            for ai, (arr, barr) in enumerate(((At, bAt), (Rt, bRt), (Bt, bBt), (Kt, bKt))):
                P.tr(pV[:, ai * 128:(ai + 1) * 128], arr[:, psl], identf, [barr, bcon], [bpV])
            if DBG_X != 1:
                P.cp("scalar", FM[:, pr, :, :], pV[:].rearrange("p (a t) -> p a t", a=4), [bpV], [bFM[pr]])
            P.cp("vector", RP[:, pr, 0:64], FM[:, pr, 1, 0:64], [bFM[pr]], [bRP[pr]])
            P.cp("vector", RP[:, pr, 192:256], FM[:, pr, 1, 64:128], [bFM[pr]], [bRP[pr]])
        if DBG_STOP < 4:
            continue
        for h in range(8):
            pr = h // 2
            ph = (h % 2) * 64
            hp = h % 2
            Gt, bGt = Gs[hp]
            A_ = FM[ph:ph + 64, pr, 0, :]
            R_ = FM[ph:ph + 64, pr, 1, :]
            B_ = FM[ph:ph + 64, pr, 2, :]
            K_ = FM[ph:ph + 64, pr, 3, :]
            bF = bFM[pr]
            P.mm(pG0[:, 0:128], B_, A_, [bF], [bpG0])
            P.mm(pG0[:, 128:256], K_, A_, [bF], [bpG0])
            P.mm(pG0[:, 256:384], B_, R_, [bF], [bpG0])
            P.mm(pG0[:, 384:512], K_, R_, [bF], [bpG0])
            P.mm(pG1[:, 0:128], A_, B_, [bF], [bpG1])
            P.tt("vector", Gt[:, 0:256], pG0[:, 0:256], con[:, O_M5:O_M5 + 256], ALU.mult, [bpG0, bcon], [bGt])
            P.tt("vector", Gt[:, 384:640], pG0[:, 256:512], con[:, O_M5 + 384:O_M5 + 640], ALU.mult, [bpG0, bcon], [bGt])
            P.tt("vector", Gt[:, 256:384], pG1[:, 0:128], con[:, O_M5 + 256:O_M5 + 384], ALU.mult, [bpG1, bcon], [bGt])
            if DBG_X == 11:
                continue
            Ncur, bNcur = Gt[:, 0:128], bGt
            Lcur, bLcur = Gt[:, 256:384], bGt
            Tcur, bTcur = Tb[hp][0]
            P.tt(rr(), Tcur[:], Gt[:, 0:128], identf, ALU.add, [bGt, bcon], [bTcur])
            Tcur = Tcur[:]
            for kx in range(1, 6):
                if DBG_X in (13, 14) and kx > 1:
                    break
                if DBG_X == 15 and kx > 2:
                    break
                Ln_, bLn = Lb[hp][kx % 2]
                i0 = 128 + (kx % 3) * 128
                P.mm(pG1[:, i0:i0 + 128], Ncur, Lcur, [bNcur, bLcur], [bpG1])
                P.cp("vector", Ln_[:], pG1[:, i0:i0 + 128], [bpG1], [bLn])
                if DBG_X == 13:
                    break
                if kx <= 4:
                    Nn_, bNn = Nb[hp][kx % 2]
                    i1 = 128 + ((kx + 1) % 3) * 128
                    P.mm(pG1[:, i1:i1 + 128], Lcur, Ncur, [bNcur, bLcur], [bpG1])
                    P.cp("vector", Nn_[:], pG1[:, i1:i1 + 128], [bpG1], [bNn])
                Tn_, bTn = Tb[hp][kx % 2]
                i2 = 128 + ((kx + 2) % 3) * 128
                P.mm(pG1[:, i2:i2 + 128], Ln_[:], Tcur, [bLn, bTcur], [bpG1])
                P.tt("vector", Tn_[:], pG1[:, i2:i2 + 128], Tcur, ALU.add, [bpG1, bTcur], [bTn])
                Lcur, bLcur = Ln_[:], bLn
                if kx <= 4:
                    Ncur, bNcur = Nn_[:], bNn
                Tcur, bTcur = Tn_[:], bTn
            if DBG_X == 12:
                continue
            S0 = ST[ph:ph + 64, pr, :]
            bS = bST[h]
            Zt, bZt = Zs[hp]
            Ut, bUt = Us[hp]
            hcol = slice(h * 64, (h + 1) * 64)
            for c in range(2):
                pv = c * 64
                pSb, sb0 = (pF, 0) if hp == 0 else (pF1, 0)
                zsl = pSb[:, sb0:sb0 + 64]
                usl = pSb[:, sb0 + 64:sb0 + 128]
                ssl = pSb[:, sb0 + 128:sb0 + 192]
                bz = bu = bs_ = (bpF if hp == 0 else bpF1)
                P.mm(zsl, A_, S0, [bF, bS], [bz], start=True, stop=False)
                P.mm(zsl, Gt[pv:pv + 64, 128:256], v_s[pv:pv + 64, hcol], [bGt, bv], [bz], start=False, stop=True)
                P.cp("vector", Zt[pv:pv + 64, :], zsl[pv:pv + 64, :], [bz], [bZt])
                P.mm(usl, Tcur[pv:pv + 64, :], Zt[pv:pv + 64, :], [bTcur, bZt], [bu])
                P.cp("vector", Ut[pv:pv + 64, :], usl[pv:pv + 64, :], [bu], [bUt])
                P.mm(pY[:, hcol], RP[ph:ph + 64, pr, c * 128:(c + 1) * 128], S0, [bRP[pr], bS], [bpY],
                     start=(c == 0), stop=False)
                P.mm(pY[:, hcol], Gt[pv:pv + 64, 512:640], v_s[pv:pv + 64, hcol], [bGt, bv], [bpY], start=False, stop=False)
                P.mm(pY[:, hcol], Gt[pv:pv + 64, 384:512], Ut[pv:pv + 64, :], [bGt, bUt], [bpY], start=False, stop=(c == 1))
                P.mm(ssl, Bh[pv:pv + 64, pr * 128:(pr + 1) * 128], Ut[pv:pv + 64, :], [bBh, bUt], [bs_], start=True, stop=False)
                P.mm(ssl, Kh[pv:pv + 64, pr * 128:(pr + 1) * 128], v_s[pv:pv + 64, hcol], [bKh, bv], [bs_], start=False, stop=True)
                P.stt("vector", S0, S0, pc[ph:ph + 64, pr, c:c + 1], ssl[ph:ph + 64, :], ALU.mult, ALU.add,
                      [bS, bpc, bs_], [bS])
        if DBG_STOP < 5:
            continue
        v3 = lambda ap: ap.rearrange("p (h j) -> p h j", h=8)
        P.cp("scalar", ysb[:], pY[:], [bpY], [bysb])
        P.op("vector", lambda e: e.tensor_reduce(out=t1[:, 0:8], in_=v3(ysb[:]), axis=AX.X, op=ALU.add), [bysb], [bt1])
        P.ts("vector", t1[:, 0:8], t1[:, 0:8], -1.0 / 64, None, ALU.mult, None, [bt1], [bt1])
        P.tt("vector", v3(ysb[:]), v3(ysb[:]), t1[:, 0:8].unsqueeze(2).to_broadcast([128, 8, 64]), ALU.add, [bysb, bt1], [bysb])
        P.tt(rr(), tm1[:], ysb[:], ysb[:], ALU.mult, [bysb], [btm1])
        P.op("vector", lambda e: e.tensor_reduce(out=t1[:, 8:16], in_=v3(tm1[:]), axis=AX.X, op=ALU.add), [btm1], [bt1])
        P.ts("vector", t1[:, 8:16], t1[:, 8:16], 1.0 / 64, 64e-5, ALU.mult, ALU.add, [bt1], [bt1])
        P.act(t1[:, 8:16], t1[:, 8:16], AF.Ln, [bt1], [bt1])
        P.act(t1[:, 8:16], t1[:, 8:16], AF.Exp, [bt1], [bt1], scale=-0.5)
        P.tt("vector", v3(ysb[:]), v3(ysb[:]), t1[:, 8:16].unsqueeze(2).to_broadcast([128, 8, 64]), ALU.mult, [bysb, bt1], [bysb])
        P.tt(rr(), ysb[:], ysb[:], prm[:, 5, :], ALU.mult, [bysb, bprm], [bysb])
        P.tt(rr(), ysb[:], ysb[:], prm[:, 6, :], ALU.add, [bysb, bprm], [bysb])
        P.tt("vector", v3(tm1[:]), v3(v_s[:]), st8[:, 0:8].unsqueeze(2).to_broadcast([128, 8, 64]), ALU.mult, [bv, bst8], [btm1])
        P.tt(rr(), ysb[:], ysb[:], tm1[:], ALU.add, [bysb, btm1], [bysb])
        P.tt("vector", yo[:], ysb[:], g_s[:], ALU.mult, [bysb, bg], [byo])
        P.dma("sync", yrw_d[tsl, :], yo[:], [byo], [b_yrw], byo)


def phase_attn(P, nc, G):
    con_d, modp_d, lamv_d, subln_d, wout_d, rtw_d, rtb_d = (G[k] for k in
        ("con_d", "modp_d", "lamv_d", "subln_d", "wout_d", "rtw_d", "rtb_d"))
    x_d, qk_d, v_d, yrw_d, x1_d, h2T_d, gat_d = (G[k] for k in ("x_d", "qk_d", "v_d", "yrw_d", "x1_d", "h2T_d", "gat_d"))
    b_modp, b_qk, b_v, b_yrw, b_x1, b_h2T, b_gat = (G[k] for k in
        ("b_modp", "b_qk", "b_v", "b_yrw", "b_x1", "b_h2T", "b_gat"))
    rr = _rr(P)
    con, bcon = P.sb("con", [128, NCONST], F32)
    P.dma("sync", con[:], con_d, [], [bcon], bcon)
    identf = con[:, O_ID:O_ID + 128]
    idb, bidb = P.sb("idb", [128, 128], BF16)
    P.cp("vector", idb[:], identf, [bcon], [bidb])
    cmk, bcmk = P.sb("cmk", [128, 4, 512], BF16)
    P.cp("vector", cmk[:].rearrange("p a t -> p (a t)"), con[:, O_CM:O_CM + 2048], [bcon], [bcmk])
    rows, brows = P.sb("rows", [128, 3, D], F32)
    P.dma("sync", rows[:].rearrange("p a d -> p (a d)"),
          modp_d[2:5, :].rearrange("(o a) d -> o (a d)", o=1).partition_broadcast(128), [b_modp], [brows], brows)
    mcol, bmcol = P.sb("mcol", [128, 6, 8], F32)
    P.dma("sync", mcol[:], modp_d.rearrange("a (k p) -> p a k", p=128), [b_modp], [bmcol], bmcol,
          allow_slow_non_contiguous=True)
    lv, blv = P.sb("lv", [128, 4, 64], F32)
    P.dma("sync", lv[:].rearrange("p a d -> p (a d)"),
          lamv_d.rearrange("(o a) d -> o (a d)", o=1).partition_broadcast(128), [], [blv], blv)
    lam, blam = P.sb("lam", [128, 8], F32)
    lt, blt = P.sb("lt", [128, 2, 64], F32)
    P.tt("vector", lt[:, 0, :], lv[:, 0, :], lv[:, 1, :], ALU.mult, [blv], [blt])
    P.tt("vector", lt[:, 1, :], lv[:, 2, :], lv[:, 3, :], ALU.mult, [blv], [blt])
    P.op("vector", lambda e: e.tensor_reduce(out=lam[:, 0:2], in_=lt[:], axis=AX.X, op=ALU.add), [blt], [blam])
    P.act(lam[:, 2:4], lam[:, 0:2], AF.Exp, [blam], [blam])
    P.tt("vector", lam[:, 4:5], lam[:, 2:3], lam[:, 3:4], ALU.subtract, [blam], [blam])
    P.ts("vector", lam[:, 5:6], lam[:, 4:5], -1.0, -LAMBDA_INIT, ALU.mult, ALU.add, [blam], [blam])
    sub, bsub = P.sb("sub", [128, 128], F32)
    P.dma("sync", sub[:], subln_d.partition_broadcast(128), [], [bsub], bsub)
    P.ts("vector", sub[:], sub[:], 1.0 - LAMBDA_INIT, None, ALU.mult, None, [bsub], [bsub])
    wo, bwo = P.sb("wo", [128, 8, D], BF16)
    P.dma("gpsimd", wo[:], wout_d.rearrange("(k p) c -> p k c", p=128), [], [bwo], bwo)
    rw, brw = P.sb("rw", [128, 8, NE], BF16)
    P.dma("gpsimd", rw[:], rtw_d.rearrange("(k p) c -> p k c", p=128), [], [brw], brw)
    rb, brb = P.sb("rb", [128, NE], F32)
    P.dma("sync", rb[:], rtb_d.partition_broadcast(128), [], [brb], brb)
    kT, bkT = P.sb("kT", [128, 4, T], BF16)
    P.dma("sync", kT[:], qk_d[512:1024, :].rearrange("(c p) t -> p c t", p=128), [b_qk], [bkT], bkT)
    vv, bvv = P.sb("vv", [128, NT, 4 * 129], BF16)
    P.dma("sync", vv[:], v_d.rearrange("(n p) f -> p n f", p=128), [b_v], [bvv], bvv)
    qT = [P.sb(f"qT{i}", [128, 4, 512], BF16) for i in range(2)]
    pt = [P.sb(f"pt{i}", [128, 512], BF16) for i in range(3)]
    psc = [P.ps(f"psc{i}", [128, 512], F32) for i in range(2)]
    po = [P.ps(f"po{i}", [128, 4, 128], F32) for i in range(2)]
    pms, bpms_ = P.ps("pms", [128, 512], F32)
    pos_ = pms[:, 0:128].rearrange("p (a b c) -> p a b c", a=2, b=4)
    bpos_ = P.buf("possum")
    pw, bpw = P.ps("pw", [128, D], F32)
    ptr, bptr = P.ps("ptr", [128, 8, 128], BF16)
    prt = pms[:, 128:256]
    bprt = bpos_
    YC, bYC_ = P.sb("YC", [128, 4, D], BF16)
    bYC = [P.buf("YCs") for _ in range(4)]
    ycT, bycT = P.sb("ycT", [128, 8, 128], BF16)
    o0, bo0 = P.sb("o0", [128, 128], F32)
    o1, bo1 = P.sb("o1", [128, 128], F32)
    rs, brs = P.sb("rs", [128, 16], F32)
    xt, bxt = P.sb("xt", [128, D], F32)
    y1, by1 = P.sb("y1", [128, D], F32)
    junk, bjunk = P.sb("junk", [128, D], BF16)
    h2, bh2 = P.sb("h2", [128, D], BF16)
    h2T, bh2T = P.sb("h2T", [128, 8, 128], BF16)
    lg, blg = P.sb("lg", [128, NE], F32)
    gt, bgt = P.sb("gt", [128, NE], F32)
    t8, bt8 = P.sb("t8", [128, 16], F32)
    yrt, byrt = P.sb("yrt", [128, 512], BF16)

    it = 0
    for qb in range(8):
        qcur, bqcur = qT[qb % 2]
        P.dma("sync", qcur[:], qk_d[0:512, qb * 512:(qb + 1) * 512].rearrange("(c p) t -> p c t", p=128),
              [b_qk], [bqcur], bqcur)
        ntk = (qb + 1) * 4
        for hd in range(4):
            for mp in range(2):
                m = hd * 2 + mp
                chn, pb = m // 2, (m % 2) * 64
                pot, bpot = po[mp]
                for tk in range(ntk):
                    ps_, bps_ = psc[it % 2]
                    ptile, bptile = pt[it % 3]
                    it += 1
                    P.mm(ps_[:], kT[pb:pb + 64, chn, tk * 128:(tk + 1) * 128], qcur[pb:pb + 64, chn, :],
                         [bkT, bqcur], [bps_])
                    P.act(ptile[:], ps_[:], AF.Exp, [bps_], [bptile], scale=0.125)
                    j = tk - qb * 4
                    if j >= 0:
                        P.tt("vector", ptile[:], ptile[:], cmk[:, j, :], ALU.mult, [bptile, bcmk], [bptile])
                    for s4 in range(4):
                        if j > s4:
                            continue
                        P.mm(pot[:, s4, :], ptile[:, s4 * 128:(s4 + 1) * 128], vv[:, tk, hd * 129:hd * 129 + 128],
                             [bptile, bvv], [bpot], start=(tk == 0 and s4 == 0), stop=(tk == ntk - 1 and s4 == 3))
                        P.mm(pos_[:, mp, s4, 0:1], ptile[:, s4 * 128:(s4 + 1) * 128], vv[:, tk, hd * 129 + 128:hd * 129 + 129],
                             [bptile, bvv], [bpos_], start=(mp == 0 and tk == 0 and s4 == 0),
                             stop=(mp == 1 and tk == ntk - 1 and s4 == 3))
            P.cp("vector", rs[:, 0:8].rearrange("p (a b) -> p a b", a=2), pos_[:, :, :, 0], [bpos_], [brs])
            P.op("vector", lambda e: e.reciprocal(out=rs[:, 8:16], in_=rs[:, 0:8]), [brs], [brs])
            P.ts("vector", rs[:, 12:16], rs[:, 12:16], lam[:, 5:6], None, ALU.mult, None, [brs, blam], [brs])
            for s4 in range(4):
                P.ts("vector", o0[:], po[0][0][:, s4, :], rs[:, 8 + s4:9 + s4], None, ALU.mult, None, [po[0][1], brs], [bo0])
                P.stt("vector", o0[:], po[1][0][:, s4, :], rs[:, 12 + s4:13 + s4], o0[:], ALU.mult, ALU.add,
                      [po[1][1], brs, bo0], [bo0])
                P.act(o1[:], o0[:], AF.Square, [bo0], [bo1, bt8], accum_out=t8[:, 0:1])
                P.ts("vector", t8[:, 1:2], t8[:, 0:1], 1.0 / 128, 1e-5, ALU.mult, ALU.add, [bt8], [bt8])
                P.act(t8[:, 2:3], t8[:, 1:2], AF.Ln, [bt8], [bt8])
                P.act(t8[:, 3:4], t8[:, 2:3], AF.Exp, [bt8], [bt8], scale=-0.5)
                P.stt("vector", YC[:, s4, hd * 128:(hd + 1) * 128], o0[:], t8[:, 3:4], sub[:], ALU.mult, ALU.mult,
                      [bo0, bt8, bsub], [bYC[s4]])
        for s4 in range(4):
            n = qb * 4 + s4
            tsl = slice(n * 128, (n + 1) * 128)
            P.dma("sync", yrt[:], yrw_d[tsl, :], [b_yrw], [byrt], byrt)
            P.cp("vector", YC[:, s4, 512:1024], yrt[:], [byrt], [bYC[s4]])
            for k in range(8):
                P.tr(ptr[:, k, :], YC[:, s4, k * 128:(k + 1) * 128], idb[:], [bYC[s4], bidb], [bptr])
            P.cp("scalar", ycT[:].rearrange("p k t -> p (k t)"), ptr[:].rearrange("p k t -> p (k t)"), [bptr], [bycT])
            for hf in range(2):
                for k in range(8):
                    P.mm(pw[:, hf * 512:(hf + 1) * 512], ycT[:, k, :], wo[:, k, hf * 512:(hf + 1) * 512],
                         [bycT, bwo], [bpw], start=(k == 0), stop=(k == 7))
            P.cp("scalar", y1[:], pw[:], [bpw], [by1])
            P.act(junk[:], y1[:], AF.Square, [by1], [bjunk, bt8], accum_out=t8[:, 4:5])
            P.ts("vector", t8[:, 5:6], t8[:, 4:5], 1.0 / D, 1e-6, ALU.mult, ALU.add, [bt8], [bt8])
            P.act(t8[:, 6:7], t8[:, 5:6], AF.Ln, [bt8], [bt8])
            P.act(t8[:, 7:8], t8[:, 6:7], AF.Exp, [bt8], [bt8], scale=-0.5)
            P.dma("sync", xt[:], x_d[tsl, :], [], [bxt], bxt)
            P.stt("vector", y1[:], y1[:], t8[:, 7:8], rows[:, 0, :], ALU.mult, ALU.mult, [by1, bt8, brows], [by1])
            P.tt("gpsimd", xt[:], xt[:], y1[:], ALU.add, [bxt, by1], [bxt])
            P.dma("sync", x1_d[tsl, :], xt[:], [bxt], [b_x1], bxt)
            P.act(junk[:], xt[:], AF.Square, [bxt], [bjunk, bt8], accum_out=t8[:, 8:9])
            P.ts("vector", t8[:, 9:10], t8[:, 8:9], 1.0 / D, 1e-6, ALU.mult, ALU.add, [bt8], [bt8])
            P.act(t8[:, 10:11], t8[:, 9:10], AF.Ln, [bt8], [bt8])
            P.act(t8[:, 11:12], t8[:, 10:11], AF.Exp, [bt8], [bt8], scale=-0.5)
            P.ts("vector", h2[:], xt[:], t8[:, 11:12], None, ALU.mult, None, [bxt, bt8], [bh2])
            for k in range(8):
                P.tr(ptr[:, k, :], h2[:, k * 128:(k + 1) * 128], idb[:], [bh2, bidb], [bptr])
            for k in range(8):
                P.act(h2T[:, k, :], ptr[:, k, :], AF.Identity, [bptr, bmcol], [bh2T],
                      bias=mcol[:, 4, k:k + 1], scale=mcol[:, 3, k:k + 1])
            P.dma("sync", h2T_d.rearrange("(k p) t -> p k t", p=128)[:, :, tsl], h2T[:], [bh2T], [b_h2T], bh2T)
            for k in range(8):
                P.mm(prt[:, 0:NE], h2T[:, k, :], rw[:, k, :], [bh2T, brw], [bprt], start=(k == 0), stop=(k == 7))
            P.tt("vector", lg[:], prt[:, 0:NE], rb[:], ALU.add, [bprt, brb], [blg])
            P.op("vector", lambda e: e.max(out=t8[:, 0:8], in_=lg[:]), [blg], [bt8])
            P.ts("vector", gt[:], lg[:], t8[:, 3:4], None, ALU.is_ge, None, [blg, bt8], [bgt])
            P.ts("vector", t8[:, 12:13], t8[:, 0:1], -1.0, None, ALU.mult, None, [bt8], [bt8])
            P.act(lg[:], lg[:], AF.Exp, [blg, bt8], [blg], bias=t8[:, 12:13], scale=1.0)
            P.tt("vector", gt[:], gt[:], lg[:], ALU.mult, [bgt, blg], [bgt])
            P.op("vector", lambda e: e.tensor_reduce(out=t8[:, 13:14], in_=gt[:], axis=AX.X, op=ALU.add), [bgt], [bt8])
            P.op("vector", lambda e: e.reciprocal(out=t8[:, 14:15], in_=t8[:, 13:14]), [bt8], [bt8])
            P.ts("vector", gt[:], gt[:], t8[:, 14:15], None, ALU.mult, None, [bgt, bt8], [bgt])
            P.dma("sync", gat_d[tsl, :], gt[:], [bgt], [b_gat], bgt)


def phase_moe(P, nc, G):
    modp_d, x1_d, h2T_d, gat_d, out_d = (G[k] for k in ("modp_d", "x1_d", "h2T_d", "gat_d", "out_d"))
    w1g_d, w1l_d, w2e_d, b1T_d, b2_d, con_d = (G[k] for k in ("w1g_d", "w1l_d", "w2e_d", "b1T_d", "b2_d", "con_d"))
    b_modp, b_x1, b_h2T, b_gat, b_out = (G[k] for k in ("b_modp", "b_x1", "b_h2T", "b_gat", "b_out"))
    idf, bidf = P.sb("idf", [128, 128], F32)
    P.dma("sync", idf[:], con_d[:, O_ID:O_ID + 128], [], [bidf], bidf)
    c2r, bc2r = P.sb("c2r", [128, D], F32)
    P.dma("sync", c2r[:], modp_d[5:6, :].partition_broadcast(128), [b_modp], [bc2r], bc2r)
    b1T, bb1T = P.sb("b1T", [128, NE, 16], F32)
    P.dma("sync", b1T[:].rearrange("p e c -> p (e c)"), b1T_d, [], [bb1T], bb1T)
    b2s, bb2s = P.sb("b2s", [NE, D], F32)
    P.dma("sync", b2s[:], b2_d, [], [bb2s], bb2s)
    W = [[P.sb(f"w{j}_{i}", [128, 8, D], BF16) for j in range(3)] for i in range(2)]
    hq, bhq = P.sb("hq", [128, 8, 1024], BF16)
    gq, bgq = P.sb("gq", [128, 8, NE], F32)
    gT, bgT = P.sb("gT", [NE, 128], F32)
    acc, bacc_ = P.sb("acc", [128, 8, D], F32)
    bacc = [P.buf("acct") for _ in range(8)]
    actT, bactT_ = P.sb("actT", [128, 8, 512], BF16)
    bactT = [P.buf("actc") for _ in range(8)]
    g_, bg_ = P.sb("mg", [128, 512], F32)
    s_, bs_ = P.sb("msg", [128, 512], F32)
    l_, bl_ = P.sb("ml", [128, 512], F32)
    xt, bxt = P.sb("mxt", [128, D], F32)
    junk, bjunk = P.sb("mjunk", [128, D], BF16)
    t8, bt8 = P.sb("mt8", [128, 8], F32)
    pg = [P.ps(f"pg{i}", [128, 512], F32) for i in range(2)]
    pl = [P.ps(f"pl{i}", [128, 512], F32) for i in range(2)]
    po = [P.ps(f"pmo{i}", [128, 512], F32) for i in range(2)]
    pm, bpm = P.ps("pmisc", [128, 512], F32)
    it = 0
    io = 0
    wi = 0
    for qt in range(4):
        q0 = qt * 1024
        P.dma("sync", hq[:], h2T_d.rearrange("(k p) t -> p k t", p=128)[:, :, q0:q0 + 1024], [b_h2T], [bhq], bhq)
        P.dma("sync", gq[:], gat_d[q0:q0 + 1024, :].rearrange("(n p) e -> p n e", p=128), [b_gat], [bgq], bgq)
        for n in range(8):
            P.tr(pm[0:NE, 0:128], gq[:, n, :], idf[:], [bgq, bidf], [bpm])
            P.cp("vector", gT[:], pm[0:NE, 0:128], [bpm], [bgT])
            for hf in range(2):
                pp, bpp = po[io % 2]
                io += 1
                P.mm(pp[:], gT[:], b2s[:, hf * 512:(hf + 1) * 512], [bgT, bb2s], [bpp])
                P.cp("scalar", acc[:, n, hf * 512:(hf + 1) * 512], pp[:], [bpp], [bacc[n]])
        for e in range(NE):
            (w1g, bw1g), (w1l, bw1l), (w2, bw2) = W[wi % 2]
            wi += 1
            P.dma("gpsimd", w1g[:], w1g_d[e].rearrange("(k p) f -> p k f", p=128), [], [bw1g], bw1g)
            P.dma("gpsimd", w1l[:], w1l_d[e].rearrange("(k p) f -> p k f", p=128), [], [bw1l], bw1l)
            P.dma("gpsimd", w2[:], w2e_d[e].rearrange("(k p) f -> p k f", p=128), [], [bw2], bw2)
            for blk in range(2):
                bsl = slice(blk * 512, (blk + 1) * 512)
                for fc in range(8):
                    pgt, bpgt = pg[it % 2]
                    plt, bplt = pl[it % 2]
                    it += 1
                    for k in range(8):
                        P.mm(pgt[:], w1g[:, k, fc * 128:(fc + 1) * 128], hq[:, k, bsl], [bw1g, bhq], [bpgt],
                             start=(k == 0), stop=(k == 7))
                    for k in range(8):
                        P.mm(plt[:], w1l[:, k, fc * 128:(fc + 1) * 128], hq[:, k, bsl], [bw1l, bhq], [bplt],
                             start=(k == 0), stop=(k == 7))
                    P.ts("vector", g_[:], pgt[:], b1T[:, e, fc:fc + 1], 7.0, ALU.add, ALU.min, [bpgt, bb1T], [bg_])
                    P.act(s_[:], g_[:], AF.Sigmoid, [bg_], [bs_], scale=1.702)
                    P.ts("vector", l_[:], plt[:], b1T[:, e, 8 + fc:9 + fc], 7.0, ALU.add, ALU.min, [bplt, bb1T], [bl_])
                    P.ts("gpsimd", l_[:], l_[:], -7.0, 1.0, ALU.max, ALU.add, [bl_], [bl_])
                    P.tt("gpsimd", s_[:], s_[:], g_[:], ALU.mult, [bs_, bg_], [bs_])
                    P.tt("vector", actT[:, fc, :], s_[:], l_[:], ALU.mult, [bs_, bl_], [bactT[fc]])
                for tt_ in range(4):
                    n = blk * 4 + tt_
                    for hf in range(2):
                        pp, bpp = po[io % 2]
                        io += 1
                        for fc in range(8):
                            P.mm(pp[:], actT[:, fc, tt_ * 128:(tt_ + 1) * 128], w2[:, fc, hf * 512:(hf + 1) * 512],
                                 [bactT[fc], bw2], [bpp], start=(fc == 0), stop=(fc == 7))
                        P.stt("vector", acc[:, n, hf * 512:(hf + 1) * 512], pp[:], gq[:, n, e:e + 1],
                              acc[:, n, hf * 512:(hf + 1) * 512], ALU.mult, ALU.add, [bpp, bgq, bacc[n]], [bacc[n]])
        for n in range(8):
            tsl = slice(q0 + n * 128, q0 + (n + 1) * 128)
            P.dma("sync", xt[:], x1_d[tsl, :], [b_x1], [bxt], bxt)
            P.act(junk[:], acc[:, n, :], AF.Square, [bacc[n]], [bjunk, bt8], accum_out=t8[:, 0:1])
            P.ts("vector", t8[:, 1:2], t8[:, 0:1], 1.0 / D, 1e-6, ALU.mult, ALU.add, [bt8], [bt8])
            P.act(t8[:, 2:3], t8[:, 1:2], AF.Ln, [bt8], [bt8])
            P.act(t8[:, 3:4], t8[:, 2:3], AF.Exp, [bt8], [bt8], scale=-0.5)
            P.stt("vector", acc[:, n, :], acc[:, n, :], t8[:, 3:4], c2r[:], ALU.mult, ALU.mult, [bacc[n], bt8, bc2r], [bacc[n]])
            P.tt("vector", xt[:], xt[:], acc[:, n, :], ALU.add, [bxt, bacc[n]], [bxt])
            P.dma("sync", out_d[tsl, :], xt[:], [bxt], [b_out], bxt)


def _consts():
    c = np.zeros((128, NCONST), np.float32)
    r = np.arange(128)[:, None]
    q = np.arange(128)[None, :]
    same = (r // 64) == (q // 64)
    su = (same & ((r % 64) < (q % 64))).astype(np.float32)
    sl = (same & ((r % 64) > (q % 64))).astype(np.float32)
    ui = (same & ((r % 64) <= (q % 64))).astype(np.float32)
    c[:, O_ID:O_ID + 128] = np.eye(128, dtype=np.float32)
    for i, m in enumerate((su, su, sl, ui, ui)):
        c[:, O_M5 + i * 128:O_M5 + (i + 1) * 128] = m
    c[:, O_ONES:O_ONES + 128] = same.astype(np.float32)
    c[:64, O_IND] = 1.0
    c[64:, O_IND + 1] = 1.0
    inv_freq = (500000.0 ** (-np.arange(0, 16, 2, dtype=np.float32) / 16)).astype(np.float32)
    for p in range(128):
        d = p % 64
        if d < 16:
            c[p, O_FREQ] = inv_freq[d % 8]
            c[p, O_SIGN] = -1.0 if d < 8 else 1.0
            pp = p + 8 if d < 8 else p - 8
            c[pp, O_PM + p] = 1.0
    tq = np.arange(512)[None, :]
    tk = np.arange(128)[:, None]
    for j in range(4):
        c[:, O_CM + j * 512:O_CM + (j + 1) * 512] = ((j * 128 + tk) <= tq).astype(np.float32)
    return c


_NC_CACHE = {}


def _in_maps(inp):
    f = lambda a: np.ascontiguousarray(np.asarray(a, dtype=np.float32))
    B = 8
    w1 = np.asarray(inp["moe_w1"])[0]
    w1g = np.ascontiguousarray(w1[:, :, 0::2])
    w1l = np.ascontiguousarray(w1[:, :, 1::2])
    b1 = np.asarray(inp["moe_b1"])[0]
    b1cat = np.concatenate([b1[:, 0::2].reshape(NE, 8, 128), b1[:, 1::2].reshape(NE, 8, 128)], axis=1)
    b1T = np.ascontiguousarray(b1cat.transpose(2, 0, 1).reshape(128, NE * 16)).astype(np.float32)
    shared = dict(
        ada_w=f(inp["ada_w"][0]), ada_b=f(inp["ada_b"]).reshape(1, 6 * D),
        norms=f(np.stack([inp["pre_mix_norm"][0], inp["post_mix_norm"][0], inp["pre_ffn_norm"][0], inp["post_ffn_norm"][0]])),
        w_in=f(inp["w_in"][0]), w_out=f(inp["w_out"][0]),
        lamv=f(np.stack([inp["da_lambda_q1"][0], inp["da_lambda_k1"][0], inp["da_lambda_q2"][0], inp["da_lambda_k2"][0]])),
        subln=f(inp["da_subln"]).reshape(1, 128), mu=f(inp["rw_mu"]).reshape(1, 1792),
        rwv=f(np.stack([inp["rw_w0"][0], inp["rw_a0"][0], inp["rw_k_k"][0], inp["rw_k_a"][0],
                        np.asarray(inp["rw_r_k"])[0].reshape(512), inp["rw_ln_w"][0], inp["rw_ln_b"][0]])),
        rw_w2=f(inp["rw_w2"][0]), rw_a2=f(inp["rw_a2"][0]), rw_g2=f(inp["rw_g2"][0]),
        router_w=f(inp["router_w"][0]), router_b=f(inp["router_b"]).reshape(1, NE),
        w1g=w1g, w1l=w1l, b1T=b1T, w2e=f(inp["moe_w2"][0]), b2=f(inp["moe_b2"][0]),
        consts=_consts(),
    )
    x = np.asarray(inp["x"], dtype=np.float32)
    c = np.asarray(inp["c"], dtype=np.float32)
    pos = np.asarray(inp["positions"]).astype(np.int32)
    in_maps = []
    for b in range(B):
        m = dict(shared)
        m["x"] = np.ascontiguousarray(x[b])
        m["cT"] = np.ascontiguousarray(c[b].reshape(8, 128).T)
        m["pos"] = np.ascontiguousarray(pos[b].reshape(1, T))
        in_maps.append(m)
    return in_maps


def kernel(**inp):
    B = 8
    if "nc" not in _NC_CACHE:
        _NC_CACHE["nc"] = build_program()
    nc = _NC_CACHE["nc"]
    in_maps = _in_maps(inp)
    res = run_bass_kernel_spmd(nc, in_maps, core_ids=list(range(B)))
    return np.stack([np.asarray(r["out"], dtype=np.float32) for r in res.results], axis=0)
```

```python
import contextlib
import math
import numpy as np
import concourse.bass as bass
import concourse.mybir as mybir
from concourse.bass_utils import run_bass_kernel_spmd

ALU = mybir.AluOpType
AF = mybir.ActivationFunctionType
F32 = mybir.dt.float32
BF16 = mybir.dt.bfloat16
I32 = mybir.dt.int32
AX = mybir.AxisListType

D = 1024
T = 4096
NT = 32
NE = 32
C0 = math.exp(-0.5)
LAMBDA_INIT = 0.8 - 0.6 * math.exp(0.0)

O_ID = 0
O_M5 = 128
O_UI = O_M5 + 384
O_SU = O_M5
O_SL = O_M5 + 256
O_ONES = 768
O_IND = 896
O_FREQ = 898
O_SIGN = 899
O_PM = 900
O_CM = 1028
NCONST = O_CM + 2048
DBG_STOP = 99
NOSELF_MOE = ('tensor',)
NOSELF_ATTN = ('tensor',)
NOSELF_FRONT = ()
DBG_X = 0
DBG_NQ = 4
DBG_NEXP = NE
DBG_NT = NT


class Buf:
    __slots__ = ("name", "w", "r", "dsem", "dcount")

    def __init__(self, name):
        self.name = name
        self.w = {}
        self.r = {}
        self.dsem = None
        self.dcount = 0


class Eng:
    def __init__(self, name, sem):
        self.name = name
        self.sem = sem
        self.count = 0
        self.waited = {}
        self.thunks = []


class Prog:
    def __init__(self, nc, stack):
        self.nc = nc
        self.gstack = stack
        self.stack = stack
        self.engs = {}
        self.sems = {}
        self.vals = {}
        for n in ("tensor", "vector", "scalar", "gpsimd", "sync"):
            sem = stack.enter_context(nc.semaphore("es_" + n))
            self.engs[n] = Eng(n, sem)
            self.sems[("e", n)] = sem
            self.vals[("e", n)] = 0
        self.nbuf = 0
        self.ninstr = 0
        self.allbufs = []
        self.gen = 0
        self.noself = ()

    def buf(self, name=None):
        self.nbuf += 1
        b = Buf(f"{name or 'b'}{self.nbuf}")
        self.allbufs.append(b)
        return b

    def new_engine_sems(self):
        self.gen += 1
        for n, eng in self.engs.items():
            old = ("e", n)
            self.vals.pop(old, None)
            sem = self.gstack.enter_context(self.nc.semaphore(f"es{self.gen}_{n}"))
            eng.sem = sem
            eng.count = 0
            eng.waited = {}
            self.sems[old] = sem
            self.vals[old] = 0
        for b in self.allbufs:
            b.w = {}
            b.r = {}

    def sb(self, name, shape, dtype):
        self.nbuf += 1
        t = self.stack.enter_context(self.nc.sbuf_tensor(f"sb{self.nbuf}_{name}", list(shape), dtype))
        return t, self.buf(name)

    def ps(self, name, shape, dtype=F32):
        self.nbuf += 1
        t = self.stack.enter_context(self.nc.psum_tensor(f"ps{self.nbuf}_{name}", list(shape), dtype))
        return t, self.buf(name)

    def _dsem(self, b):
        if b.dsem is None:
            s = self.gstack.enter_context(self.nc.semaphore("ds_" + b.name))
            b.dsem = ("d", b.name)
            self.sems[b.dsem] = s
            self.vals[b.dsem] = 0
        return b.dsem

    def _collect(self, eng, reads, writes):
        need = {}
        for b in reads:
            for k, v in b.w.items():
                if need.get(k, 0) < v:
                    need[k] = v
        for b in writes:
            for k, v in b.w.items():
                if need.get(k, 0) < v:
                    need[k] = v
            for k, v in b.r.items():
                if need.get(k, 0) < v:
                    need[k] = v
        own = ("e", eng.name)
        for k, v in need.items():
            if k == own and eng.name in self.noself:
                continue
            if eng.waited.get(k, 0) < v:
                eng.waited[k] = v
                sem = self.sems[k]
                eng.thunks.append(lambda e, sem=sem, v=v: e.wait_ge(sem, v))

    def op(self, engname, fn, reads=(), writes=()):
        eng = self.engs[engname]
        self._collect(eng, reads, writes)
        eng.count += 1
        c = eng.count
        sem = eng.sem
        eng.thunks.append(lambda e, fn=fn, sem=sem: fn(e).then_inc(sem, 1))
        key = ("e", engname)
        self.vals[key] = c
        for b in reads:
            b.r[key] = c
        for b in writes:
            b.w = {key: c}
            b.r = {}
        self.ninstr += 1

    def dma(self, q, out_ap, in_ap, reads, writes, sbuf_buf, **kw):
        eng = self.engs[q]
        self._collect(eng, reads, writes)
        key = self._dsem(sbuf_buf)
        sbuf_buf.dcount += 16
        c = sbuf_buf.dcount
        self.vals[key] = c
        sem = self.sems[key]
        eng.thunks.append(
            lambda e, o=out_ap, i=in_ap, sem=sem, kw=kw: e.dma_start(out=o, in_=i, **kw).then_inc(sem, 16))
        for b in reads:
            b.r[key] = c
        for b in writes:
            if b is sbuf_buf:
                b.w = {key: c}
                b.r = {}
            else:
                b.w[key] = c
        self.ninstr += 1

    def barrier(self):
        for eng in self.engs.values():
            for k, v in self.vals.items():
                if v > 0 and eng.waited.get(k, 0) < v:
                    eng.waited[k] = v
                    sem = self.sems[k]
                    eng.thunks.append(lambda e, sem=sem, v=v: e.wait_ge(sem, v))

    def flush(self):
        nc = self.nc
        engs = self.engs
        with nc.Block() as block:
            @block.tensor
            def _(e):
                for t in engs["tensor"].thunks:
                    t(e)

            @block.vector
            def _(e):
                for t in engs["vector"].thunks:
                    t(e)

            @block.scalar
            def _(e):
                for t in engs["scalar"].thunks:
                    t(e)

            @block.gpsimd
            def _(e):
                for t in engs["gpsimd"].thunks:
                    t(e)

            @block.sync
            def _(e):
                for t in engs["sync"].thunks:
                    t(e)
        for e in engs.values():
            e.thunks = []

    @contextlib.contextmanager
    def phase(self):
        with contextlib.ExitStack() as ph:
            self.stack = ph
            yield
            self.barrier()
            self.flush()
        self.stack = self.gstack
        self.new_engine_sems()

    def mm(self, out, lhsT, rhs, R, W, start=True, stop=True):
        self.op("tensor", lambda e: e.matmul(out, lhsT=lhsT, rhs=rhs, start=start, stop=stop), R, W)

    def tr(self, out, in_, ident, R, W):
        self.op("tensor", lambda e: e.transpose(out, in_, ident), R, W)

    def tt(self, eng, out, in0, in1, op, R, W):
        self.op(eng, lambda e: e.tensor_tensor(out=out, in0=in0, in1=in1, op=op), R, W)

    def ts(self, eng, out, in0, s1, s2, op0, op1, R, W):
        if s2 is None:
            self.op(eng, lambda e: e.tensor_scalar(out=out, in0=in0, scalar1=s1, scalar2=None, op0=op0), R, W)
        else:
            self.op(eng, lambda e: e.tensor_scalar(out=out, in0=in0, scalar1=s1, scalar2=s2, op0=op0, op1=op1), R, W)

    def stt(self, eng, out, in0, scalar, in1, op0, op1, R, W):
        eng = "vector"
        self.op(eng, lambda e: e.scalar_tensor_tensor(out=out, in0=in0, scalar=scalar, in1=in1, op0=op0, op1=op1), R, W)

    def act(self, out, in_, func, R, W, bias=None, scale=None, accum_out=None):
        kw = {}
        if bias is not None:
            kw["bias"] = bias
        if scale is not None:
            kw["scale"] = scale
        if accum_out is not None:
            kw["accum_out"] = accum_out
        self.op("scalar", lambda e: e.activation(out=out, in_=in_, func=func, **kw), R, W)

    def cp(self, eng, out, in_, R, W):
        if eng == "scalar":
            self.op("scalar", lambda e: e.copy(out=out, in_=in_), R, W)
        else:
            self.op(eng, lambda e: e.tensor_copy(out=out, in_=in_), R, W)

    def ms(self, eng, ap, val, W):
        self.op(eng, lambda e: e.memset(ap, val), [], W)


def _rr(P):
    state = {"i": 0}

    def nxt():
        state["i"] += 1
        return "vector" if state["i"] % 3 else "gpsimd"
    return nxt


def build_program(debug=False, upto=3):
    nc = bass.Bass("TRN2", target_bir_lowering=False)
    skind = "ExternalOutput" if debug else "Internal"

    def din(name, shape, dt=F32):
        return nc.dram_tensor(name, list(shape), dt, kind="ExternalInput").ap()

    x_d = din("x", [T, D])
    cT_d = din("cT", [128, 8])
    pos_d = din("pos", [1, T], I32)
    adaw_d = din("ada_w", [D, 6 * D])
    adab_d = din("ada_b", [1, 6 * D])
    norms_d = din("norms", [4, D])
    win_d = din("w_in", [D, 3328])
    wout_d = din("w_out", [D, D])
    lamv_d = din("lamv", [4, 64])
    subln_d = din("subln", [1, 128])
    mu_d = din("mu", [1, 1792])
    rwv_d = din("rwv", [7, 512])
    w2_d = din("rw_w2", [64, 512])
    a2_d = din("rw_a2", [64, 512])
    g2_d = din("rw_g2", [128, 512])
    rtw_d = din("router_w", [D, NE])
    rtb_d = din("router_b", [1, NE])
    w1g_d = din("w1g", [NE, D, D]) if upto >= 3 else None
    w1l_d = din("w1l", [NE, D, D]) if upto >= 3 else None
    b1T_d = din("b1T", [128, NE * 16])
    w2e_d = din("w2e", [NE, D, D]) if upto >= 3 else None
    b2_d = din("b2", [NE, D])
    con_d = din("consts", [128, NCONST])
    out_d = nc.dram_tensor("out", [T, D], F32, kind="ExternalOutput").ap()

    modp_d = nc.dram_tensor("modp", [6, D], F32, kind=skind).ap()
    qk_d = nc.dram_tensor("qk_s", [D, T], BF16, kind=skind).ap()
    v_d = nc.dram_tensor("v_s", [T, 4 * 129], BF16, kind=skind).ap()
    yrw_d = nc.dram_tensor("yrw_s", [T, 512], BF16, kind=skind).ap()
    x1_d = nc.dram_tensor("x1_s", [T, D], F32, kind=skind).ap()
    h2T_d = nc.dram_tensor("h2T_s", [D, T], BF16, kind=skind).ap()
    gat_d = nc.dram_tensor("gat_s", [T, NE], F32, kind=skind).ap()

    with contextlib.ExitStack() as gst:
        P = Prog(nc, gst)
        b_modp = P.buf("modp")
        b_qk = P.buf("qkd")
        b_v = P.buf("vd")
        b_yrw = P.buf("yrwd")
        b_x1 = P.buf("x1d")
        b_h2T = P.buf("h2Td")
        b_gat = P.buf("gatd")
        b_out = P.buf("outd")

        with P.phase():
            cT, bcT = P.sb("cT", [128, 8], F32)
            sc, bsc = P.sb("sc", [128, 8], F32)
            P.dma("sync", cT[:], cT_d, [], [bcT], bcT)
            P.act(sc[:], cT[:], AF.Silu, [bcT], [bsc])
            aw = [P.sb(f"aw{i}", [128, 3072], F32) for i in range(2)]
            pm, bpm = P.ps("pmod", [128, 3072], F32)
            mrow, bmrow = P.sb("mrow", [1, 6 * D], F32)
            brow, bbrow = P.sb("brow", [1, 6 * D], F32)
            nrm, bnrm = P.sb("nrm", [1, 4 * D], F32)
            orow, borow = P.sb("orow", [1, 6 * D], F32)
            P.dma("sync", brow[:], adab_d, [], [bbrow], bbrow)
            P.dma("sync", nrm[:], norms_d.rearrange("(o a) d -> o (a d)", o=1), [], [bnrm], bnrm)
            i = 0
            for half in range(2):
                for k in range(8):
                    t_, b_ = aw[i % 2]
                    i += 1
                    P.dma("sync", t_[:], adaw_d[k * 128:(k + 1) * 128, half * 3072:(half + 1) * 3072], [], [b_], b_)
                    for j in range(6):
                        P.mm(pm[0:1, j * 512:(j + 1) * 512], sc[:, k:k + 1], t_[:, j * 512:(j + 1) * 512],
                             [bsc, b_], [bpm], start=(k == 0), stop=(k == 7))
                P.tt("vector", mrow[:, half * 3072:(half + 1) * 3072], pm[0:1, :], brow[:, half * 3072:(half + 1) * 3072],
                     ALU.add, [bpm, bbrow], [bmrow])

            def mseg(i_):
                return mrow[:, i_ * D:(i_ + 1) * D]

            def nseg(i_):
                return nrm[:, i_ * D:(i_ + 1) * D]
            P.stt("vector", orow[:, 0:D], mseg(1), 1.0, nseg(0), ALU.add, ALU.mult, [bmrow, bnrm], [borow])
            P.cp("vector", orow[:, D:2 * D], mseg(0), [bmrow], [borow])
            P.tt("vector", orow[:, 2 * D:3 * D], mseg(2), nseg(1), ALU.mult, [bmrow, bnrm], [borow])
            P.stt("vector", orow[:, 3 * D:4 * D], mseg(4), 1.0, nseg(2), ALU.add, ALU.mult, [bmrow, bnrm], [borow])
            P.cp("vector", orow[:, 4 * D:5 * D], mseg(3), [bmrow], [borow])
            P.tt("vector", orow[:, 5 * D:6 * D], mseg(5), nseg(3), ALU.mult, [bmrow, bnrm], [borow])
            P.dma("sync", modp_d.rearrange("(o a) d -> o (a d)", o=1), orow[:], [borow], [b_modp], borow)

        if upto >= 1:
          with P.phase():
            phase_front(P, nc, locals())

        if upto >= 2:
          with P.phase():
            phase_attn(P, nc, locals())

        if upto >= 3:
          with P.phase():
            phase_moe(P, nc, locals())
    return nc


def phase_front(P, nc, G):
    P.noself = NOSELF_FRONT
    x_d, pos_d, win_d, mu_d, rwv_d = G["x_d"], G["pos_d"], G["win_d"], G["mu_d"], G["rwv_d"]
    w2_d, a2_d, g2_d, con_d, modp_d = G["w2_d"], G["a2_d"], G["g2_d"], G["con_d"], G["modp_d"]
    qk_d, v_d, yrw_d = G["qk_d"], G["v_d"], G["yrw_d"]
    b_modp, b_qk, b_v, b_yrw = G["b_modp"], G["b_qk"], G["b_v"], G["b_yrw"]
    rr = _rr(P)

    con, bcon = P.sb("con", [128, NCONST], F32)
    P.dma("sync", con[:], con_d, [], [bcon], bcon)
    identf = con[:, O_ID:O_ID + 128]
    idb, bidb = P.sb("idb", [128, 128], BF16)
    P.cp("vector", idb[:], identf, [bcon], [bidb])

    mcol, bmcol = P.sb("mcol", [128, 6, 8], F32)
    P.dma("sync", mcol[:], modp_d.rearrange("a (k p) -> p a k", p=128), [b_modp], [bmcol], bmcol,
          allow_slow_non_contiguous=True)

    wda, bwda = P.sb("wda", [128, 8, 1536], BF16)
    P.dma("gpsimd", wda[:], win_d[:, 0:1536].rearrange("(k p) c -> p k c", p=128), [], [bwda], bwda)
    w1, bw1 = P.sb("w1", [128, 8, 1792], BF16)
    w2m, bw2m = P.sb("w2m", [128, 8, 1792], BF16)
    prm, bprm = P.sb("prm", [128, 7, 512], F32)
    P.dma("sync", prm[:].rearrange("p a d -> p (a d)"),
          rwv_d.rearrange("(o a) d -> o (a d)", o=1).partition_broadcast(128), [], [bprm], bprm)
    lw2, blw2 = P.sb("lw2", [128, 512], F32)
    lg2, blg2 = P.sb("lg2", [128, 512], F32)
    P.dma("sync", lw2[0:64, :], w2_d, [], [blw2], blw2)
    P.dma("sync", lw2[64:128, :], a2_d, [], [blw2], blw2)
    P.dma("sync", lg2[:], g2_d, [], [blg2], blg2)

    ctab, bctab = P.sb("ctab", [128, T], BF16)
    stab, bstab = P.sb("stab", [128, T], BF16)
    with contextlib.ExitStack() as tmp:
        old = P.stack
        P.stack = tmp
        mub, bmub = P.sb("mub", [128, 1792], F32)
        omu, bomu = P.sb("omu", [128, 1792], F32)
        P.dma("sync", mub[:], mu_d.partition_broadcast(128), [], [bmub], bmub)
        P.ts("vector", omu[:], mub[:], -1.0, 1.0, ALU.mult, ALU.add, [bmub], [bomu])
        wst = [P.sb(f"wst{i}", [128, 1792], F32) for i in range(2)]
        for k in range(8):
            t_, b_ = wst[k % 2]
            P.dma("sync", t_[:], win_d[k * 128:(k + 1) * 128, 1536:3328], [], [b_], b_)
            P.tt("vector", w1[:, k, :], t_[:], omu[:], ALU.mult, [b_, bomu], [bw1])
            P.tt("gpsimd", w2m[:, k, :], t_[:], mub[:], ALU.mult, [b_, bmub], [bw2m])
        posi, bposi = P.sb("posi", [128, 1024], I32)
        ang, bang = P.sb("ang", [128, 1024], F32)
        y_, by_ = P.sb("ry", [128, 1024], F32)
        kf, bkf = P.sb("rkf", [128, 1024], F32)
        ki, bki = P.sb("rki", [128, 1024], I32)
        for q4 in range(4):
            sl = slice(q4 * 1024, (q4 + 1) * 1024)
            P.dma("sync", posi[:], pos_d[:, sl].partition_broadcast(128), [], [bposi], bposi)
            P.cp("vector", ang[:], posi[:], [bposi], [bang])
            P.ts("vector", ang[:], ang[:], con[:, O_FREQ:O_FREQ + 1], None, ALU.mult, None, [bang, bcon], [bang])
            for which, shift in ((0, math.pi * 1.5), (1, math.pi)):
                P.ts("vector", y_[:], ang[:], shift, None, ALU.add, None, [bang], [by_])
                P.ts("vector", kf[:], y_[:], 1.0 / (2 * math.pi), None, ALU.mult, None, [by_], [bkf])
                P.cp("vector", ki[:], kf[:], [bkf], [bki])
                P.cp("vector", kf[:], ki[:], [bki], [bkf])
                P.stt("vector", y_[:], kf[:], -2 * math.pi, y_[:], ALU.mult, ALU.add, [bkf, by_], [by_])
                P.ts("vector", kf[:], y_[:], 0.0, 2 * math.pi, ALU.is_lt, ALU.mult, [by_], [bkf])
                P.tt("vector", y_[:], y_[:], kf[:], ALU.add, [by_, bkf], [by_])
                P.ts("vector", y_[:], y_[:], -math.pi, None, ALU.add, None, [by_], [by_])
                P.ts("vector", y_[:], y_[:], -math.pi, math.pi, ALU.max, ALU.min, [by_], [by_])
                if which == 0:
                    P.act(ctab[:, sl], y_[:], AF.Sin, [by_], [bctab])
                else:
                    P.act(kf[:], y_[:], AF.Sin, [by_], [bkf])
                    P.ts("vector", stab[:, sl], kf[:], con[:, O_SIGN:O_SIGN + 1], None, ALU.mult, None, [bkf, bcon], [bstab])
        P.barrier()
        P.flush()
        P.stack = old

    def f32t(name, w=512):
        return P.sb(name, [128, w], F32)

    xt, bxt = P.sb("xt", [128, D], F32)
    xn, bxn = P.sb("xn", [128, D], BF16)
    junk, bjunk = xn, bxn
    st8, bst8 = P.sb("st8", [128, 8], F32)
    hT = [P.sb(f"hT{i}", [128, 8, 129], BF16) for i in range(2)]
    P.ms("vector", hT[1][0][:, :, 128:129], 0.0, [hT[1][1]])
    qf, bqf = P.sb("qf", [128, 128], F32)
    t1, bt1 = P.sb("t1", [128, 128], F32)
    t2, bt2 = P.sb("t2", [128, 128], F32)
    qko, bqko = P.sb("qko", [128, 8, 128], BF16)
    vo, bvo = P.sb("vo", [128, 4, 129], BF16)
    P.ms("vector", vo[:, :, 128:129], 1.0, [bvo])
    r_s, br = f32t("r_s")
    k_s, bk = f32t("k_s")
    v_s, bv = f32t("v_s")
    lo0, blo0 = P.sb("lo0", [128, 128], F32)
    lo1, blo1 = P.sb("lo1", [128, 128], F32)
    sig, bsig = f32t("sig")
    a_s, ba = f32t("a_s")
    g_s, bg = f32t("g_s")
    kk, bkk = f32t("kk")
    km, bkm = f32t("km")
    bb, bbb = f32t("bb")
    e1, be1 = f32t("e1")
    e2, be2 = f32t("e2")
    tm1, btm1 = f32t("tm1")
    At, bAt = f32t("At")
    Rt, bRt = f32t("Rt")
    Bt, bBt = f32t("Bt")
    Kt, bKt = f32t("Kt")
    Bh, bBh = f32t("Bh")
    Kh, bKh = f32t("Kh")
    FM, bFM_ = P.sb("FM", [128, 4, 4, 128], F32)
    bFM = [P.buf("FMp") for _ in range(4)]
    RP, bRP_ = P.sb("RP", [128, 4, 384], F32)
    bRP = [P.buf("RPp") for _ in range(4)]
    P.ms("vector", RP[:], 0.0, bRP)
    pc, bpc = P.sb("pc", [128, 4, 2], F32)
    ST, bST_ = P.sb("ST", [128, 4, 64], F32)
    bST = [P.buf("STh") for _ in range(8)]
    P.ms("vector", ST[:], 0.0, bST)
    Gs = [P.sb(f"Gs{i}", [128, 640], F32) for i in range(2)]
    Nb = [[P.sb(f"N{i}_{j}", [128, 128], F32) for j in range(2)] for i in range(2)]
    Lb = [[P.sb(f"L{i}_{j}", [128, 128], F32) for j in range(2)] for i in range(2)]
    Tb = [[P.sb(f"T{i}_{j}", [128, 128], F32) for j in range(2)] for i in range(2)]
    Zs = [P.sb(f"Zs{i}", [128, 64], F32) for i in range(2)]
    Us = [P.sb(f"Us{i}", [128, 64], F32) for i in range(2)]
    ysb, bysb = xt[:, 0:512], bxt
    yo, byo = P.sb("yo", [128, 512], BF16)

    pT, bpT = P.ps("pT", [128, 8, 128], BF16)
    pQ, bpQ = P.ps("pQ", [128, 512], F32)
    pV, bpV = P.ps("pV", [128, 512], F32)
    pF, bpF = P.ps("pF", [128, 512], F32)
    pF1, bpF1 = P.ps("pF1", [128, 512], F32)
    pG0, bpG0 = P.ps("pG0", [128, 512], F32)
    pG1, bpG1 = P.ps("pG1", [128, 512], F32)
    pY, bpY = P.ps("pY", [128, 512], F32)

    A1c = mcol[:, 0, :]
    B1c = mcol[:, 1, :]

    if DBG_STOP < 1:
        return
    for n in range(DBG_NT):
        tsl = slice(n * 128, (n + 1) * 128)
        hcur, bhcur = hT[n % 2]
        hprev, bhprev = hT[(n + 1) % 2]
        P.dma("sync", xt[:], x_d[tsl, :], [], [bxt], bxt)
        P.act(junk[:], xt[:], AF.Square, [bxt], [bjunk, bst8], accum_out=st8[:, 0:1])
        P.ts("vector", st8[:, 1:2], st8[:, 0:1], 1.0 / D, 1e-6, ALU.mult, ALU.add, [bst8], [bst8])
        P.act(st8[:, 2:3], st8[:, 1:2], AF.Ln, [bst8], [bst8])
        P.act(st8[:, 3:4], st8[:, 2:3], AF.Exp, [bst8], [bst8], scale=-0.5)
        P.ts("vector", xn[:], xt[:], st8[:, 3:4], None, ALU.mult, None, [bxt, bst8], [bxn])
        for k in range(8):
            P.tr(pT[:, k, :], xn[:, k * 128:(k + 1) * 128], idb[:], [bxn, bidb], [bpT])
        P.cp("vector", hcur[:, :, 0:1], hprev[:, :, 128:129], [bhprev], [bhcur])
        for k in range(8):
            P.act(hcur[:, k, 1:129], pT[:, k, :], AF.Identity, [bpT, bmcol], [bhcur],
                  bias=B1c[:, k:k + 1], scale=A1c[:, k:k + 1])
        hx = lambda k: hcur[:, k, 1:129]
        hs = lambda k: hcur[:, k, 0:128]
        for cq in range(8):
            for k in range(8):
                P.mm(pQ[:, 0:128], wda[:, k, cq * 128:(cq + 1) * 128], hx(k), [bwda, bhcur], [bpQ],
                     start=(k == 0), stop=(k == 7))
            P.cp("scalar", qf[:], pQ[:, 0:128], [bpQ], [bqf])
            P.mm(pQ[:, 128:256], con[:, O_PM:O_PM + 128], qf[:], [bcon, bqf], [bpQ])
            P.cp("scalar", t2[:], pQ[:, 128:256], [bpQ], [bt2])
            P.tt("vector", t1[:], qf[:], ctab[:, tsl], ALU.mult, [bqf, bctab], [bt1])
            P.tt("gpsimd", qf[:], t2[:], stab[:, tsl], ALU.mult, [bt2, bstab], [bqf])
            P.tt("vector", qko[:, cq, :], t1[:], qf[:], ALU.add, [bt1, bqf], [bqko])
        P.dma("sync", qk_d.rearrange("(c p) t -> p c t", p=128)[:, :, tsl], qko[:], [bqko], [b_qk], bqko)
        for k in range(8):
            P.mm(pV[:], hx(k), wda[:, k, 1024:1536], [bhcur, bwda], [bpV], start=(k == 0), stop=(k == 7))
        P.cp("scalar", vo[:, :, 0:128], pV[:].rearrange("p (h d) -> p h d", h=4), [bpV], [bvo])
        P.dma("sync", v_d[tsl, :], vo[:].rearrange("p h d -> p (h d)"), [bvo], [b_v], bvo)
        if DBG_STOP < 2:
            continue
        for cc, (dst, bdst) in enumerate(((r_s, br), (k_s, bk), (v_s, bv))):
            pp, bpp = pV, bpV
            for k in range(8):
                P.mm(pp[:], hx(k), w1[:, k, cc * 512:(cc + 1) * 512], [bhcur, bw1], [bpp], start=(k == 0), stop=False)
            for k in range(8):
                P.mm(pp[:], hs(k), w2m[:, k, cc * 512:(cc + 1) * 512], [bhcur, bw2m], [bpp], start=False, stop=(k == 7))
            P.cp("scalar", dst[:], pp[:], [bpp], [bdst])
        for lc in range(2):
            cs_ = slice(1536 + lc * 128, 1536 + (lc + 1) * 128)
            osl = pQ[:, 256:384]
            for k in range(8):
                P.mm(osl, w1[:, k, cs_], hx(k), [bw1, bhcur], [bpQ], start=(k == 0), stop=False)
            for k in range(8):
                P.mm(osl, w2m[:, k, cs_], hs(k), [bw2m, bhcur], [bpQ], start=False, stop=(k == 7))
            if lc == 0:
                P.act(lo0[0:64, :], pQ[0:64, 256:384], AF.Tanh, [bpQ], [blo0])
                P.cp("scalar", lo0[64:128, :], pQ[64:128, 256:384], [bpQ], [blo0])
            else:
                P.act(lo1[:], pQ[:, 256:384], AF.Sigmoid, [bpQ], [blo1])
        P.mm(pV[:], lo0[0:64, :], lw2[0:64, :], [blo0, blw2], [bpV])
        P.tt("vector", sig[:], pV[:], prm[:, 0, :], ALU.add, [bpV, bprm], [bsig])
        P.act(sig[:], sig[:], AF.Sigmoid, [bsig], [bsig])
        P.mm(pV[:], lo0[64:128, :], lw2[64:128, :], [blo0, blw2], [bpV])
        P.tt("vector", a_s[:], pV[:], prm[:, 1, :], ALU.add, [bpV, bprm], [ba])
        P.act(a_s[:], a_s[:], AF.Sigmoid, [ba], [ba])
        P.mm(pV[:], lo1[:], lg2[:], [blo1, blg2], [bpV])
        P.cp("scalar", g_s[:], pV[:], [bpV], [bg])
        P.tt(rr(), kk[:], k_s[:], prm[:, 2, :], ALU.mult, [bk, bprm], [bkk])
        P.tt(rr(), tm1[:], kk[:], kk[:], ALU.mult, [bkk], [btm1])
        P.op("vector", lambda e: e.tensor_reduce(out=st8[:, 0:8], in_=tm1[:].rearrange("p (h j) -> p h j", h=8),
                                                 axis=AX.X, op=ALU.add), [btm1], [bst8])
        P.ts("vector", st8[:, 0:8], st8[:, 0:8], 1e-24, None, ALU.max, None, [bst8], [bst8])
        P.act(st8[:, 0:8], st8[:, 0:8], AF.Ln, [bst8], [bst8])
        P.act(st8[:, 0:8], st8[:, 0:8], AF.Exp, [bst8], [bst8], scale=-0.5)
        P.tt("vector", kk[:].rearrange("p (h j) -> p h j", h=8), kk[:].rearrange("p (h j) -> p h j", h=8),
             st8[:, 0:8].unsqueeze(2).to_broadcast([128, 8, 64]), ALU.mult, [bkk, bst8], [bkk])
        P.stt(rr(), tm1[:], a_s[:], -1.0, prm[:, 3, :], ALU.add, ALU.mult, [ba, bprm], [btm1])
        P.stt(rr(), km[:], tm1[:], 1.0, k_s[:], ALU.add, ALU.mult, [btm1, bk], [bkm])
        P.tt(rr(), bb[:], kk[:], a_s[:], ALU.mult, [bkk, ba], [bbb])
        P.mm(pV[:], con[:, O_UI:O_UI + 128], sig[:], [bcon, bsig], [bpV])
        P.act(e1[:], pV[:], AF.Exp, [bpV], [be1], scale=-C0)
        P.act(e2[:], pV[:], AF.Exp, [bpV], [be2], scale=C0)
        P.tt(rr(), Rt[:], r_s[:], e1[:], ALU.mult, [br, be1], [bRt])
        P.tt(rr(), Bt[:], bb[:], e2[:], ALU.mult, [bbb, be2], [bBt])
        P.tt(rr(), Kt[:], km[:], e2[:], ALU.mult, [bkm, be2], [bKt])
        P.mm(pV[:], con[:, O_SU:O_SU + 128], sig[:], [bcon, bsig], [bpV])
        P.act(e1[:], pV[:], AF.Exp, [bpV], [be1], scale=-C0)
        P.stt(rr(), At[:], kk[:], -1.0, e1[:], ALU.mult, ALU.mult, [bkk, be1], [bAt])
        P.mm(pV[:], con[:, O_SL:O_SL + 128], sig[:], [bcon, bsig], [bpV])
        P.act(e2[:], pV[:], AF.Exp, [bpV], [be2], scale=-C0)
        P.tt(rr(), Bh[:], bb[:], e2[:], ALU.mult, [bbb, be2], [bBh])
        P.tt(rr(), Kh[:], km[:], e2[:], ALU.mult, [bkm, be2], [bKh])
        for pr in range(4):
            P.mm(pQ[:, 384 + pr * 2:384 + pr * 2 + 2], sig[:, pr * 128:(pr + 1) * 128], con[:, O_IND:O_IND + 2],
                 [bsig, bcon], [bpQ])
        P.act(pc[:].rearrange("p a c -> p (a c)"), pQ[:, 384:392], AF.Exp, [bpQ], [bpc], scale=-C0)
        P.tt(rr(), tm1[:], r_s[:], km[:], ALU.mult, [br, bkm], [btm1])
        P.tt(rr(), tm1[:], tm1[:], prm[:, 4, :], ALU.mult, [btm1, bprm], [btm1])
        P.op("vector", lambda e: e.tensor_reduce(out=st8[:, 0:8], in_=tm1[:].rearrange("p (h j) -> p h j", h=8),
                                                 axis=AX.X, op=ALU.add), [btm1], [bst8])
        if DBG_STOP < 3:
            continue
        for pr in range(4):
            psl = slice(pr * 128, (pr + 1) * 128)
            for ai, (arr, barr) in enumerate(((At, bAt), (Rt, bRt), (Bt, bBt), (Kt, bKt))):
                P.tr(pV[:, ai * 128:(ai + 1) * 128], arr[:, psl], identf, [barr, bcon], [bpV])
            if DBG_X != 1:
                P.cp("scalar", FM[:, pr, :, :], pV[:].rearrange("p (a t) -> p a t", a=4), [bpV], [bFM[pr]])
            P.cp("vector", RP[:, pr, 0:64], FM[:, pr, 1, 0:64], [bFM[pr]], [bRP[pr]])
            P.cp("vector", RP[:, pr, 192:256], FM[:, pr, 1, 64:128], [bFM[pr]], [bRP[pr]])
        if DBG_STOP < 4:
            continue
        for h in range(8):
            pr = h // 2
            ph = (h % 2) * 64
            hp = h % 2
            Gt, bGt = Gs[hp]
            A_ = FM[ph:ph + 64, pr, 0, :]
            R_ = FM[ph:ph + 64, pr, 1, :]
            B_ = FM[ph:ph + 64, pr, 2, :]
            K_ = FM[ph:ph + 64, pr, 3, :]
            bF = bFM[pr]
            P.mm(pG0[:, 0:128], B_, A_, [bF], [bpG0])
            P.mm(pG0[:, 128:256], K_, A_, [bF], [bpG0])
            P.mm(pG0[:, 256:384], B_, R_, [bF], [bpG0])
            P.mm(pG0[:, 384:512], K_, R_, [bF], [bpG0])
            P.mm(pG1[:, 0:128], A_, B_, [bF], [bpG1])
            P.tt("vector", Gt[:, 0:256], pG0[:, 0:256], con[:, O_M5:O_M5 + 256], ALU.mult, [bpG0, bcon], [bGt])
            P.tt("vector", Gt[:, 384:640], pG0[:, 256:512], con[:, O_M5 + 384:O_M5 + 640], ALU.mult, [bpG0, bcon], [bGt])
            P.tt("vector", Gt[:, 256:384], pG1[:, 0:128], con[:, O_M5 + 256:O_M5 + 384], ALU.mult, [bpG1, bcon], [bGt])
            if DBG_X == 11:
                continue
            Ncur, bNcur = Gt[:, 0:128], bGt
            Lcur, bLcur = Gt[:, 256:384], bGt
            Tcur, bTcur = Tb[hp][0]
            P.tt(rr(), Tcur[:], Gt[:, 0:128], identf, ALU.add, [bGt, bcon], [bTcur])
            Tcur = Tcur[:]
            for kx in range(1, 6):
                if DBG_X in (13, 14) and kx > 1:
                    break
                if DBG_X == 15 and kx > 2:
                    break
                Ln_, bLn = Lb[hp][kx % 2]
                i0 = 128 + (kx % 3) * 128
                P.mm(pG1[:, i0:i0 + 128], Ncur, Lcur, [bNcur, bLcur], [bpG1])
                P.cp("vector", Ln_[:], pG1[:, i0:i0 + 128], [bpG1], [bLn])
                if DBG_X == 13:
                    break
                if kx <= 4:
                    Nn_, bNn = Nb[hp][kx % 2]
                    i1 = 128 + ((kx + 1) % 3) * 128
                    P.mm(pG1[:, i1:i1 + 128], Lcur, Ncur, [bNcur, bLcur], [bpG1])
                    P.cp("vector", Nn_[:], pG1[:, i1:i1 + 128], [bpG1], [bNn])
                Tn_, bTn = Tb[hp][kx % 2]
                i2 = 128 + ((kx + 2) % 3) * 128
                P.mm(pG1[:, i2:i2 + 128], Ln_[:], Tcur, [bLn, bTcur], [bpG1])
                P.tt("vector", Tn_[:], pG1[:, i2:i2 + 128], Tcur, ALU.add, [bpG1, bTcur], [bTn])
                Lcur, bLcur = Ln_[:], bLn
                if kx <= 4:
                    Ncur, bNcur = Nn_[:], bNn
                Tcur, bTcur = Tn_[:], bTn
            if DBG_X == 12:
                continue
            S0 = ST[ph:ph + 64, pr, :]
            bS = bST[h]
            Zt, bZt = Zs[hp]
            Ut, bUt = Us[hp]
            hcol = slice(h * 64, (h + 1) * 64)
            for c in range(2):
                pv = c * 64
                pSb, sb0 = (pF, 0) if hp == 0 else (pF1, 0)
                zsl = pSb[:, sb0:sb0 + 64]
                usl = pSb[:, sb0 + 64:sb0 + 128]
                ssl = pSb[:, sb0 + 128:sb0 + 192]
                bz = bu = bs_ = (bpF if hp == 0 else bpF1)
                P.mm(zsl, A_, S0, [bF, bS], [bz], start=True, stop=False)
                P.mm(zsl, Gt[pv:pv + 64, 128:256], v_s[pv:pv + 64, hcol], [bGt, bv], [bz], start=False, stop=True)
                P.cp("vector", Zt[pv:pv + 64, :], zsl[pv:pv + 64, :], [bz], [bZt])
                P.mm(usl, Tcur[pv:pv + 64, :], Zt[pv:pv + 64, :], [bTcur, bZt], [bu])
                P.cp("vector", Ut[pv:pv + 64, :], usl[pv:pv + 64, :], [bu], [bUt])
                P.mm(pY[:, hcol], RP[ph:ph + 64, pr, c * 128:(c + 1) * 128], S0, [bRP[pr], bS], [bpY],
                     start=(c == 0), stop=False)
                P.mm(pY[:, hcol], Gt[pv:pv + 64, 512:640], v_s[pv:pv + 64, hcol], [bGt, bv], [bpY], start=False, stop=False)
                P.mm(pY[:, hcol], Gt[pv:pv + 64, 384:512], Ut[pv:pv + 64, :], [bGt, bUt], [bpY], start=False, stop=(c == 1))
                P.mm(ssl, Bh[pv:pv + 64, pr * 128:(pr + 1) * 128], Ut[pv:pv + 64, :], [bBh, bUt], [bs_], start=True, stop=False)
                P.mm(ssl, Kh[pv:pv + 64, pr * 128:(pr + 1) * 128], v_s[pv:pv + 64, hcol], [bKh, bv], [bs_], start=False, stop=True)
                P.stt("vector", S0, S0, pc[ph:ph + 64, pr, c:c + 1], ssl[ph:ph + 64, :], ALU.mult, ALU.add,
                      [bS, bpc, bs_], [bS])
        if DBG_STOP < 5:
            continue
        v3 = lambda ap: ap.rearrange("p (h j) -> p h j", h=8)
        P.cp("scalar", ysb[:], pY[:], [bpY], [bysb])
        P.op("vector", lambda e: e.tensor_reduce(out=t1[:, 0:8], in_=v3(ysb[:]), axis=AX.X, op=ALU.add), [bysb], [bt1])
        P.ts("vector", t1[:, 0:8], t1[:, 0:8], -1.0 / 64, None, ALU.mult, None, [bt1], [bt1])
        P.tt("vector", v3(ysb[:]), v3(ysb[:]), t1[:, 0:8].unsqueeze(2).to_broadcast([128, 8, 64]), ALU.add, [bysb, bt1], [bysb])
        P.tt(rr(), tm1[:], ysb[:], ysb[:], ALU.mult, [bysb], [btm1])
        P.op("vector", lambda e: e.tensor_reduce(out=t1[:, 8:16], in_=v3(tm1[:]), axis=AX.X, op=ALU.add), [btm1], [bt1])
        P.ts("vector", t1[:, 8:16], t1[:, 8:16], 1.0 / 64, 64e-5, ALU.mult, ALU.add, [bt1], [bt1])
        P.act(t1[:, 8:16], t1[:, 8:16], AF.Ln, [bt1], [bt1])
        P.act(t1[:, 8:16], t1[:, 8:16], AF.Exp, [bt1], [bt1], scale=-0.5)
        P.tt("vector", v3(ysb[:]), v3(ysb[:]), t1[:, 8:16].unsqueeze(2).to_broadcast([128, 8, 64]), ALU.mult, [bysb, bt1], [bysb])
        P.tt(rr(), ysb[:], ysb[:], prm[:, 5, :], ALU.mult, [bysb, bprm], [bysb])
        P.tt(rr(), ysb[:], ysb[:], prm[:, 6, :], ALU.add, [bysb, bprm], [bysb])
        P.tt("vector", v3(tm1[:]), v3(v_s[:]), st8[:, 0:8].unsqueeze(2).to_broadcast([128, 8, 64]), ALU.mult, [bv, bst8], [btm1])
        P.tt(rr(), ysb[:], ysb[:], tm1[:], ALU.add, [bysb, btm1], [bysb])
        P.tt("vector", yo[:], ysb[:], g_s[:], ALU.mult, [bysb, bg], [byo])
        P.dma("sync", yrw_d[tsl, :], yo[:], [byo], [b_yrw], byo)


def phase_attn(P, nc, G):
    P.noself = NOSELF_ATTN
    con_d, modp_d, lamv_d, subln_d, wout_d, rtw_d, rtb_d = (G[k] for k in
        ("con_d", "modp_d", "lamv_d", "subln_d", "wout_d", "rtw_d", "rtb_d"))
    x_d, qk_d, v_d, yrw_d, x1_d, h2T_d, gat_d = (G[k] for k in ("x_d", "qk_d", "v_d", "yrw_d", "x1_d", "h2T_d", "gat_d"))
    b_modp, b_qk, b_v, b_yrw, b_x1, b_h2T, b_gat = (G[k] for k in
        ("b_modp", "b_qk", "b_v", "b_yrw", "b_x1", "b_h2T", "b_gat"))
    rr = _rr(P)
    con, bcon = P.sb("con", [128, NCONST], F32)
    P.dma("sync", con[:], con_d, [], [bcon], bcon)
    identf = con[:, O_ID:O_ID + 128]
    idb, bidb = P.sb("idb", [128, 128], BF16)
    P.cp("vector", idb[:], identf, [bcon], [bidb])
    cmk, bcmk = P.sb("cmk", [128, 4, 512], BF16)
    P.cp("vector", cmk[:].rearrange("p a t -> p (a t)"), con[:, O_CM:O_CM + 2048], [bcon], [bcmk])
    rows, brows = P.sb("rows", [128, 3, D], F32)
    P.dma("sync", rows[:].rearrange("p a d -> p (a d)"),
          modp_d[2:5, :].rearrange("(o a) d -> o (a d)", o=1).partition_broadcast(128), [b_modp], [brows], brows)
    mcol, bmcol = P.sb("mcol", [128, 6, 8], F32)
    P.dma("sync", mcol[:], modp_d.rearrange("a (k p) -> p a k", p=128), [b_modp], [bmcol], bmcol,
          allow_slow_non_contiguous=True)
    lv, blv = P.sb("lv", [128, 4, 64], F32)
    P.dma("sync", lv[:].rearrange("p a d -> p (a d)"),
          lamv_d.rearrange("(o a) d -> o (a d)", o=1).partition_broadcast(128), [], [blv], blv)
    lam, blam = P.sb("lam", [128, 8], F32)
    lt, blt = P.sb("lt", [128, 2, 64], F32)
    P.tt("vector", lt[:, 0, :], lv[:, 0, :], lv[:, 1, :], ALU.mult, [blv], [blt])
    P.tt("vector", lt[:, 1, :], lv[:, 2, :], lv[:, 3, :], ALU.mult, [blv], [blt])
    P.op("vector", lambda e: e.tensor_reduce(out=lam[:, 0:2], in_=lt[:], axis=AX.X, op=ALU.add), [blt], [blam])
    P.act(lam[:, 2:4], lam[:, 0:2], AF.Exp, [blam], [blam])
    P.tt("vector", lam[:, 4:5], lam[:, 2:3], lam[:, 3:4], ALU.subtract, [blam], [blam])
    P.ts("vector", lam[:, 5:6], lam[:, 4:5], -1.0, -LAMBDA_INIT, ALU.mult, ALU.add, [blam], [blam])
    sub, bsub = P.sb("sub", [128, 128], F32)
    P.dma("sync", sub[:], subln_d.partition_broadcast(128), [], [bsub], bsub)
    P.ts("vector", sub[:], sub[:], 1.0 - LAMBDA_INIT, None, ALU.mult, None, [bsub], [bsub])
    wo, bwo = P.sb("wo", [128, 8, D], BF16)
    P.dma("gpsimd", wo[:], wout_d.rearrange("(k p) c -> p k c", p=128), [], [bwo], bwo)
    rw, brw = P.sb("rw", [128, 8, NE], BF16)
    P.dma("gpsimd", rw[:], rtw_d.rearrange("(k p) c -> p k c", p=128), [], [brw], brw)
    rb, brb = P.sb("rb", [128, NE], F32)
    P.dma("sync", rb[:], rtb_d.partition_broadcast(128), [], [brb], brb)
    kT, bkT = P.sb("kT", [128, 4, T], BF16)
    P.dma("sync", kT[:], qk_d[512:1024, :].rearrange("(c p) t -> p c t", p=128), [b_qk], [bkT], bkT)
    vv, bvv = P.sb("vv", [128, NT, 4 * 129], BF16)
    P.dma("sync", vv[:], v_d.rearrange("(n p) f -> p n f", p=128), [b_v], [bvv], bvv)
    qT = [P.sb(f"qT{i}", [128, 4, 512], BF16) for i in range(2)]
    pt = [P.sb(f"pt{i}", [128, 512], BF16) for i in range(3)]
    psc = [P.ps(f"psc{i}", [128, 512], F32) for i in range(2)]
    po = [P.ps(f"po{i}", [128, 4, 128], F32) for i in range(2)]
    pms, bpms_ = P.ps("pms", [128, 512], F32)
    pos_ = pms[:, 0:128].rearrange("p (a b c) -> p a b c", a=2, b=4)
    bpos_ = P.buf("possum")
    pw, bpw = P.ps("pw", [128, D], F32)
    ptr, bptr = P.ps("ptr", [128, 8, 128], BF16)
    prt = pms[:, 128:256]
    bprt = bpos_
    YC, bYC_ = P.sb("YC", [128, 4, D], BF16)
    bYC = [P.buf("YCs") for _ in range(4)]
    ycT, bycT = P.sb("ycT", [128, 8, 128], BF16)
    o0, bo0 = P.sb("o0", [128, 128], F32)
    o1, bo1 = P.sb("o1", [128, 128], F32)
    rs, brs = P.sb("rs", [128, 16], F32)
    xt, bxt = P.sb("xt", [128, D], F32)
    y1, by1 = P.sb("y1", [128, D], F32)
    junk, bjunk = P.sb("junk", [128, D], BF16)
    h2, bh2 = P.sb("h2", [128, D], BF16)
    h2T, bh2T = P.sb("h2T", [128, 8, 128], BF16)
    lg, blg = P.sb("lg", [128, NE], F32)
    gt, bgt = P.sb("gt", [128, NE], F32)
    t8, bt8 = P.sb("t8", [128, 16], F32)
    yrt, byrt = P.sb("yrt", [128, 512], BF16)

    it = 0
    for qb in range(8):
        qcur, bqcur = qT[qb % 2]
        P.dma("sync", qcur[:], qk_d[0:512, qb * 512:(qb + 1) * 512].rearrange("(c p) t -> p c t", p=128),
              [b_qk], [bqcur], bqcur)
        ntk = (qb + 1) * 4
        for hd in range(4):
            for mp in range(2):
                m = hd * 2 + mp
                chn, pb = m // 2, (m % 2) * 64
                pot, bpot = po[mp]
                for tk in range(ntk):
                    ps_, bps_ = psc[it % 2]
                    ptile, bptile = pt[it % 3]
                    it += 1
                    P.mm(ps_[:], kT[pb:pb + 64, chn, tk * 128:(tk + 1) * 128], qcur[pb:pb + 64, chn, :],
                         [bkT, bqcur], [bps_])
                    P.act(ptile[:], ps_[:], AF.Exp, [bps_], [bptile], scale=0.125)
                    j = tk - qb * 4
                    if j >= 0:
                        P.tt("vector", ptile[:], ptile[:], cmk[:, j, :], ALU.mult, [bptile, bcmk], [bptile])
                    for s4 in range(4):
                        if j > s4:
                            continue
                        P.mm(pot[:, s4, :], ptile[:, s4 * 128:(s4 + 1) * 128], vv[:, tk, hd * 129:hd * 129 + 128],
                             [bptile, bvv], [bpot], start=(tk == 0 and s4 == 0), stop=(tk == ntk - 1 and s4 == 3))
                        P.mm(pos_[:, mp, s4, 0:1], ptile[:, s4 * 128:(s4 + 1) * 128], vv[:, tk, hd * 129 + 128:hd * 129 + 129],
                             [bptile, bvv], [bpos_], start=(mp == 0 and tk == 0 and s4 == 0),
                             stop=(mp == 1 and tk == ntk - 1 and s4 == 3))
            P.cp("vector", rs[:, 0:8].rearrange("p (a b) -> p a b", a=2), pos_[:, :, :, 0], [bpos_], [brs])
            P.op("vector", lambda e: e.reciprocal(out=rs[:, 8:16], in_=rs[:, 0:8]), [brs], [brs])
            P.ts("vector", rs[:, 12:16], rs[:, 12:16], lam[:, 5:6], None, ALU.mult, None, [brs, blam], [brs])
            for s4 in range(4):
                P.ts("vector", o0[:], po[0][0][:, s4, :], rs[:, 8 + s4:9 + s4], None, ALU.mult, None, [po[0][1], brs], [bo0])
                P.stt("vector", o0[:], po[1][0][:, s4, :], rs[:, 12 + s4:13 + s4], o0[:], ALU.mult, ALU.add,
                      [po[1][1], brs, bo0], [bo0])
                P.act(o1[:], o0[:], AF.Square, [bo0], [bo1, bt8], accum_out=t8[:, 0:1])
                P.ts("vector", t8[:, 1:2], t8[:, 0:1], 1.0 / 128, 1e-5, ALU.mult, ALU.add, [bt8], [bt8])
                P.act(t8[:, 2:3], t8[:, 1:2], AF.Ln, [bt8], [bt8])
                P.act(t8[:, 3:4], t8[:, 2:3], AF.Exp, [bt8], [bt8], scale=-0.5)
                P.stt("vector", YC[:, s4, hd * 128:(hd + 1) * 128], o0[:], t8[:, 3:4], sub[:], ALU.mult, ALU.mult,
                      [bo0, bt8, bsub], [bYC[s4]])
        for s4 in range(4):
            n = qb * 4 + s4
            tsl = slice(n * 128, (n + 1) * 128)
            P.dma("sync", yrt[:], yrw_d[tsl, :], [b_yrw], [byrt], byrt)
            P.cp("vector", YC[:, s4, 512:1024], yrt[:], [byrt], [bYC[s4]])
            for k in range(8):
                P.tr(ptr[:, k, :], YC[:, s4, k * 128:(k + 1) * 128], idb[:], [bYC[s4], bidb], [bptr])
            P.cp("scalar", ycT[:].rearrange("p k t -> p (k t)"), ptr[:].rearrange("p k t -> p (k t)"), [bptr], [bycT])
            for hf in range(2):
                for k in range(8):
                    P.mm(pw[:, hf * 512:(hf + 1) * 512], ycT[:, k, :], wo[:, k, hf * 512:(hf + 1) * 512],
                         [bycT, bwo], [bpw], start=(k == 0), stop=(k == 7))
            P.cp("scalar", y1[:], pw[:], [bpw], [by1])
            P.act(junk[:], y1[:], AF.Square, [by1], [bjunk, bt8], accum_out=t8[:, 4:5])
            P.ts("vector", t8[:, 5:6], t8[:, 4:5], 1.0 / D, 1e-6, ALU.mult, ALU.add, [bt8], [bt8])
            P.act(t8[:, 6:7], t8[:, 5:6], AF.Ln, [bt8], [bt8])
            P.act(t8[:, 7:8], t8[:, 6:7], AF.Exp, [bt8], [bt8], scale=-0.5)
            P.dma("sync", xt[:], x_d[tsl, :], [], [bxt], bxt)
            P.stt("vector", y1[:], y1[:], t8[:, 7:8], rows[:, 0, :], ALU.mult, ALU.mult, [by1, bt8, brows], [by1])
            P.tt("gpsimd", xt[:], xt[:], y1[:], ALU.add, [bxt, by1], [bxt])
            P.dma("sync", x1_d[tsl, :], xt[:], [bxt], [b_x1], bxt)
            P.act(junk[:], xt[:], AF.Square, [bxt], [bjunk, bt8], accum_out=t8[:, 8:9])
            P.ts("vector", t8[:, 9:10], t8[:, 8:9], 1.0 / D, 1e-6, ALU.mult, ALU.add, [bt8], [bt8])
            P.act(t8[:, 10:11], t8[:, 9:10], AF.Ln, [bt8], [bt8])
            P.act(t8[:, 11:12], t8[:, 10:11], AF.Exp, [bt8], [bt8], scale=-0.5)
            P.ts("vector", h2[:], xt[:], t8[:, 11:12], None, ALU.mult, None, [bxt, bt8], [bh2])
            for k in range(8):
                P.tr(ptr[:, k, :], h2[:, k * 128:(k + 1) * 128], idb[:], [bh2, bidb], [bptr])
            for k in range(8):
                P.act(h2T[:, k, :], ptr[:, k, :], AF.Identity, [bptr, bmcol], [bh2T],
                      bias=mcol[:, 4, k:k + 1], scale=mcol[:, 3, k:k + 1])
            P.dma("sync", h2T_d.rearrange("(k p) t -> p k t", p=128)[:, :, tsl], h2T[:], [bh2T], [b_h2T], bh2T)
            for k in range(8):
                P.mm(prt[:, 0:NE], h2T[:, k, :], rw[:, k, :], [bh2T, brw], [bprt], start=(k == 0), stop=(k == 7))
            P.tt("vector", lg[:], prt[:, 0:NE], rb[:], ALU.add, [bprt, brb], [blg])
            P.op("vector", lambda e: e.max(out=t8[:, 0:8], in_=lg[:]), [blg], [bt8])
            P.ts("vector", gt[:], lg[:], t8[:, 3:4], None, ALU.is_ge, None, [blg, bt8], [bgt])
            P.ts("vector", t8[:, 12:13], t8[:, 0:1], -1.0, None, ALU.mult, None, [bt8], [bt8])
            P.act(lg[:], lg[:], AF.Exp, [blg, bt8], [blg], bias=t8[:, 12:13], scale=1.0)
            P.tt("vector", gt[:], gt[:], lg[:], ALU.mult, [bgt, blg], [bgt])
            P.op("vector", lambda e: e.tensor_reduce(out=t8[:, 13:14], in_=gt[:], axis=AX.X, op=ALU.add), [bgt], [bt8])
            P.op("vector", lambda e: e.reciprocal(out=t8[:, 14:15], in_=t8[:, 13:14]), [bt8], [bt8])
            P.ts("vector", gt[:], gt[:], t8[:, 14:15], None, ALU.mult, None, [bgt, bt8], [bgt])
            P.dma("sync", gat_d[tsl, :], gt[:], [bgt], [b_gat], bgt)


def phase_moe(P, nc, G):
    P.noself = NOSELF_MOE
    modp_d, x1_d, h2T_d, gat_d, out_d = (G[k] for k in ("modp_d", "x1_d", "h2T_d", "gat_d", "out_d"))
    w1g_d, w1l_d, w2e_d, b1T_d, b2_d, con_d = (G[k] for k in ("w1g_d", "w1l_d", "w2e_d", "b1T_d", "b2_d", "con_d"))
    b_modp, b_x1, b_h2T, b_gat, b_out = (G[k] for k in ("b_modp", "b_x1", "b_h2T", "b_gat", "b_out"))
    idf, bidf = P.sb("idf", [128, 128], F32)
    P.dma("sync", idf[:], con_d[:, O_ID:O_ID + 128], [], [bidf], bidf)
    c2r, bc2r = P.sb("c2r", [128, D], F32)
    P.dma("sync", c2r[:], modp_d[5:6, :].partition_broadcast(128), [b_modp], [bc2r], bc2r)
    b1T, bb1T = P.sb("b1T", [128, NE, 16], F32)
    P.dma("sync", b1T[:].rearrange("p e c -> p (e c)"), b1T_d, [], [bb1T], bb1T)
    b2s, bb2s = P.sb("b2s", [NE, D], F32)
    P.dma("sync", b2s[:], b2_d, [], [bb2s], bb2s)
    W = [[P.sb(f"w{j}_{i}", [128, 8, D], BF16) for j in range(3)] for i in range(2)]
    hq, bhq = P.sb("hq", [128, 8, 1024], BF16)
    gq, bgq = P.sb("gq", [128, 8, NE], F32)
    gT, bgT = P.sb("gT", [NE, 128], F32)
    acc, bacc_ = P.sb("acc", [128, 8, D], F32)
    bacc = [P.buf("acct") for _ in range(8)]
    actT, bactT_ = P.sb("actT", [128, 8, 512], BF16)
    bactT = [P.buf("actc") for _ in range(8)]
    GG = [P.sb(f"mg{i}", [128, 512], F32) for i in range(2)]
    SS = [P.sb(f"msg{i}", [128, 512], F32) for i in range(2)]
    LL = [P.sb(f"ml{i}", [128, 512], F32) for i in range(2)]
    b1p, bb1p = P.sb("b1p", [128, NE, 8], F32)
    P.ts("vector", b1p[:], b1T[:, :, 8:16], 1.0, None, ALU.add, None, [bb1T], [bb1p])
    xt, bxt = P.sb("mxt", [128, D], F32)
    junk, bjunk = P.sb("mjunk", [128, D], BF16)
    t8, bt8 = P.sb("mt8", [128, 8], F32)
    pg = [P.ps(f"pg{i}", [128, 512], F32) for i in range(2)]
    pl = [P.ps(f"pl{i}", [128, 512], F32) for i in range(2)]
    po = [P.ps(f"pmo{i}", [128, 512], F32) for i in range(2)]
    pm, bpm = P.ps("pmisc", [128, 512], F32)
    it = 0
    io = 0
    wi = 0
    for qt in range(DBG_NQ):
        q0 = qt * 1024
        P.dma("sync", hq[:], h2T_d.rearrange("(k p) t -> p k t", p=128)[:, :, q0:q0 + 1024], [b_h2T], [bhq], bhq)
        P.dma("sync", gq[:], gat_d[q0:q0 + 1024, :].rearrange("(n p) e -> p n e", p=128), [b_gat], [bgq], bgq)
        for n in range(8):
            P.tr(pm[0:NE, 0:128], gq[:, n, :], idf[:], [bgq, bidf], [bpm])
            P.cp("vector", gT[:], pm[0:NE, 0:128], [bpm], [bgT])
            for hf in range(2):
                pp, bpp = po[io % 2]
                io += 1
                P.mm(pp[:], gT[:], b2s[:, hf * 512:(hf + 1) * 512], [bgT, bb2s], [bpp])
                P.cp("scalar", acc[:, n, hf * 512:(hf + 1) * 512], pp[:], [bpp], [bacc[n]])
        for e in range(DBG_NEXP):
            (w1g, bw1g), (w1l, bw1l), (w2, bw2) = W[wi % 2]
            wi += 1
            P.dma("gpsimd", w1g[:], w1g_d[e].rearrange("(k p) f -> p k f", p=128), [], [bw1g], bw1g)
            P.dma("gpsimd", w1l[:], w1l_d[e].rearrange("(k p) f -> p k f", p=128), [], [bw1l], bw1l)
            P.dma("gpsimd", w2[:], w2e_d[e].rearrange("(k p) f -> p k f", p=128), [], [bw2], bw2)
            for blk in range(2):
                bsl = slice(blk * 512, (blk + 1) * 512)
                for fc in range(8):
                    pgt, bpgt = pg[it % 2]
                    plt, bplt = pl[it % 2]
                    it += 1
                    for k in range(8):
                        P.mm(pgt[:], w1g[:, k, fc * 128:(fc + 1) * 128], hq[:, k, bsl], [bw1g, bhq], [bpgt],
                             start=(k == 0), stop=(k == 7))
                    for k in range(8):
                        P.mm(plt[:], w1l[:, k, fc * 128:(fc + 1) * 128], hq[:, k, bsl], [bw1l, bhq], [bplt],
                             start=(k == 0), stop=(k == 7))
                    gi, bgi = GG[it % 2]
                    si, bsi = SS[it % 2]
                    li, bli = LL[it % 2]
                    P.ts("vector", gi[:], pgt[:], b1T[:, e, fc:fc + 1], 7.0, ALU.add, ALU.min, [bpgt, bb1T], [bgi])
                    P.act(si[:], gi[:], AF.Sigmoid, [bgi], [bsi], scale=1.702)
                    P.act(li[:], plt[:], AF.Identity, [bplt, bb1p], [bli], bias=b1p[:, e, fc:fc + 1], scale=1.0)
                    P.ts("vector", li[:], li[:], -6.0, 8.0, ALU.max, ALU.min, [bli], [bli])
                    P.tt("gpsimd", si[:], si[:], gi[:], ALU.mult, [bsi, bgi], [bsi])
                    P.tt("vector", actT[:, fc, :], si[:], li[:], ALU.mult, [bsi, bli], [bactT[fc]])
                for tt_ in range(4):
                    n = blk * 4 + tt_
                    for hf in range(2):
                        pp, bpp = po[io % 2]
                        io += 1
                        for fc in range(8):
                            P.mm(pp[:], actT[:, fc, tt_ * 128:(tt_ + 1) * 128], w2[:, fc, hf * 512:(hf + 1) * 512],
                                 [bactT[fc], bw2], [bpp], start=(fc == 0), stop=(fc == 7))
                        P.stt("vector", acc[:, n, hf * 512:(hf + 1) * 512], pp[:], gq[:, n, e:e + 1],
                              acc[:, n, hf * 512:(hf + 1) * 512], ALU.mult, ALU.add, [bpp, bgq, bacc[n]], [bacc[n]])
        for n in range(8):
            tsl = slice(q0 + n * 128, q0 + (n + 1) * 128)
            P.dma("sync", xt[:], x1_d[tsl, :], [b_x1], [bxt], bxt)
            P.act(junk[:], acc[:, n, :], AF.Square, [bacc[n]], [bjunk, bt8], accum_out=t8[:, 0:1])
            P.ts("vector", t8[:, 1:2], t8[:, 0:1], 1.0 / D, 1e-6, ALU.mult, ALU.add, [bt8], [bt8])
            P.act(t8[:, 2:3], t8[:, 1:2], AF.Ln, [bt8], [bt8])
            P.act(t8[:, 3:4], t8[:, 2:3], AF.Exp, [bt8], [bt8], scale=-0.5)
            P.stt("vector", acc[:, n, :], acc[:, n, :], t8[:, 3:4], c2r[:], ALU.mult, ALU.mult, [bacc[n], bt8, bc2r], [bacc[n]])
            P.tt("vector", xt[:], xt[:], acc[:, n, :], ALU.add, [bxt, bacc[n]], [bxt])
            P.dma("sync", out_d[tsl, :], xt[:], [bxt], [b_out], bxt)


def _consts():
    c = np.zeros((128, NCONST), np.float32)
    r = np.arange(128)[:, None]
    q = np.arange(128)[None, :]
    same = (r // 64) == (q // 64)
    su = (same & ((r % 64) < (q % 64))).astype(np.float32)
    sl = (same & ((r % 64) > (q % 64))).astype(np.float32)
    ui = (same & ((r % 64) <= (q % 64))).astype(np.float32)
    c[:, O_ID:O_ID + 128] = np.eye(128, dtype=np.float32)
    for i, m in enumerate((su, su, sl, ui, ui)):
        c[:, O_M5 + i * 128:O_M5 + (i + 1) * 128] = m
    c[:, O_ONES:O_ONES + 128] = same.astype(np.float32)
    c[:64, O_IND] = 1.0
    c[64:, O_IND + 1] = 1.0
    inv_freq = (500000.0 ** (-np.arange(0, 16, 2, dtype=np.float32) / 16)).astype(np.float32)
    for p in range(128):
        d = p % 64
        if d < 16:
            c[p, O_FREQ] = inv_freq[d % 8]
            c[p, O_SIGN] = -1.0 if d < 8 else 1.0
            pp = p + 8 if d < 8 else p - 8
            c[pp, O_PM + p] = 1.0
    tq = np.arange(512)[None, :]
    tk = np.arange(128)[:, None]
    for j in range(4):
        c[:, O_CM + j * 512:O_CM + (j + 1) * 512] = ((j * 128 + tk) <= tq).astype(np.float32)
    return c


_NC_CACHE = {}


def _in_maps(inp):
    f = lambda a: np.ascontiguousarray(np.asarray(a, dtype=np.float32))
    B = 8
    w1 = np.asarray(inp["moe_w1"])[0]
    w1g = np.ascontiguousarray(w1[:, :, 0::2])
    w1l = np.ascontiguousarray(w1[:, :, 1::2])
    b1 = np.asarray(inp["moe_b1"])[0]
    b1cat = np.concatenate([b1[:, 0::2].reshape(NE, 8, 128), b1[:, 1::2].reshape(NE, 8, 128)], axis=1)
    b1T = np.ascontiguousarray(b1cat.transpose(2, 0, 1).reshape(128, NE * 16)).astype(np.float32)
    shared = dict(
        ada_w=f(inp["ada_w"][0]), ada_b=f(inp["ada_b"]).reshape(1, 6 * D),
        norms=f(np.stack([inp["pre_mix_norm"][0], inp["post_mix_norm"][0], inp["pre_ffn_norm"][0], inp["post_ffn_norm"][0]])),
        w_in=f(inp["w_in"][0]), w_out=f(inp["w_out"][0]),
        lamv=f(np.stack([inp["da_lambda_q1"][0], inp["da_lambda_k1"][0], inp["da_lambda_q2"][0], inp["da_lambda_k2"][0]])),
        subln=f(inp["da_subln"]).reshape(1, 128), mu=f(inp["rw_mu"]).reshape(1, 1792),
        rwv=f(np.stack([inp["rw_w0"][0], inp["rw_a0"][0], inp["rw_k_k"][0], inp["rw_k_a"][0],
                        np.asarray(inp["rw_r_k"])[0].reshape(512), inp["rw_ln_w"][0], inp["rw_ln_b"][0]])),
        rw_w2=f(inp["rw_w2"][0]), rw_a2=f(inp["rw_a2"][0]), rw_g2=f(inp["rw_g2"][0]),
        router_w=f(inp["router_w"][0]), router_b=f(inp["router_b"]).reshape(1, NE),
        w1g=w1g, w1l=w1l, b1T=b1T, w2e=f(inp["moe_w2"][0]), b2=f(inp["moe_b2"][0]),
        consts=_consts(),
    )
    x = np.asarray(inp["x"], dtype=np.float32)
    c = np.asarray(inp["c"], dtype=np.float32)
    pos = np.asarray(inp["positions"]).astype(np.int32)
    in_maps = []
    for b in range(B):
        m = dict(shared)
        m["x"] = np.ascontiguousarray(x[b])
        m["cT"] = np.ascontiguousarray(c[b].reshape(8, 128).T)
        m["pos"] = np.ascontiguousarray(pos[b].reshape(1, T))
        in_maps.append(m)
    return in_maps


def kernel(**inp):
    B = 8
    if "nc" not in _NC_CACHE:
        _NC_CACHE["nc"] = build_program()
    nc = _NC_CACHE["nc"]
    in_maps = _in_maps(inp)
    res = run_bass_kernel_spmd(nc, in_maps, core_ids=list(range(B)))
    return np.stack([np.asarray(r["out"], dtype=np.float32) for r in res.results], axis=0)
```

```python
import contextlib
import math
import numpy as np
import concourse.bass as bass
import concourse.mybir as mybir
from concourse.bass_utils import run_bass_kernel_spmd

ALU = mybir.AluOpType
AF = mybir.ActivationFunctionType
F32 = mybir.dt.float32
BF16 = mybir.dt.bfloat16
I32 = mybir.dt.int32
AX = mybir.AxisListType

D = 1024
T = 4096
NT = 32
NE = 32
C0 = math.exp(-0.5)
LAMBDA_INIT = 0.8 - 0.6 * math.exp(0.0)

O_ID = 0
O_M5 = 128
O_UI = O_M5 + 384
O_SU = O_M5
O_SL = O_M5 + 256
O_ONES = 768
O_IND = 896
O_FREQ = 898
O_SIGN = 899
O_PM = 900
O_CM = 1028
NCONST = O_CM + 2048
DBG_STOP = 99
NOSELF_MOE = ('tensor',)
NOSELF_ATTN = ('tensor',)
NOSELF_FRONT = ('tensor',)
DBG_X = 0
DBG_NQ = 4
DBG_NEXP = NE
DBG_NT = NT


class Buf:
    __slots__ = ("name", "w", "r", "dsem", "dcount")

    def __init__(self, name):
        self.name = name
        self.w = {}
        self.r = {}
        self.dsem = None
        self.dcount = 0


class Eng:
    def __init__(self, name, sem):
        self.name = name
        self.sem = sem
        self.count = 0
        self.waited = {}
        self.thunks = []


class Prog:
    def __init__(self, nc, stack):
        self.nc = nc
        self.gstack = stack
        self.stack = stack
        self.engs = {}
        self.sems = {}
        self.vals = {}
        for n in ("tensor", "vector", "scalar", "gpsimd", "sync"):
            sem = stack.enter_context(nc.semaphore("es_" + n))
            self.engs[n] = Eng(n, sem)
            self.sems[("e", n)] = sem
            self.vals[("e", n)] = 0
        self.nbuf = 0
        self.ninstr = 0
        self.allbufs = []
        self.gen = 0
        self.noself = ()
        self.last_f32 = False

    def buf(self, name=None):
        self.nbuf += 1
        b = Buf(f"{name or 'b'}{self.nbuf}")
        self.allbufs.append(b)
        return b

    def new_engine_sems(self):
        self.gen += 1
        for n, eng in self.engs.items():
            old = ("e", n)
            self.vals.pop(old, None)
            sem = self.gstack.enter_context(self.nc.semaphore(f"es{self.gen}_{n}"))
            eng.sem = sem
            eng.count = 0
            eng.waited = {}
            self.sems[old] = sem
            self.vals[old] = 0
        for b in self.allbufs:
            b.w = {}
            b.r = {}

    def sb(self, name, shape, dtype):
        self.nbuf += 1
        t = self.stack.enter_context(self.nc.sbuf_tensor(f"sb{self.nbuf}_{name}", list(shape), dtype))
        return t, self.buf(name)

    def ps(self, name, shape, dtype=F32):
        self.nbuf += 1
        t = self.stack.enter_context(self.nc.psum_tensor(f"ps{self.nbuf}_{name}", list(shape), dtype))
        return t, self.buf(name)

    def _dsem(self, b):
        if b.dsem is None:
            s = self.gstack.enter_context(self.nc.semaphore("ds_" + b.name))
            b.dsem = ("d", b.name)
            self.sems[b.dsem] = s
            self.vals[b.dsem] = 0
        return b.dsem

    def _collect(self, eng, reads, writes):
        need = {}
        for b in reads:
            for k, v in b.w.items():
                if need.get(k, 0) < v:
                    need[k] = v
        for b in writes:
            for k, v in b.w.items():
                if need.get(k, 0) < v:
                    need[k] = v
            for k, v in b.r.items():
                if need.get(k, 0) < v:
                    need[k] = v
        own = ("e", eng.name)
        for k, v in need.items():
            if k == own and eng.name in self.noself:
                continue
            if eng.waited.get(k, 0) < v:
                eng.waited[k] = v
                sem = self.sems[k]
                eng.thunks.append(lambda e, sem=sem, v=v: e.wait_ge(sem, v))

    def op(self, engname, fn, reads=(), writes=(), selfwait=False):
        eng = self.engs[engname]
        self._collect(eng, reads, writes)
        if selfwait and eng.count > 0:
            own = ("e", engname)
            if eng.waited.get(own, 0) < eng.count:
                eng.waited[own] = eng.count
                eng.thunks.append(lambda e, sem=eng.sem, v=eng.count: e.wait_ge(sem, v))
        eng.count += 1
        c = eng.count
        sem = eng.sem
        eng.thunks.append(lambda e, fn=fn, sem=sem: fn(e).then_inc(sem, 1))
        key = ("e", engname)
        self.vals[key] = c
        for b in reads:
            b.r[key] = c
        for b in writes:
            b.w = {key: c}
            b.r = {}
        self.ninstr += 1

    def dma(self, q, out_ap, in_ap, reads, writes, sbuf_buf, **kw):
        eng = self.engs[q]
        self._collect(eng, reads, writes)
        key = self._dsem(sbuf_buf)
        sbuf_buf.dcount += 16
        c = sbuf_buf.dcount
        self.vals[key] = c
        sem = self.sems[key]
        eng.thunks.append(
            lambda e, o=out_ap, i=in_ap, sem=sem, kw=kw: e.dma_start(out=o, in_=i, **kw).then_inc(sem, 16))
        for b in reads:
            b.r[key] = c
        for b in writes:
            if b is sbuf_buf:
                b.w = {key: c}
                b.r = {}
            else:
                b.w[key] = c
        self.ninstr += 1

    def barrier(self):
        for eng in self.engs.values():
            for k, v in self.vals.items():
                if v > 0 and eng.waited.get(k, 0) < v:
                    eng.waited[k] = v
                    sem = self.sems[k]
                    eng.thunks.append(lambda e, sem=sem, v=v: e.wait_ge(sem, v))

    def flush(self):
        nc = self.nc
        engs = self.engs
        with nc.Block() as block:
            @block.tensor
            def _(e):
                for t in engs["tensor"].thunks:
                    t(e)

            @block.vector
            def _(e):
                for t in engs["vector"].thunks:
                    t(e)

            @block.scalar
            def _(e):
                for t in engs["scalar"].thunks:
                    t(e)

            @block.gpsimd
            def _(e):
                for t in engs["gpsimd"].thunks:
                    t(e)

            @block.sync
            def _(e):
                for t in engs["sync"].thunks:
                    t(e)
        for e in engs.values():
            e.thunks = []

    @contextlib.contextmanager
    def phase(self):
        with contextlib.ExitStack() as ph:
            self.stack = ph
            yield
            self.barrier()
            self.flush()
        self.stack = self.gstack
        self.new_engine_sems()

    def mm(self, out, lhsT, rhs, R, W, start=True, stop=True, f32=False):
        sw = f32 or self.last_f32
        self.last_f32 = f32
        self.op("tensor", lambda e: e.matmul(out, lhsT=lhsT, rhs=rhs, start=start, stop=stop), R, W, selfwait=sw)

    def tr(self, out, in_, ident, R, W, f32=False):
        sw = f32 or self.last_f32
        self.last_f32 = f32
        self.op("tensor", lambda e: e.transpose(out, in_, ident), R, W, selfwait=sw)

    def tt(self, eng, out, in0, in1, op, R, W):
        self.op(eng, lambda e: e.tensor_tensor(out=out, in0=in0, in1=in1, op=op), R, W)

    def ts(self, eng, out, in0, s1, s2, op0, op1, R, W):
        if s2 is None:
            self.op(eng, lambda e: e.tensor_scalar(out=out, in0=in0, scalar1=s1, scalar2=None, op0=op0), R, W)
        else:
            self.op(eng, lambda e: e.tensor_scalar(out=out, in0=in0, scalar1=s1, scalar2=s2, op0=op0, op1=op1), R, W)

    def stt(self, eng, out, in0, scalar, in1, op0, op1, R, W):
        eng = "vector"
        self.op(eng, lambda e: e.scalar_tensor_tensor(out=out, in0=in0, scalar=scalar, in1=in1, op0=op0, op1=op1), R, W)

    def act(self, out, in_, func, R, W, bias=None, scale=None, accum_out=None):
        kw = {}
        if bias is not None:
            kw["bias"] = bias
        if scale is not None:
            kw["scale"] = scale
        if accum_out is not None:
            kw["accum_out"] = accum_out
        self.op("scalar", lambda e: e.activation(out=out, in_=in_, func=func, **kw), R, W)

    def cp(self, eng, out, in_, R, W):
        if eng == "scalar":
            self.op("scalar", lambda e: e.copy(out=out, in_=in_), R, W)
        else:
            self.op(eng, lambda e: e.tensor_copy(out=out, in_=in_), R, W)

    def ms(self, eng, ap, val, W):
        self.op(eng, lambda e: e.memset(ap, val), [], W)


def _rr(P):
    state = {"i": 0}

    def nxt():
        state["i"] += 1
        return "vector" if state["i"] % 3 else "gpsimd"
    return nxt


def build_program(debug=False, upto=3):
    nc = bass.Bass("TRN2", target_bir_lowering=False)
    skind = "ExternalOutput" if debug else "Internal"

    def din(name, shape, dt=F32):
        return nc.dram_tensor(name, list(shape), dt, kind="ExternalInput").ap()

    x_d = din("x", [T, D])
    cT_d = din("cT", [128, 8])
    pos_d = din("pos", [1, T], I32)
    adaw_d = din("ada_w", [D, 6 * D])
    adab_d = din("ada_b", [1, 6 * D])
    norms_d = din("norms", [4, D])
    win_d = din("w_in", [D, 3328])
    wout_d = din("w_out", [D, D])
    lamv_d = din("lamv", [4, 64])
    subln_d = din("subln", [1, 128])
    mu_d = din("mu", [1, 1792])
    rwv_d = din("rwv", [7, 512])
    w2_d = din("rw_w2", [64, 512])
    a2_d = din("rw_a2", [64, 512])
    g2_d = din("rw_g2", [128, 512])
    rtw_d = din("router_w", [D, NE])
    rtb_d = din("router_b", [1, NE])
    w1g_d = din("w1g", [NE, D, D]) if upto >= 3 else None
    w1l_d = din("w1l", [NE, D, D]) if upto >= 3 else None
    b1T_d = din("b1T", [128, NE * 16])
    w2e_d = din("w2e", [NE, D, D]) if upto >= 3 else None
    b2_d = din("b2", [NE, D])
    con_d = din("consts", [128, NCONST])
    out_d = nc.dram_tensor("out", [T, D], F32, kind="ExternalOutput").ap()

    modp_d = nc.dram_tensor("modp", [6, D], F32, kind=skind).ap()
    qk_d = nc.dram_tensor("qk_s", [D, T], BF16, kind=skind).ap()
    v_d = nc.dram_tensor("v_s", [T, 4 * 129], BF16, kind=skind).ap()
    yrw_d = nc.dram_tensor("yrw_s", [T, 512], BF16, kind=skind).ap()
    x1_d = nc.dram_tensor("x1_s", [T, D], F32, kind=skind).ap()
    h2T_d = nc.dram_tensor("h2T_s", [D, T], BF16, kind=skind).ap()
    gat_d = nc.dram_tensor("gat_s", [T, NE], F32, kind=skind).ap()

    with contextlib.ExitStack() as gst:
        P = Prog(nc, gst)
        b_modp = P.buf("modp")
        b_qk = P.buf("qkd")
        b_v = P.buf("vd")
        b_yrw = P.buf("yrwd")
        b_x1 = P.buf("x1d")
        b_h2T = P.buf("h2Td")
        b_gat = P.buf("gatd")
        b_out = P.buf("outd")

        with P.phase():
            cT, bcT = P.sb("cT", [128, 8], F32)
            sc, bsc = P.sb("sc", [128, 8], F32)
            P.dma("sync", cT[:], cT_d, [], [bcT], bcT)
            P.act(sc[:], cT[:], AF.Silu, [bcT], [bsc])
            aw = [P.sb(f"aw{i}", [128, 3072], F32) for i in range(2)]
            pm, bpm = P.ps("pmod", [128, 3072], F32)
            mrow, bmrow = P.sb("mrow", [1, 6 * D], F32)
            brow, bbrow = P.sb("brow", [1, 6 * D], F32)
            nrm, bnrm = P.sb("nrm", [1, 4 * D], F32)
            orow, borow = P.sb("orow", [1, 6 * D], F32)
            P.dma("sync", brow[:], adab_d, [], [bbrow], bbrow)
            P.dma("sync", nrm[:], norms_d.rearrange("(o a) d -> o (a d)", o=1), [], [bnrm], bnrm)
            i = 0
            for half in range(2):
                for k in range(8):
                    t_, b_ = aw[i % 2]
                    i += 1
                    P.dma("sync", t_[:], adaw_d[k * 128:(k + 1) * 128, half * 3072:(half + 1) * 3072], [], [b_], b_)
                    for j in range(6):
                        P.mm(pm[0:1, j * 512:(j + 1) * 512], sc[:, k:k + 1], t_[:, j * 512:(j + 1) * 512],
                             [bsc, b_], [bpm], start=(k == 0), stop=(k == 7))
                P.tt("vector", mrow[:, half * 3072:(half + 1) * 3072], pm[0:1, :], brow[:, half * 3072:(half + 1) * 3072],
                     ALU.add, [bpm, bbrow], [bmrow])

            def mseg(i_):
                return mrow[:, i_ * D:(i_ + 1) * D]

            def nseg(i_):
                return nrm[:, i_ * D:(i_ + 1) * D]
            P.stt("vector", orow[:, 0:D], mseg(1), 1.0, nseg(0), ALU.add, ALU.mult, [bmrow, bnrm], [borow])
            P.cp("vector", orow[:, D:2 * D], mseg(0), [bmrow], [borow])
            P.tt("vector", orow[:, 2 * D:3 * D], mseg(2), nseg(1), ALU.mult, [bmrow, bnrm], [borow])
            P.stt("vector", orow[:, 3 * D:4 * D], mseg(4), 1.0, nseg(2), ALU.add, ALU.mult, [bmrow, bnrm], [borow])
            P.cp("vector", orow[:, 4 * D:5 * D], mseg(3), [bmrow], [borow])
            P.tt("vector", orow[:, 5 * D:6 * D], mseg(5), nseg(3), ALU.mult, [bmrow, bnrm], [borow])
            P.dma("sync", modp_d.rearrange("(o a) d -> o (a d)", o=1), orow[:], [borow], [b_modp], borow)

        if upto >= 1:
          with P.phase():
            phase_front(P, nc, locals())

        if upto >= 2:
          with P.phase():
            phase_attn(P, nc, locals())

        if upto >= 3:
          with P.phase():
            phase_moe(P, nc, locals())
    return nc


def phase_front(P, nc, G):
    P.noself = NOSELF_FRONT
    x_d, pos_d, win_d, mu_d, rwv_d = G["x_d"], G["pos_d"], G["win_d"], G["mu_d"], G["rwv_d"]
    w2_d, a2_d, g2_d, con_d, modp_d = G["w2_d"], G["a2_d"], G["g2_d"], G["con_d"], G["modp_d"]
    qk_d, v_d, yrw_d = G["qk_d"], G["v_d"], G["yrw_d"]
    b_modp, b_qk, b_v, b_yrw = G["b_modp"], G["b_qk"], G["b_v"], G["b_yrw"]
    rr = _rr(P)

    con, bcon = P.sb("con", [128, NCONST], F32)
    P.dma("sync", con[:], con_d, [], [bcon], bcon)
    identf = con[:, O_ID:O_ID + 128]
    idb, bidb = P.sb("idb", [128, 128], BF16)
    P.cp("vector", idb[:], identf, [bcon], [bidb])

    mcol, bmcol = P.sb("mcol", [128, 6, 8], F32)
    P.dma("sync", mcol[:], modp_d.rearrange("a (k p) -> p a k", p=128), [b_modp], [bmcol], bmcol,
          allow_slow_non_contiguous=True)

    wda, bwda = P.sb("wda", [128, 8, 1536], BF16)
    P.dma("gpsimd", wda[:], win_d[:, 0:1536].rearrange("(k p) c -> p k c", p=128), [], [bwda], bwda)
    w1, bw1 = P.sb("w1", [128, 8, 1792], BF16)
    w2m, bw2m = P.sb("w2m", [128, 8, 1792], BF16)
    prm, bprm = P.sb("prm", [128, 7, 512], F32)
    P.dma("sync", prm[:].rearrange("p a d -> p (a d)"),
          rwv_d.rearrange("(o a) d -> o (a d)", o=1).partition_broadcast(128), [], [bprm], bprm)
    lw2, blw2 = P.sb("lw2", [128, 512], BF16)
    lg2, blg2 = P.sb("lg2", [128, 512], BF16)
    P.dma("gpsimd", lw2[0:64, :], w2_d, [], [blw2], blw2)
    P.dma("gpsimd", lw2[64:128, :], a2_d, [], [blw2], blw2)
    P.dma("gpsimd", lg2[:], g2_d, [], [blg2], blg2)
    pmb, bpmb = P.sb("pmb", [128, 128], BF16)
    P.cp("vector", pmb[:], con[:, O_PM:O_PM + 128], [bcon], [bpmb])

    ctab, bctab = P.sb("ctab", [128, T], BF16)
    stab, bstab = P.sb("stab", [128, T], BF16)
    with contextlib.ExitStack() as tmp:
        old = P.stack
        P.stack = tmp
        mub, bmub = P.sb("mub", [128, 1792], F32)
        omu, bomu = P.sb("omu", [128, 1792], F32)
        P.dma("sync", mub[:], mu_d.partition_broadcast(128), [], [bmub], bmub)
        P.ts("vector", omu[:], mub[:], -1.0, 1.0, ALU.mult, ALU.add, [bmub], [bomu])
        wst = [P.sb(f"wst{i}", [128, 1792], F32) for i in range(2)]
        for k in range(8):
            t_, b_ = wst[k % 2]
            P.dma("sync", t_[:], win_d[k * 128:(k + 1) * 128, 1536:3328], [], [b_], b_)
            P.tt("vector", w1[:, k, :], t_[:], omu[:], ALU.mult, [b_, bomu], [bw1])
            P.tt("gpsimd", w2m[:, k, :], t_[:], mub[:], ALU.mult, [b_, bmub], [bw2m])
        posi, bposi = P.sb("posi", [128, 1024], I32)
        ang, bang = P.sb("ang", [128, 1024], F32)
        y_, by_ = P.sb("ry", [128, 1024], F32)
        kf, bkf = P.sb("rkf", [128, 1024], F32)
        ki, bki = P.sb("rki", [128, 1024], I32)
        for q4 in range(4):
            sl = slice(q4 * 1024, (q4 + 1) * 1024)
            P.dma("sync", posi[:], pos_d[:, sl].partition_broadcast(128), [], [bposi], bposi)
            P.cp("vector", ang[:], posi[:], [bposi], [bang])
            P.ts("vector", ang[:], ang[:], con[:, O_FREQ:O_FREQ + 1], None, ALU.mult, None, [bang, bcon], [bang])
            for which, shift in ((0, math.pi * 1.5), (1, math.pi)):
                P.ts("vector", y_[:], ang[:], shift, None, ALU.add, None, [bang], [by_])
                P.ts("vector", kf[:], y_[:], 1.0 / (2 * math.pi), None, ALU.mult, None, [by_], [bkf])
                P.cp("vector", ki[:], kf[:], [bkf], [bki])
                P.cp("vector", kf[:], ki[:], [bki], [bkf])
                P.stt("vector", y_[:], kf[:], -2 * math.pi, y_[:], ALU.mult, ALU.add, [bkf, by_], [by_])
                P.ts("vector", kf[:], y_[:], 0.0, 2 * math.pi, ALU.is_lt, ALU.mult, [by_], [bkf])
                P.tt("vector", y_[:], y_[:], kf[:], ALU.add, [by_, bkf], [by_])
                P.ts("vector", y_[:], y_[:], -math.pi, None, ALU.add, None, [by_], [by_])
                P.ts("vector", y_[:], y_[:], -math.pi, math.pi, ALU.max, ALU.min, [by_], [by_])
                if which == 0:
                    P.act(ctab[:, sl], y_[:], AF.Sin, [by_], [bctab])
                else:
                    P.act(kf[:], y_[:], AF.Sin, [by_], [bkf])
                    P.ts("vector", stab[:, sl], kf[:], con[:, O_SIGN:O_SIGN + 1], None, ALU.mult, None, [bkf, bcon], [bstab])
        P.barrier()
        P.flush()
        P.stack = old

    def f32t(name, w=512):
        return P.sb(name, [128, w], F32)

    xt, bxt = P.sb("xt", [128, D], F32)
    xn, bxn = P.sb("xn", [128, D], BF16)
    junk, bjunk = xn, bxn
    st8, bst8 = P.sb("st8", [128, 8], F32)
    hT = [P.sb(f"hT{i}", [128, 8, 129], BF16) for i in range(2)]
    P.ms("vector", hT[1][0][:, :, 128:129], 0.0, [hT[1][1]])
    qf, bqf = P.sb("qf", [128, 128], BF16)
    qf2, bqf2 = P.sb("qf2", [128, 128], F32)
    t1, bt1 = P.sb("t1", [128, 128], F32)
    t2, bt2 = P.sb("t2", [128, 128], F32)
    qko, bqko = P.sb("qko", [128, 8, 128], BF16)
    vo, bvo = P.sb("vo", [128, 4, 129], BF16)
    P.ms("vector", vo[:, :, 128:129], 1.0, [bvo])
    r_s, br = f32t("r_s")
    k_s, bk = f32t("k_s")
    v_s, bv = f32t("v_s")
    lo0, blo0 = P.sb("lo0", [128, 128], BF16)
    lo1, blo1 = P.sb("lo1", [128, 128], BF16)
    v_b, bvb = P.sb("v_b", [128, 512], BF16)
    STb, bSTb_ = P.sb("STb", [128, 4, 64], BF16)
    P.ms("vector", STb[:], 0.0, [bSTb_])
    sig, bsig = f32t("sig")
    a_s, ba = f32t("a_s")
    g_s, bg = f32t("g_s")
    kk, bkk = f32t("kk")
    km, bkm = f32t("km")
    bb, bbb = f32t("bb")
    e1, be1 = f32t("e1")
    e2, be2 = f32t("e2")
    tm1, btm1 = f32t("tm1")
    At, bAt = f32t("At")
    Rt, bRt = f32t("Rt")
    Bt, bBt = f32t("Bt")
    Kt, bKt = f32t("Kt")
    Bh, bBh = P.sb("Bh", [128, 512], BF16)
    Kh, bKh = P.sb("Kh", [128, 512], BF16)
    FM, bFM_ = P.sb("FM", [128, 4, 4, 128], BF16)
    bFM = [P.buf("FMp") for _ in range(4)]
    RP, bRP_ = P.sb("RP", [128, 4, 384], BF16)
    bRP = [P.buf("RPp") for _ in range(4)]
    P.ms("vector", RP[:], 0.0, bRP)
    pc, bpc = P.sb("pc", [128, 4, 2], F32)
    ST, bST_ = P.sb("ST", [128, 4, 64], F32)
    bST = [P.buf("STh") for _ in range(8)]
    P.ms("vector", ST[:], 0.0, bST)
    Gs = [P.sb(f"Gs{i}", [128, 640], BF16) for i in range(2)]
    Nb = [[P.sb(f"N{i}_{j}", [128, 128], BF16) for j in range(2)] for i in range(2)]
    Lb = [[P.sb(f"L{i}_{j}", [128, 128], BF16) for j in range(2)] for i in range(2)]
    Tb = [[P.sb(f"T{i}_{j}", [128, 128], BF16) for j in range(2)] for i in range(2)]
    Zs = [P.sb(f"Zs{i}", [128, 64], BF16) for i in range(2)]
    Us = [P.sb(f"Us{i}", [128, 64], BF16) for i in range(2)]
    ysb, bysb = xt[:, 0:512], bxt
    yo, byo = P.sb("yo", [128, 512], BF16)

    pT, bpT = P.ps("pT", [128, 8, 128], BF16)
    pQ, bpQ = P.ps("pQ", [128, 512], F32)
    pV, bpV = P.ps("pV", [128, 512], F32)
    pF, bpF = P.ps("pF", [128, 512], F32)
    pF1, bpF1 = P.ps("pF1", [128, 512], F32)
    pG0, bpG0 = P.ps("pG0", [128, 512], F32)
    pG1, bpG1 = P.ps("pG1", [128, 512], F32)
    pY, bpY = P.ps("pY", [128, 512], F32)

    A1c = mcol[:, 0, :]
    B1c = mcol[:, 1, :]

    if DBG_STOP < 1:
        return
    for n in range(DBG_NT):
        tsl = slice(n * 128, (n + 1) * 128)
        hcur, bhcur = hT[n % 2]
        hprev, bhprev = hT[(n + 1) % 2]
        P.dma("sync", xt[:], x_d[tsl, :], [], [bxt], bxt)
        P.act(junk[:], xt[:], AF.Square, [bxt], [bjunk, bst8], accum_out=st8[:, 0:1])
        P.ts("vector", st8[:, 1:2], st8[:, 0:1], 1.0 / D, 1e-6, ALU.mult, ALU.add, [bst8], [bst8])
        P.act(st8[:, 2:3], st8[:, 1:2], AF.Ln, [bst8], [bst8])
        P.act(st8[:, 3:4], st8[:, 2:3], AF.Exp, [bst8], [bst8], scale=-0.5)
        P.ts("vector", xn[:], xt[:], st8[:, 3:4], None, ALU.mult, None, [bxt, bst8], [bxn])
        for k in range(8):
            P.tr(pT[:, k, :], xn[:, k * 128:(k + 1) * 128], idb[:], [bxn, bidb], [bpT])
        P.cp("vector", hcur[:, :, 0:1], hprev[:, :, 128:129], [bhprev], [bhcur])
        for k in range(8):
            P.act(hcur[:, k, 1:129], pT[:, k, :], AF.Identity, [bpT, bmcol], [bhcur],
                  bias=B1c[:, k:k + 1], scale=A1c[:, k:k + 1])
        hx = lambda k: hcur[:, k, 1:129]
        hs = lambda k: hcur[:, k, 0:128]
        for cq in range(8):
            for k in range(8):
                P.mm(pQ[:, 0:128], wda[:, k, cq * 128:(cq + 1) * 128], hx(k), [bwda, bhcur], [bpQ],
                     start=(k == 0), stop=(k == 7))
            P.cp("scalar", qf[:], pQ[:, 0:128], [bpQ], [bqf])
            P.mm(pQ[:, 128:256], pmb[:], qf[:], [bpmb, bqf], [bpQ])
            P.cp("scalar", t2[:], pQ[:, 128:256], [bpQ], [bt2])
            P.tt("vector", t1[:], qf[:], ctab[:, tsl], ALU.mult, [bqf, bctab], [bt1])
            P.tt("gpsimd", qf2[:], t2[:], stab[:, tsl], ALU.mult, [bt2, bstab], [bqf2])
            P.tt("vector", qko[:, cq, :], t1[:], qf2[:], ALU.add, [bt1, bqf2], [bqko])
        P.dma("sync", qk_d.rearrange("(c p) t -> p c t", p=128)[:, :, tsl], qko[:], [bqko], [b_qk], bqko)
        for k in range(8):
            P.mm(pV[:], hx(k), wda[:, k, 1024:1536], [bhcur, bwda], [bpV], start=(k == 0), stop=(k == 7))
        P.cp("scalar", vo[:, :, 0:128], pV[:].rearrange("p (h d) -> p h d", h=4), [bpV], [bvo])
        P.dma("sync", v_d[tsl, :], vo[:].rearrange("p h d -> p (h d)"), [bvo], [b_v], bvo)
        if DBG_STOP < 2:
            continue
        for cc, (dst, bdst) in enumerate(((r_s, br), (k_s, bk), (v_s, bv))):
            pp, bpp = pV, bpV
            for k in range(8):
                P.mm(pp[:], hx(k), w1[:, k, cc * 512:(cc + 1) * 512], [bhcur, bw1], [bpp], start=(k == 0), stop=False)
            for k in range(8):
                P.mm(pp[:], hs(k), w2m[:, k, cc * 512:(cc + 1) * 512], [bhcur, bw2m], [bpp], start=False, stop=(k == 7))
            P.cp("scalar", dst[:], pp[:], [bpp], [bdst])
            if cc == 2:
                P.cp("gpsimd", v_b[:], v_s[:], [bv], [bvb])
        for lc in range(2):
            cs_ = slice(1536 + lc * 128, 1536 + (lc + 1) * 128)
            osl = pQ[:, 256:384]
            for k in range(8):
                P.mm(osl, w1[:, k, cs_], hx(k), [bw1, bhcur], [bpQ], start=(k == 0), stop=False)
            for k in range(8):
                P.mm(osl, w2m[:, k, cs_], hs(k), [bw2m, bhcur], [bpQ], start=False, stop=(k == 7))
            if lc == 0:
                P.act(lo0[0:64, :], pQ[0:64, 256:384], AF.Tanh, [bpQ], [blo0])
                P.cp("scalar", lo0[64:128, :], pQ[64:128, 256:384], [bpQ], [blo0])
            else:
                P.act(lo1[:], pQ[:, 256:384], AF.Sigmoid, [bpQ], [blo1])
        P.mm(pV[:], lo0[0:64, :], lw2[0:64, :], [blo0, blw2], [bpV])
        P.tt("vector", sig[:], pV[:], prm[:, 0, :], ALU.add, [bpV, bprm], [bsig])
        P.act(sig[:], sig[:], AF.Sigmoid, [bsig], [bsig])
        P.mm(pV[:], lo0[64:128, :], lw2[64:128, :], [blo0, blw2], [bpV])
        P.tt("vector", a_s[:], pV[:], prm[:, 1, :], ALU.add, [bpV, bprm], [ba])
        P.act(a_s[:], a_s[:], AF.Sigmoid, [ba], [ba])
        P.mm(pV[:], lo1[:], lg2[:], [blo1, blg2], [bpV])
        P.cp("scalar", g_s[:], pV[:], [bpV], [bg])
        P.tt(rr(), kk[:], k_s[:], prm[:, 2, :], ALU.mult, [bk, bprm], [bkk])
        P.tt(rr(), tm1[:], kk[:], kk[:], ALU.mult, [bkk], [btm1])
        P.op("vector", lambda e: e.tensor_reduce(out=st8[:, 0:8], in_=tm1[:].rearrange("p (h j) -> p h j", h=8),
                                                 axis=AX.X, op=ALU.add), [btm1], [bst8])
        P.ts("vector", st8[:, 0:8], st8[:, 0:8], 1e-24, None, ALU.max, None, [bst8], [bst8])
        P.act(st8[:, 0:8], st8[:, 0:8], AF.Ln, [bst8], [bst8])
        P.act(st8[:, 0:8], st8[:, 0:8], AF.Exp, [bst8], [bst8], scale=-0.5)
        P.tt("vector", kk[:].rearrange("p (h j) -> p h j", h=8), kk[:].rearrange("p (h j) -> p h j", h=8),
             st8[:, 0:8].unsqueeze(2).to_broadcast([128, 8, 64]), ALU.mult, [bkk, bst8], [bkk])
        P.stt(rr(), tm1[:], a_s[:], -1.0, prm[:, 3, :], ALU.add, ALU.mult, [ba, bprm], [btm1])
        P.stt(rr(), km[:], tm1[:], 1.0, k_s[:], ALU.add, ALU.mult, [btm1, bk], [bkm])
        P.tt(rr(), bb[:], kk[:], a_s[:], ALU.mult, [bkk, ba], [bbb])
        P.mm(pV[:], con[:, O_UI:O_UI + 128], sig[:], [bcon, bsig], [bpV], f32=True)
        P.act(e1[:], pV[:], AF.Exp, [bpV], [be1], scale=-C0)
        P.act(e2[:], pV[:], AF.Exp, [bpV], [be2], scale=C0)
        P.tt(rr(), Rt[:], r_s[:], e1[:], ALU.mult, [br, be1], [bRt])
        P.tt(rr(), Bt[:], bb[:], e2[:], ALU.mult, [bbb, be2], [bBt])
        P.tt(rr(), Kt[:], km[:], e2[:], ALU.mult, [bkm, be2], [bKt])
        P.mm(pV[:], con[:, O_SU:O_SU + 128], sig[:], [bcon, bsig], [bpV], f32=True)
        P.act(e1[:], pV[:], AF.Exp, [bpV], [be1], scale=-C0)
        P.stt(rr(), At[:], kk[:], -1.0, e1[:], ALU.mult, ALU.mult, [bkk, be1], [bAt])
        P.mm(pV[:], con[:, O_SL:O_SL + 128], sig[:], [bcon, bsig], [bpV], f32=True)
        P.act(e2[:], pV[:], AF.Exp, [bpV], [be2], scale=-C0)
        P.tt(rr(), Bh[:], bb[:], e2[:], ALU.mult, [bbb, be2], [bBh])
        P.tt(rr(), Kh[:], km[:], e2[:], ALU.mult, [bkm, be2], [bKh])
        for pr in range(4):
            P.mm(pQ[:, 384 + pr * 2:384 + pr * 2 + 2], sig[:, pr * 128:(pr + 1) * 128], con[:, O_IND:O_IND + 2],
                 [bsig, bcon], [bpQ], f32=True)
        P.act(pc[:].rearrange("p a c -> p (a c)"), pQ[:, 384:392], AF.Exp, [bpQ], [bpc], scale=-C0)
        P.tt(rr(), tm1[:], r_s[:], km[:], ALU.mult, [br, bkm], [btm1])
        P.tt(rr(), tm1[:], tm1[:], prm[:, 4, :], ALU.mult, [btm1, bprm], [btm1])
        P.op("vector", lambda e: e.tensor_reduce(out=st8[:, 0:8], in_=tm1[:].rearrange("p (h j) -> p h j", h=8),
                                                 axis=AX.X, op=ALU.add), [btm1], [bst8])
        if DBG_STOP < 3:
            continue
        for pr in range(4):
            psl = slice(pr * 128, (pr + 1) * 128)
            for ai, (arr, barr) in enumerate(((At, bAt), (Rt, bRt), (Bt, bBt), (Kt, bKt))):
                P.tr(pV[:, ai * 128:(ai + 1) * 128], arr[:, psl], identf, [barr, bcon], [bpV], f32=True)
            if DBG_X != 1:
                P.cp("scalar", FM[:, pr, :, :], pV[:].rearrange("p (a t) -> p a t", a=4), [bpV], [bFM[pr]])
            P.cp("vector", RP[:, pr, 0:64], FM[:, pr, 1, 0:64], [bFM[pr]], [bRP[pr]])
            P.cp("vector", RP[:, pr, 192:256], FM[:, pr, 1, 64:128], [bFM[pr]], [bRP[pr]])
        if DBG_STOP < 4:
            continue
        for h in range(8):
            pr = h // 2
            ph = (h % 2) * 64
            hp = h % 2
            Gt, bGt = Gs[hp]
            A_ = FM[ph:ph + 64, pr, 0, :]
            R_ = FM[ph:ph + 64, pr, 1, :]
            B_ = FM[ph:ph + 64, pr, 2, :]
            K_ = FM[ph:ph + 64, pr, 3, :]
            bF = bFM[pr]
            P.mm(pG0[:, 0:128], B_, A_, [bF], [bpG0])
            P.mm(pG0[:, 128:256], K_, A_, [bF], [bpG0])
            P.mm(pG0[:, 256:384], B_, R_, [bF], [bpG0])
            P.mm(pG0[:, 384:512], K_, R_, [bF], [bpG0])
            P.mm(pG1[:, 0:128], A_, B_, [bF], [bpG1])
            P.tt("vector", Gt[:, 0:256], pG0[:, 0:256], con[:, O_M5:O_M5 + 256], ALU.mult, [bpG0, bcon], [bGt])
            P.tt("vector", Gt[:, 384:640], pG0[:, 256:512], con[:, O_M5 + 384:O_M5 + 640], ALU.mult, [bpG0, bcon], [bGt])
            P.tt("vector", Gt[:, 256:384], pG1[:, 0:128], con[:, O_M5 + 256:O_M5 + 384], ALU.mult, [bpG1, bcon], [bGt])
            if DBG_X == 11:
                continue
            Ncur, bNcur = Gt[:, 0:128], bGt
            Lcur, bLcur = Gt[:, 256:384], bGt
            Tcur, bTcur = Tb[hp][0]
            P.tt(rr(), Tcur[:], Gt[:, 0:128], identf, ALU.add, [bGt, bcon], [bTcur])
            Tcur = Tcur[:]
            for kx in range(1, 6):
                if DBG_X in (13, 14) and kx > 1:
                    break
                if DBG_X == 15 and kx > 2:
                    break
                Ln_, bLn = Lb[hp][kx % 2]
                i0 = 128 + (kx % 3) * 128
                P.mm(pG1[:, i0:i0 + 128], Ncur, Lcur, [bNcur, bLcur], [bpG1])
                P.cp("vector", Ln_[:], pG1[:, i0:i0 + 128], [bpG1], [bLn])
                if DBG_X == 13:
                    break
                if kx <= 4:
                    Nn_, bNn = Nb[hp][kx % 2]
                    i1 = 128 + ((kx + 1) % 3) * 128
                    P.mm(pG1[:, i1:i1 + 128], Lcur, Ncur, [bNcur, bLcur], [bpG1])
                    P.cp("vector", Nn_[:], pG1[:, i1:i1 + 128], [bpG1], [bNn])
                Tn_, bTn = Tb[hp][kx % 2]
                i2 = 128 + ((kx + 2) % 3) * 128
                P.mm(pG1[:, i2:i2 + 128], Ln_[:], Tcur, [bLn, bTcur], [bpG1])
                P.tt("vector", Tn_[:], pG1[:, i2:i2 + 128], Tcur, ALU.add, [bpG1, bTcur], [bTn])
                Lcur, bLcur = Ln_[:], bLn
                if kx <= 4:
                    Ncur, bNcur = Nn_[:], bNn
                Tcur, bTcur = Tn_[:], bTn
            if DBG_X == 12:
                continue
            S0 = ST[ph:ph + 64, pr, :]
            S0b = STb[ph:ph + 64, pr, :]
            bS = bST[h]
            Zt, bZt = Zs[hp]
            Ut, bUt = Us[hp]
            hcol = slice(h * 64, (h + 1) * 64)
            for c in range(2):
                pv = c * 64
                pSb, sb0 = (pF, 0) if hp == 0 else (pF1, 0)
                zsl = pSb[:, sb0:sb0 + 64]
                usl = pSb[:, sb0 + 64:sb0 + 128]
                ssl = pSb[:, sb0 + 128:sb0 + 192]
                bz = bu = bs_ = (bpF if hp == 0 else bpF1)
                P.mm(zsl, A_, S0b, [bF, bS], [bz], start=True, stop=False, f32=True)
                P.mm(zsl, Gt[pv:pv + 64, 128:256], v_b[pv:pv + 64, hcol], [bGt, bvb], [bz], start=False, stop=True, f32=True)
                P.cp("vector", Zt[pv:pv + 64, :], zsl[pv:pv + 64, :], [bz], [bZt])
                P.mm(usl, Tcur[pv:pv + 64, :], Zt[pv:pv + 64, :], [bTcur, bZt], [bu], f32=True)
                P.cp("vector", Ut[pv:pv + 64, :], usl[pv:pv + 64, :], [bu], [bUt])
                P.mm(pY[:, hcol], RP[ph:ph + 64, pr, c * 128:(c + 1) * 128], S0b, [bRP[pr], bS], [bpY],
                     start=(c == 0), stop=False, f32=True)
                P.mm(pY[:, hcol], Gt[pv:pv + 64, 512:640], v_b[pv:pv + 64, hcol], [bGt, bvb], [bpY], start=False, stop=False, f32=True)
                P.mm(pY[:, hcol], Gt[pv:pv + 64, 384:512], Ut[pv:pv + 64, :], [bGt, bUt], [bpY], start=False, stop=(c == 1), f32=True)
                P.mm(ssl, Bh[pv:pv + 64, pr * 128:(pr + 1) * 128], Ut[pv:pv + 64, :], [bBh, bUt], [bs_], start=True, stop=False, f32=True)
                P.mm(ssl, Kh[pv:pv + 64, pr * 128:(pr + 1) * 128], v_b[pv:pv + 64, hcol], [bKh, bvb], [bs_], start=False, stop=True, f32=True)
                P.stt("vector", S0, S0, pc[ph:ph + 64, pr, c:c + 1], ssl[ph:ph + 64, :], ALU.mult, ALU.add,
                      [bS, bpc, bs_], [bS])
                P.cp("gpsimd", S0b, S0, [bS], [bS])
        if DBG_STOP < 5:
            continue
        v3 = lambda ap: ap.rearrange("p (h j) -> p h j", h=8)
        P.cp("scalar", ysb[:], pY[:], [bpY], [bysb])
        P.op("vector", lambda e: e.tensor_reduce(out=t1[:, 0:8], in_=v3(ysb[:]), axis=AX.X, op=ALU.add), [bysb], [bt1])
        P.ts("vector", t1[:, 0:8], t1[:, 0:8], -1.0 / 64, None, ALU.mult, None, [bt1], [bt1])
        P.tt("vector", v3(ysb[:]), v3(ysb[:]), t1[:, 0:8].unsqueeze(2).to_broadcast([128, 8, 64]), ALU.add, [bysb, bt1], [bysb])
        P.tt(rr(), tm1[:], ysb[:], ysb[:], ALU.mult, [bysb], [btm1])
        P.op("vector", lambda e: e.tensor_reduce(out=t1[:, 8:16], in_=v3(tm1[:]), axis=AX.X, op=ALU.add), [btm1], [bt1])
        P.ts("vector", t1[:, 8:16], t1[:, 8:16], 1.0 / 64, 64e-5, ALU.mult, ALU.add, [bt1], [bt1])
        P.act(t1[:, 8:16], t1[:, 8:16], AF.Ln, [bt1], [bt1])
        P.act(t1[:, 8:16], t1[:, 8:16], AF.Exp, [bt1], [bt1], scale=-0.5)
        P.tt("vector", v3(ysb[:]), v3(ysb[:]), t1[:, 8:16].unsqueeze(2).to_broadcast([128, 8, 64]), ALU.mult, [bysb, bt1], [bysb])
        P.tt(rr(), ysb[:], ysb[:], prm[:, 5, :], ALU.mult, [bysb, bprm], [bysb])
        P.tt(rr(), ysb[:], ysb[:], prm[:, 6, :], ALU.add, [bysb, bprm], [bysb])
        P.tt("vector", v3(tm1[:]), v3(v_s[:]), st8[:, 0:8].unsqueeze(2).to_broadcast([128, 8, 64]), ALU.mult, [bv, bst8], [btm1])
        P.tt(rr(), ysb[:], ysb[:], tm1[:], ALU.add, [bysb, btm1], [bysb])
        P.tt("vector", yo[:], ysb[:], g_s[:], ALU.mult, [bysb, bg], [byo])
        P.dma("sync", yrw_d[tsl, :], yo[:], [byo], [b_yrw], byo)


def phase_attn(P, nc, G):
    P.noself = NOSELF_ATTN
    con_d, modp_d, lamv_d, subln_d, wout_d, rtw_d, rtb_d = (G[k] for k in
        ("con_d", "modp_d", "lamv_d", "subln_d", "wout_d", "rtw_d", "rtb_d"))
    x_d, qk_d, v_d, yrw_d, x1_d, h2T_d, gat_d = (G[k] for k in ("x_d", "qk_d", "v_d", "yrw_d", "x1_d", "h2T_d", "gat_d"))
    b_modp, b_qk, b_v, b_yrw, b_x1, b_h2T, b_gat = (G[k] for k in
        ("b_modp", "b_qk", "b_v", "b_yrw", "b_x1", "b_h2T", "b_gat"))
    rr = _rr(P)
    con, bcon = P.sb("con", [128, NCONST], F32)
    P.dma("sync", con[:], con_d, [], [bcon], bcon)
    identf = con[:, O_ID:O_ID + 128]
    idb, bidb = P.sb("idb", [128, 128], BF16)
    P.cp("vector", idb[:], identf, [bcon], [bidb])
    cmk, bcmk = P.sb("cmk", [128, 4, 512], BF16)
    P.cp("vector", cmk[:].rearrange("p a t -> p (a t)"), con[:, O_CM:O_CM + 2048], [bcon], [bcmk])
    rows, brows = P.sb("rows", [128, 3, D], F32)
    P.dma("sync", rows[:].rearrange("p a d -> p (a d)"),
          modp_d[2:5, :].rearrange("(o a) d -> o (a d)", o=1).partition_broadcast(128), [b_modp], [brows], brows)
    mcol, bmcol = P.sb("mcol", [128, 6, 8], F32)
    P.dma("sync", mcol[:], modp_d.rearrange("a (k p) -> p a k", p=128), [b_modp], [bmcol], bmcol,
          allow_slow_non_contiguous=True)
    lv, blv = P.sb("lv", [128, 4, 64], F32)
    P.dma("sync", lv[:].rearrange("p a d -> p (a d)"),
          lamv_d.rearrange("(o a) d -> o (a d)", o=1).partition_broadcast(128), [], [blv], blv)
    lam, blam = P.sb("lam", [128, 8], F32)
    lt, blt = P.sb("lt", [128, 2, 64], F32)
    P.tt("vector", lt[:, 0, :], lv[:, 0, :], lv[:, 1, :], ALU.mult, [blv], [blt])
    P.tt("vector", lt[:, 1, :], lv[:, 2, :], lv[:, 3, :], ALU.mult, [blv], [blt])
    P.op("vector", lambda e: e.tensor_reduce(out=lam[:, 0:2], in_=lt[:], axis=AX.X, op=ALU.add), [blt], [blam])
    P.act(lam[:, 2:4], lam[:, 0:2], AF.Exp, [blam], [blam])
    P.tt("vector", lam[:, 4:5], lam[:, 2:3], lam[:, 3:4], ALU.subtract, [blam], [blam])
    P.ts("vector", lam[:, 5:6], lam[:, 4:5], -1.0, -LAMBDA_INIT, ALU.mult, ALU.add, [blam], [blam])
    sub, bsub = P.sb("sub", [128, 128], F32)
    P.dma("sync", sub[:], subln_d.partition_broadcast(128), [], [bsub], bsub)
    P.ts("vector", sub[:], sub[:], 1.0 - LAMBDA_INIT, None, ALU.mult, None, [bsub], [bsub])
    wo, bwo = P.sb("wo", [128, 8, D], BF16)
    P.dma("gpsimd", wo[:], wout_d.rearrange("(k p) c -> p k c", p=128), [], [bwo], bwo)
    rw, brw = P.sb("rw", [128, 8, NE], BF16)
    P.dma("gpsimd", rw[:], rtw_d.rearrange("(k p) c -> p k c", p=128), [], [brw], brw)
    rb, brb = P.sb("rb", [128, NE], F32)
    P.dma("sync", rb[:], rtb_d.partition_broadcast(128), [], [brb], brb)
    kT, bkT = P.sb("kT", [128, 4, T], BF16)
    P.dma("sync", kT[:], qk_d[512:1024, :].rearrange("(c p) t -> p c t", p=128), [b_qk], [bkT], bkT)
    vv, bvv = P.sb("vv", [128, NT, 4 * 129], BF16)
    P.dma("sync", vv[:], v_d.rearrange("(n p) f -> p n f", p=128), [b_v], [bvv], bvv)
    qT = [P.sb(f"qT{i}", [128, 4, 512], BF16) for i in range(2)]
    pt = [P.sb(f"pt{i}", [128, 512], BF16) for i in range(3)]
    psc = [P.ps(f"psc{i}", [128, 512], F32) for i in range(2)]
    po = [P.ps(f"po{i}", [128, 4, 128], F32) for i in range(2)]
    pms, bpms_ = P.ps("pms", [128, 512], F32)
    pos_ = pms[:, 0:128].rearrange("p (a b c) -> p a b c", a=2, b=4)
    bpos_ = P.buf("possum")
    pw, bpw = P.ps("pw", [128, D], F32)
    ptr, bptr = P.ps("ptr", [128, 8, 128], BF16)
    prt = pms[:, 128:256]
    bprt = bpos_
    YC, bYC_ = P.sb("YC", [128, 4, D], BF16)
    bYC = [P.buf("YCs") for _ in range(4)]
    ycT, bycT = P.sb("ycT", [128, 8, 128], BF16)
    o0, bo0 = P.sb("o0", [128, 128], F32)
    o1, bo1 = P.sb("o1", [128, 128], F32)
    rs, brs = P.sb("rs", [128, 16], F32)
    xt, bxt = P.sb("xt", [128, D], F32)
    y1, by1 = P.sb("y1", [128, D], F32)
    junk, bjunk = P.sb("junk", [128, D], BF16)
    h2, bh2 = P.sb("h2", [128, D], BF16)
    h2T, bh2T = P.sb("h2T", [128, 8, 128], BF16)
    lg, blg = P.sb("lg", [128, NE], F32)
    gt, bgt = P.sb("gt", [128, NE], F32)
    t8, bt8 = P.sb("t8", [128, 16], F32)
    yrt, byrt = P.sb("yrt", [128, 512], BF16)

    it = 0
    for qb in range(8):
        qcur, bqcur = qT[qb % 2]
        P.dma("sync", qcur[:], qk_d[0:512, qb * 512:(qb + 1) * 512].rearrange("(c p) t -> p c t", p=128),
              [b_qk], [bqcur], bqcur)
        ntk = (qb + 1) * 4
        for hd in range(4):
            for mp in range(2):
                m = hd * 2 + mp
                chn, pb = m // 2, (m % 2) * 64
                pot, bpot = po[mp]
                for tk in range(ntk):
                    ps_, bps_ = psc[it % 2]
                    ptile, bptile = pt[it % 3]
                    it += 1
                    P.mm(ps_[:], kT[pb:pb + 64, chn, tk * 128:(tk + 1) * 128], qcur[pb:pb + 64, chn, :],
                         [bkT, bqcur], [bps_])
                    P.act(ptile[:], ps_[:], AF.Exp, [bps_], [bptile], scale=0.125)
                    j = tk - qb * 4
                    if j >= 0:
                        P.tt("vector", ptile[:], ptile[:], cmk[:, j, :], ALU.mult, [bptile, bcmk], [bptile])
                    for s4 in range(4):
                        if j > s4:
                            continue
                        P.mm(pot[:, s4, :], ptile[:, s4 * 128:(s4 + 1) * 128], vv[:, tk, hd * 129:hd * 129 + 128],
                             [bptile, bvv], [bpot], start=(tk == 0 and s4 == 0), stop=(tk == ntk - 1 and s4 == 3))
                        P.mm(pos_[:, mp, s4, 0:1], ptile[:, s4 * 128:(s4 + 1) * 128], vv[:, tk, hd * 129 + 128:hd * 129 + 129],
                             [bptile, bvv], [bpos_], start=(mp == 0 and tk == 0 and s4 == 0),
                             stop=(mp == 1 and tk == ntk - 1 and s4 == 3))
            P.cp("vector", rs[:, 0:8].rearrange("p (a b) -> p a b", a=2), pos_[:, :, :, 0], [bpos_], [brs])
            P.op("vector", lambda e: e.reciprocal(out=rs[:, 8:16], in_=rs[:, 0:8]), [brs], [brs])
            P.ts("vector", rs[:, 12:16], rs[:, 12:16], lam[:, 5:6], None, ALU.mult, None, [brs, blam], [brs])
            for s4 in range(4):
                P.ts("vector", o0[:], po[0][0][:, s4, :], rs[:, 8 + s4:9 + s4], None, ALU.mult, None, [po[0][1], brs], [bo0])
                P.stt("vector", o0[:], po[1][0][:, s4, :], rs[:, 12 + s4:13 + s4], o0[:], ALU.mult, ALU.add,
                      [po[1][1], brs, bo0], [bo0])
                P.act(o1[:], o0[:], AF.Square, [bo0], [bo1, bt8], accum_out=t8[:, 0:1])
                P.ts("vector", t8[:, 1:2], t8[:, 0:1], 1.0 / 128, 1e-5, ALU.mult, ALU.add, [bt8], [bt8])
                P.act(t8[:, 2:3], t8[:, 1:2], AF.Ln, [bt8], [bt8])
                P.act(t8[:, 3:4], t8[:, 2:3], AF.Exp, [bt8], [bt8], scale=-0.5)
                P.stt("vector", YC[:, s4, hd * 128:(hd + 1) * 128], o0[:], t8[:, 3:4], sub[:], ALU.mult, ALU.mult,
                      [bo0, bt8, bsub], [bYC[s4]])
        for s4 in range(4):
            n = qb * 4 + s4
            tsl = slice(n * 128, (n + 1) * 128)
            P.dma("sync", yrt[:], yrw_d[tsl, :], [b_yrw], [byrt], byrt)
            P.cp("vector", YC[:, s4, 512:1024], yrt[:], [byrt], [bYC[s4]])
            for k in range(8):
                P.tr(ptr[:, k, :], YC[:, s4, k * 128:(k + 1) * 128], idb[:], [bYC[s4], bidb], [bptr])
            P.cp("scalar", ycT[:].rearrange("p k t -> p (k t)"), ptr[:].rearrange("p k t -> p (k t)"), [bptr], [bycT])
            for hf in range(2):
                for k in range(8):
                    P.mm(pw[:, hf * 512:(hf + 1) * 512], ycT[:, k, :], wo[:, k, hf * 512:(hf + 1) * 512],
                         [bycT, bwo], [bpw], start=(k == 0), stop=(k == 7))
            P.cp("scalar", y1[:], pw[:], [bpw], [by1])
            P.act(junk[:], y1[:], AF.Square, [by1], [bjunk, bt8], accum_out=t8[:, 4:5])
            P.ts("vector", t8[:, 5:6], t8[:, 4:5], 1.0 / D, 1e-6, ALU.mult, ALU.add, [bt8], [bt8])
            P.act(t8[:, 6:7], t8[:, 5:6], AF.Ln, [bt8], [bt8])
            P.act(t8[:, 7:8], t8[:, 6:7], AF.Exp, [bt8], [bt8], scale=-0.5)
            P.dma("sync", xt[:], x_d[tsl, :], [], [bxt], bxt)
            P.stt("vector", y1[:], y1[:], t8[:, 7:8], rows[:, 0, :], ALU.mult, ALU.mult, [by1, bt8, brows], [by1])
            P.tt("gpsimd", xt[:], xt[:], y1[:], ALU.add, [bxt, by1], [bxt])
            P.dma("sync", x1_d[tsl, :], xt[:], [bxt], [b_x1], bxt)
            P.act(junk[:], xt[:], AF.Square, [bxt], [bjunk, bt8], accum_out=t8[:, 8:9])
            P.ts("vector", t8[:, 9:10], t8[:, 8:9], 1.0 / D, 1e-6, ALU.mult, ALU.add, [bt8], [bt8])
            P.act(t8[:, 10:11], t8[:, 9:10], AF.Ln, [bt8], [bt8])
            P.act(t8[:, 11:12], t8[:, 10:11], AF.Exp, [bt8], [bt8], scale=-0.5)
            P.ts("vector", h2[:], xt[:], t8[:, 11:12], None, ALU.mult, None, [bxt, bt8], [bh2])
            for k in range(8):
                P.tr(ptr[:, k, :], h2[:, k * 128:(k + 1) * 128], idb[:], [bh2, bidb], [bptr])
            for k in range(8):
                P.act(h2T[:, k, :], ptr[:, k, :], AF.Identity, [bptr, bmcol], [bh2T],
                      bias=mcol[:, 4, k:k + 1], scale=mcol[:, 3, k:k + 1])
            P.dma("sync", h2T_d.rearrange("(k p) t -> p k t", p=128)[:, :, tsl], h2T[:], [bh2T], [b_h2T], bh2T)
            for k in range(8):
                P.mm(prt[:, 0:NE], h2T[:, k, :], rw[:, k, :], [bh2T, brw], [bprt], start=(k == 0), stop=(k == 7))
            P.tt("vector", lg[:], prt[:, 0:NE], rb[:], ALU.add, [bprt, brb], [blg])
            P.op("vector", lambda e: e.max(out=t8[:, 0:8], in_=lg[:]), [blg], [bt8])
            P.ts("vector", gt[:], lg[:], t8[:, 3:4], None, ALU.is_ge, None, [blg, bt8], [bgt])
            P.ts("vector", t8[:, 12:13], t8[:, 0:1], -1.0, None, ALU.mult, None, [bt8], [bt8])
            P.act(lg[:], lg[:], AF.Exp, [blg, bt8], [blg], bias=t8[:, 12:13], scale=1.0)
            P.tt("vector", gt[:], gt[:], lg[:], ALU.mult, [bgt, blg], [bgt])
            P.op("vector", lambda e: e.tensor_reduce(out=t8[:, 13:14], in_=gt[:], axis=AX.X, op=ALU.add), [bgt], [bt8])
            P.op("vector", lambda e: e.reciprocal(out=t8[:, 14:15], in_=t8[:, 13:14]), [bt8], [bt8])
            P.ts("vector", gt[:], gt[:], t8[:, 14:15], None, ALU.mult, None, [bgt, bt8], [bgt])
            P.dma("sync", gat_d[tsl, :], gt[:], [bgt], [b_gat], bgt)


def phase_moe(P, nc, G):
    P.noself = NOSELF_MOE
    modp_d, x1_d, h2T_d, gat_d, out_d = (G[k] for k in ("modp_d", "x1_d", "h2T_d", "gat_d", "out_d"))
    w1g_d, w1l_d, w2e_d, b1T_d, b2_d, con_d = (G[k] for k in ("w1g_d", "w1l_d", "w2e_d", "b1T_d", "b2_d", "con_d"))
    b_modp, b_x1, b_h2T, b_gat, b_out = (G[k] for k in ("b_modp", "b_x1", "b_h2T", "b_gat", "b_out"))
    idf, bidf = P.sb("idf", [128, 128], F32)
    P.dma("sync", idf[:], con_d[:, O_ID:O_ID + 128], [], [bidf], bidf)
    c2r, bc2r = P.sb("c2r", [128, D], F32)
    P.dma("sync", c2r[:], modp_d[5:6, :].partition_broadcast(128), [b_modp], [bc2r], bc2r)
    b1T, bb1T = P.sb("b1T", [128, NE, 16], F32)
    P.dma("sync", b1T[:].rearrange("p e c -> p (e c)"), b1T_d, [], [bb1T], bb1T)
    b2s, bb2s = P.sb("b2s", [NE, D], F32)
    P.dma("sync", b2s[:], b2_d, [], [bb2s], bb2s)
    W = [[P.sb(f"w{j}_{i}", [128, 8, D], BF16) for j in range(3)] for i in range(2)]
    hq, bhq = P.sb("hq", [128, 8, 1024], BF16)
    gq, bgq = P.sb("gq", [128, 8, NE], F32)
    gT, bgT = P.sb("gT", [NE, 128], F32)
    acc, bacc_ = P.sb("acc", [128, 8, D], F32)
    bacc = [P.buf("acct") for _ in range(8)]
    actT, bactT_ = P.sb("actT", [128, 8, 512], BF16)
    bactT = [P.buf("actc") for _ in range(8)]
    GG = [P.sb(f"mg{i}", [128, 512], F32) for i in range(2)]
    SS = [P.sb(f"msg{i}", [128, 512], F32) for i in range(2)]
    LL = [P.sb(f"ml{i}", [128, 512], F32) for i in range(2)]
    b1p, bb1p = P.sb("b1p", [128, NE, 8], F32)
    P.ts("vector", b1p[:], b1T[:, :, 8:16], 1.0, None, ALU.add, None, [bb1T], [bb1p])
    xt, bxt = P.sb("mxt", [128, D], F32)
    junk, bjunk = P.sb("mjunk", [128, D], BF16)
    t8, bt8 = P.sb("mt8", [128, 8], F32)
    pg = [P.ps(f"pg{i}", [128, 512], F32) for i in range(2)]
    pl = [P.ps(f"pl{i}", [128, 512], F32) for i in range(2)]
    po = [P.ps(f"pmo{i}", [128, 512], F32) for i in range(2)]
    pm, bpm = P.ps("pmisc", [128, 512], F32)
    it = 0
    io = 0
    wi = 0
    for qt in range(DBG_NQ):
        q0 = qt * 1024
        P.dma("sync", hq[:], h2T_d.rearrange("(k p) t -> p k t", p=128)[:, :, q0:q0 + 1024], [b_h2T], [bhq], bhq)
        P.dma("sync", gq[:], gat_d[q0:q0 + 1024, :].rearrange("(n p) e -> p n e", p=128), [b_gat], [bgq], bgq)
        for n in range(8):
            P.tr(pm[0:NE, 0:128], gq[:, n, :], idf[:], [bgq, bidf], [bpm], f32=True)
            P.cp("vector", gT[:], pm[0:NE, 0:128], [bpm], [bgT])
            for hf in range(2):
                pp, bpp = po[io % 2]
                io += 1
                P.mm(pp[:], gT[:], b2s[:, hf * 512:(hf + 1) * 512], [bgT, bb2s], [bpp], f32=True)
                P.cp("scalar", acc[:, n, hf * 512:(hf + 1) * 512], pp[:], [bpp], [bacc[n]])
        for e in range(DBG_NEXP):
            (w1g, bw1g), (w1l, bw1l), (w2, bw2) = W[wi % 2]
            wi += 1
            P.dma("gpsimd", w1g[:], w1g_d[e].rearrange("(k p) f -> p k f", p=128), [], [bw1g], bw1g)
            P.dma("gpsimd", w1l[:], w1l_d[e].rearrange("(k p) f -> p k f", p=128), [], [bw1l], bw1l)
            P.dma("gpsimd", w2[:], w2e_d[e].rearrange("(k p) f -> p k f", p=128), [], [bw2], bw2)
            for blk in range(2):
                bsl = slice(blk * 512, (blk + 1) * 512)
                for fc in range(8):
                    pgt, bpgt = pg[it % 2]
                    plt, bplt = pl[it % 2]
                    it += 1
                    for k in range(8):
                        P.mm(pgt[:], w1g[:, k, fc * 128:(fc + 1) * 128], hq[:, k, bsl], [bw1g, bhq], [bpgt],
                             start=(k == 0), stop=(k == 7))
                    for k in range(8):
                        P.mm(plt[:], w1l[:, k, fc * 128:(fc + 1) * 128], hq[:, k, bsl], [bw1l, bhq], [bplt],
                             start=(k == 0), stop=(k == 7))
                    gi, bgi = GG[it % 2]
                    si, bsi = SS[it % 2]
                    li, bli = LL[it % 2]
                    P.ts("vector", gi[:], pgt[:], b1T[:, e, fc:fc + 1], 7.0, ALU.add, ALU.min, [bpgt, bb1T], [bgi])
                    P.act(si[:], gi[:], AF.Sigmoid, [bgi], [bsi], scale=1.702)
                    P.act(li[:], plt[:], AF.Identity, [bplt, bb1p], [bli], bias=b1p[:, e, fc:fc + 1], scale=1.0)
                    P.ts("vector", li[:], li[:], -6.0, 8.0, ALU.max, ALU.min, [bli], [bli])
                    P.tt("gpsimd", si[:], si[:], gi[:], ALU.mult, [bsi, bgi], [bsi])
                    P.tt("vector", actT[:, fc, :], si[:], li[:], ALU.mult, [bsi, bli], [bactT[fc]])
                for tt_ in range(4):
                    n = blk * 4 + tt_
                    for hf in range(2):
                        pp, bpp = po[io % 2]
                        io += 1
                        for fc in range(8):
                            P.mm(pp[:], actT[:, fc, tt_ * 128:(tt_ + 1) * 128], w2[:, fc, hf * 512:(hf + 1) * 512],
                                 [bactT[fc], bw2], [bpp], start=(fc == 0), stop=(fc == 7))
                        P.stt("vector", acc[:, n, hf * 512:(hf + 1) * 512], pp[:], gq[:, n, e:e + 1],
                              acc[:, n, hf * 512:(hf + 1) * 512], ALU.mult, ALU.add, [bpp, bgq, bacc[n]], [bacc[n]])
        for n in range(8):
            tsl = slice(q0 + n * 128, q0 + (n + 1) * 128)
            P.dma("sync", xt[:], x1_d[tsl, :], [b_x1], [bxt], bxt)
            P.act(junk[:], acc[:, n, :], AF.Square, [bacc[n]], [bjunk, bt8], accum_out=t8[:, 0:1])
            P.ts("vector", t8[:, 1:2], t8[:, 0:1], 1.0 / D, 1e-6, ALU.mult, ALU.add, [bt8], [bt8])
            P.act(t8[:, 2:3], t8[:, 1:2], AF.Ln, [bt8], [bt8])
            P.act(t8[:, 3:4], t8[:, 2:3], AF.Exp, [bt8], [bt8], scale=-0.5)
            P.stt("vector", acc[:, n, :], acc[:, n, :], t8[:, 3:4], c2r[:], ALU.mult, ALU.mult, [bacc[n], bt8, bc2r], [bacc[n]])
            P.tt("vector", xt[:], xt[:], acc[:, n, :], ALU.add, [bxt, bacc[n]], [bxt])
            P.dma("sync", out_d[tsl, :], xt[:], [bxt], [b_out], bxt)


def _consts():
    c = np.zeros((128, NCONST), np.float32)
    r = np.arange(128)[:, None]
    q = np.arange(128)[None, :]
    same = (r // 64) == (q // 64)
    su = (same & ((r % 64) < (q % 64))).astype(np.float32)
    sl = (same & ((r % 64) > (q % 64))).astype(np.float32)
    ui = (same & ((r % 64) <= (q % 64))).astype(np.float32)
    c[:, O_ID:O_ID + 128] = np.eye(128, dtype=np.float32)
    for i, m in enumerate((su, su, sl, ui, ui)):
        c[:, O_M5 + i * 128:O_M5 + (i + 1) * 128] = m
    c[:, O_ONES:O_ONES + 128] = same.astype(np.float32)
    c[:64, O_IND] = 1.0
    c[64:, O_IND + 1] = 1.0
    inv_freq = (500000.0 ** (-np.arange(0, 16, 2, dtype=np.float32) / 16)).astype(np.float32)
    for p in range(128):
        d = p % 64
        if d < 16:
            c[p, O_FREQ] = inv_freq[d % 8]
            c[p, O_SIGN] = -1.0 if d < 8 else 1.0
            pp = p + 8 if d < 8 else p - 8
            c[pp, O_PM + p] = 1.0
    tq = np.arange(512)[None, :]
    tk = np.arange(128)[:, None]
    for j in range(4):
        c[:, O_CM + j * 512:O_CM + (j + 1) * 512] = ((j * 128 + tk) <= tq).astype(np.float32)
    return c


_NC_CACHE = {}


def _in_maps(inp):
    f = lambda a: np.ascontiguousarray(np.asarray(a, dtype=np.float32))
    B = 8
    w1 = np.asarray(inp["moe_w1"])[0]
    w1g = np.ascontiguousarray(w1[:, :, 0::2])
    w1l = np.ascontiguousarray(w1[:, :, 1::2])
    b1 = np.asarray(inp["moe_b1"])[0]
    b1cat = np.concatenate([b1[:, 0::2].reshape(NE, 8, 128), b1[:, 1::2].reshape(NE, 8, 128)], axis=1)
    b1T = np.ascontiguousarray(b1cat.transpose(2, 0, 1).reshape(128, NE * 16)).astype(np.float32)
    shared = dict(
        ada_w=f(inp["ada_w"][0]), ada_b=f(inp["ada_b"]).reshape(1, 6 * D),
        norms=f(np.stack([inp["pre_mix_norm"][0], inp["post_mix_norm"][0], inp["pre_ffn_norm"][0], inp["post_ffn_norm"][0]])),
        w_in=f(inp["w_in"][0]), w_out=f(inp["w_out"][0]),
        lamv=f(np.stack([inp["da_lambda_q1"][0], inp["da_lambda_k1"][0], inp["da_lambda_q2"][0], inp["da_lambda_k2"][0]])),
        subln=f(inp["da_subln"]).reshape(1, 128), mu=f(inp["rw_mu"]).reshape(1, 1792),
        rwv=f(np.stack([inp["rw_w0"][0], inp["rw_a0"][0], inp["rw_k_k"][0], inp["rw_k_a"][0],
                        np.asarray(inp["rw_r_k"])[0].reshape(512), inp["rw_ln_w"][0], inp["rw_ln_b"][0]])),
        rw_w2=f(inp["rw_w2"][0]), rw_a2=f(inp["rw_a2"][0]), rw_g2=f(inp["rw_g2"][0]),
        router_w=f(inp["router_w"][0]), router_b=f(inp["router_b"]).reshape(1, NE),
        w1g=w1g, w1l=w1l, b1T=b1T, w2e=f(inp["moe_w2"][0]), b2=f(inp["moe_b2"][0]),
        consts=_consts(),
    )
    x = np.asarray(inp["x"], dtype=np.float32)
    c = np.asarray(inp["c"], dtype=np.float32)
    pos = np.asarray(inp["positions"]).astype(np.int32)
    in_maps = []
    for b in range(B):
        m = dict(shared)
        m["x"] = np.ascontiguousarray(x[b])
        m["cT"] = np.ascontiguousarray(c[b].reshape(8, 128).T)
        m["pos"] = np.ascontiguousarray(pos[b].reshape(1, T))
        in_maps.append(m)
    return in_maps


def kernel(**inp):
    B = 8
    if "nc" not in _NC_CACHE:
        _NC_CACHE["nc"] = build_program()
    nc = _NC_CACHE["nc"]
    in_maps = _in_maps(inp)
    res = run_bass_kernel_spmd(nc, in_maps, core_ids=list(range(B)))
    return np.stack([np.asarray(r["out"], dtype=np.float32) for r in res.results], axis=0)
```

```python
import contextlib
import math
import numpy as np
import concourse.bass as bass
import concourse.mybir as mybir
from concourse.bass_utils import run_bass_kernel_spmd

ALU = mybir.AluOpType
AF = mybir.ActivationFunctionType
F32 = mybir.dt.float32
BF16 = mybir.dt.bfloat16
I32 = mybir.dt.int32
AX = mybir.AxisListType

D = 1024
T = 4096
NT = 32
NE = 32
C0 = math.exp(-0.5)
LAMBDA_INIT = 0.8 - 0.6 * math.exp(0.0)

O_ID = 0
O_M5 = 128
O_UI = O_M5 + 384
O_SU = O_M5
O_SL = O_M5 + 256
O_ONES = 768
O_IND = 896
O_FREQ = 898
O_SIGN = 899
O_PM = 900
O_CM = 1028
NCONST = O_CM + 2048
DBG_STOP = 99
NOSELF_MOE = ('tensor',)
NOSELF_ATTN = ('tensor',)
NOSELF_FRONT = ('tensor',)
DBG_X = 0
DBG_NQ = 4
DBG_NEXP = NE
DBG_NT = NT


class Buf:
    __slots__ = ("name", "w", "r", "dsem", "dcount")

    def __init__(self, name):
        self.name = name
        self.w = {}
        self.r = {}
        self.dsem = None
        self.dcount = 0


class Eng:
    def __init__(self, name, sem):
        self.name = name
        self.sem = sem
        self.count = 0
        self.waited = {}
        self.thunks = []


class Prog:
    def __init__(self, nc, stack):
        self.nc = nc
        self.gstack = stack
        self.stack = stack
        self.engs = {}
        self.sems = {}
        self.vals = {}
        for n in ("tensor", "vector", "scalar", "gpsimd", "sync"):
            sem = stack.enter_context(nc.semaphore("es_" + n))
            self.engs[n] = Eng(n, sem)
            self.sems[("e", n)] = sem
            self.vals[("e", n)] = 0
        self.nbuf = 0
        self.ninstr = 0
        self.allbufs = []
        self.gen = 0
        self.noself = ()
        self.last_f32 = False

    def buf(self, name=None):
        self.nbuf += 1
        b = Buf(f"{name or 'b'}{self.nbuf}")
        self.allbufs.append(b)
        return b

    def new_engine_sems(self):
        self.gen += 1
        for n, eng in self.engs.items():
            old = ("e", n)
            self.vals.pop(old, None)
            sem = self.gstack.enter_context(self.nc.semaphore(f"es{self.gen}_{n}"))
            eng.sem = sem
            eng.count = 0
            eng.waited = {}
            self.sems[old] = sem
            self.vals[old] = 0
        for b in self.allbufs:
            b.w = {}
            b.r = {}

    def sb(self, name, shape, dtype):
        self.nbuf += 1
        t = self.stack.enter_context(self.nc.sbuf_tensor(f"sb{self.nbuf}_{name}", list(shape), dtype))
        return t, self.buf(name)

    def ps(self, name, shape, dtype=F32):
        self.nbuf += 1
        t = self.stack.enter_context(self.nc.psum_tensor(f"ps{self.nbuf}_{name}", list(shape), dtype))
        return t, self.buf(name)

    def _dsem(self, b):
        if b.dsem is None:
            s = self.gstack.enter_context(self.nc.semaphore("ds_" + b.name))
            b.dsem = ("d", b.name)
            self.sems[b.dsem] = s
            self.vals[b.dsem] = 0
        return b.dsem

    def _collect(self, eng, reads, writes):
        need = {}
        for b in reads:
            for k, v in b.w.items():
                if need.get(k, 0) < v:
                    need[k] = v
        for b in writes:
            for k, v in b.w.items():
                if need.get(k, 0) < v:
                    need[k] = v
            for k, v in b.r.items():
                if need.get(k, 0) < v:
                    need[k] = v
        own = ("e", eng.name)
        for k, v in need.items():
            if k == own and eng.name in self.noself:
                continue
            if eng.waited.get(k, 0) < v:
                eng.waited[k] = v
                sem = self.sems[k]
                eng.thunks.append(lambda e, sem=sem, v=v: e.wait_ge(sem, v))

    def op(self, engname, fn, reads=(), writes=(), selfwait=False):
        eng = self.engs[engname]
        self._collect(eng, reads, writes)
        if selfwait and eng.count > 0:
            own = ("e", engname)
            if eng.waited.get(own, 0) < eng.count:
                eng.waited[own] = eng.count
                eng.thunks.append(lambda e, sem=eng.sem, v=eng.count: e.wait_ge(sem, v))
        eng.count += 1
        c = eng.count
        sem = eng.sem
        eng.thunks.append(lambda e, fn=fn, sem=sem: fn(e).then_inc(sem, 1))
        key = ("e", engname)
        self.vals[key] = c
        for b in reads:
            b.r[key] = c
        for b in writes:
            b.w = {key: c}
            b.r = {}
        self.ninstr += 1

    def dma(self, q, out_ap, in_ap, reads, writes, sbuf_buf, **kw):
        eng = self.engs[q]
        self._collect(eng, reads, writes)
        key = self._dsem(sbuf_buf)
        sbuf_buf.dcount += 16
        c = sbuf_buf.dcount
        self.vals[key] = c
        sem = self.sems[key]
        eng.thunks.append(
            lambda e, o=out_ap, i=in_ap, sem=sem, kw=kw: e.dma_start(out=o, in_=i, **kw).then_inc(sem, 16))
        for b in reads:
            b.r[key] = c
        for b in writes:
            if b is sbuf_buf:
                b.w = {key: c}
                b.r = {}
            else:
                b.w[key] = c
        self.ninstr += 1

    def barrier(self):
        for eng in self.engs.values():
            for k, v in self.vals.items():
                if v > 0 and eng.waited.get(k, 0) < v:
                    eng.waited[k] = v
                    sem = self.sems[k]
                    eng.thunks.append(lambda e, sem=sem, v=v: e.wait_ge(sem, v))

    def flush(self):
        nc = self.nc
        engs = self.engs
        with nc.Block() as block:
            @block.tensor
            def _(e):
                for t in engs["tensor"].thunks:
                    t(e)

            @block.vector
            def _(e):
                for t in engs["vector"].thunks:
                    t(e)

            @block.scalar
            def _(e):
                for t in engs["scalar"].thunks:
                    t(e)

            @block.gpsimd
            def _(e):
                for t in engs["gpsimd"].thunks:
                    t(e)

            @block.sync
            def _(e):
                for t in engs["sync"].thunks:
                    t(e)
        for e in engs.values():
            e.thunks = []

    @contextlib.contextmanager
    def phase(self):
        with contextlib.ExitStack() as ph:
            self.stack = ph
            yield
            self.barrier()
            self.flush()
        self.stack = self.gstack
        self.new_engine_sems()

    def mm(self, out, lhsT, rhs, R, W, start=True, stop=True, f32=False):
        sw = f32 or self.last_f32
        self.last_f32 = f32
        self.op("tensor", lambda e: e.matmul(out, lhsT=lhsT, rhs=rhs, start=start, stop=stop), R, W, selfwait=sw)

    def tr(self, out, in_, ident, R, W, f32=False):
        sw = f32 or self.last_f32
        self.last_f32 = f32
        self.op("tensor", lambda e: e.transpose(out, in_, ident), R, W, selfwait=sw)

    def tt(self, eng, out, in0, in1, op, R, W):
        self.op(eng, lambda e: e.tensor_tensor(out=out, in0=in0, in1=in1, op=op), R, W)

    def ts(self, eng, out, in0, s1, s2, op0, op1, R, W):
        if s2 is None:
            self.op(eng, lambda e: e.tensor_scalar(out=out, in0=in0, scalar1=s1, scalar2=None, op0=op0), R, W)
        else:
            self.op(eng, lambda e: e.tensor_scalar(out=out, in0=in0, scalar1=s1, scalar2=s2, op0=op0, op1=op1), R, W)

    def stt(self, eng, out, in0, scalar, in1, op0, op1, R, W):
        eng = "vector"
        self.op(eng, lambda e: e.scalar_tensor_tensor(out=out, in0=in0, scalar=scalar, in1=in1, op0=op0, op1=op1), R, W)

    def act(self, out, in_, func, R, W, bias=None, scale=None, accum_out=None):
        kw = {}
        if bias is not None:
            kw["bias"] = bias
        if scale is not None:
            kw["scale"] = scale
        if accum_out is not None:
            kw["accum_out"] = accum_out
        self.op("scalar", lambda e: e.activation(out=out, in_=in_, func=func, **kw), R, W)

    def cp(self, eng, out, in_, R, W):
        if eng == "scalar":
            self.op("scalar", lambda e: e.copy(out=out, in_=in_), R, W)
        else:
            self.op(eng, lambda e: e.tensor_copy(out=out, in_=in_), R, W)

    def ms(self, eng, ap, val, W):
        self.op(eng, lambda e: e.memset(ap, val), [], W)


def _rr(P):
    state = {"i": 0}

    def nxt():
        state["i"] += 1
        return "vector" if state["i"] % 3 else "gpsimd"
    return nxt


def build_program(debug=False, upto=3):
    nc = bass.Bass("TRN2", target_bir_lowering=False)
    skind = "ExternalOutput" if debug else "Internal"

    def din(name, shape, dt=F32):
        return nc.dram_tensor(name, list(shape), dt, kind="ExternalInput").ap()

    x_d = din("x", [T, D])
    cT_d = din("cT", [128, 8])
    pos_d = din("pos", [1, T], I32)
    adaw_d = din("ada_w", [D, 6 * D])
    adab_d = din("ada_b", [1, 6 * D])
    norms_d = din("norms", [4, D])
    win_d = din("w_in", [D, 3328])
    wout_d = din("w_out", [D, D])
    lamv_d = din("lamv", [4, 64])
    subln_d = din("subln", [1, 128])
    mu_d = din("mu", [1, 1792])
    rwv_d = din("rwv", [7, 512])
    w2_d = din("rw_w2", [64, 512])
    a2_d = din("rw_a2", [64, 512])
    g2_d = din("rw_g2", [128, 512])
    rtw_d = din("router_w", [D, NE])
    rtb_d = din("router_b", [1, NE])
    w1g_d = din("w1g", [NE, D, D]) if upto >= 3 else None
    w1l_d = din("w1l", [NE, D, D]) if upto >= 3 else None
    b1T_d = din("b1T", [128, NE * 16])
    w2e_d = din("w2e", [NE, D, D]) if upto >= 3 else None
    b2_d = din("b2", [NE, D])
    con_d = din("consts", [128, NCONST])
    out_d = nc.dram_tensor("out", [T, D], F32, kind="ExternalOutput").ap()

    modp_d = nc.dram_tensor("modp", [6, D], F32, kind=skind).ap()
    qk_d = nc.dram_tensor("qk_s", [D, T], BF16, kind=skind).ap()
    v_d = nc.dram_tensor("v_s", [T, 4 * 129], BF16, kind=skind).ap()
    yrw_d = nc.dram_tensor("yrw_s", [T, 512], BF16, kind=skind).ap()
    x1_d = nc.dram_tensor("x1_s", [T, D], F32, kind=skind).ap()
    h2T_d = nc.dram_tensor("h2T_s", [D, T], BF16, kind=skind).ap()
    gat_d = nc.dram_tensor("gat_s", [T, NE], F32, kind=skind).ap()

    with contextlib.ExitStack() as gst:
        P = Prog(nc, gst)
        b_modp = P.buf("modp")
        b_qk = P.buf("qkd")
        b_v = P.buf("vd")
        b_yrw = P.buf("yrwd")
        b_x1 = P.buf("x1d")
        b_h2T = P.buf("h2Td")
        b_gat = P.buf("gatd")
        b_out = P.buf("outd")

        with P.phase():
            cT, bcT = P.sb("cT", [128, 8], F32)
            sc, bsc = P.sb("sc", [128, 8], F32)
            P.dma("sync", cT[:], cT_d, [], [bcT], bcT)
            P.act(sc[:], cT[:], AF.Silu, [bcT], [bsc])
            aw = [P.sb(f"aw{i}", [128, 3072], F32) for i in range(2)]
            pm, bpm = P.ps("pmod", [128, 3072], F32)
            mrow, bmrow = P.sb("mrow", [1, 6 * D], F32)
            brow, bbrow = P.sb("brow", [1, 6 * D], F32)
            nrm, bnrm = P.sb("nrm", [1, 4 * D], F32)
            orow, borow = P.sb("orow", [1, 6 * D], F32)
            P.dma("sync", brow[:], adab_d, [], [bbrow], bbrow)
            P.dma("sync", nrm[:], norms_d.rearrange("(o a) d -> o (a d)", o=1), [], [bnrm], bnrm)
            i = 0
            for half in range(2):
                for k in range(8):
                    t_, b_ = aw[i % 2]
                    i += 1
                    P.dma("sync", t_[:], adaw_d[k * 128:(k + 1) * 128, half * 3072:(half + 1) * 3072], [], [b_], b_)
                    for j in range(6):
                        P.mm(pm[0:1, j * 512:(j + 1) * 512], sc[:, k:k + 1], t_[:, j * 512:(j + 1) * 512],
                             [bsc, b_], [bpm], start=(k == 0), stop=(k == 7))
                P.tt("vector", mrow[:, half * 3072:(half + 1) * 3072], pm[0:1, :], brow[:, half * 3072:(half + 1) * 3072],
                     ALU.add, [bpm, bbrow], [bmrow])

            def mseg(i_):
                return mrow[:, i_ * D:(i_ + 1) * D]

            def nseg(i_):
                return nrm[:, i_ * D:(i_ + 1) * D]
            P.stt("vector", orow[:, 0:D], mseg(1), 1.0, nseg(0), ALU.add, ALU.mult, [bmrow, bnrm], [borow])
            P.cp("vector", orow[:, D:2 * D], mseg(0), [bmrow], [borow])
            P.tt("vector", orow[:, 2 * D:3 * D], mseg(2), nseg(1), ALU.mult, [bmrow, bnrm], [borow])
            P.stt("vector", orow[:, 3 * D:4 * D], mseg(4), 1.0, nseg(2), ALU.add, ALU.mult, [bmrow, bnrm], [borow])
            P.cp("vector", orow[:, 4 * D:5 * D], mseg(3), [bmrow], [borow])
            P.tt("vector", orow[:, 5 * D:6 * D], mseg(5), nseg(3), ALU.mult, [bmrow, bnrm], [borow])
            P.dma("sync", modp_d.rearrange("(o a) d -> o (a d)", o=1), orow[:], [borow], [b_modp], borow)

        if upto >= 1:
          with P.phase():
            phase_front(P, nc, locals())

        if upto >= 2:
          with P.phase():
            phase_attn(P, nc, locals())

        if upto >= 3:
          with P.phase():
            phase_moe(P, nc, locals())
    return nc


def phase_front(P, nc, G):
    P.noself = NOSELF_FRONT
    x_d, pos_d, win_d, mu_d, rwv_d = G["x_d"], G["pos_d"], G["win_d"], G["mu_d"], G["rwv_d"]
    w2_d, a2_d, g2_d, con_d, modp_d = G["w2_d"], G["a2_d"], G["g2_d"], G["con_d"], G["modp_d"]
    qk_d, v_d, yrw_d = G["qk_d"], G["v_d"], G["yrw_d"]
    b_modp, b_qk, b_v, b_yrw = G["b_modp"], G["b_qk"], G["b_v"], G["b_yrw"]
    rr = _rr(P)

    con, bcon = P.sb("con", [128, NCONST], F32)
    P.dma("sync", con[:], con_d, [], [bcon], bcon)
    identf = con[:, O_ID:O_ID + 128]
    idb, bidb = P.sb("idb", [128, 128], BF16)
    P.cp("vector", idb[:], identf, [bcon], [bidb])

    mcol, bmcol = P.sb("mcol", [128, 6, 8], F32)
    P.dma("sync", mcol[:], modp_d.rearrange("a (k p) -> p a k", p=128), [b_modp], [bmcol], bmcol,
          allow_slow_non_contiguous=True)

    wda, bwda = P.sb("wda", [128, 8, 1536], BF16)
    P.dma("gpsimd", wda[:], win_d[:, 0:1536].rearrange("(k p) c -> p k c", p=128), [], [bwda], bwda)
    w1, bw1 = P.sb("w1", [128, 8, 1792], BF16)
    w2m, bw2m = P.sb("w2m", [128, 8, 1792], BF16)
    prm, bprm = P.sb("prm", [128, 7, 512], F32)
    P.dma("sync", prm[:].rearrange("p a d -> p (a d)"),
          rwv_d.rearrange("(o a) d -> o (a d)", o=1).partition_broadcast(128), [], [bprm], bprm)
    lw2, blw2 = P.sb("lw2", [128, 512], BF16)
    lg2, blg2 = P.sb("lg2", [128, 512], BF16)
    P.dma("gpsimd", lw2[0:64, :], w2_d, [], [blw2], blw2)
    P.dma("gpsimd", lw2[64:128, :], a2_d, [], [blw2], blw2)
    P.dma("gpsimd", lg2[:], g2_d, [], [blg2], blg2)
    pmb, bpmb = P.sb("pmb", [128, 128], BF16)
    P.cp("vector", pmb[:], con[:, O_PM:O_PM + 128], [bcon], [bpmb])

    ctab, bctab = P.sb("ctab", [128, T], BF16)
    stab, bstab = P.sb("stab", [128, T], BF16)
    with contextlib.ExitStack() as tmp:
        old = P.stack
        P.stack = tmp
        mub, bmub = P.sb("mub", [128, 1792], F32)
        omu, bomu = P.sb("omu", [128, 1792], F32)
        P.dma("sync", mub[:], mu_d.partition_broadcast(128), [], [bmub], bmub)
        P.ts("vector", omu[:], mub[:], -1.0, 1.0, ALU.mult, ALU.add, [bmub], [bomu])
        wst = [P.sb(f"wst{i}", [128, 1792], F32) for i in range(2)]
        for k in range(8):
            t_, b_ = wst[k % 2]
            P.dma("sync", t_[:], win_d[k * 128:(k + 1) * 128, 1536:3328], [], [b_], b_)
            P.tt("vector", w1[:, k, :], t_[:], omu[:], ALU.mult, [b_, bomu], [bw1])
            P.tt("gpsimd", w2m[:, k, :], t_[:], mub[:], ALU.mult, [b_, bmub], [bw2m])
        posi, bposi = P.sb("posi", [128, 1024], I32)
        ang, bang = P.sb("ang", [128, 1024], F32)
        y_, by_ = P.sb("ry", [128, 1024], F32)
        kf, bkf = P.sb("rkf", [128, 1024], F32)
        ki, bki = P.sb("rki", [128, 1024], I32)
        for q4 in range(4):
            sl = slice(q4 * 1024, (q4 + 1) * 1024)
            P.dma("sync", posi[:], pos_d[:, sl].partition_broadcast(128), [], [bposi], bposi)
            P.cp("vector", ang[:], posi[:], [bposi], [bang])
            P.ts("vector", ang[:], ang[:], con[:, O_FREQ:O_FREQ + 1], None, ALU.mult, None, [bang, bcon], [bang])
            for which, shift in ((0, math.pi * 1.5), (1, math.pi)):
                P.ts("vector", y_[:], ang[:], shift, None, ALU.add, None, [bang], [by_])
                P.ts("vector", kf[:], y_[:], 1.0 / (2 * math.pi), None, ALU.mult, None, [by_], [bkf])
                P.cp("vector", ki[:], kf[:], [bkf], [bki])
                P.cp("vector", kf[:], ki[:], [bki], [bkf])
                P.stt("vector", y_[:], kf[:], -2 * math.pi, y_[:], ALU.mult, ALU.add, [bkf, by_], [by_])
                P.ts("vector", kf[:], y_[:], 0.0, 2 * math.pi, ALU.is_lt, ALU.mult, [by_], [bkf])
                P.tt("vector", y_[:], y_[:], kf[:], ALU.add, [by_, bkf], [by_])
                P.ts("vector", y_[:], y_[:], -math.pi, None, ALU.add, None, [by_], [by_])
                P.ts("vector", y_[:], y_[:], -math.pi, math.pi, ALU.max, ALU.min, [by_], [by_])
                if which == 0:
                    P.act(ctab[:, sl], y_[:], AF.Sin, [by_], [bctab])
                else:
                    P.act(kf[:], y_[:], AF.Sin, [by_], [bkf])
                    P.ts("vector", stab[:, sl], kf[:], con[:, O_SIGN:O_SIGN + 1], None, ALU.mult, None, [bkf, bcon], [bstab])
        P.barrier()
        P.flush()
        P.stack = old

    def f32t(name, w=512):
        return P.sb(name, [128, w], F32)

    XT = [P.sb(f"xt{i}", [128, D], F32) for i in range(2)]
    xn, bxn = P.sb("xn", [128, D], BF16)
    junk, bjunk = xn, bxn
    st8, bst8 = P.sb("st8", [128, 8], F32)
    hT = [P.sb(f"hT{i}", [128, 8, 129], BF16) for i in range(2)]
    P.ms("vector", hT[1][0][:, :, 128:129], 0.0, [hT[1][1]])
    qf, bqf = P.sb("qf", [128, 128], BF16)
    qf2, bqf2 = P.sb("qf2", [128, 128], F32)
    t1, bt1 = P.sb("t1", [128, 128], F32)
    t2, bt2 = P.sb("t2", [128, 128], F32)
    qko, bqko = P.sb("qko", [128, 8, 128], BF16)
    vo, bvo = P.sb("vo", [128, 4, 129], BF16)
    P.ms("vector", vo[:, :, 128:129], 1.0, [bvo])
    r_s, br = f32t("r_s")
    k_s, bk = f32t("k_s")
    v_s, bv = f32t("v_s")
    lo0, blo0 = P.sb("lo0", [128, 128], BF16)
    lo1, blo1 = P.sb("lo1", [128, 128], BF16)
    v_b, bvb = P.sb("v_b", [128, 512], BF16)
    STb, bSTb_ = P.sb("STb", [128, 4, 64], BF16)
    P.ms("vector", STb[:], 0.0, [bSTb_])
    sig, bsig = f32t("sig")
    a_s, ba = f32t("a_s")
    g_s, bg = f32t("g_s")
    kk, bkk = f32t("kk")
    km, bkm = f32t("km")
    bb, bbb = f32t("bb")
    e1, be1 = f32t("e1")
    e2, be2 = f32t("e2")
    tm1, btm1 = f32t("tm1")
    At, bAt = f32t("At")
    Rt, bRt = f32t("Rt")
    Bt, bBt = f32t("Bt")
    Kt, bKt = f32t("Kt")
    Bh, bBh = P.sb("Bh", [128, 512], BF16)
    Kh, bKh = P.sb("Kh", [128, 512], BF16)
    FM, bFM_ = P.sb("FM", [128, 4, 4, 128], BF16)
    bFM = [P.buf("FMp") for _ in range(4)]
    RP, bRP_ = P.sb("RP", [128, 4, 384], BF16)
    bRP = [P.buf("RPp") for _ in range(4)]
    P.ms("vector", RP[:], 0.0, bRP)
    pc, bpc = P.sb("pc", [128, 4, 2], F32)
    ST, bST_ = P.sb("ST", [128, 4, 64], F32)
    bST = [P.buf("STh") for _ in range(8)]
    P.ms("vector", ST[:], 0.0, bST)
    Gs = [P.sb(f"Gs{i}", [128, 640], BF16) for i in range(2)]
    Nb = [[P.sb(f"N{i}_{j}", [128, 128], BF16) for j in range(2)] for i in range(2)]
    Lb = [[P.sb(f"L{i}_{j}", [128, 128], BF16) for j in range(2)] for i in range(2)]
    Tb = [[P.sb(f"T{i}_{j}", [128, 128], BF16) for j in range(2)] for i in range(2)]
    Zs = [P.sb(f"Zs{i}", [128, 64], BF16) for i in range(2)]
    Us = [P.sb(f"Us{i}", [128, 64], BF16) for i in range(2)]
    yo, byo = P.sb("yo", [128, 512], BF16)

    pT, bpT = P.ps("pT", [128, 8, 128], BF16)
    pQ, bpQ = P.ps("pQ", [128, 512], F32)
    pV, bpV = P.ps("pV", [128, 512], F32)
    pF, bpF = P.ps("pF", [128, 512], F32)
    pF1, bpF1 = P.ps("pF1", [128, 512], F32)
    pG0, bpG0 = P.ps("pG0", [128, 512], F32)
    pG1, bpG1 = P.ps("pG1", [128, 512], F32)
    pY, bpY = P.ps("pY", [128, 512], F32)

    A1c = mcol[:, 0, :]
    B1c = mcol[:, 1, :]

    if DBG_STOP < 1:
        return
    for n in range(DBG_NT):
        tsl = slice(n * 128, (n + 1) * 128)
        hcur, bhcur = hT[n % 2]
        hprev, bhprev = hT[(n + 1) % 2]
        xt, bxt = XT[n % 2]
        ysb, bysb = xt[:, 0:512], bxt
        if n == 0:
            P.dma("sync", xt[:], x_d[tsl, :], [], [bxt], bxt)
        if n + 1 < DBG_NT:
            xtn, bxtn = XT[(n + 1) % 2]
            P.dma("sync", xtn[:], x_d[(n + 1) * 128:(n + 2) * 128, :], [], [bxtn], bxtn)
        P.act(junk[:], xt[:], AF.Square, [bxt], [bjunk, bst8], accum_out=st8[:, 0:1])
        P.ts("vector", st8[:, 1:2], st8[:, 0:1], 1.0 / D, 1e-6, ALU.mult, ALU.add, [bst8], [bst8])
        P.act(st8[:, 2:3], st8[:, 1:2], AF.Ln, [bst8], [bst8])
        P.act(st8[:, 3:4], st8[:, 2:3], AF.Exp, [bst8], [bst8], scale=-0.5)
        P.ts("vector", xn[:], xt[:], st8[:, 3:4], None, ALU.mult, None, [bxt, bst8], [bxn])
        for k in range(8):
            P.tr(pT[:, k, :], xn[:, k * 128:(k + 1) * 128], idb[:], [bxn, bidb], [bpT])
        P.cp("vector", hcur[:, :, 0:1], hprev[:, :, 128:129], [bhprev], [bhcur])
        for k in range(8):
            P.act(hcur[:, k, 1:129], pT[:, k, :], AF.Identity, [bpT, bmcol], [bhcur],
                  bias=B1c[:, k:k + 1], scale=A1c[:, k:k + 1])
        hx = lambda k: hcur[:, k, 1:129]
        hs = lambda k: hcur[:, k, 0:128]
        for cq in range(8):
            for k in range(8):
                P.mm(pQ[:, 0:128], wda[:, k, cq * 128:(cq + 1) * 128], hx(k), [bwda, bhcur], [bpQ],
                     start=(k == 0), stop=(k == 7))
            P.cp("scalar", qf[:], pQ[:, 0:128], [bpQ], [bqf])
            P.mm(pQ[:, 128:256], pmb[:], qf[:], [bpmb, bqf], [bpQ])
            P.cp("scalar", t2[:], pQ[:, 128:256], [bpQ], [bt2])
            P.tt("vector", t1[:], qf[:], ctab[:, tsl], ALU.mult, [bqf, bctab], [bt1])
            P.tt("gpsimd", qf2[:], t2[:], stab[:, tsl], ALU.mult, [bt2, bstab], [bqf2])
            P.tt("vector", qko[:, cq, :], t1[:], qf2[:], ALU.add, [bt1, bqf2], [bqko])
        P.dma("sync", qk_d.rearrange("(c p) t -> p c t", p=128)[:, :, tsl], qko[:], [bqko], [b_qk], bqko)
        for k in range(8):
            P.mm(pV[:], hx(k), wda[:, k, 1024:1536], [bhcur, bwda], [bpV], start=(k == 0), stop=(k == 7))
        P.cp("scalar", vo[:, :, 0:128], pV[:].rearrange("p (h d) -> p h d", h=4), [bpV], [bvo])
        P.dma("sync", v_d[tsl, :], vo[:].rearrange("p h d -> p (h d)"), [bvo], [b_v], bvo)
        if DBG_STOP < 2:
            continue
        for cc, (dst, bdst) in enumerate(((r_s, br), (k_s, bk), (v_s, bv))):
            pp, bpp = pV, bpV
            for k in range(8):
                P.mm(pp[:], hx(k), w1[:, k, cc * 512:(cc + 1) * 512], [bhcur, bw1], [bpp], start=(k == 0), stop=False)
            for k in range(8):
                P.mm(pp[:], hs(k), w2m[:, k, cc * 512:(cc + 1) * 512], [bhcur, bw2m], [bpp], start=False, stop=(k == 7))
            P.cp("scalar", dst[:], pp[:], [bpp], [bdst])
            if cc == 2:
                P.cp("gpsimd", v_b[:], v_s[:], [bv], [bvb])
        for lc in range(2):
            cs_ = slice(1536 + lc * 128, 1536 + (lc + 1) * 128)
            osl = pQ[:, 256:384]
            for k in range(8):
                P.mm(osl, w1[:, k, cs_], hx(k), [bw1, bhcur], [bpQ], start=(k == 0), stop=False)
            for k in range(8):
                P.mm(osl, w2m[:, k, cs_], hs(k), [bw2m, bhcur], [bpQ], start=False, stop=(k == 7))
            if lc == 0:
                P.act(lo0[0:64, :], pQ[0:64, 256:384], AF.Tanh, [bpQ], [blo0])
                P.cp("scalar", lo0[64:128, :], pQ[64:128, 256:384], [bpQ], [blo0])
            else:
                P.act(lo1[:], pQ[:, 256:384], AF.Sigmoid, [bpQ], [blo1])
        P.mm(pV[:], lo0[0:64, :], lw2[0:64, :], [blo0, blw2], [bpV])
        P.tt("vector", sig[:], pV[:], prm[:, 0, :], ALU.add, [bpV, bprm], [bsig])
        P.act(sig[:], sig[:], AF.Sigmoid, [bsig], [bsig])
        P.mm(pV[:], lo0[64:128, :], lw2[64:128, :], [blo0, blw2], [bpV])
        P.tt("vector", a_s[:], pV[:], prm[:, 1, :], ALU.add, [bpV, bprm], [ba])
        P.act(a_s[:], a_s[:], AF.Sigmoid, [ba], [ba])
        P.mm(pV[:], lo1[:], lg2[:], [blo1, blg2], [bpV])
        P.cp("scalar", g_s[:], pV[:], [bpV], [bg])
        P.tt(rr(), kk[:], k_s[:], prm[:, 2, :], ALU.mult, [bk, bprm], [bkk])
        P.tt(rr(), tm1[:], kk[:], kk[:], ALU.mult, [bkk], [btm1])
        P.op("vector", lambda e: e.tensor_reduce(out=st8[:, 0:8], in_=tm1[:].rearrange("p (h j) -> p h j", h=8),
                                                 axis=AX.X, op=ALU.add), [btm1], [bst8])
        P.ts("vector", st8[:, 0:8], st8[:, 0:8], 1e-24, None, ALU.max, None, [bst8], [bst8])
        P.act(st8[:, 0:8], st8[:, 0:8], AF.Ln, [bst8], [bst8])
        P.act(st8[:, 0:8], st8[:, 0:8], AF.Exp, [bst8], [bst8], scale=-0.5)
        P.tt("vector", kk[:].rearrange("p (h j) -> p h j", h=8), kk[:].rearrange("p (h j) -> p h j", h=8),
             st8[:, 0:8].unsqueeze(2).to_broadcast([128, 8, 64]), ALU.mult, [bkk, bst8], [bkk])
        P.stt(rr(), tm1[:], a_s[:], -1.0, prm[:, 3, :], ALU.add, ALU.mult, [ba, bprm], [btm1])
        P.stt(rr(), km[:], tm1[:], 1.0, k_s[:], ALU.add, ALU.mult, [btm1, bk], [bkm])
        P.tt(rr(), bb[:], kk[:], a_s[:], ALU.mult, [bkk, ba], [bbb])
        P.mm(pV[:], con[:, O_UI:O_UI + 128], sig[:], [bcon, bsig], [bpV], f32=True)
        P.act(e1[:], pV[:], AF.Exp, [bpV], [be1], scale=-C0)
        P.act(e2[:], pV[:], AF.Exp, [bpV], [be2], scale=C0)
        P.tt(rr(), Rt[:], r_s[:], e1[:], ALU.mult, [br, be1], [bRt])
        P.tt(rr(), Bt[:], bb[:], e2[:], ALU.mult, [bbb, be2], [bBt])
        P.tt(rr(), Kt[:], km[:], e2[:], ALU.mult, [bkm, be2], [bKt])
        P.mm(pV[:], con[:, O_SU:O_SU + 128], sig[:], [bcon, bsig], [bpV], f32=True)
        P.act(e1[:], pV[:], AF.Exp, [bpV], [be1], scale=-C0)
        P.stt(rr(), At[:], kk[:], -1.0, e1[:], ALU.mult, ALU.mult, [bkk, be1], [bAt])
        P.mm(pV[:], con[:, O_SL:O_SL + 128], sig[:], [bcon, bsig], [bpV], f32=True)
        P.act(e2[:], pV[:], AF.Exp, [bpV], [be2], scale=-C0)
        P.tt(rr(), Bh[:], bb[:], e2[:], ALU.mult, [bbb, be2], [bBh])
        P.tt(rr(), Kh[:], km[:], e2[:], ALU.mult, [bkm, be2], [bKh])
        for pr in range(4):
            P.mm(pQ[:, 384 + pr * 2:384 + pr * 2 + 2], sig[:, pr * 128:(pr + 1) * 128], con[:, O_IND:O_IND + 2],
                 [bsig, bcon], [bpQ], f32=True)
        P.act(pc[:].rearrange("p a c -> p (a c)"), pQ[:, 384:392], AF.Exp, [bpQ], [bpc], scale=-C0)
        P.tt(rr(), tm1[:], r_s[:], km[:], ALU.mult, [br, bkm], [btm1])
        P.tt(rr(), tm1[:], tm1[:], prm[:, 4, :], ALU.mult, [btm1, bprm], [btm1])
        P.op("vector", lambda e: e.tensor_reduce(out=st8[:, 0:8], in_=tm1[:].rearrange("p (h j) -> p h j", h=8),
                                                 axis=AX.X, op=ALU.add), [btm1], [bst8])
        if DBG_STOP < 3:
            continue
        for pr in range(4):
            psl = slice(pr * 128, (pr + 1) * 128)
            for ai, (arr, barr) in enumerate(((At, bAt), (Rt, bRt), (Bt, bBt), (Kt, bKt))):
                P.tr(pV[:, ai * 128:(ai + 1) * 128], arr[:, psl], identf, [barr, bcon], [bpV], f32=True)
            if DBG_X != 1:
                P.cp("scalar", FM[:, pr, :, :], pV[:].rearrange("p (a t) -> p a t", a=4), [bpV], [bFM[pr]])
            P.cp("vector", RP[:, pr, 0:64], FM[:, pr, 1, 0:64], [bFM[pr]], [bRP[pr]])
            P.cp("vector", RP[:, pr, 192:256], FM[:, pr, 1, 64:128], [bFM[pr]], [bRP[pr]])
        if DBG_STOP < 4:
            continue
        for h in range(8):
            pr = h // 2
            ph = (h % 2) * 64
            hp = h % 2
            Gt, bGt = Gs[hp]
            A_ = FM[ph:ph + 64, pr, 0, :]
            R_ = FM[ph:ph + 64, pr, 1, :]
            B_ = FM[ph:ph + 64, pr, 2, :]
            K_ = FM[ph:ph + 64, pr, 3, :]
            bF = bFM[pr]
            P.mm(pG0[:, 0:128], B_, A_, [bF], [bpG0])
            P.mm(pG0[:, 128:256], K_, A_, [bF], [bpG0])
            P.mm(pG0[:, 256:384], B_, R_, [bF], [bpG0])
            P.mm(pG0[:, 384:512], K_, R_, [bF], [bpG0])
            P.mm(pG1[:, 0:128], A_, B_, [bF], [bpG1])
            P.tt("vector", Gt[:, 0:256], pG0[:, 0:256], con[:, O_M5:O_M5 + 256], ALU.mult, [bpG0, bcon], [bGt])
            P.tt("vector", Gt[:, 384:640], pG0[:, 256:512], con[:, O_M5 + 384:O_M5 + 640], ALU.mult, [bpG0, bcon], [bGt])
            P.tt("vector", Gt[:, 256:384], pG1[:, 0:128], con[:, O_M5 + 256:O_M5 + 384], ALU.mult, [bpG1, bcon], [bGt])
            if DBG_X == 11:
                continue
            Ncur, bNcur = Gt[:, 0:128], bGt
            Lcur, bLcur = Gt[:, 256:384], bGt
            Tcur, bTcur = Tb[hp][0]
            P.tt(rr(), Tcur[:], Gt[:, 0:128], identf, ALU.add, [bGt, bcon], [bTcur])
            Tcur = Tcur[:]
            for kx in range(1, 6):
                if DBG_X in (13, 14) and kx > 1:
                    break
                if DBG_X == 15 and kx > 2:
                    break
                Ln_, bLn = Lb[hp][kx % 2]
                i0 = 128 + (kx % 3) * 128
                P.mm(pG1[:, i0:i0 + 128], Ncur, Lcur, [bNcur, bLcur], [bpG1])
                P.cp("vector", Ln_[:], pG1[:, i0:i0 + 128], [bpG1], [bLn])
                if DBG_X == 13:
                    break
                if kx <= 4:
                    Nn_, bNn = Nb[hp][kx % 2]
                    i1 = 128 + ((kx + 1) % 3) * 128
                    P.mm(pG1[:, i1:i1 + 128], Lcur, Ncur, [bNcur, bLcur], [bpG1])
                    P.cp("vector", Nn_[:], pG1[:, i1:i1 + 128], [bpG1], [bNn])
                Tn_, bTn = Tb[hp][kx % 2]
                i2 = 128 + ((kx + 2) % 3) * 128
                P.mm(pG1[:, i2:i2 + 128], Ln_[:], Tcur, [bLn, bTcur], [bpG1])
                P.tt("vector", Tn_[:], pG1[:, i2:i2 + 128], Tcur, ALU.add, [bpG1, bTcur], [bTn])
                Lcur, bLcur = Ln_[:], bLn
                if kx <= 4:
                    Ncur, bNcur = Nn_[:], bNn
                Tcur, bTcur = Tn_[:], bTn
            if DBG_X == 12:
                continue
            S0 = ST[ph:ph + 64, pr, :]
            S0b = STb[ph:ph + 64, pr, :]
            bS = bST[h]
            Zt, bZt = Zs[hp]
            Ut, bUt = Us[hp]
            hcol = slice(h * 64, (h + 1) * 64)
            for c in range(2):
                pv = c * 64
                pSb, sb0 = (pF, 0) if hp == 0 else (pF1, 0)
                zsl = pSb[:, sb0:sb0 + 64]
                usl = pSb[:, sb0 + 64:sb0 + 128]
                ssl = pSb[:, sb0 + 128:sb0 + 192]
                bz = bu = bs_ = (bpF if hp == 0 else bpF1)
                P.mm(zsl, A_, S0b, [bF, bS], [bz], start=True, stop=False, f32=True)
                P.mm(zsl, Gt[pv:pv + 64, 128:256], v_b[pv:pv + 64, hcol], [bGt, bvb], [bz], start=False, stop=True, f32=True)
                P.cp("vector", Zt[pv:pv + 64, :], zsl[pv:pv + 64, :], [bz], [bZt])
                P.mm(usl, Tcur[pv:pv + 64, :], Zt[pv:pv + 64, :], [bTcur, bZt], [bu], f32=True)
                P.cp("vector", Ut[pv:pv + 64, :], usl[pv:pv + 64, :], [bu], [bUt])
                P.mm(pY[:, hcol], RP[ph:ph + 64, pr, c * 128:(c + 1) * 128], S0b, [bRP[pr], bS], [bpY],
                     start=(c == 0), stop=False, f32=True)
                P.mm(pY[:, hcol], Gt[pv:pv + 64, 512:640], v_b[pv:pv + 64, hcol], [bGt, bvb], [bpY], start=False, stop=False, f32=True)
                P.mm(pY[:, hcol], Gt[pv:pv + 64, 384:512], Ut[pv:pv + 64, :], [bGt, bUt], [bpY], start=False, stop=(c == 1), f32=True)
                P.mm(ssl, Bh[pv:pv + 64, pr * 128:(pr + 1) * 128], Ut[pv:pv + 64, :], [bBh, bUt], [bs_], start=True, stop=False, f32=True)
                P.mm(ssl, Kh[pv:pv + 64, pr * 128:(pr + 1) * 128], v_b[pv:pv + 64, hcol], [bKh, bvb], [bs_], start=False, stop=True, f32=True)
                P.stt("vector", S0, S0, pc[ph:ph + 64, pr, c:c + 1], ssl[ph:ph + 64, :], ALU.mult, ALU.add,
                      [bS, bpc, bs_], [bS])
                P.cp("gpsimd", S0b, S0, [bS], [bS])
        if DBG_STOP < 5:
            continue
        v3 = lambda ap: ap.rearrange("p (h j) -> p h j", h=8)
        P.cp("scalar", ysb[:], pY[:], [bpY], [bysb])
        P.op("vector", lambda e, ysb=ysb: e.tensor_reduce(out=t1[:, 0:8], in_=ysb.rearrange("p (h j) -> p h j", h=8),
                                                          axis=AX.X, op=ALU.add), [bysb], [bt1])
        P.ts("vector", t1[:, 0:8], t1[:, 0:8], -1.0 / 64, None, ALU.mult, None, [bt1], [bt1])
        P.tt("vector", v3(ysb[:]), v3(ysb[:]), t1[:, 0:8].unsqueeze(2).to_broadcast([128, 8, 64]), ALU.add, [bysb, bt1], [bysb])
        P.tt(rr(), tm1[:], ysb[:], ysb[:], ALU.mult, [bysb], [btm1])
        P.op("vector", lambda e: e.tensor_reduce(out=t1[:, 8:16], in_=v3(tm1[:]), axis=AX.X, op=ALU.add), [btm1], [bt1])
        P.ts("vector", t1[:, 8:16], t1[:, 8:16], 1.0 / 64, 64e-5, ALU.mult, ALU.add, [bt1], [bt1])
        P.act(t1[:, 8:16], t1[:, 8:16], AF.Ln, [bt1], [bt1])
        P.act(t1[:, 8:16], t1[:, 8:16], AF.Exp, [bt1], [bt1], scale=-0.5)
        P.tt("vector", v3(ysb[:]), v3(ysb[:]), t1[:, 8:16].unsqueeze(2).to_broadcast([128, 8, 64]), ALU.mult, [bysb, bt1], [bysb])
        P.tt(rr(), ysb[:], ysb[:], prm[:, 5, :], ALU.mult, [bysb, bprm], [bysb])
        P.tt(rr(), ysb[:], ysb[:], prm[:, 6, :], ALU.add, [bysb, bprm], [bysb])
        P.tt("vector", v3(tm1[:]), v3(v_s[:]), st8[:, 0:8].unsqueeze(2).to_broadcast([128, 8, 64]), ALU.mult, [bv, bst8], [btm1])
        P.tt(rr(), ysb[:], ysb[:], tm1[:], ALU.add, [bysb, btm1], [bysb])
        P.tt("vector", yo[:], ysb[:], g_s[:], ALU.mult, [bysb, bg], [byo])
        P.dma("sync", yrw_d[tsl, :], yo[:], [byo], [b_yrw], byo)


def phase_attn(P, nc, G):
    P.noself = NOSELF_ATTN
    con_d, modp_d, lamv_d, subln_d, wout_d, rtw_d, rtb_d = (G[k] for k in
        ("con_d", "modp_d", "lamv_d", "subln_d", "wout_d", "rtw_d", "rtb_d"))
    x_d, qk_d, v_d, yrw_d, x1_d, h2T_d, gat_d = (G[k] for k in ("x_d", "qk_d", "v_d", "yrw_d", "x1_d", "h2T_d", "gat_d"))
    b_modp, b_qk, b_v, b_yrw, b_x1, b_h2T, b_gat = (G[k] for k in
        ("b_modp", "b_qk", "b_v", "b_yrw", "b_x1", "b_h2T", "b_gat"))
    rr = _rr(P)
    con, bcon = P.sb("con", [128, NCONST], F32)
    P.dma("sync", con[:], con_d, [], [bcon], bcon)
    identf = con[:, O_ID:O_ID + 128]
    idb, bidb = P.sb("idb", [128, 128], BF16)
    P.cp("vector", idb[:], identf, [bcon], [bidb])
    cmk, bcmk = P.sb("cmk", [128, 4, 512], BF16)
    P.cp("vector", cmk[:].rearrange("p a t -> p (a t)"), con[:, O_CM:O_CM + 2048], [bcon], [bcmk])
    rows, brows = P.sb("rows", [128, 3, D], F32)
    P.dma("sync", rows[:].rearrange("p a d -> p (a d)"),
          modp_d[2:5, :].rearrange("(o a) d -> o (a d)", o=1).partition_broadcast(128), [b_modp], [brows], brows)
    mcol, bmcol = P.sb("mcol", [128, 6, 8], F32)
    P.dma("sync", mcol[:], modp_d.rearrange("a (k p) -> p a k", p=128), [b_modp], [bmcol], bmcol,
          allow_slow_non_contiguous=True)
    lv, blv = P.sb("lv", [128, 4, 64], F32)
    P.dma("sync", lv[:].rearrange("p a d -> p (a d)"),
          lamv_d.rearrange("(o a) d -> o (a d)", o=1).partition_broadcast(128), [], [blv], blv)
    lam, blam = P.sb("lam", [128, 8], F32)
    lt, blt = P.sb("lt", [128, 2, 64], F32)
    P.tt("vector", lt[:, 0, :], lv[:, 0, :], lv[:, 1, :], ALU.mult, [blv], [blt])
    P.tt("vector", lt[:, 1, :], lv[:, 2, :], lv[:, 3, :], ALU.mult, [blv], [blt])
    P.op("vector", lambda e: e.tensor_reduce(out=lam[:, 0:2], in_=lt[:], axis=AX.X, op=ALU.add), [blt], [blam])
    P.act(lam[:, 2:4], lam[:, 0:2], AF.Exp, [blam], [blam])
    P.tt("vector", lam[:, 4:5], lam[:, 2:3], lam[:, 3:4], ALU.subtract, [blam], [blam])
    P.ts("vector", lam[:, 5:6], lam[:, 4:5], -1.0, -LAMBDA_INIT, ALU.mult, ALU.add, [blam], [blam])
    sub, bsub = P.sb("sub", [128, 128], F32)
    P.dma("sync", sub[:], subln_d.partition_broadcast(128), [], [bsub], bsub)
    P.ts("vector", sub[:], sub[:], 1.0 - LAMBDA_INIT, None, ALU.mult, None, [bsub], [bsub])
    wo, bwo = P.sb("wo", [128, 8, D], BF16)
    P.dma("gpsimd", wo[:], wout_d.rearrange("(k p) c -> p k c", p=128), [], [bwo], bwo)
    rw, brw = P.sb("rw", [128, 8, NE], BF16)
    P.dma("gpsimd", rw[:], rtw_d.rearrange("(k p) c -> p k c", p=128), [], [brw], brw)
    rb, brb = P.sb("rb", [128, NE], F32)
    P.dma("sync", rb[:], rtb_d.partition_broadcast(128), [], [brb], brb)
    kT, bkT = P.sb("kT", [128, 4, T], BF16)
    P.dma("sync", kT[:], qk_d[512:1024, :].rearrange("(c p) t -> p c t", p=128), [b_qk], [bkT], bkT)
    vv, bvv = P.sb("vv", [128, NT, 4 * 129], BF16)
    P.dma("sync", vv[:], v_d.rearrange("(n p) f -> p n f", p=128), [b_v], [bvv], bvv)
    qT = [P.sb(f"qT{i}", [128, 4, 512], BF16) for i in range(2)]
    pt = [P.sb(f"pt{i}", [128, 512], BF16) for i in range(3)]
    psc = [P.ps(f"psc{i}", [128, 512], F32) for i in range(2)]
    po = [P.ps(f"po{i}", [128, 4, 128], F32) for i in range(2)]
    pms, bpms_ = P.ps("pms", [128, 512], F32)
    pos_ = pms[:, 0:128].rearrange("p (a b c) -> p a b c", a=2, b=4)
    bpos_ = P.buf("possum")
    pw, bpw = P.ps("pw", [128, D], F32)
    ptr, bptr = P.ps("ptr", [128, 8, 128], BF16)
    prt = pms[:, 128:256]
    bprt = bpos_
    YC, bYC_ = P.sb("YC", [128, 4, D], BF16)
    bYC = [P.buf("YCs") for _ in range(4)]
    ycT, bycT = P.sb("ycT", [128, 8, 128], BF16)
    o0, bo0 = P.sb("o0", [128, 128], F32)
    o1, bo1 = P.sb("o1", [128, 128], F32)
    rs, brs = P.sb("rs", [128, 16], F32)
    XT4 = [P.sb(f"axt{i}", [128, D], F32) for i in range(4)]
    YR4 = [P.sb(f"ayr{i}", [128, 512], BF16) for i in range(4)]
    y1, by1 = P.sb("y1", [128, D], F32)
    junk, bjunk = P.sb("junk", [128, D], BF16)
    h2, bh2 = P.sb("h2", [128, D], BF16)
    h2T, bh2T = P.sb("h2T", [128, 8, 128], BF16)
    lg, blg = P.sb("lg", [128, NE], F32)
    gt, bgt = P.sb("gt", [128, NE], F32)
    t8, bt8 = P.sb("t8", [128, 16], F32)

    it = 0
    for qb in range(8):
        qcur, bqcur = qT[qb % 2]
        if qb == 0:
            P.dma("sync", qcur[:], qk_d[0:512, 0:512].rearrange("(c p) t -> p c t", p=128), [b_qk], [bqcur], bqcur)
        if qb + 1 < 8:
            qn_, bqn_ = qT[(qb + 1) % 2]
            P.dma("sync", qn_[:], qk_d[0:512, (qb + 1) * 512:(qb + 2) * 512].rearrange("(c p) t -> p c t", p=128),
                  [b_qk], [bqn_], bqn_)
        for s4 in range(4):
            n_ = qb * 4 + s4
            P.dma("sync", YR4[s4][0][:], yrw_d[n_ * 128:(n_ + 1) * 128, :], [b_yrw], [YR4[s4][1]], YR4[s4][1])
            P.dma("sync", XT4[s4][0][:], x_d[n_ * 128:(n_ + 1) * 128, :], [], [XT4[s4][1]], XT4[s4][1])
        ntk = (qb + 1) * 4
        for hd in range(4):
            for mp in range(2):
                m = hd * 2 + mp
                chn, pb = m // 2, (m % 2) * 64
                pot, bpot = po[mp]
                for tk in range(ntk):
                    ps_, bps_ = psc[it % 2]
                    ptile, bptile = pt[it % 3]
                    it += 1
                    P.mm(ps_[:], kT[pb:pb + 64, chn, tk * 128:(tk + 1) * 128], qcur[pb:pb + 64, chn, :],
                         [bkT, bqcur], [bps_])
                    P.act(ptile[:], ps_[:], AF.Exp, [bps_], [bptile], scale=0.125)
                    j = tk - qb * 4
                    if j >= 0:
                        P.tt("vector", ptile[:], ptile[:], cmk[:, j, :], ALU.mult, [bptile, bcmk], [bptile])
                    for s4 in range(4):
                        if j > s4:
                            continue
                        P.mm(pot[:, s4, :], ptile[:, s4 * 128:(s4 + 1) * 128], vv[:, tk, hd * 129:hd * 129 + 128],
                             [bptile, bvv], [bpot], start=(tk == 0 and s4 == 0), stop=(tk == ntk - 1 and s4 == 3))
                        P.mm(pos_[:, mp, s4, 0:1], ptile[:, s4 * 128:(s4 + 1) * 128], vv[:, tk, hd * 129 + 128:hd * 129 + 129],
                             [bptile, bvv], [bpos_], start=(mp == 0 and tk == 0 and s4 == 0),
                             stop=(mp == 1 and tk == ntk - 1 and s4 == 3))
            P.cp("vector", rs[:, 0:8].rearrange("p (a b) -> p a b", a=2), pos_[:, :, :, 0], [bpos_], [brs])
            P.op("vector", lambda e: e.reciprocal(out=rs[:, 8:16], in_=rs[:, 0:8]), [brs], [brs])
            P.ts("vector", rs[:, 12:16], rs[:, 12:16], lam[:, 5:6], None, ALU.mult, None, [brs, blam], [brs])
            for s4 in range(4):
                P.ts("vector", o0[:], po[0][0][:, s4, :], rs[:, 8 + s4:9 + s4], None, ALU.mult, None, [po[0][1], brs], [bo0])
                P.stt("vector", o0[:], po[1][0][:, s4, :], rs[:, 12 + s4:13 + s4], o0[:], ALU.mult, ALU.add,
                      [po[1][1], brs, bo0], [bo0])
                P.act(o1[:], o0[:], AF.Square, [bo0], [bo1, bt8], accum_out=t8[:, 0:1])
                P.ts("vector", t8[:, 1:2], t8[:, 0:1], 1.0 / 128, 1e-5, ALU.mult, ALU.add, [bt8], [bt8])
                P.act(t8[:, 2:3], t8[:, 1:2], AF.Ln, [bt8], [bt8])
                P.act(t8[:, 3:4], t8[:, 2:3], AF.Exp, [bt8], [bt8], scale=-0.5)
                P.stt("vector", YC[:, s4, hd * 128:(hd + 1) * 128], o0[:], t8[:, 3:4], sub[:], ALU.mult, ALU.mult,
                      [bo0, bt8, bsub], [bYC[s4]])
        for s4 in range(4):
            n = qb * 4 + s4
            tsl = slice(n * 128, (n + 1) * 128)
            yrt, byrt = YR4[s4]
            xt, bxt = XT4[s4]
            P.cp("vector", YC[:, s4, 512:1024], yrt[:], [byrt], [bYC[s4]])
            for k in range(8):
                P.tr(ptr[:, k, :], YC[:, s4, k * 128:(k + 1) * 128], idb[:], [bYC[s4], bidb], [bptr])
            P.cp("scalar", ycT[:].rearrange("p k t -> p (k t)"), ptr[:].rearrange("p k t -> p (k t)"), [bptr], [bycT])
            for hf in range(2):
                for k in range(8):
                    P.mm(pw[:, hf * 512:(hf + 1) * 512], ycT[:, k, :], wo[:, k, hf * 512:(hf + 1) * 512],
                         [bycT, bwo], [bpw], start=(k == 0), stop=(k == 7))
            P.cp("scalar", y1[:], pw[:], [bpw], [by1])
            P.act(junk[:], y1[:], AF.Square, [by1], [bjunk, bt8], accum_out=t8[:, 4:5])
            P.ts("vector", t8[:, 5:6], t8[:, 4:5], 1.0 / D, 1e-6, ALU.mult, ALU.add, [bt8], [bt8])
            P.act(t8[:, 6:7], t8[:, 5:6], AF.Ln, [bt8], [bt8])
            P.act(t8[:, 7:8], t8[:, 6:7], AF.Exp, [bt8], [bt8], scale=-0.5)
            P.stt("vector", y1[:], y1[:], t8[:, 7:8], rows[:, 0, :], ALU.mult, ALU.mult, [by1, bt8, brows], [by1])
            P.tt("gpsimd", xt[:], xt[:], y1[:], ALU.add, [bxt, by1], [bxt])
            P.dma("sync", x1_d[tsl, :], xt[:], [bxt], [b_x1], bxt)
            P.act(junk[:], xt[:], AF.Square, [bxt], [bjunk, bt8], accum_out=t8[:, 8:9])
            P.ts("vector", t8[:, 9:10], t8[:, 8:9], 1.0 / D, 1e-6, ALU.mult, ALU.add, [bt8], [bt8])
            P.act(t8[:, 10:11], t8[:, 9:10], AF.Ln, [bt8], [bt8])
            P.act(t8[:, 11:12], t8[:, 10:11], AF.Exp, [bt8], [bt8], scale=-0.5)
            P.ts("vector", h2[:], xt[:], t8[:, 11:12], None, ALU.mult, None, [bxt, bt8], [bh2])
            for k in range(8):
                P.tr(ptr[:, k, :], h2[:, k * 128:(k + 1) * 128], idb[:], [bh2, bidb], [bptr])
            for k in range(8):
                P.act(h2T[:, k, :], ptr[:, k, :], AF.Identity, [bptr, bmcol], [bh2T],
                      bias=mcol[:, 4, k:k + 1], scale=mcol[:, 3, k:k + 1])
            P.dma("sync", h2T_d.rearrange("(k p) t -> p k t", p=128)[:, :, tsl], h2T[:], [bh2T], [b_h2T], bh2T)
            for k in range(8):
                P.mm(prt[:, 0:NE], h2T[:, k, :], rw[:, k, :], [bh2T, brw], [bprt], start=(k == 0), stop=(k == 7))
            P.tt("vector", lg[:], prt[:, 0:NE], rb[:], ALU.add, [bprt, brb], [blg])
            P.op("vector", lambda e: e.max(out=t8[:, 0:8], in_=lg[:]), [blg], [bt8])
            P.ts("vector", gt[:], lg[:], t8[:, 3:4], None, ALU.is_ge, None, [blg, bt8], [bgt])
            P.ts("vector", t8[:, 12:13], t8[:, 0:1], -1.0, None, ALU.mult, None, [bt8], [bt8])
            P.act(lg[:], lg[:], AF.Exp, [blg, bt8], [blg], bias=t8[:, 12:13], scale=1.0)
            P.tt("vector", gt[:], gt[:], lg[:], ALU.mult, [bgt, blg], [bgt])
            P.op("vector", lambda e: e.tensor_reduce(out=t8[:, 13:14], in_=gt[:], axis=AX.X, op=ALU.add), [bgt], [bt8])
            P.op("vector", lambda e: e.reciprocal(out=t8[:, 14:15], in_=t8[:, 13:14]), [bt8], [bt8])
            P.ts("vector", gt[:], gt[:], t8[:, 14:15], None, ALU.mult, None, [bgt, bt8], [bgt])
            P.dma("sync", gat_d[tsl, :], gt[:], [bgt], [b_gat], bgt)


def phase_moe(P, nc, G):
    P.noself = NOSELF_MOE
    modp_d, x1_d, h2T_d, gat_d, out_d = (G[k] for k in ("modp_d", "x1_d", "h2T_d", "gat_d", "out_d"))
    w1g_d, w1l_d, w2e_d, b1T_d, b2_d, con_d = (G[k] for k in ("w1g_d", "w1l_d", "w2e_d", "b1T_d", "b2_d", "con_d"))
    b_modp, b_x1, b_h2T, b_gat, b_out = (G[k] for k in ("b_modp", "b_x1", "b_h2T", "b_gat", "b_out"))
    idf, bidf = P.sb("idf", [128, 128], F32)
    P.dma("sync", idf[:], con_d[:, O_ID:O_ID + 128], [], [bidf], bidf)
    c2r, bc2r = P.sb("c2r", [128, D], F32)
    P.dma("sync", c2r[:], modp_d[5:6, :].partition_broadcast(128), [b_modp], [bc2r], bc2r)
    b1T, bb1T = P.sb("b1T", [128, NE, 16], F32)
    P.dma("sync", b1T[:].rearrange("p e c -> p (e c)"), b1T_d, [], [bb1T], bb1T)
    b2s, bb2s = P.sb("b2s", [NE, D], F32)
    P.dma("sync", b2s[:], b2_d, [], [bb2s], bb2s)
    W = [[P.sb(f"w{j}_{i}", [128, 8, D], BF16) for j in range(3)] for i in range(2)]
    hq, bhq = P.sb("hq", [128, 8, 1024], BF16)
    gq, bgq = P.sb("gq", [128, 8, NE], F32)
    gT, bgT = P.sb("gT", [NE, 128], F32)
    acc, bacc_ = P.sb("acc", [128, 8, D], F32)
    bacc = [P.buf("acct") for _ in range(8)]
    actT, bactT_ = P.sb("actT", [128, 2, 8, 512], BF16)
    bactT = [[P.buf("actc") for _ in range(8)] for _ in range(2)]
    GG = [P.sb(f"mg{i}", [128, 512], F32) for i in range(2)]
    SS = [P.sb(f"msg{i}", [128, 512], F32) for i in range(2)]
    LL = [P.sb(f"ml{i}", [128, 512], F32) for i in range(2)]
    b1p, bb1p = P.sb("b1p", [128, NE, 8], F32)
    P.ts("vector", b1p[:], b1T[:, :, 8:16], 1.0, None, ALU.add, None, [bb1T], [bb1p])
    xt, bxt = P.sb("mxt", [128, D], F32)
    junk, bjunk = P.sb("mjunk", [128, D], BF16)
    t8, bt8 = P.sb("mt8", [128, 8], F32)
    pg = [P.ps(f"pg{i}", [128, 512], F32) for i in range(2)]
    pl = [P.ps(f"pl{i}", [128, 512], F32) for i in range(2)]
    po = [P.ps(f"pmo{i}", [128, 512], F32) for i in range(2)]
    pm, bpm = P.ps("pmisc", [128, 512], F32)
    it = 0
    io = 0
    wi = 0
    stepi = 0
    for qt in range(DBG_NQ):
        q0 = qt * 1024
        P.dma("sync", hq[:], h2T_d.rearrange("(k p) t -> p k t", p=128)[:, :, q0:q0 + 1024], [b_h2T], [bhq], bhq)
        P.dma("sync", gq[:], gat_d[q0:q0 + 1024, :].rearrange("(n p) e -> p n e", p=128), [b_gat], [bgq], bgq)
        for n in range(8):
            P.tr(pm[0:NE, 0:128], gq[:, n, :], idf[:], [bgq, bidf], [bpm], f32=True)
            P.cp("vector", gT[:], pm[0:NE, 0:128], [bpm], [bgT])
            for hf in range(2):
                pp, bpp = po[io % 2]
                io += 1
                P.mm(pp[:], gT[:], b2s[:, hf * 512:(hf + 1) * 512], [bgT, bb2s], [bpp], f32=True)
                P.cp("scalar", acc[:, n, hf * 512:(hf + 1) * 512], pp[:], [bpp], [bacc[n]])
        def hid(e, blk, Wset, ab):
            nonlocal it
            (w1g, bw1g), (w1l, bw1l), (w2, bw2) = Wset
            bsl = slice(blk * 512, (blk + 1) * 512)
            for fc in range(8):
                pgt, bpgt = pg[it % 2]
                plt, bplt = pl[it % 2]
                it += 1
                for k in range(8):
                    P.mm(pgt[:], w1g[:, k, fc * 128:(fc + 1) * 128], hq[:, k, bsl], [bw1g, bhq], [bpgt],
                         start=(k == 0), stop=(k == 7))
                for k in range(8):
                    P.mm(plt[:], w1l[:, k, fc * 128:(fc + 1) * 128], hq[:, k, bsl], [bw1l, bhq], [bplt],
                         start=(k == 0), stop=(k == 7))
                gi, bgi = GG[it % 2]
                si, bsi = SS[it % 2]
                li, bli = LL[it % 2]
                P.ts("vector", gi[:], pgt[:], b1T[:, e, fc:fc + 1], 7.0, ALU.add, ALU.min, [bpgt, bb1T], [bgi])
                P.act(si[:], gi[:], AF.Sigmoid, [bgi], [bsi], scale=1.702)
                P.act(li[:], plt[:], AF.Identity, [bplt, bb1p], [bli], bias=b1p[:, e, fc:fc + 1], scale=1.0)
                P.ts("vector", li[:], li[:], -6.0, 8.0, ALU.max, ALU.min, [bli], [bli])
                P.tt("gpsimd", si[:], si[:], gi[:], ALU.mult, [bsi, bgi], [bsi])
                P.tt("vector", actT[:, ab, fc, :], si[:], li[:], ALU.mult, [bsi, bli], [bactT[ab][fc]])

        def second(e, blk, Wset, ab):
            nonlocal io
            (w1g, bw1g), (w1l, bw1l), (w2, bw2) = Wset
            for tt_ in range(4):
                n = blk * 4 + tt_
                for hf in range(2):
                    pp, bpp = po[io % 2]
                    io += 1
                    for fc in range(8):
                        P.mm(pp[:], actT[:, ab, fc, tt_ * 128:(tt_ + 1) * 128], w2[:, fc, hf * 512:(hf + 1) * 512],
                             [bactT[ab][fc], bw2], [bpp], start=(fc == 0), stop=(fc == 7))
                    P.stt("vector", acc[:, n, hf * 512:(hf + 1) * 512], pp[:], gq[:, n, e:e + 1],
                          acc[:, n, hf * 512:(hf + 1) * 512], ALU.mult, ALU.add, [bpp, bgq, bacc[n]], [bacc[n]])

        prev = None
        for e in range(DBG_NEXP):
            Wset = W[wi % 2]
            (w1g, bw1g), (w1l, bw1l), (w2, bw2) = Wset
            wi += 1
            P.dma("gpsimd", w1g[:], w1g_d[e].rearrange("(k p) f -> p k f", p=128), [], [bw1g], bw1g)
            P.dma("gpsimd", w1l[:], w1l_d[e].rearrange("(k p) f -> p k f", p=128), [], [bw1l], bw1l)
            P.dma("gpsimd", w2[:], w2e_d[e].rearrange("(k p) f -> p k f", p=128), [], [bw2], bw2)
            for blk in range(2):
                ab = stepi % 2
                stepi += 1
                hid(e, blk, Wset, ab)
                if prev is not None:
                    second(*prev)
                prev = (e, blk, Wset, ab)
        second(*prev)
        for n in range(8):
            tsl = slice(q0 + n * 128, q0 + (n + 1) * 128)
            P.dma("sync", xt[:], x1_d[tsl, :], [b_x1], [bxt], bxt)
            P.act(junk[:], acc[:, n, :], AF.Square, [bacc[n]], [bjunk, bt8], accum_out=t8[:, 0:1])
            P.ts("vector", t8[:, 1:2], t8[:, 0:1], 1.0 / D, 1e-6, ALU.mult, ALU.add, [bt8], [bt8])
            P.act(t8[:, 2:3], t8[:, 1:2], AF.Ln, [bt8], [bt8])
            P.act(t8[:, 3:4], t8[:, 2:3], AF.Exp, [bt8], [bt8], scale=-0.5)
            P.stt("vector", acc[:, n, :], acc[:, n, :], t8[:, 3:4], c2r[:], ALU.mult, ALU.mult, [bacc[n], bt8, bc2r], [bacc[n]])
            P.tt("vector", xt[:], xt[:], acc[:, n, :], ALU.add, [bxt, bacc[n]], [bxt])
            P.dma("sync", out_d[tsl, :], xt[:], [bxt], [b_out], bxt)


def _consts():
    c = np.zeros((128, NCONST), np.float32)
    r = np.arange(128)[:, None]
    q = np.arange(128)[None, :]
    same = (r // 64) == (q // 64)
    su = (same & ((r % 64) < (q % 64))).astype(np.float32)
    sl = (same & ((r % 64) > (q % 64))).astype(np.float32)
    ui = (same & ((r % 64) <= (q % 64))).astype(np.float32)
    c[:, O_ID:O_ID + 128] = np.eye(128, dtype=np.float32)
    for i, m in enumerate((su, su, sl, ui, ui)):
        c[:, O_M5 + i * 128:O_M5 + (i + 1) * 128] = m
    c[:, O_ONES:O_ONES + 128] = same.astype(np.float32)
    c[:64, O_IND] = 1.0
    c[64:, O_IND + 1] = 1.0
    inv_freq = (500000.0 ** (-np.arange(0, 16, 2, dtype=np.float32) / 16)).astype(np.float32)
    for p in range(128):
        d = p % 64
        if d < 16:
            c[p, O_FREQ] = inv_freq[d % 8]
            c[p, O_SIGN] = -1.0 if d < 8 else 1.0
            pp = p + 8 if d < 8 else p - 8
            c[pp, O_PM + p] = 1.0
    tq = np.arange(512)[None, :]
    tk = np.arange(128)[:, None]
    for j in range(4):
        c[:, O_CM + j * 512:O_CM + (j + 1) * 512] = ((j * 128 + tk) <= tq).astype(np.float32)
    return c


_NC_CACHE = {}


def _in_maps(inp):
    f = lambda a: np.ascontiguousarray(np.asarray(a, dtype=np.float32))
    B = 8
    w1 = np.asarray(inp["moe_w1"])[0]
    w1g = np.ascontiguousarray(w1[:, :, 0::2])
    w1l = np.ascontiguousarray(w1[:, :, 1::2])
    b1 = np.asarray(inp["moe_b1"])[0]
    b1cat = np.concatenate([b1[:, 0::2].reshape(NE, 8, 128), b1[:, 1::2].reshape(NE, 8, 128)], axis=1)
    b1T = np.ascontiguousarray(b1cat.transpose(2, 0, 1).reshape(128, NE * 16)).astype(np.float32)
    shared = dict(
        ada_w=f(inp["ada_w"][0]), ada_b=f(inp["ada_b"]).reshape(1, 6 * D),
        norms=f(np.stack([inp["pre_mix_norm"][0], inp["post_mix_norm"][0], inp["pre_ffn_norm"][0], inp["post_ffn_norm"][0]])),
        w_in=f(inp["w_in"][0]), w_out=f(inp["w_out"][0]),
        lamv=f(np.stack([inp["da_lambda_q1"][0], inp["da_lambda_k1"][0], inp["da_lambda_q2"][0], inp["da_lambda_k2"][0]])),
        subln=f(inp["da_subln"]).reshape(1, 128), mu=f(inp["rw_mu"]).reshape(1, 1792),
        rwv=f(np.stack([inp["rw_w0"][0], inp["rw_a0"][0], inp["rw_k_k"][0], inp["rw_k_a"][0],
                        np.asarray(inp["rw_r_k"])[0].reshape(512), inp["rw_ln_w"][0], inp["rw_ln_b"][0]])),
        rw_w2=f(inp["rw_w2"][0]), rw_a2=f(inp["rw_a2"][0]), rw_g2=f(inp["rw_g2"][0]),
        router_w=f(inp["router_w"][0]), router_b=f(inp["router_b"]).reshape(1, NE),
        w1g=w1g, w1l=w1l, b1T=b1T, w2e=f(inp["moe_w2"][0]), b2=f(inp["moe_b2"][0]),
        consts=_consts(),
    )
    x = np.asarray(inp["x"], dtype=np.float32)
    c = np.asarray(inp["c"], dtype=np.float32)
    pos = np.asarray(inp["positions"]).astype(np.int32)
    in_maps = []
    for b in range(B):
        m = dict(shared)
        m["x"] = np.ascontiguousarray(x[b])
        m["cT"] = np.ascontiguousarray(c[b].reshape(8, 128).T)
        m["pos"] = np.ascontiguousarray(pos[b].reshape(1, T))
        in_maps.append(m)
    return in_maps


def kernel(**inp):
    B = 8
    if "nc" not in _NC_CACHE:
        _NC_CACHE["nc"] = build_program()
    nc = _NC_CACHE["nc"]
    in_maps = _in_maps(inp)
    res = run_bass_kernel_spmd(nc, in_maps, core_ids=list(range(B)))
    return np.stack([np.asarray(r["out"], dtype=np.float32) for r in res.results], axis=0)
```

```python
import contextlib
import math
import numpy as np
import concourse.bass as bass
import concourse.mybir as mybir
from concourse.bass_utils import run_bass_kernel_spmd

ALU = mybir.AluOpType
AF = mybir.ActivationFunctionType
F32 = mybir.dt.float32
BF16 = mybir.dt.bfloat16
I32 = mybir.dt.int32
AX = mybir.AxisListType

D = 1024
T = 4096
NT = 32
NE = 32
C0 = math.exp(-0.5)
LAMBDA_INIT = 0.8 - 0.6 * math.exp(0.0)

O_ID = 0
O_M5 = 128
O_UI = O_M5 + 384
O_SU = O_M5
O_SL = O_M5 + 256
O_ONES = 768
O_IND = 896
O_FREQ = 898
O_SIGN = 899
O_PM = 900
O_CM = 1028
NCONST = O_CM + 2048
DBG_STOP = 99
NOSELF_MOE = ('tensor',)
NOSELF_ATTN = ('tensor',)
NOSELF_FRONT = ('tensor',)
DBG_X = 0
DBG_NQ = 4
DBG_NEXP = NE
DBG_NT = NT


class Buf:
    __slots__ = ("name", "w", "r", "dsem", "dcount")

    def __init__(self, name):
        self.name = name
        self.w = {}
        self.r = {}
        self.dsem = None
        self.dcount = 0


class Eng:
    def __init__(self, name, sem):
        self.name = name
        self.sem = sem
        self.count = 0
        self.waited = {}
        self.thunks = []


class Prog:
    def __init__(self, nc, stack):
        self.nc = nc
        self.gstack = stack
        self.stack = stack
        self.engs = {}
        self.sems = {}
        self.vals = {}
        for n in ("tensor", "vector", "scalar", "gpsimd", "sync"):
            sem = stack.enter_context(nc.semaphore("es_" + n))
            self.engs[n] = Eng(n, sem)
            self.sems[("e", n)] = sem
            self.vals[("e", n)] = 0
        self.nbuf = 0
        self.ninstr = 0
        self.allbufs = []
        self.gen = 0
        self.noself = ()
        self.last_f32 = False

    def buf(self, name=None):
        self.nbuf += 1
        b = Buf(f"{name or 'b'}{self.nbuf}")
        self.allbufs.append(b)
        return b

    def new_engine_sems(self):
        self.gen += 1
        for n, eng in self.engs.items():
            old = ("e", n)
            self.vals.pop(old, None)
            sem = self.gstack.enter_context(self.nc.semaphore(f"es{self.gen}_{n}"))
            eng.sem = sem
            eng.count = 0
            eng.waited = {}
            self.sems[old] = sem
            self.vals[old] = 0
        for b in self.allbufs:
            b.w = {}
            b.r = {}

    def sb(self, name, shape, dtype):
        self.nbuf += 1
        t = self.stack.enter_context(self.nc.sbuf_tensor(f"sb{self.nbuf}_{name}", list(shape), dtype))
        return t, self.buf(name)

    def ps(self, name, shape, dtype=F32):
        self.nbuf += 1
        t = self.stack.enter_context(self.nc.psum_tensor(f"ps{self.nbuf}_{name}", list(shape), dtype))
        return t, self.buf(name)

    def _dsem(self, b):
        if b.dsem is None:
            s = self.gstack.enter_context(self.nc.semaphore("ds_" + b.name))
            b.dsem = ("d", b.name)
            self.sems[b.dsem] = s
            self.vals[b.dsem] = 0
        return b.dsem

    def _collect(self, eng, reads, writes):
        need = {}
        for b in reads:
            for k, v in b.w.items():
                if need.get(k, 0) < v:
                    need[k] = v
        for b in writes:
            for k, v in b.w.items():
                if need.get(k, 0) < v:
                    need[k] = v
            for k, v in b.r.items():
                if need.get(k, 0) < v:
                    need[k] = v
        own = ("e", eng.name)
        for k, v in need.items():
            if k == own and eng.name in self.noself:
                continue
            if eng.waited.get(k, 0) < v:
                eng.waited[k] = v
                sem = self.sems[k]
                eng.thunks.append(lambda e, sem=sem, v=v: e.wait_ge(sem, v))

    def op(self, engname, fn, reads=(), writes=(), selfwait=False):
        eng = self.engs[engname]
        self._collect(eng, reads, writes)
        if selfwait and eng.count > 0:
            own = ("e", engname)
            if eng.waited.get(own, 0) < eng.count:
                eng.waited[own] = eng.count
                eng.thunks.append(lambda e, sem=eng.sem, v=eng.count: e.wait_ge(sem, v))
        eng.count += 1
        c = eng.count
        sem = eng.sem
        eng.thunks.append(lambda e, fn=fn, sem=sem: fn(e).then_inc(sem, 1))
        key = ("e", engname)
        self.vals[key] = c
        for b in reads:
            b.r[key] = c
        for b in writes:
            b.w = {key: c}
            b.r = {}
        self.ninstr += 1

    def dma(self, q, out_ap, in_ap, reads, writes, sbuf_buf, **kw):
        eng = self.engs[q]
        self._collect(eng, reads, writes)
        key = self._dsem(sbuf_buf)
        sbuf_buf.dcount += 16
        c = sbuf_buf.dcount
        self.vals[key] = c
        sem = self.sems[key]
        eng.thunks.append(
            lambda e, o=out_ap, i=in_ap, sem=sem, kw=kw: e.dma_start(out=o, in_=i, **kw).then_inc(sem, 16))
        for b in reads:
            b.r[key] = c
        for b in writes:
            if b is sbuf_buf:
                b.w = {key: c}
                b.r = {}
            else:
                b.w[key] = c
        self.ninstr += 1

    def barrier(self):
        for eng in self.engs.values():
            for k, v in self.vals.items():
                if v > 0 and eng.waited.get(k, 0) < v:
                    eng.waited[k] = v
                    sem = self.sems[k]
                    eng.thunks.append(lambda e, sem=sem, v=v: e.wait_ge(sem, v))

    def flush(self):
        nc = self.nc
        engs = self.engs
        with nc.Block() as block:
            @block.tensor
            def _(e):
                for t in engs["tensor"].thunks:
                    t(e)

            @block.vector
            def _(e):
                for t in engs["vector"].thunks:
                    t(e)

            @block.scalar
            def _(e):
                for t in engs["scalar"].thunks:
                    t(e)

            @block.gpsimd
            def _(e):
                for t in engs["gpsimd"].thunks:
                    t(e)

            @block.sync
            def _(e):
                for t in engs["sync"].thunks:
                    t(e)
        for e in engs.values():
            e.thunks = []

    @contextlib.contextmanager
    def phase(self):
        with contextlib.ExitStack() as ph:
            self.stack = ph
            yield
            self.barrier()
            self.flush()
        self.stack = self.gstack
        self.new_engine_sems()

    def mm(self, out, lhsT, rhs, R, W, start=True, stop=True, f32=False):
        sw = f32 or self.last_f32
        self.last_f32 = f32
        self.op("tensor", lambda e: e.matmul(out, lhsT=lhsT, rhs=rhs, start=start, stop=stop), R, W, selfwait=sw)

    def tr(self, out, in_, ident, R, W, f32=False):
        sw = f32 or self.last_f32
        self.last_f32 = f32
        self.op("tensor", lambda e: e.transpose(out, in_, ident), R, W, selfwait=sw)

    def tt(self, eng, out, in0, in1, op, R, W):
        self.op(eng, lambda e: e.tensor_tensor(out=out, in0=in0, in1=in1, op=op), R, W)

    def ts(self, eng, out, in0, s1, s2, op0, op1, R, W):
        if s2 is None:
            self.op(eng, lambda e: e.tensor_scalar(out=out, in0=in0, scalar1=s1, scalar2=None, op0=op0), R, W)
        else:
            self.op(eng, lambda e: e.tensor_scalar(out=out, in0=in0, scalar1=s1, scalar2=s2, op0=op0, op1=op1), R, W)

    def stt(self, eng, out, in0, scalar, in1, op0, op1, R, W):
        eng = "vector"
        self.op(eng, lambda e: e.scalar_tensor_tensor(out=out, in0=in0, scalar=scalar, in1=in1, op0=op0, op1=op1), R, W)

    def act(self, out, in_, func, R, W, bias=None, scale=None, accum_out=None):
        kw = {}
        if bias is not None:
            kw["bias"] = bias
        if scale is not None:
            kw["scale"] = scale
        if accum_out is not None:
            kw["accum_out"] = accum_out
        self.op("scalar", lambda e: e.activation(out=out, in_=in_, func=func, **kw), R, W)

    def cp(self, eng, out, in_, R, W):
        if eng == "scalar":
            self.op("scalar", lambda e: e.copy(out=out, in_=in_), R, W)
        else:
            self.op(eng, lambda e: e.tensor_copy(out=out, in_=in_), R, W)

    def ms(self, eng, ap, val, W):
        self.op(eng, lambda e: e.memset(ap, val), [], W)


def _rr(P):
    state = {"i": 0}

    def nxt():
        state["i"] += 1
        return "vector" if state["i"] % 3 else "gpsimd"
    return nxt


def build_program(debug=False, upto=3):
    nc = bass.Bass("TRN2", target_bir_lowering=False)
    skind = "ExternalOutput" if debug else "Internal"

    def din(name, shape, dt=F32):
        return nc.dram_tensor(name, list(shape), dt, kind="ExternalInput").ap()

    x_d = din("x", [T, D])
    cT_d = din("cT", [128, 8])
    pos_d = din("pos", [1, T], I32)
    adaw_d = din("ada_w", [D, 6 * D])
    adab_d = din("ada_b", [1, 6 * D])
    norms_d = din("norms", [4, D])
    win_d = din("w_in", [D, 3328])
    wout_d = din("w_out", [D, D])
    lamv_d = din("lamv", [4, 64])
    subln_d = din("subln", [1, 128])
    mu_d = din("mu", [1, 1792])
    rwv_d = din("rwv", [7, 512])
    w2_d = din("rw_w2", [64, 512])
    a2_d = din("rw_a2", [64, 512])
    g2_d = din("rw_g2", [128, 512])
    rtw_d = din("router_w", [D, NE])
    rtb_d = din("router_b", [1, NE])
    w1g_d = din("w1g", [NE, D, D]) if upto >= 3 else None
    w1l_d = din("w1l", [NE, D, D]) if upto >= 3 else None
    b1T_d = din("b1T", [128, NE * 16])
    w2e_d = din("w2e", [NE, D, D]) if upto >= 3 else None
    b2_d = din("b2", [NE, D])
    con_d = din("consts", [128, NCONST])
    out_d = nc.dram_tensor("out", [T, D], F32, kind="ExternalOutput").ap()

    modp_d = nc.dram_tensor("modp", [6, D], F32, kind=skind).ap()
    qk_d = nc.dram_tensor("qk_s", [D, T], BF16, kind=skind).ap()
    v_d = nc.dram_tensor("v_s", [T, 4 * 129], BF16, kind=skind).ap()
    yrw_d = nc.dram_tensor("yrw_s", [T, 512], BF16, kind=skind).ap()
    x1_d = nc.dram_tensor("x1_s", [T, D], F32, kind=skind).ap()
    h2T_d = nc.dram_tensor("h2T_s", [D, T], BF16, kind=skind).ap()
    gat_d = nc.dram_tensor("gat_s", [T, NE], F32, kind=skind).ap()
    wbf_d = nc.dram_tensor("wbf_s", [NE, 3, D, D], BF16).ap() if upto >= 3 else None

    with contextlib.ExitStack() as gst:
        P = Prog(nc, gst)
        b_modp = P.buf("modp")
        b_qk = P.buf("qkd")
        b_v = P.buf("vd")
        b_yrw = P.buf("yrwd")
        b_x1 = P.buf("x1d")
        b_h2T = P.buf("h2Td")
        b_gat = P.buf("gatd")
        b_out = P.buf("outd")
        b_wbf = P.buf("wbfd")
        b_conv = P.buf("wconv")

        with P.phase():
            cT, bcT = P.sb("cT", [128, 8], F32)
            sc, bsc = P.sb("sc", [128, 8], F32)
            P.dma("sync", cT[:], cT_d, [], [bcT], bcT)
            P.act(sc[:], cT[:], AF.Silu, [bcT], [bsc])
            aw = [P.sb(f"aw{i}", [128, 3072], F32) for i in range(2)]
            pm, bpm = P.ps("pmod", [128, 3072], F32)
            mrow, bmrow = P.sb("mrow", [1, 6 * D], F32)
            brow, bbrow = P.sb("brow", [1, 6 * D], F32)
            nrm, bnrm = P.sb("nrm", [1, 4 * D], F32)
            orow, borow = P.sb("orow", [1, 6 * D], F32)
            P.dma("sync", brow[:], adab_d, [], [bbrow], bbrow)
            P.dma("sync", nrm[:], norms_d.rearrange("(o a) d -> o (a d)", o=1), [], [bnrm], bnrm)
            i = 0
            for half in range(2):
                for k in range(8):
                    t_, b_ = aw[i % 2]
                    i += 1
                    P.dma("sync", t_[:], adaw_d[k * 128:(k + 1) * 128, half * 3072:(half + 1) * 3072], [], [b_], b_)
                    for j in range(6):
                        P.mm(pm[0:1, j * 512:(j + 1) * 512], sc[:, k:k + 1], t_[:, j * 512:(j + 1) * 512],
                             [bsc, b_], [bpm], start=(k == 0), stop=(k == 7))
                P.tt("vector", mrow[:, half * 3072:(half + 1) * 3072], pm[0:1, :], brow[:, half * 3072:(half + 1) * 3072],
                     ALU.add, [bpm, bbrow], [bmrow])

            def mseg(i_):
                return mrow[:, i_ * D:(i_ + 1) * D]

            def nseg(i_):
                return nrm[:, i_ * D:(i_ + 1) * D]
            P.stt("vector", orow[:, 0:D], mseg(1), 1.0, nseg(0), ALU.add, ALU.mult, [bmrow, bnrm], [borow])
            P.cp("vector", orow[:, D:2 * D], mseg(0), [bmrow], [borow])
            P.tt("vector", orow[:, 2 * D:3 * D], mseg(2), nseg(1), ALU.mult, [bmrow, bnrm], [borow])
            P.stt("vector", orow[:, 3 * D:4 * D], mseg(4), 1.0, nseg(2), ALU.add, ALU.mult, [bmrow, bnrm], [borow])
            P.cp("vector", orow[:, 4 * D:5 * D], mseg(3), [bmrow], [borow])
            P.tt("vector", orow[:, 5 * D:6 * D], mseg(5), nseg(3), ALU.mult, [bmrow, bnrm], [borow])
            P.dma("sync", modp_d.rearrange("(o a) d -> o (a d)", o=1), orow[:], [borow], [b_modp], borow)

        if upto >= 1:
          with P.phase():
            phase_front(P, nc, locals())

        if upto >= 2:
          with P.phase():
            phase_attn(P, nc, locals())

        if upto >= 3:
          with P.phase():
            phase_moe(P, nc, locals())
    return nc


def phase_front(P, nc, G):
    P.noself = NOSELF_FRONT
    x_d, pos_d, win_d, mu_d, rwv_d = G["x_d"], G["pos_d"], G["win_d"], G["mu_d"], G["rwv_d"]
    w2_d, a2_d, g2_d, con_d, modp_d = G["w2_d"], G["a2_d"], G["g2_d"], G["con_d"], G["modp_d"]
    qk_d, v_d, yrw_d = G["qk_d"], G["v_d"], G["yrw_d"]
    b_modp, b_qk, b_v, b_yrw = G["b_modp"], G["b_qk"], G["b_v"], G["b_yrw"]
    rr = _rr(P)

    con, bcon = P.sb("con", [128, NCONST], F32)
    P.dma("sync", con[:], con_d, [], [bcon], bcon)
    identf = con[:, O_ID:O_ID + 128]
    idb, bidb = P.sb("idb", [128, 128], BF16)
    P.cp("vector", idb[:], identf, [bcon], [bidb])
    mcol, bmcol = P.sb("mcol", [128, 6, 8], F32)
    P.dma("sync", mcol[:], modp_d.rearrange("a (k p) -> p a k", p=128), [b_modp], [bmcol], bmcol,
          allow_slow_non_contiguous=True)

    wda, bwda = P.sb("wda", [128, 8, 1536], BF16)
    P.dma("gpsimd", wda[:], win_d[:, 0:1536].rearrange("(k p) c -> p k c", p=128), [], [bwda], bwda)
    w1, bw1 = P.sb("w1", [128, 8, 1792], BF16)
    w2m, bw2m = P.sb("w2m", [128, 8, 1792], BF16)
    prm, bprm = P.sb("prm", [128, 7, 512], F32)
    P.dma("sync", prm[:].rearrange("p a d -> p (a d)"),
          rwv_d.rearrange("(o a) d -> o (a d)", o=1).partition_broadcast(128), [], [bprm], bprm)
    lw2, blw2 = P.sb("lw2", [128, 512], BF16)
    lg2, blg2 = P.sb("lg2", [128, 512], BF16)
    P.dma("gpsimd", lw2[0:64, :], w2_d, [], [blw2], blw2)
    P.dma("gpsimd", lw2[64:128, :], a2_d, [], [blw2], blw2)
    P.dma("gpsimd", lg2[:], g2_d, [], [blg2], blg2)
    pmb, bpmb = P.sb("pmb", [128, 128], BF16)
    P.cp("vector", pmb[:], con[:, O_PM:O_PM + 128], [bcon], [bpmb])

    ctab, bctab = P.sb("ctab", [128, T], BF16)
    stab, bstab = P.sb("stab", [128, T], BF16)
    with contextlib.ExitStack() as tmp:
        old = P.stack
        P.stack = tmp
        mub, bmub = P.sb("mub", [128, 1792], F32)
        omu, bomu = P.sb("omu", [128, 1792], F32)
        P.dma("sync", mub[:], mu_d.partition_broadcast(128), [], [bmub], bmub)
        P.ts("vector", omu[:], mub[:], -1.0, 1.0, ALU.mult, ALU.add, [bmub], [bomu])
        wst = [P.sb(f"wst{i}", [128, 1792], F32) for i in range(2)]
        for k in range(8):
            t_, b_ = wst[k % 2]
            P.dma("sync", t_[:], win_d[k * 128:(k + 1) * 128, 1536:3328], [], [b_], b_)
            P.tt("vector", w1[:, k, :], t_[:], omu[:], ALU.mult, [b_, bomu], [bw1])
            P.tt("gpsimd", w2m[:, k, :], t_[:], mub[:], ALU.mult, [b_, bmub], [bw2m])
        posi, bposi = P.sb("posi", [128, 1024], I32)
        ang, bang = P.sb("ang", [128, 1024], F32)
        y_, by_ = P.sb("ry", [128, 1024], F32)
        kf, bkf = P.sb("rkf", [128, 1024], F32)
        ki, bki = P.sb("rki", [128, 1024], I32)
        for q4 in range(4):
            sl = slice(q4 * 1024, (q4 + 1) * 1024)
            P.dma("sync", posi[:], pos_d[:, sl].partition_broadcast(128), [], [bposi], bposi)
            P.cp("vector", ang[:], posi[:], [bposi], [bang])
            P.ts("vector", ang[:], ang[:], con[:, O_FREQ:O_FREQ + 1], None, ALU.mult, None, [bang, bcon], [bang])
            for which, shift in ((0, math.pi * 1.5), (1, math.pi)):
                P.ts("vector", y_[:], ang[:], shift, None, ALU.add, None, [bang], [by_])
                P.ts("vector", kf[:], y_[:], 1.0 / (2 * math.pi), None, ALU.mult, None, [by_], [bkf])
                P.cp("vector", ki[:], kf[:], [bkf], [bki])
                P.cp("vector", kf[:], ki[:], [bki], [bkf])
                P.stt("vector", y_[:], kf[:], -2 * math.pi, y_[:], ALU.mult, ALU.add, [bkf, by_], [by_])
                P.ts("vector", kf[:], y_[:], 0.0, 2 * math.pi, ALU.is_lt, ALU.mult, [by_], [bkf])
                P.tt("vector", y_[:], y_[:], kf[:], ALU.add, [by_, bkf], [by_])
                P.ts("vector", y_[:], y_[:], -math.pi, None, ALU.add, None, [by_], [by_])
                P.ts("vector", y_[:], y_[:], -math.pi, math.pi, ALU.max, ALU.min, [by_], [by_])
                if which == 0:
                    P.act(ctab[:, sl], y_[:], AF.Sin, [by_], [bctab])
                else:
                    P.act(kf[:], y_[:], AF.Sin, [by_], [bkf])
                    P.ts("vector", stab[:, sl], kf[:], con[:, O_SIGN:O_SIGN + 1], None, ALU.mult, None, [bkf, bcon], [bstab])
        P.barrier()
        P.flush()
        P.stack = old

    if G.get("wbf_d") is not None:
        for e_ in range(NE):
            for j_, src in enumerate((G["w1g_d"], G["w1l_d"], G["w2e_d"])):
                P.dma("gpsimd", G["wbf_d"][e_, j_], src[e_], [], [G["b_wbf"]], G["b_conv"])

    def f32t(name, w=512):
        return P.sb(name, [128, w], F32)

    XT = [P.sb(f"xt{i}", [128, D], F32) for i in range(2)]
    xn, bxn = P.sb("xn", [128, D], BF16)
    junk, bjunk = xn, bxn
    st8, bst8 = P.sb("st8", [128, 8], F32)
    hT = [P.sb(f"hT{i}", [128, 8, 129], BF16) for i in range(2)]
    P.ms("vector", hT[1][0][:, :, 128:129], 0.0, [hT[1][1]])
    qf, bqf = P.sb("qf", [128, 128], BF16)
    qf2, bqf2 = P.sb("qf2", [128, 128], F32)
    t1, bt1 = P.sb("t1", [128, 128], F32)
    t2, bt2 = P.sb("t2", [128, 128], F32)
    qko, bqko = P.sb("qko", [128, 8, 128], BF16)
    vo, bvo = P.sb("vo", [128, 4, 129], BF16)
    P.ms("vector", vo[:, :, 128:129], 1.0, [bvo])
    r_s, br = f32t("r_s")
    k_s, bk = f32t("k_s")
    VS2 = [f32t(f"v_s{i}") for i in range(2)]
    lo0, blo0 = P.sb("lo0", [128, 128], BF16)
    lo1, blo1 = P.sb("lo1", [128, 128], BF16)
    VB2 = [P.sb(f"v_b{i}", [128, 512], BF16) for i in range(2)]
    STb, bSTb_ = P.sb("STb", [128, 4, 64], BF16)
    P.ms("vector", STb[:], 0.0, [bSTb_])
    sig, bsig = f32t("sig")
    a_s, ba = f32t("a_s")
    GS2 = [f32t(f"g_s{i}") for i in range(2)]
    BON2 = [P.sb(f"bon{i}", [128, 8], F32) for i in range(2)]
    gn, bgn = P.sb("gn", [128, 16], F32)
    kk, bkk = f32t("kk")
    km, bkm = f32t("km")
    bb, bbb = f32t("bb")
    e1, be1 = f32t("e1")
    e2, be2 = f32t("e2")
    tm1, btm1 = f32t("tm1")
    tm2, btm2 = f32t("tm2")
    At, bAt = P.sb("At", [128, 512], BF16)
    Rt, bRt = P.sb("Rt", [128, 512], BF16)
    Bt, bBt = P.sb("Bt", [128, 512], BF16)
    Kt, bKt = P.sb("Kt", [128, 512], BF16)
    BH2 = [P.sb(f"Bh{i}", [128, 512], BF16) for i in range(2)]
    KH2 = [P.sb(f"Kh{i}", [128, 512], BF16) for i in range(2)]
    FM2 = [P.sb(f"FM{i}", [128, 4, 4, 128], BF16)[0] for i in range(2)]
    bFM2 = [[P.buf("FMp") for _ in range(4)] for _ in range(2)]
    RP2 = [P.sb(f"RP{i}", [128, 4, 384], BF16)[0] for i in range(2)]
    bRP2 = [[P.buf("RPp") for _ in range(4)] for _ in range(2)]
    for i in range(2):
        P.ms("vector", RP2[i][:], 0.0, bRP2[i])
    PC2 = [P.sb(f"pc{i}", [128, 4, 2], F32) for i in range(2)]
    ST, bST_ = P.sb("ST", [128, 4, 64], F32)
    bST = [P.buf("STh") for _ in range(8)]
    P.ms("vector", ST[:], 0.0, bST)
    Gs = [P.sb(f"Gs{i}", [128, 640], BF16) for i in range(2)]
    Nb = [[P.sb(f"N{i}_{j}", [128, 128], BF16) for j in range(2)] for i in range(2)]
    Lb = [[P.sb(f"L{i}_{j}", [128, 128], BF16) for j in range(2)] for i in range(2)]
    Tb = [[P.sb(f"T{i}_{j}", [128, 128], BF16) for j in range(2)] for i in range(2)]
    Zs = [P.sb(f"Zs{i}", [128, 64], BF16) for i in range(2)]
    Us = [P.sb(f"Us{i}", [128, 64], BF16) for i in range(2)]
    yo, byo = P.sb("yo", [128, 512], BF16)

    pT, bpT = P.ps("pT", [128, 8, 128], BF16)
    pQ, bpQ = P.ps("pQ", [128, 512], F32)
    pV, bpV = P.ps("pV", [128, 512], F32)
    pF, bpF = P.ps("pF", [128, 512], F32)
    pF1, bpF1 = P.ps("pF1", [128, 512], F32)
    pG0, bpG0 = P.ps("pG0", [128, 512], F32)
    pG1, bpG1 = P.ps("pG1", [128, 512], F32)
    pY, bpY = P.ps("pY", [128, 512], F32)

    A1c = mcol[:, 0, :]
    B1c = mcol[:, 1, :]

    if DBG_STOP < 1:
        return
    for n in range(DBG_NT):
        tsl = slice(n * 128, (n + 1) * 128)
        hcur, bhcur = hT[n % 2]
        hprev, bhprev = hT[(n + 1) % 2]
        xt, bxt = XT[n % 2]
        ysb, bysb = xt[:, 0:512], bxt
        v_s, bv = VS2[n % 2]
        g_s, bg = GS2[n % 2]
        bon, bbon = BON2[n % 2]
        v_b, bvb = VB2[n % 2]
        Bh, bBh = BH2[n % 2]
        Kh, bKh = KH2[n % 2]
        FM, bFM = FM2[n % 2], bFM2[n % 2]
        RP, bRP = RP2[n % 2], bRP2[n % 2]
        pc, bpc = PC2[n % 2]
        if n == 0:
            P.dma("sync", xt[:], x_d[tsl, :], [], [bxt], bxt)
        if n + 1 < DBG_NT:
            xtn, bxtn = XT[(n + 1) % 2]
            P.dma("sync", xtn[:], x_d[(n + 1) * 128:(n + 2) * 128, :], [], [bxtn], bxtn)
        P.act(junk[:], xt[:], AF.Square, [bxt], [bjunk, bst8], accum_out=st8[:, 0:1])
        P.ts("vector", st8[:, 1:2], st8[:, 0:1], 1.0 / D, 1e-6, ALU.mult, ALU.add, [bst8], [bst8])
        P.act(st8[:, 2:3], st8[:, 1:2], AF.Ln, [bst8], [bst8])
        P.act(st8[:, 3:4], st8[:, 2:3], AF.Exp, [bst8], [bst8], scale=-0.5)
        P.ts("vector", xn[:], xt[:], st8[:, 3:4], None, ALU.mult, None, [bxt, bst8], [bxn])
        for k in range(8):
            P.tr(pT[:, k, :], xn[:, k * 128:(k + 1) * 128], idb[:], [bxn, bidb], [bpT])
        P.cp("vector", hcur[:, :, 0:1], hprev[:, :, 128:129], [bhprev], [bhcur])
        for k in range(8):
            P.act(hcur[:, k, 1:129], pT[:, k, :], AF.Identity, [bpT, bmcol], [bhcur],
                  bias=B1c[:, k:k + 1], scale=A1c[:, k:k + 1])
        hx = lambda k: hcur[:, k, 1:129]
        hs = lambda k: hcur[:, k, 0:128]
        for cq in range(8):
            for k in range(8):
                P.mm(pQ[:, 0:128], wda[:, k, cq * 128:(cq + 1) * 128], hx(k), [bwda, bhcur], [bpQ],
                     start=(k == 0), stop=(k == 7))
            P.cp("scalar", qf[:], pQ[:, 0:128], [bpQ], [bqf])
            P.mm(pQ[:, 128:256], pmb[:], qf[:], [bpmb, bqf], [bpQ])
            P.cp("scalar", t2[:], pQ[:, 128:256], [bpQ], [bt2])
            P.tt("vector", t1[:], qf[:], ctab[:, tsl], ALU.mult, [bqf, bctab], [bt1])
            P.tt("gpsimd", qf2[:], t2[:], stab[:, tsl], ALU.mult, [bt2, bstab], [bqf2])
            P.tt("vector", qko[:, cq, :], t1[:], qf2[:], ALU.add, [bt1, bqf2], [bqko])
        P.dma("sync", qk_d.rearrange("(c p) t -> p c t", p=128)[:, :, tsl], qko[:], [bqko], [b_qk], bqko)
        for k in range(8):
            P.mm(pV[:], hx(k), wda[:, k, 1024:1536], [bhcur, bwda], [bpV], start=(k == 0), stop=(k == 7))
        P.cp("scalar", vo[:, :, 0:128], pV[:].rearrange("p (h d) -> p h d", h=4), [bpV], [bvo])
        P.dma("sync", v_d[tsl, :], vo[:].rearrange("p h d -> p (h d)"), [bvo], [b_v], bvo)
        if DBG_STOP < 2:
            continue
        for cc, (dst, bdst) in enumerate(((r_s, br), (k_s, bk), (v_s, bv))):
            pp, bpp = pV, bpV
            for k in range(8):
                P.mm(pp[:], hx(k), w1[:, k, cc * 512:(cc + 1) * 512], [bhcur, bw1], [bpp], start=(k == 0), stop=False)
            for k in range(8):
                P.mm(pp[:], hs(k), w2m[:, k, cc * 512:(cc + 1) * 512], [bhcur, bw2m], [bpp], start=False, stop=(k == 7))
            P.cp("scalar", dst[:], pp[:], [bpp], [bdst])
            if cc == 2:
                P.cp("gpsimd", v_b[:], v_s[:], [bv], [bvb])
        for lc in range(2):
            cs_ = slice(1536 + lc * 128, 1536 + (lc + 1) * 128)
            osl = pQ[:, 256:384]
            for k in range(8):
                P.mm(osl, w1[:, k, cs_], hx(k), [bw1, bhcur], [bpQ], start=(k == 0), stop=False)
            for k in range(8):
                P.mm(osl, w2m[:, k, cs_], hs(k), [bw2m, bhcur], [bpQ], start=False, stop=(k == 7))
            if lc == 0:
                P.act(lo0[0:64, :], pQ[0:64, 256:384], AF.Tanh, [bpQ], [blo0])
                P.cp("scalar", lo0[64:128, :], pQ[64:128, 256:384], [bpQ], [blo0])
            else:
                P.act(lo1[:], pQ[:, 256:384], AF.Sigmoid, [bpQ], [blo1])
        P.mm(pV[:], lo0[0:64, :], lw2[0:64, :], [blo0, blw2], [bpV])
        P.tt("vector", sig[:], pV[:], prm[:, 0, :], ALU.add, [bpV, bprm], [bsig])
        P.act(sig[:], sig[:], AF.Sigmoid, [bsig], [bsig])
        P.mm(pV[:], lo0[64:128, :], lw2[64:128, :], [blo0, blw2], [bpV])
        P.tt("vector", a_s[:], pV[:], prm[:, 1, :], ALU.add, [bpV, bprm], [ba])
        P.act(a_s[:], a_s[:], AF.Sigmoid, [ba], [ba])
        P.mm(pV[:], lo1[:], lg2[:], [blo1, blg2], [bpV])
        P.cp("scalar", g_s[:], pV[:], [bpV], [bg])
        P.tt(rr(), kk[:], k_s[:], prm[:, 2, :], ALU.mult, [bk, bprm], [bkk])
        P.tt(rr(), tm1[:], kk[:], kk[:], ALU.mult, [bkk], [btm1])
        P.op("vector", lambda e: e.tensor_reduce(out=st8[:, 0:8], in_=tm1[:].rearrange("p (h j) -> p h j", h=8),
                                                 axis=AX.X, op=ALU.add), [btm1], [bst8])
        P.ts("vector", st8[:, 0:8], st8[:, 0:8], 1e-24, None, ALU.max, None, [bst8], [bst8])
        P.act(st8[:, 0:8], st8[:, 0:8], AF.Ln, [bst8], [bst8])
        P.act(st8[:, 0:8], st8[:, 0:8], AF.Exp, [bst8], [bst8], scale=-0.5)
        P.tt("vector", kk[:].rearrange("p (h j) -> p h j", h=8), kk[:].rearrange("p (h j) -> p h j", h=8),
             st8[:, 0:8].unsqueeze(2).to_broadcast([128, 8, 64]), ALU.mult, [bkk, bst8], [bkk])
        P.stt(rr(), tm1[:], a_s[:], -1.0, prm[:, 3, :], ALU.add, ALU.mult, [ba, bprm], [btm1])
        P.stt(rr(), km[:], tm1[:], 1.0, k_s[:], ALU.add, ALU.mult, [btm1, bk], [bkm])
        P.tt(rr(), bb[:], kk[:], a_s[:], ALU.mult, [bkk, ba], [bbb])
        P.mm(pV[:], con[:, O_UI:O_UI + 128], sig[:], [bcon, bsig], [bpV], f32=True)
        P.act(e1[:], pV[:], AF.Exp, [bpV], [be1], scale=-C0)
        P.act(e2[:], pV[:], AF.Exp, [bpV], [be2], scale=C0)
        P.tt(rr(), Rt[:], r_s[:], e1[:], ALU.mult, [br, be1], [bRt])
        P.tt(rr(), Bt[:], bb[:], e2[:], ALU.mult, [bbb, be2], [bBt])
        P.tt(rr(), Kt[:], km[:], e2[:], ALU.mult, [bkm, be2], [bKt])
        P.mm(pV[:], con[:, O_SU:O_SU + 128], sig[:], [bcon, bsig], [bpV], f32=True)
        P.act(e1[:], pV[:], AF.Exp, [bpV], [be1], scale=-C0)
        P.stt(rr(), At[:], kk[:], -1.0, e1[:], ALU.mult, ALU.mult, [bkk, be1], [bAt])
        P.mm(pV[:], con[:, O_SL:O_SL + 128], sig[:], [bcon, bsig], [bpV], f32=True)
        P.act(e2[:], pV[:], AF.Exp, [bpV], [be2], scale=-C0)
        P.tt(rr(), Bh[:], bb[:], e2[:], ALU.mult, [bbb, be2], [bBh])
        P.tt(rr(), Kh[:], km[:], e2[:], ALU.mult, [bkm, be2], [bKh])
        for pr in range(4):
            P.mm(pQ[:, 384 + pr * 2:384 + pr * 2 + 2], sig[:, pr * 128:(pr + 1) * 128], con[:, O_IND:O_IND + 2],
                 [bsig, bcon], [bpQ], f32=True)
        P.act(pc[:].rearrange("p a c -> p (a c)"), pQ[:, 384:392], AF.Exp, [bpQ], [bpc], scale=-C0)
        P.tt(rr(), tm1[:], r_s[:], km[:], ALU.mult, [br, bkm], [btm1])
        P.tt(rr(), tm1[:], tm1[:], prm[:, 4, :], ALU.mult, [btm1, bprm], [btm1])
        P.op("vector", lambda e, bon=bon: e.tensor_reduce(out=bon[:, 0:8], in_=tm1[:].rearrange("p (h j) -> p h j", h=8),
                                                          axis=AX.X, op=ALU.add), [btm1], [bbon])
        if DBG_STOP < 3:
            continue
        for pr in range(4):
            psl = slice(pr * 128, (pr + 1) * 128)
            for ai, (arr, barr) in enumerate(((At, bAt), (Rt, bRt), (Bt, bBt), (Kt, bKt))):
                P.tr(pT[:, ai, :], arr[:, psl], idb[:], [barr, bidb], [bpT])
            P.cp("scalar", FM[:, pr, :, :], pT[:, 0:4, :], [bpT], [bFM[pr]])
            P.cp("vector", RP[:, pr, 0:64], FM[:, pr, 1, 0:64], [bFM[pr]], [bRP[pr]])
            P.cp("vector", RP[:, pr, 192:256], FM[:, pr, 1, 64:128], [bFM[pr]], [bRP[pr]])
        if DBG_STOP < 4:
            continue
        for h in range(8):
            pr = h // 2
            ph = (h % 2) * 64
            hp = h % 2
            Gt, bGt = Gs[hp]
            A_ = FM[ph:ph + 64, pr, 0, :]
            R_ = FM[ph:ph + 64, pr, 1, :]
            B_ = FM[ph:ph + 64, pr, 2, :]
            K_ = FM[ph:ph + 64, pr, 3, :]
            bF = bFM[pr]
            P.mm(pG0[:, 0:128], B_, A_, [bF], [bpG0])
            P.mm(pG0[:, 128:256], K_, A_, [bF], [bpG0])
            P.mm(pG0[:, 256:384], B_, R_, [bF], [bpG0])
            P.mm(pG0[:, 384:512], K_, R_, [bF], [bpG0])
            P.mm(pG1[:, 0:128], A_, B_, [bF], [bpG1])
            P.tt("vector", Gt[:, 0:256], pG0[:, 0:256], con[:, O_M5:O_M5 + 256], ALU.mult, [bpG0, bcon], [bGt])
            P.tt("vector", Gt[:, 384:640], pG0[:, 256:512], con[:, O_M5 + 384:O_M5 + 640], ALU.mult, [bpG0, bcon], [bGt])
            P.tt("vector", Gt[:, 256:384], pG1[:, 0:128], con[:, O_M5 + 256:O_M5 + 384], ALU.mult, [bpG1, bcon], [bGt])
            if DBG_X == 11:
                continue
            Ncur, bNcur = Gt[:, 0:128], bGt
            Lcur, bLcur = Gt[:, 256:384], bGt
            Tcur, bTcur = Tb[hp][0]
            P.tt(rr(), Tcur[:], Gt[:, 0:128], identf, ALU.add, [bGt, bcon], [bTcur])
            Tcur = Tcur[:]
            for kx in range(1, 6):
                if DBG_X in (13, 14) and kx > 1:
                    break
                if DBG_X == 15 and kx > 2:
                    break
                Ln_, bLn = Lb[hp][kx % 2]
                i0 = 128 + (kx % 3) * 128
                P.mm(pG1[:, i0:i0 + 128], Ncur, Lcur, [bNcur, bLcur], [bpG1])
                P.cp("vector", Ln_[:], pG1[:, i0:i0 + 128], [bpG1], [bLn])
                if DBG_X == 13:
                    break
                if kx <= 4:
                    Nn_, bNn = Nb[hp][kx % 2]
                    i1 = 128 + ((kx + 1) % 3) * 128
                    P.mm(pG1[:, i1:i1 + 128], Lcur, Ncur, [bNcur, bLcur], [bpG1])
                    P.cp("vector", Nn_[:], pG1[:, i1:i1 + 128], [bpG1], [bNn])
                Tn_, bTn = Tb[hp][kx % 2]
                i2 = 128 + ((kx + 2) % 3) * 128
                P.mm(pG1[:, i2:i2 + 128], Ln_[:], Tcur, [bLn, bTcur], [bpG1])
                P.tt("vector", Tn_[:], pG1[:, i2:i2 + 128], Tcur, ALU.add, [bpG1, bTcur], [bTn])
                Lcur, bLcur = Ln_[:], bLn
                if kx <= 4:
                    Ncur, bNcur = Nn_[:], bNn
                Tcur, bTcur = Tn_[:], bTn
            if DBG_X == 12:
                continue
            S0 = ST[ph:ph + 64, pr, :]
            S0b = STb[ph:ph + 64, pr, :]
            bS = bST[h]
            Zt, bZt = Zs[hp]
            Ut, bUt = Us[hp]
            hcol = slice(h * 64, (h + 1) * 64)
            for c in range(2):
                pv = c * 64
                pSb, sb0 = (pF, 0) if hp == 0 else (pF1, 0)
                zsl = pSb[:, sb0:sb0 + 64]
                usl = pSb[:, sb0 + 64:sb0 + 128]
                ssl = pSb[:, sb0 + 128:sb0 + 192]
                bz = bu = bs_ = (bpF if hp == 0 else bpF1)
                P.mm(zsl, A_, S0b, [bF, bS], [bz], start=True, stop=False, f32=True)
                P.mm(zsl, Gt[pv:pv + 64, 128:256], v_b[pv:pv + 64, hcol], [bGt, bvb], [bz], start=False, stop=True, f32=True)
                P.cp("vector", Zt[pv:pv + 64, :], zsl[pv:pv + 64, :], [bz], [bZt])
                P.mm(usl, Tcur[pv:pv + 64, :], Zt[pv:pv + 64, :], [bTcur, bZt], [bu], f32=True)
                P.cp("vector", Ut[pv:pv + 64, :], usl[pv:pv + 64, :], [bu], [bUt])
                P.mm(pY[:, hcol], RP[ph:ph + 64, pr, c * 128:(c + 1) * 128], S0b, [bRP[pr], bS], [bpY],
                     start=(c == 0), stop=False, f32=True)
                P.mm(pY[:, hcol], Gt[pv:pv + 64, 512:640], v_b[pv:pv + 64, hcol], [bGt, bvb], [bpY], start=False, stop=False, f32=True)
                P.mm(pY[:, hcol], Gt[pv:pv + 64, 384:512], Ut[pv:pv + 64, :], [bGt, bUt], [bpY], start=False, stop=(c == 1), f32=True)
                P.mm(ssl, Bh[pv:pv + 64, pr * 128:(pr + 1) * 128], Ut[pv:pv + 64, :], [bBh, bUt], [bs_], start=True, stop=False, f32=True)
                P.mm(ssl, Kh[pv:pv + 64, pr * 128:(pr + 1) * 128], v_b[pv:pv + 64, hcol], [bKh, bvb], [bs_], start=False, stop=True, f32=True)
                P.stt("vector", S0, S0, pc[ph:ph + 64, pr, c:c + 1], ssl[ph:ph + 64, :], ALU.mult, ALU.add,
                      [bS, bpc, bs_], [bS])
                P.cp("gpsimd", S0b, S0, [bS], [bS])
        if DBG_STOP < 5:
            continue
        v3 = lambda ap: ap.rearrange("p (h j) -> p h j", h=8)
        P.cp("scalar", ysb[:], pY[:], [bpY], [bysb])
        P.op("vector", lambda e, ysb=ysb: e.tensor_reduce(out=gn[:, 0:8], in_=ysb.rearrange("p (h j) -> p h j", h=8),
                                                          axis=AX.X, op=ALU.add), [bysb], [bgn])
        P.ts("vector", gn[:, 0:8], gn[:, 0:8], -1.0 / 64, None, ALU.mult, None, [bgn], [bgn])
        P.tt("vector", v3(ysb[:]), v3(ysb[:]), gn[:, 0:8].unsqueeze(2).to_broadcast([128, 8, 64]), ALU.add, [bysb, bgn], [bysb])
        P.tt(rr(), tm2[:], ysb[:], ysb[:], ALU.mult, [bysb], [btm2])
        P.op("vector", lambda e: e.tensor_reduce(out=gn[:, 8:16], in_=v3(tm2[:]), axis=AX.X, op=ALU.add), [btm2], [bgn])
        P.ts("vector", gn[:, 8:16], gn[:, 8:16], 1.0 / 64, 64e-5, ALU.mult, ALU.add, [bgn], [bgn])
        P.act(gn[:, 8:16], gn[:, 8:16], AF.Ln, [bgn], [bgn])
        P.act(gn[:, 8:16], gn[:, 8:16], AF.Exp, [bgn], [bgn], scale=-0.5)
        P.tt("vector", v3(ysb[:]), v3(ysb[:]), gn[:, 8:16].unsqueeze(2).to_broadcast([128, 8, 64]), ALU.mult, [bysb, bgn], [bysb])
        P.tt(rr(), ysb[:], ysb[:], prm[:, 5, :], ALU.mult, [bysb, bprm], [bysb])
        P.tt(rr(), ysb[:], ysb[:], prm[:, 6, :], ALU.add, [bysb, bprm], [bysb])
        P.tt("vector", v3(tm2[:]), v3(v_s[:]), bon[:, 0:8].unsqueeze(2).to_broadcast([128, 8, 64]), ALU.mult, [bv, bbon], [btm2])
        P.tt(rr(), ysb[:], ysb[:], tm2[:], ALU.add, [bysb, btm2], [bysb])
        P.tt("vector", yo[:], ysb[:], g_s[:], ALU.mult, [bysb, bg], [byo])
        P.dma("sync", yrw_d[tsl, :], yo[:], [byo], [b_yrw], byo)


def phase_attn(P, nc, G):
    P.noself = NOSELF_ATTN
    con_d, modp_d, lamv_d, subln_d, wout_d, rtw_d, rtb_d = (G[k] for k in
        ("con_d", "modp_d", "lamv_d", "subln_d", "wout_d", "rtw_d", "rtb_d"))
    x_d, qk_d, v_d, yrw_d, x1_d, h2T_d, gat_d = (G[k] for k in ("x_d", "qk_d", "v_d", "yrw_d", "x1_d", "h2T_d", "gat_d"))
    b_modp, b_qk, b_v, b_yrw, b_x1, b_h2T, b_gat = (G[k] for k in
        ("b_modp", "b_qk", "b_v", "b_yrw", "b_x1", "b_h2T", "b_gat"))
    rr = _rr(P)
    con, bcon = P.sb("con", [128, NCONST], F32)
    P.dma("sync", con[:], con_d, [], [bcon], bcon)
    identf = con[:, O_ID:O_ID + 128]
    idb, bidb = P.sb("idb", [128, 128], BF16)
    P.cp("vector", idb[:], identf, [bcon], [bidb])
    cmk, bcmk = P.sb("cmk", [128, 4, 512], BF16)
    P.cp("vector", cmk[:].rearrange("p a t -> p (a t)"), con[:, O_CM:O_CM + 2048], [bcon], [bcmk])
    rows, brows = P.sb("rows", [128, 3, D], F32)
    P.dma("sync", rows[:].rearrange("p a d -> p (a d)"),
          modp_d[2:5, :].rearrange("(o a) d -> o (a d)", o=1).partition_broadcast(128), [b_modp], [brows], brows)
    mcol, bmcol = P.sb("mcol", [128, 6, 8], F32)
    P.dma("sync", mcol[:], modp_d.rearrange("a (k p) -> p a k", p=128), [b_modp], [bmcol], bmcol,
          allow_slow_non_contiguous=True)
    lv, blv = P.sb("lv", [128, 4, 64], F32)
    P.dma("sync", lv[:].rearrange("p a d -> p (a d)"),
          lamv_d.rearrange("(o a) d -> o (a d)", o=1).partition_broadcast(128), [], [blv], blv)
    lam, blam = P.sb("lam", [128, 8], F32)
    lt, blt = P.sb("lt", [128, 2, 64], F32)
    P.tt("vector", lt[:, 0, :], lv[:, 0, :], lv[:, 1, :], ALU.mult, [blv], [blt])
    P.tt("vector", lt[:, 1, :], lv[:, 2, :], lv[:, 3, :], ALU.mult, [blv], [blt])
    P.op("vector", lambda e: e.tensor_reduce(out=lam[:, 0:2], in_=lt[:], axis=AX.X, op=ALU.add), [blt], [blam])
    P.act(lam[:, 2:4], lam[:, 0:2], AF.Exp, [blam], [blam])
    P.tt("vector", lam[:, 4:5], lam[:, 2:3], lam[:, 3:4], ALU.subtract, [blam], [blam])
    P.ts("vector", lam[:, 5:6], lam[:, 4:5], -1.0, -LAMBDA_INIT, ALU.mult, ALU.add, [blam], [blam])
    sub, bsub = P.sb("sub", [128, 128], F32)
    P.dma("sync", sub[:], subln_d.partition_broadcast(128), [], [bsub], bsub)
    P.ts("vector", sub[:], sub[:], 1.0 - LAMBDA_INIT, None, ALU.mult, None, [bsub], [bsub])
    wo, bwo = P.sb("wo", [128, 8, D], BF16)
    P.dma("gpsimd", wo[:], wout_d.rearrange("(k p) c -> p k c", p=128), [], [bwo], bwo)
    rw, brw = P.sb("rw", [128, 8, NE], BF16)
    P.dma("gpsimd", rw[:], rtw_d.rearrange("(k p) c -> p k c", p=128), [], [brw], brw)
    rb, brb = P.sb("rb", [128, NE], F32)
    P.dma("sync", rb[:], rtb_d.partition_broadcast(128), [], [brb], brb)
    kT, bkT = P.sb("kT", [128, 4, T], BF16)
    P.dma("sync", kT[:], qk_d[512:1024, :].rearrange("(c p) t -> p c t", p=128), [b_qk], [bkT], bkT)
    vv, bvv = P.sb("vv", [128, NT, 4 * 129], BF16)
    P.dma("sync", vv[:], v_d.rearrange("(n p) f -> p n f", p=128), [b_v], [bvv], bvv)
    qT = [P.sb(f"qT{i}", [128, 4, 512], BF16) for i in range(2)]
    pt = [P.sb(f"pt{i}", [128, 512], BF16) for i in range(3)]
    psc = [P.ps(f"psc{i}", [128, 512], F32) for i in range(2)]
    po = [P.ps(f"po{i}", [128, 4, 128], F32) for i in range(2)]
    pms, bpms_ = P.ps("pms", [128, 512], F32)
    pos_ = pms[:, 0:128].rearrange("p (a b c) -> p a b c", a=2, b=4)
    bpos_ = P.buf("possum")
    pw, bpw = P.ps("pw", [128, D], F32)
    ptr, bptr = P.ps("ptr", [128, 8, 128], BF16)
    prt = pms[:, 128:256]
    bprt = bpos_
    YC, bYC_ = P.sb("YC", [128, 4, D], BF16)
    bYC = [P.buf("YCs") for _ in range(4)]
    ycT, bycT = P.sb("ycT", [128, 8, 128], BF16)
    o0, bo0 = P.sb("o0", [128, 128], F32)
    o1, bo1 = P.sb("o1", [128, 128], F32)
    rs, brs = P.sb("rs", [128, 16], F32)
    XT4 = [P.sb(f"axt{i}", [128, D], F32) for i in range(4)]
    YR4 = [P.sb(f"ayr{i}", [128, 512], BF16) for i in range(4)]
    y1, by1 = P.sb("y1", [128, D], F32)
    junk, bjunk = P.sb("junk", [128, D], BF16)
    h2, bh2 = P.sb("h2", [128, D], BF16)
    h2T, bh2T = P.sb("h2T", [128, 8, 128], BF16)
    lg, blg = P.sb("lg", [128, NE], F32)
    gt, bgt = P.sb("gt", [128, NE], F32)
    t8, bt8 = P.sb("t8", [128, 16], F32)

    it = 0
    for qb in range(8):
        qcur, bqcur = qT[qb % 2]
        if qb == 0:
            P.dma("sync", qcur[:], qk_d[0:512, 0:512].rearrange("(c p) t -> p c t", p=128), [b_qk], [bqcur], bqcur)
        if qb + 1 < 8:
            qn_, bqn_ = qT[(qb + 1) % 2]
            P.dma("sync", qn_[:], qk_d[0:512, (qb + 1) * 512:(qb + 2) * 512].rearrange("(c p) t -> p c t", p=128),
                  [b_qk], [bqn_], bqn_)
        for s4 in range(4):
            n_ = qb * 4 + s4
            P.dma("sync", YR4[s4][0][:], yrw_d[n_ * 128:(n_ + 1) * 128, :], [b_yrw], [YR4[s4][1]], YR4[s4][1])
            P.dma("sync", XT4[s4][0][:], x_d[n_ * 128:(n_ + 1) * 128, :], [], [XT4[s4][1]], XT4[s4][1])
        ntk = (qb + 1) * 4
        for hd in range(4):
            for mp in range(2):
                m = hd * 2 + mp
                chn, pb = m // 2, (m % 2) * 64
                pot, bpot = po[mp]
                for tk in range(ntk):
                    ps_, bps_ = psc[it % 2]
                    ptile, bptile = pt[it % 3]
                    it += 1
                    P.mm(ps_[:], kT[pb:pb + 64, chn, tk * 128:(tk + 1) * 128], qcur[pb:pb + 64, chn, :],
                         [bkT, bqcur], [bps_])
                    P.act(ptile[:], ps_[:], AF.Exp, [bps_], [bptile], scale=0.125)
                    j = tk - qb * 4
                    if j >= 0:
                        P.tt("vector", ptile[:], ptile[:], cmk[:, j, :], ALU.mult, [bptile, bcmk], [bptile])
                    for s4 in range(4):
                        if j > s4:
                            continue
                        P.mm(pot[:, s4, :], ptile[:, s4 * 128:(s4 + 1) * 128], vv[:, tk, hd * 129:hd * 129 + 128],
                             [bptile, bvv], [bpot], start=(tk == 0 and s4 == 0), stop=(tk == ntk - 1 and s4 == 3))
                        P.mm(pos_[:, mp, s4, 0:1], ptile[:, s4 * 128:(s4 + 1) * 128], vv[:, tk, hd * 129 + 128:hd * 129 + 129],
                             [bptile, bvv], [bpos_], start=(mp == 0 and tk == 0 and s4 == 0),
                             stop=(mp == 1 and tk == ntk - 1 and s4 == 3))
            P.cp("vector", rs[:, 0:8].rearrange("p (a b) -> p a b", a=2), pos_[:, :, :, 0], [bpos_], [brs])
            P.op("vector", lambda e: e.reciprocal(out=rs[:, 8:16], in_=rs[:, 0:8]), [brs], [brs])
            P.ts("vector", rs[:, 12:16], rs[:, 12:16], lam[:, 5:6], None, ALU.mult, None, [brs, blam], [brs])
            for s4 in range(4):
                P.ts("vector", o0[:], po[0][0][:, s4, :], rs[:, 8 + s4:9 + s4], None, ALU.mult, None, [po[0][1], brs], [bo0])
                P.stt("vector", o0[:], po[1][0][:, s4, :], rs[:, 12 + s4:13 + s4], o0[:], ALU.mult, ALU.add,
                      [po[1][1], brs, bo0], [bo0])
                P.act(o1[:], o0[:], AF.Square, [bo0], [bo1, bt8], accum_out=t8[:, 0:1])
                P.ts("vector", t8[:, 1:2], t8[:, 0:1], 1.0 / 128, 1e-5, ALU.mult, ALU.add, [bt8], [bt8])
                P.act(t8[:, 2:3], t8[:, 1:2], AF.Ln, [bt8], [bt8])
                P.act(t8[:, 3:4], t8[:, 2:3], AF.Exp, [bt8], [bt8], scale=-0.5)
                P.stt("vector", YC[:, s4, hd * 128:(hd + 1) * 128], o0[:], t8[:, 3:4], sub[:], ALU.mult, ALU.mult,
                      [bo0, bt8, bsub], [bYC[s4]])
        for s4 in range(4):
            n = qb * 4 + s4
            tsl = slice(n * 128, (n + 1) * 128)
            yrt, byrt = YR4[s4]
            xt, bxt = XT4[s4]
            P.cp("vector", YC[:, s4, 512:1024], yrt[:], [byrt], [bYC[s4]])
            for k in range(8):
                P.tr(ptr[:, k, :], YC[:, s4, k * 128:(k + 1) * 128], idb[:], [bYC[s4], bidb], [bptr])
            P.cp("scalar", ycT[:].rearrange("p k t -> p (k t)"), ptr[:].rearrange("p k t -> p (k t)"), [bptr], [bycT])
            for hf in range(2):
                for k in range(8):
                    P.mm(pw[:, hf * 512:(hf + 1) * 512], ycT[:, k, :], wo[:, k, hf * 512:(hf + 1) * 512],
                         [bycT, bwo], [bpw], start=(k == 0), stop=(k == 7))
            P.cp("scalar", y1[:], pw[:], [bpw], [by1])
            P.act(junk[:], y1[:], AF.Square, [by1], [bjunk, bt8], accum_out=t8[:, 4:5])
            P.ts("vector", t8[:, 5:6], t8[:, 4:5], 1.0 / D, 1e-6, ALU.mult, ALU.add, [bt8], [bt8])
            P.act(t8[:, 6:7], t8[:, 5:6], AF.Ln, [bt8], [bt8])
            P.act(t8[:, 7:8], t8[:, 6:7], AF.Exp, [bt8], [bt8], scale=-0.5)
            P.stt("vector", y1[:], y1[:], t8[:, 7:8], rows[:, 0, :], ALU.mult, ALU.mult, [by1, bt8, brows], [by1])
            P.tt("gpsimd", xt[:], xt[:], y1[:], ALU.add, [bxt, by1], [bxt])
            P.dma("sync", x1_d[tsl, :], xt[:], [bxt], [b_x1], bxt)
            P.act(junk[:], xt[:], AF.Square, [bxt], [bjunk, bt8], accum_out=t8[:, 8:9])
            P.ts("vector", t8[:, 9:10], t8[:, 8:9], 1.0 / D, 1e-6, ALU.mult, ALU.add, [bt8], [bt8])
            P.act(t8[:, 10:11], t8[:, 9:10], AF.Ln, [bt8], [bt8])
            P.act(t8[:, 11:12], t8[:, 10:11], AF.Exp, [bt8], [bt8], scale=-0.5)
            P.ts("vector", h2[:], xt[:], t8[:, 11:12], None, ALU.mult, None, [bxt, bt8], [bh2])
            for k in range(8):
                P.tr(ptr[:, k, :], h2[:, k * 128:(k + 1) * 128], idb[:], [bh2, bidb], [bptr])
            for k in range(8):
                P.act(h2T[:, k, :], ptr[:, k, :], AF.Identity, [bptr, bmcol], [bh2T],
                      bias=mcol[:, 4, k:k + 1], scale=mcol[:, 3, k:k + 1])
            P.dma("sync", h2T_d.rearrange("(k p) t -> p k t", p=128)[:, :, tsl], h2T[:], [bh2T], [b_h2T], bh2T)
            for k in range(8):
                P.mm(prt[:, 0:NE], h2T[:, k, :], rw[:, k, :], [bh2T, brw], [bprt], start=(k == 0), stop=(k == 7))
            P.tt("vector", lg[:], prt[:, 0:NE], rb[:], ALU.add, [bprt, brb], [blg])
            P.op("vector", lambda e: e.max(out=t8[:, 0:8], in_=lg[:]), [blg], [bt8])
            P.ts("vector", gt[:], lg[:], t8[:, 3:4], None, ALU.is_ge, None, [blg, bt8], [bgt])
            P.ts("vector", t8[:, 12:13], t8[:, 0:1], -1.0, None, ALU.mult, None, [bt8], [bt8])
            P.act(lg[:], lg[:], AF.Exp, [blg, bt8], [blg], bias=t8[:, 12:13], scale=1.0)
            P.tt("vector", gt[:], gt[:], lg[:], ALU.mult, [bgt, blg], [bgt])
            P.op("vector", lambda e: e.tensor_reduce(out=t8[:, 13:14], in_=gt[:], axis=AX.X, op=ALU.add), [bgt], [bt8])
            P.op("vector", lambda e: e.reciprocal(out=t8[:, 14:15], in_=t8[:, 13:14]), [bt8], [bt8])
            P.ts("vector", gt[:], gt[:], t8[:, 14:15], None, ALU.mult, None, [bgt, bt8], [bgt])
            P.dma("sync", gat_d[tsl, :], gt[:], [bgt], [b_gat], bgt)


def phase_moe(P, nc, G):
    P.noself = NOSELF_MOE
    modp_d, x1_d, h2T_d, gat_d, out_d = (G[k] for k in ("modp_d", "x1_d", "h2T_d", "gat_d", "out_d"))
    w1g_d, w1l_d, w2e_d, b1T_d, b2_d, con_d = (G[k] for k in ("w1g_d", "w1l_d", "w2e_d", "b1T_d", "b2_d", "con_d"))
    wbf_d, b_wbf = G["wbf_d"], G["b_wbf"]
    b_modp, b_x1, b_h2T, b_gat, b_out = (G[k] for k in ("b_modp", "b_x1", "b_h2T", "b_gat", "b_out"))
    idf, bidf = P.sb("idf", [128, 128], F32)
    P.dma("sync", idf[:], con_d[:, O_ID:O_ID + 128], [], [bidf], bidf)
    c2r, bc2r = P.sb("c2r", [128, D], F32)
    P.dma("sync", c2r[:], modp_d[5:6, :].partition_broadcast(128), [b_modp], [bc2r], bc2r)
    b1T, bb1T = P.sb("b1T", [128, NE, 16], F32)
    P.dma("sync", b1T[:].rearrange("p e c -> p (e c)"), b1T_d, [], [bb1T], bb1T)
    b2s, bb2s = P.sb("b2s", [NE, D], F32)
    P.dma("sync", b2s[:], b2_d, [], [bb2s], bb2s)
    W = [[P.sb(f"w{j}_{i}", [128, 8, D], BF16) for j in range(3)] for i in range(2)]
    hq, bhq = P.sb("hq", [128, 8, 1024], BF16)
    gq, bgq = P.sb("gq", [128, 8, NE], F32)
    gT, bgT = P.sb("gT", [NE, 128], F32)
    acc, bacc_ = P.sb("acc", [128, 8, D], F32)
    bacc = [P.buf("acct") for _ in range(8)]
    actT, bactT_ = P.sb("actT", [128, 2, 8, 512], BF16)
    bactT = [[P.buf("actc") for _ in range(8)] for _ in range(2)]
    GG = [P.sb(f"mg{i}", [128, 512], F32) for i in range(2)]
    SS = [P.sb(f"msg{i}", [128, 512], F32) for i in range(2)]
    LL = [P.sb(f"ml{i}", [128, 512], F32) for i in range(2)]
    b1p, bb1p = P.sb("b1p", [128, NE, 8], F32)
    P.ts("vector", b1p[:], b1T[:, :, 8:16], 1.0, None, ALU.add, None, [bb1T], [bb1p])
    xt, bxt = P.sb("mxt", [128, D], F32)
    junk, bjunk = P.sb("mjunk", [128, D], BF16)
    t8, bt8 = P.sb("mt8", [128, 8], F32)
    pg = [P.ps(f"pg{i}", [128, 512], F32) for i in range(2)]
    pl = [P.ps(f"pl{i}", [128, 512], F32) for i in range(2)]
    po = [P.ps(f"pmo{i}", [128, 512], F32) for i in range(2)]
    pm, bpm = P.ps("pmisc", [128, 512], F32)
    it = 0
    io = 0
    wi = 0
    stepi = 0
    for qt in range(DBG_NQ):
        q0 = qt * 1024
        P.dma("sync", hq[:], h2T_d.rearrange("(k p) t -> p k t", p=128)[:, :, q0:q0 + 1024], [b_h2T], [bhq], bhq)
        P.dma("sync", gq[:], gat_d[q0:q0 + 1024, :].rearrange("(n p) e -> p n e", p=128), [b_gat], [bgq], bgq)
        for n in range(8):
            P.tr(pm[0:NE, 0:128], gq[:, n, :], idf[:], [bgq, bidf], [bpm], f32=True)
            P.cp("vector", gT[:], pm[0:NE, 0:128], [bpm], [bgT])
            for hf in range(2):
                pp, bpp = po[io % 2]
                io += 1
                P.mm(pp[:], gT[:], b2s[:, hf * 512:(hf + 1) * 512], [bgT, bb2s], [bpp], f32=True)
                P.cp("scalar", acc[:, n, hf * 512:(hf + 1) * 512], pp[:], [bpp], [bacc[n]])
        def hid(e, blk, Wset, ab):
            nonlocal it
            (w1g, bw1g), (w1l, bw1l), (w2, bw2) = Wset
            bsl = slice(blk * 512, (blk + 1) * 512)
            for fc in range(8):
                pgt, bpgt = pg[it % 2]
                plt, bplt = pl[it % 2]
                it += 1
                for k in range(8):
                    P.mm(pgt[:], w1g[:, k, fc * 128:(fc + 1) * 128], hq[:, k, bsl], [bw1g, bhq], [bpgt],
                         start=(k == 0), stop=(k == 7))
                for k in range(8):
                    P.mm(plt[:], w1l[:, k, fc * 128:(fc + 1) * 128], hq[:, k, bsl], [bw1l, bhq], [bplt],
                         start=(k == 0), stop=(k == 7))
                gi, bgi = GG[it % 2]
                si, bsi = SS[it % 2]
                li, bli = LL[it % 2]
                P.ts("vector", gi[:], pgt[:], b1T[:, e, fc:fc + 1], 7.0, ALU.add, ALU.min, [bpgt, bb1T], [bgi])
                P.act(si[:], gi[:], AF.Sigmoid, [bgi], [bsi], scale=1.702)
                P.act(li[:], plt[:], AF.Identity, [bplt, bb1p], [bli], bias=b1p[:, e, fc:fc + 1], scale=1.0)
                P.ts("vector", li[:], li[:], -6.0, 8.0, ALU.max, ALU.min, [bli], [bli])
                P.tt("gpsimd", si[:], si[:], gi[:], ALU.mult, [bsi, bgi], [bsi])
                P.tt("vector", actT[:, ab, fc, :], si[:], li[:], ALU.mult, [bsi, bli], [bactT[ab][fc]])

        def second(e, blk, Wset, ab):
            nonlocal io
            (w1g, bw1g), (w1l, bw1l), (w2, bw2) = Wset
            for tt_ in range(4):
                n = blk * 4 + tt_
                for hf in range(2):
                    pp, bpp = po[io % 2]
                    io += 1
                    for fc in range(8):
                        P.mm(pp[:], actT[:, ab, fc, tt_ * 128:(tt_ + 1) * 128], w2[:, fc, hf * 512:(hf + 1) * 512],
                             [bactT[ab][fc], bw2], [bpp], start=(fc == 0), stop=(fc == 7))
                    P.stt("vector", acc[:, n, hf * 512:(hf + 1) * 512], pp[:], gq[:, n, e:e + 1],
                          acc[:, n, hf * 512:(hf + 1) * 512], ALU.mult, ALU.add, [bpp, bgq, bacc[n]], [bacc[n]])

        prev = None
        for e in range(DBG_NEXP):
            Wset = W[wi % 2]
            (w1g, bw1g), (w1l, bw1l), (w2, bw2) = Wset
            wi += 1
            P.dma("sync", w1g[:], wbf_d[e, 0].rearrange("(k p) f -> p k f", p=128), [b_wbf], [bw1g], bw1g)
            P.dma("sync", w1l[:], wbf_d[e, 1].rearrange("(k p) f -> p k f", p=128), [b_wbf], [bw1l], bw1l)
            P.dma("sync", w2[:], wbf_d[e, 2].rearrange("(k p) f -> p k f", p=128), [b_wbf], [bw2], bw2)
            for blk in range(2):
                ab = stepi % 2
                stepi += 1
                hid(e, blk, Wset, ab)
                if prev is not None:
                    second(*prev)
                prev = (e, blk, Wset, ab)
        second(*prev)
        for n in range(8):
            tsl = slice(q0 + n * 128, q0 + (n + 1) * 128)
            P.dma("gpsimd", xt[:], x1_d[tsl, :], [b_x1], [bxt], bxt)
            P.act(junk[:], acc[:, n, :], AF.Square, [bacc[n]], [bjunk, bt8], accum_out=t8[:, 0:1])
            P.ts("vector", t8[:, 1:2], t8[:, 0:1], 1.0 / D, 1e-6, ALU.mult, ALU.add, [bt8], [bt8])
            P.act(t8[:, 2:3], t8[:, 1:2], AF.Ln, [bt8], [bt8])
            P.act(t8[:, 3:4], t8[:, 2:3], AF.Exp, [bt8], [bt8], scale=-0.5)
            P.stt("vector", acc[:, n, :], acc[:, n, :], t8[:, 3:4], c2r[:], ALU.mult, ALU.mult, [bacc[n], bt8, bc2r], [bacc[n]])
            P.tt("vector", xt[:], xt[:], acc[:, n, :], ALU.add, [bxt, bacc[n]], [bxt])
            P.dma("gpsimd", out_d[tsl, :], xt[:], [bxt], [b_out], bxt)


def _consts():
    c = np.zeros((128, NCONST), np.float32)
    r = np.arange(128)[:, None]
    q = np.arange(128)[None, :]
    same = (r // 64) == (q // 64)
    su = (same & ((r % 64) < (q % 64))).astype(np.float32)
    sl = (same & ((r % 64) > (q % 64))).astype(np.float32)
    ui = (same & ((r % 64) <= (q % 64))).astype(np.float32)
    c[:, O_ID:O_ID + 128] = np.eye(128, dtype=np.float32)
    for i, m in enumerate((su, su, sl, ui, ui)):
        c[:, O_M5 + i * 128:O_M5 + (i + 1) * 128] = m
    c[:, O_ONES:O_ONES + 128] = same.astype(np.float32)
    c[:64, O_IND] = 1.0
    c[64:, O_IND + 1] = 1.0
    inv_freq = (500000.0 ** (-np.arange(0, 16, 2, dtype=np.float32) / 16)).astype(np.float32)
    for p in range(128):
        d = p % 64
        if d < 16:
            c[p, O_FREQ] = inv_freq[d % 8]
            c[p, O_SIGN] = -1.0 if d < 8 else 1.0
            pp = p + 8 if d < 8 else p - 8
            c[pp, O_PM + p] = 1.0
    tq = np.arange(512)[None, :]
    tk = np.arange(128)[:, None]
    for j in range(4):
        c[:, O_CM + j * 512:O_CM + (j + 1) * 512] = ((j * 128 + tk) <= tq).astype(np.float32)
    return c


_NC_CACHE = {}


def _in_maps(inp):
    f = lambda a: np.ascontiguousarray(np.asarray(a, dtype=np.float32))
    B = 8
    w1 = np.asarray(inp["moe_w1"])[0]
    w1g = np.ascontiguousarray(w1[:, :, 0::2])
    w1l = np.ascontiguousarray(w1[:, :, 1::2])
    b1 = np.asarray(inp["moe_b1"])[0]
    b1cat = np.concatenate([b1[:, 0::2].reshape(NE, 8, 128), b1[:, 1::2].reshape(NE, 8, 128)], axis=1)
    b1T = np.ascontiguousarray(b1cat.transpose(2, 0, 1).reshape(128, NE * 16)).astype(np.float32)
    shared = dict(
        ada_w=f(inp["ada_w"][0]), ada_b=f(inp["ada_b"]).reshape(1, 6 * D),
        norms=f(np.stack([inp["pre_mix_norm"][0], inp["post_mix_norm"][0], inp["pre_ffn_norm"][0], inp["post_ffn_norm"][0]])),
        w_in=f(inp["w_in"][0]), w_out=f(inp["w_out"][0]),
        lamv=f(np.stack([inp["da_lambda_q1"][0], inp["da_lambda_k1"][0], inp["da_lambda_q2"][0], inp["da_lambda_k2"][0]])),
        subln=f(inp["da_subln"]).reshape(1, 128), mu=f(inp["rw_mu"]).reshape(1, 1792),
        rwv=f(np.stack([inp["rw_w0"][0], inp["rw_a0"][0], inp["rw_k_k"][0], inp["rw_k_a"][0],
                        np.asarray(inp["rw_r_k"])[0].reshape(512), inp["rw_ln_w"][0], inp["rw_ln_b"][0]])),
        rw_w2=f(inp["rw_w2"][0]), rw_a2=f(inp["rw_a2"][0]), rw_g2=f(inp["rw_g2"][0]),
        router_w=f(inp["router_w"][0]), router_b=f(inp["router_b"]).reshape(1, NE),
        w1g=w1g, w1l=w1l, b1T=b1T, w2e=f(inp["moe_w2"][0]), b2=f(inp["moe_b2"][0]),
        consts=_consts(),
    )
    x = np.asarray(inp["x"], dtype=np.float32)
    c = np.asarray(inp["c"], dtype=np.float32)
    pos = np.asarray(inp["positions"]).astype(np.int32)
    in_maps = []
    for b in range(B):
        m = dict(shared)
        m["x"] = np.ascontiguousarray(x[b])
        m["cT"] = np.ascontiguousarray(c[b].reshape(8, 128).T)
        m["pos"] = np.ascontiguousarray(pos[b].reshape(1, T))
        in_maps.append(m)
    return in_maps


def kernel(**inp):
    B = 8
    if "nc" not in _NC_CACHE:
        _NC_CACHE["nc"] = build_program()
    nc = _NC_CACHE["nc"]
    in_maps = _in_maps(inp)
    res = run_bass_kernel_spmd(nc, in_maps, core_ids=list(range(B)))
    return np.stack([np.asarray(r["out"], dtype=np.float32) for r in res.results], axis=0)
```

```python
import contextlib
import math
import numpy as np
import concourse.bass as bass
import concourse.mybir as mybir
from concourse.bass_utils import run_bass_kernel_spmd

ALU = mybir.AluOpType
AF = mybir.ActivationFunctionType
F32 = mybir.dt.float32
BF16 = mybir.dt.bfloat16
I32 = mybir.dt.int32
AX = mybir.AxisListType

D = 1024
T = 4096
NT = 32
NE = 32
C0 = math.exp(-0.5)
LAMBDA_INIT = 0.8 - 0.6 * math.exp(0.0)

O_ID = 0
O_M5 = 128
O_UI = O_M5 + 384
O_SU = O_M5
O_SL = O_M5 + 256
O_ONES = 768
O_IND = 896
O_FREQ = 898
O_SIGN = 899
O_PM = 900
O_CM = 1028
NCONST = O_CM + 2048
DBG_STOP = 99
NOSELF_MOE = ('tensor',)
NOSELF_ATTN = ('tensor',)
NOSELF_FRONT = ('tensor',)
DBG_X = 0
DBG_NQ = 4
DBG_NEXP = NE
DBG_NT = NT


class Buf:
    __slots__ = ("name", "w", "r", "dsem", "dcount")

    def __init__(self, name):
        self.name = name
        self.w = {}
        self.r = {}
        self.dsem = None
        self.dcount = 0


class Eng:
    def __init__(self, name, sem):
        self.name = name
        self.sem = sem
        self.count = 0
        self.waited = {}
        self.thunks = []


class Prog:
    def __init__(self, nc, stack):
        self.nc = nc
        self.gstack = stack
        self.stack = stack
        self.engs = {}
        self.sems = {}
        self.vals = {}
        for n in ("tensor", "vector", "scalar", "gpsimd", "sync"):
            sem = stack.enter_context(nc.semaphore("es_" + n))
            self.engs[n] = Eng(n, sem)
            self.sems[("e", n)] = sem
            self.vals[("e", n)] = 0
        self.nbuf = 0
        self.ninstr = 0
        self.allbufs = []
        self.gen = 0
        self.noself = ()
        self.last_f32 = False
        self.skip_keys = set()
        self.persist = []

    def buf(self, name=None):
        self.nbuf += 1
        b = Buf(f"{name or 'b'}{self.nbuf}")
        self.allbufs.append(b)
        return b

    def new_engine_sems(self):
        self.gen += 1
        for n, eng in self.engs.items():
            old = ("e", n)
            self.vals.pop(old, None)
            sem = self.gstack.enter_context(self.nc.semaphore(f"es{self.gen}_{n}"))
            eng.sem = sem
            eng.count = 0
            eng.waited = {}
            self.sems[old] = sem
            self.vals[old] = 0
        for b in self.allbufs:
            if b in self.persist:
                continue
            b.w = {}
            b.r = {}

    def sb(self, name, shape, dtype):
        self.nbuf += 1
        t = self.stack.enter_context(self.nc.sbuf_tensor(f"sb{self.nbuf}_{name}", list(shape), dtype))
        return t, self.buf(name)

    def ps(self, name, shape, dtype=F32):
        self.nbuf += 1
        t = self.stack.enter_context(self.nc.psum_tensor(f"ps{self.nbuf}_{name}", list(shape), dtype))
        return t, self.buf(name)

    def _dsem(self, b):
        if b.dsem is None:
            s = self.gstack.enter_context(self.nc.semaphore("ds_" + b.name))
            b.dsem = ("d", b.name)
            self.sems[b.dsem] = s
            self.vals[b.dsem] = 0
        return b.dsem

    def _collect(self, eng, reads, writes):
        need = {}
        for b in reads:
            for k, v in b.w.items():
                if need.get(k, 0) < v:
                    need[k] = v
        for b in writes:
            for k, v in b.w.items():
                if need.get(k, 0) < v:
                    need[k] = v
            for k, v in b.r.items():
                if need.get(k, 0) < v:
                    need[k] = v
        own = ("e", eng.name)
        for k, v in need.items():
            if k == own and eng.name in self.noself:
                continue
            if eng.waited.get(k, 0) < v:
                eng.waited[k] = v
                sem = self.sems[k]
                eng.thunks.append(lambda e, sem=sem, v=v: e.wait_ge(sem, v))

    def op(self, engname, fn, reads=(), writes=(), selfwait=False):
        eng = self.engs[engname]
        self._collect(eng, reads, writes)
        if selfwait and eng.count > 0:
            own = ("e", engname)
            if eng.waited.get(own, 0) < eng.count:
                eng.waited[own] = eng.count
                eng.thunks.append(lambda e, sem=eng.sem, v=eng.count: e.wait_ge(sem, v))
        eng.count += 1
        c = eng.count
        sem = eng.sem
        eng.thunks.append(lambda e, fn=fn, sem=sem: fn(e).then_inc(sem, 1))
        key = ("e", engname)
        self.vals[key] = c
        for b in reads:
            b.r[key] = c
        for b in writes:
            b.w = {key: c}
            b.r = {}
        self.ninstr += 1

    def dma(self, q, out_ap, in_ap, reads, writes, sbuf_buf, **kw):
        eng = self.engs[q]
        self._collect(eng, reads, writes)
        key = self._dsem(sbuf_buf)
        sbuf_buf.dcount += 16
        c = sbuf_buf.dcount
        self.vals[key] = c
        sem = self.sems[key]
        eng.thunks.append(
            lambda e, o=out_ap, i=in_ap, sem=sem, kw=kw: e.dma_start(out=o, in_=i, **kw).then_inc(sem, 16))
        for b in reads:
            b.r[key] = c
        for b in writes:
            if b is sbuf_buf:
                b.w = {key: c}
                b.r = {}
            else:
                b.w[key] = c
        self.ninstr += 1

    def barrier(self):
        for eng in self.engs.values():
            for k, v in self.vals.items():
                if k in self.skip_keys:
                    continue
                if v > 0 and eng.waited.get(k, 0) < v:
                    eng.waited[k] = v
                    sem = self.sems[k]
                    eng.thunks.append(lambda e, sem=sem, v=v: e.wait_ge(sem, v))

    def flush(self):
        nc = self.nc
        engs = self.engs
        with nc.Block() as block:
            @block.tensor
            def _(e):
                for t in engs["tensor"].thunks:
                    t(e)

            @block.vector
            def _(e):
                for t in engs["vector"].thunks:
                    t(e)

            @block.scalar
            def _(e):
                for t in engs["scalar"].thunks:
                    t(e)

            @block.gpsimd
            def _(e):
                for t in engs["gpsimd"].thunks:
                    t(e)

            @block.sync
            def _(e):
                for t in engs["sync"].thunks:
                    t(e)
        for e in engs.values():
            e.thunks = []

    @contextlib.contextmanager
    def phase(self):
        with contextlib.ExitStack() as ph:
            self.stack = ph
            yield
            self.barrier()
            self.flush()
        self.stack = self.gstack
        self.new_engine_sems()

    def mm(self, out, lhsT, rhs, R, W, start=True, stop=True, f32=False):
        sw = f32 or self.last_f32
        self.last_f32 = f32
        self.op("tensor", lambda e: e.matmul(out, lhsT=lhsT, rhs=rhs, start=start, stop=stop), R, W, selfwait=sw)

    def tr(self, out, in_, ident, R, W, f32=False):
        sw = f32 or self.last_f32
        self.last_f32 = f32
        self.op("tensor", lambda e: e.transpose(out, in_, ident), R, W, selfwait=sw)

    def tt(self, eng, out, in0, in1, op, R, W):
        self.op(eng, lambda e: e.tensor_tensor(out=out, in0=in0, in1=in1, op=op), R, W)

    def ts(self, eng, out, in0, s1, s2, op0, op1, R, W):
        if s2 is None:
            self.op(eng, lambda e: e.tensor_scalar(out=out, in0=in0, scalar1=s1, scalar2=None, op0=op0), R, W)
        else:
            self.op(eng, lambda e: e.tensor_scalar(out=out, in0=in0, scalar1=s1, scalar2=s2, op0=op0, op1=op1), R, W)

    def stt(self, eng, out, in0, scalar, in1, op0, op1, R, W):
        eng = "vector"
        self.op(eng, lambda e: e.scalar_tensor_tensor(out=out, in0=in0, scalar=scalar, in1=in1, op0=op0, op1=op1), R, W)

    def act(self, out, in_, func, R, W, bias=None, scale=None, accum_out=None):
        kw = {}
        if bias is not None:
            kw["bias"] = bias
        if scale is not None:
            kw["scale"] = scale
        if accum_out is not None:
            kw["accum_out"] = accum_out
        self.op("scalar", lambda e: e.activation(out=out, in_=in_, func=func, **kw), R, W)

    def cp(self, eng, out, in_, R, W):
        if eng == "scalar":
            self.op("scalar", lambda e: e.copy(out=out, in_=in_), R, W)
        else:
            self.op(eng, lambda e: e.tensor_copy(out=out, in_=in_), R, W)

    def ms(self, eng, ap, val, W):
        self.op(eng, lambda e: e.memset(ap, val), [], W)


def _rr(P):
    state = {"i": 0}

    def nxt():
        state["i"] += 1
        return "vector" if state["i"] % 3 else "gpsimd"
    return nxt


def build_program(debug=False, upto=3):
    nc = bass.Bass("TRN2", target_bir_lowering=False)
    skind = "ExternalOutput" if debug else "Internal"

    def din(name, shape, dt=F32):
        return nc.dram_tensor(name, list(shape), dt, kind="ExternalInput").ap()

    x_d = din("x", [T, D])
    cT_d = din("cT", [128, 8])
    pos_d = din("pos", [1, T], I32)
    adaw_d = din("ada_w", [D, 6 * D])
    adab_d = din("ada_b", [1, 6 * D])
    norms_d = din("norms", [4, D])
    win_d = din("w_in", [D, 3328])
    wout_d = din("w_out", [D, D])
    lamv_d = din("lamv", [4, 64])
    subln_d = din("subln", [1, 128])
    mu_d = din("mu", [1, 1792])
    rwv_d = din("rwv", [7, 512])
    w2_d = din("rw_w2", [64, 512])
    a2_d = din("rw_a2", [64, 512])
    g2_d = din("rw_g2", [128, 512])
    rtw_d = din("router_w", [D, NE])
    rtb_d = din("router_b", [1, NE])
    w1g_d = din("w1g", [NE, D, D]) if upto >= 3 else None
    w1l_d = din("w1l", [NE, D, D]) if upto >= 3 else None
    b1T_d = din("b1T", [128, NE * 16])
    w2e_d = din("w2e", [NE, D, D]) if upto >= 3 else None
    b2_d = din("b2", [NE, D])
    con_d = din("consts", [128, NCONST])
    out_d = nc.dram_tensor("out", [T, D], F32, kind="ExternalOutput").ap()

    modp_d = nc.dram_tensor("modp", [6, D], F32, kind=skind).ap()
    qk_d = nc.dram_tensor("qk_s", [D, T], BF16, kind=skind).ap()
    v_d = nc.dram_tensor("v_s", [T, 4 * 129], BF16, kind=skind).ap()
    yrw_d = nc.dram_tensor("yrw_s", [T, 512], BF16, kind=skind).ap()
    x1_d = nc.dram_tensor("x1_s", [T, D], F32, kind=skind).ap()
    h2T_d = nc.dram_tensor("h2T_s", [D, T], BF16, kind=skind).ap()
    gat_d = nc.dram_tensor("gat_s", [T, NE], F32, kind=skind).ap()
    wbf_d = nc.dram_tensor("wbf_s", [NE, 3, D, D], BF16).ap() if upto >= 3 else None

    with contextlib.ExitStack() as gst:
        P = Prog(nc, gst)
        b_modp = P.buf("modp")
        b_qk = P.buf("qkd")
        b_v = P.buf("vd")
        b_yrw = P.buf("yrwd")
        b_x1 = P.buf("x1d")
        b_h2T = P.buf("h2Td")
        b_gat = P.buf("gatd")
        b_out = P.buf("outd")
        b_wbf = P.buf("wbfd")
        b_conv = P.buf("wconv")

        with P.phase():
            cT, bcT = P.sb("cT", [128, 8], F32)
            sc, bsc = P.sb("sc", [128, 8], F32)
            P.dma("sync", cT[:], cT_d, [], [bcT], bcT)
            P.act(sc[:], cT[:], AF.Silu, [bcT], [bsc])
            aw = [P.sb(f"aw{i}", [128, 3072], F32) for i in range(2)]
            pm, bpm = P.ps("pmod", [128, 3072], F32)
            mrow, bmrow = P.sb("mrow", [1, 6 * D], F32)
            brow, bbrow = P.sb("brow", [1, 6 * D], F32)
            nrm, bnrm = P.sb("nrm", [1, 4 * D], F32)
            orow, borow = P.sb("orow", [1, 6 * D], F32)
            P.dma("sync", brow[:], adab_d, [], [bbrow], bbrow)
            P.dma("sync", nrm[:], norms_d.rearrange("(o a) d -> o (a d)", o=1), [], [bnrm], bnrm)
            i = 0
            for half in range(2):
                for k in range(8):
                    t_, b_ = aw[i % 2]
                    i += 1
                    P.dma("sync", t_[:], adaw_d[k * 128:(k + 1) * 128, half * 3072:(half + 1) * 3072], [], [b_], b_)
                    for j in range(6):
                        P.mm(pm[0:1, j * 512:(j + 1) * 512], sc[:, k:k + 1], t_[:, j * 512:(j + 1) * 512],
                             [bsc, b_], [bpm], start=(k == 0), stop=(k == 7))
                P.tt("vector", mrow[:, half * 3072:(half + 1) * 3072], pm[0:1, :], brow[:, half * 3072:(half + 1) * 3072],
                     ALU.add, [bpm, bbrow], [bmrow])

            def mseg(i_):
                return mrow[:, i_ * D:(i_ + 1) * D]

            def nseg(i_):
                return nrm[:, i_ * D:(i_ + 1) * D]
            P.stt("vector", orow[:, 0:D], mseg(1), 1.0, nseg(0), ALU.add, ALU.mult, [bmrow, bnrm], [borow])
            P.cp("vector", orow[:, D:2 * D], mseg(0), [bmrow], [borow])
            P.tt("vector", orow[:, 2 * D:3 * D], mseg(2), nseg(1), ALU.mult, [bmrow, bnrm], [borow])
            P.stt("vector", orow[:, 3 * D:4 * D], mseg(4), 1.0, nseg(2), ALU.add, ALU.mult, [bmrow, bnrm], [borow])
            P.cp("vector", orow[:, 4 * D:5 * D], mseg(3), [bmrow], [borow])
            P.tt("vector", orow[:, 5 * D:6 * D], mseg(5), nseg(3), ALU.mult, [bmrow, bnrm], [borow])
            P.dma("sync", modp_d.rearrange("(o a) d -> o (a d)", o=1), orow[:], [borow], [b_modp], borow)

        if upto >= 1:
          with P.phase():
            phase_front(P, nc, locals())

        if upto >= 2:
          with P.phase():
            phase_attn(P, nc, locals())

        if upto >= 3:
          with P.phase():
            phase_moe(P, nc, locals())
    return nc


def phase_front(P, nc, G):
    P.noself = NOSELF_FRONT
    x_d, pos_d, win_d, mu_d, rwv_d = G["x_d"], G["pos_d"], G["win_d"], G["mu_d"], G["rwv_d"]
    w2_d, a2_d, g2_d, con_d, modp_d = G["w2_d"], G["a2_d"], G["g2_d"], G["con_d"], G["modp_d"]
    qk_d, v_d, yrw_d = G["qk_d"], G["v_d"], G["yrw_d"]
    b_modp, b_qk, b_v, b_yrw = G["b_modp"], G["b_qk"], G["b_v"], G["b_yrw"]
    rr = _rr(P)

    con, bcon = P.sb("con", [128, NCONST], F32)
    P.dma("sync", con[:], con_d, [], [bcon], bcon)
    identf = con[:, O_ID:O_ID + 128]
    idb, bidb = P.sb("idb", [128, 128], BF16)
    P.cp("vector", idb[:], identf, [bcon], [bidb])
    mcol, bmcol = P.sb("mcol", [128, 6, 8], F32)
    P.dma("sync", mcol[:], modp_d.rearrange("a (k p) -> p a k", p=128), [b_modp], [bmcol], bmcol,
          allow_slow_non_contiguous=True)

    wda, bwda = P.sb("wda", [128, 8, 1536], BF16)
    P.dma("gpsimd", wda[:], win_d[:, 0:1536].rearrange("(k p) c -> p k c", p=128), [], [bwda], bwda)
    w1, bw1 = P.sb("w1", [128, 8, 1792], BF16)
    w2m, bw2m = P.sb("w2m", [128, 8, 1792], BF16)
    prm, bprm = P.sb("prm", [128, 7, 512], F32)
    P.dma("sync", prm[:].rearrange("p a d -> p (a d)"),
          rwv_d.rearrange("(o a) d -> o (a d)", o=1).partition_broadcast(128), [], [bprm], bprm)
    lw2, blw2 = P.sb("lw2", [128, 512], BF16)
    lg2, blg2 = P.sb("lg2", [128, 512], BF16)
    P.dma("gpsimd", lw2[0:64, :], w2_d, [], [blw2], blw2)
    P.dma("gpsimd", lw2[64:128, :], a2_d, [], [blw2], blw2)
    P.dma("gpsimd", lg2[:], g2_d, [], [blg2], blg2)
    pmb, bpmb = P.sb("pmb", [128, 128], BF16)
    P.cp("vector", pmb[:], con[:, O_PM:O_PM + 128], [bcon], [bpmb])

    ctab, bctab = P.sb("ctab", [128, T], BF16)
    stab, bstab = P.sb("stab", [128, T], BF16)
    with contextlib.ExitStack() as tmp:
        old = P.stack
        P.stack = tmp
        mub, bmub = P.sb("mub", [128, 1792], F32)
        omu, bomu = P.sb("omu", [128, 1792], F32)
        P.dma("sync", mub[:], mu_d.partition_broadcast(128), [], [bmub], bmub)
        P.ts("vector", omu[:], mub[:], -1.0, 1.0, ALU.mult, ALU.add, [bmub], [bomu])
        wst = [P.sb(f"wst{i}", [128, 1792], F32) for i in range(2)]
        for k in range(8):
            t_, b_ = wst[k % 2]
            P.dma("sync", t_[:], win_d[k * 128:(k + 1) * 128, 1536:3328], [], [b_], b_)
            P.tt("vector", w1[:, k, :], t_[:], omu[:], ALU.mult, [b_, bomu], [bw1])
            P.tt("gpsimd", w2m[:, k, :], t_[:], mub[:], ALU.mult, [b_, bmub], [bw2m])
        posi, bposi = P.sb("posi", [128, 1024], I32)
        ang, bang = P.sb("ang", [128, 1024], F32)
        y_, by_ = P.sb("ry", [128, 1024], F32)
        kf, bkf = P.sb("rkf", [128, 1024], F32)
        ki, bki = P.sb("rki", [128, 1024], I32)
        for q4 in range(4):
            sl = slice(q4 * 1024, (q4 + 1) * 1024)
            P.dma("sync", posi[:], pos_d[:, sl].partition_broadcast(128), [], [bposi], bposi)
            P.cp("vector", ang[:], posi[:], [bposi], [bang])
            P.ts("vector", ang[:], ang[:], con[:, O_FREQ:O_FREQ + 1], None, ALU.mult, None, [bang, bcon], [bang])
            for which, shift in ((0, math.pi * 1.5), (1, math.pi)):
                P.ts("vector", y_[:], ang[:], shift, None, ALU.add, None, [bang], [by_])
                P.ts("vector", kf[:], y_[:], 1.0 / (2 * math.pi), None, ALU.mult, None, [by_], [bkf])
                P.cp("vector", ki[:], kf[:], [bkf], [bki])
                P.cp("vector", kf[:], ki[:], [bki], [bkf])
                P.stt("vector", y_[:], kf[:], -2 * math.pi, y_[:], ALU.mult, ALU.add, [bkf, by_], [by_])
                P.ts("vector", kf[:], y_[:], 0.0, 2 * math.pi, ALU.is_lt, ALU.mult, [by_], [bkf])
                P.tt("vector", y_[:], y_[:], kf[:], ALU.add, [by_, bkf], [by_])
                P.ts("vector", y_[:], y_[:], -math.pi, None, ALU.add, None, [by_], [by_])
                P.ts("vector", y_[:], y_[:], -math.pi, math.pi, ALU.max, ALU.min, [by_], [by_])
                if which == 0:
                    P.act(ctab[:, sl], y_[:], AF.Sin, [by_], [bctab])
                else:
                    P.act(kf[:], y_[:], AF.Sin, [by_], [bkf])
                    P.ts("vector", stab[:, sl], kf[:], con[:, O_SIGN:O_SIGN + 1], None, ALU.mult, None, [bkf, bcon], [bstab])
        P.barrier()
        P.flush()
        P.stack = old

    if G.get("wbf_d") is not None:
        for e_ in range(NE):
            for j_, src in enumerate((G["w1g_d"], G["w1l_d"], G["w2e_d"])):
                P.dma("gpsimd", G["wbf_d"][e_, j_], src[e_], [], [G["b_wbf"]], G["b_conv"])
        P.skip_keys.add(G["b_conv"].dsem)
        P.persist.append(G["b_wbf"])

    def f32t(name, w=512):
        return P.sb(name, [128, w], F32)

    XT = [P.sb(f"xt{i}", [128, D], F32) for i in range(2)]
    xn, bxn = P.sb("xn", [128, D], BF16)
    junk, bjunk = xn, bxn
    st8, bst8 = P.sb("st8", [128, 8], F32)
    hT = [P.sb(f"hT{i}", [128, 8, 129], BF16) for i in range(2)]
    P.ms("vector", hT[1][0][:, :, 128:129], 0.0, [hT[1][1]])
    qf, bqf = P.sb("qf", [128, 128], BF16)
    qf2, bqf2 = P.sb("qf2", [128, 128], F32)
    t1, bt1 = P.sb("t1", [128, 128], F32)
    t2, bt2 = P.sb("t2", [128, 128], F32)
    qko, bqko = P.sb("qko", [128, 8, 128], BF16)
    vo, bvo = P.sb("vo", [128, 4, 129], BF16)
    P.ms("vector", vo[:, :, 128:129], 1.0, [bvo])
    r_s, br = f32t("r_s")
    k_s, bk = f32t("k_s")
    VS2 = [f32t(f"v_s{i}") for i in range(2)]
    lo0, blo0 = P.sb("lo0", [128, 128], BF16)
    lo1, blo1 = P.sb("lo1", [128, 128], BF16)
    VB2 = [P.sb(f"v_b{i}", [128, 512], BF16) for i in range(2)]
    STb, bSTb_ = P.sb("STb", [128, 4, 64], BF16)
    P.ms("vector", STb[:], 0.0, [bSTb_])
    sig, bsig = f32t("sig")
    a_s, ba = f32t("a_s")
    GS2 = [f32t(f"g_s{i}") for i in range(2)]
    BON2 = [P.sb(f"bon{i}", [128, 8], F32) for i in range(2)]
    gn, bgn = P.sb("gn", [128, 16], F32)
    kk, bkk = f32t("kk")
    km, bkm = f32t("km")
    bb, bbb = f32t("bb")
    e1, be1 = f32t("e1")
    e2, be2 = f32t("e2")
    tm1, btm1 = f32t("tm1")
    tm2, btm2 = f32t("tm2")
    At, bAt = P.sb("At", [128, 512], BF16)
    Rt, bRt = P.sb("Rt", [128, 512], BF16)
    Bt, bBt = P.sb("Bt", [128, 512], BF16)
    Kt, bKt = P.sb("Kt", [128, 512], BF16)
    BH2 = [P.sb(f"Bh{i}", [128, 512], BF16) for i in range(2)]
    KH2 = [P.sb(f"Kh{i}", [128, 512], BF16) for i in range(2)]
    FM2 = [P.sb(f"FM{i}", [128, 4, 4, 128], BF16)[0] for i in range(2)]
    bFM2 = [[P.buf("FMp") for _ in range(4)] for _ in range(2)]
    RP2 = [P.sb(f"RP{i}", [128, 4, 384], BF16)[0] for i in range(2)]
    bRP2 = [[P.buf("RPp") for _ in range(4)] for _ in range(2)]
    for i in range(2):
        P.ms("vector", RP2[i][:], 0.0, bRP2[i])
    PC2 = [P.sb(f"pc{i}", [128, 4, 2], F32) for i in range(2)]
    ST, bST_ = P.sb("ST", [128, 4, 64], F32)
    bST = [P.buf("STh") for _ in range(8)]
    P.ms("vector", ST[:], 0.0, bST)
    Gs = [P.sb(f"Gs{i}", [128, 640], BF16) for i in range(2)]
    Nb = [[P.sb(f"N{i}_{j}", [128, 128], BF16) for j in range(2)] for i in range(2)]
    Lb = [[P.sb(f"L{i}_{j}", [128, 128], BF16) for j in range(2)] for i in range(2)]
    Tb = [[P.sb(f"T{i}_{j}", [128, 128], BF16) for j in range(2)] for i in range(2)]
    Zs = [P.sb(f"Zs{i}", [128, 64], BF16) for i in range(2)]
    Us = [P.sb(f"Us{i}", [128, 64], BF16) for i in range(2)]
    yo, byo = P.sb("yo", [128, 512], BF16)

    pT, bpT = P.ps("pT", [128, 8, 128], BF16)
    pQ, bpQ = P.ps("pQ", [128, 512], F32)
    pV, bpV = P.ps("pV", [128, 512], F32)
    pF, bpF = P.ps("pF", [128, 512], F32)
    pF1, bpF1 = P.ps("pF1", [128, 512], F32)
    pG0, bpG0 = P.ps("pG0", [128, 512], F32)
    pG1, bpG1 = P.ps("pG1", [128, 512], F32)
    pY, bpY = P.ps("pY", [128, 512], F32)

    A1c = mcol[:, 0, :]
    B1c = mcol[:, 1, :]

    if DBG_STOP < 1:
        return
    for n in range(DBG_NT):
        tsl = slice(n * 128, (n + 1) * 128)
        hcur, bhcur = hT[n % 2]
        hprev, bhprev = hT[(n + 1) % 2]
        xt, bxt = XT[n % 2]
        ysb, bysb = xt[:, 0:512], bxt
        v_s, bv = VS2[n % 2]
        g_s, bg = GS2[n % 2]
        bon, bbon = BON2[n % 2]
        v_b, bvb = VB2[n % 2]
        Bh, bBh = BH2[n % 2]
        Kh, bKh = KH2[n % 2]
        FM, bFM = FM2[n % 2], bFM2[n % 2]
        RP, bRP = RP2[n % 2], bRP2[n % 2]
        pc, bpc = PC2[n % 2]
        if n == 0:
            P.dma("sync", xt[:], x_d[tsl, :], [], [bxt], bxt)
        if n + 1 < DBG_NT:
            xtn, bxtn = XT[(n + 1) % 2]
            P.dma("sync", xtn[:], x_d[(n + 1) * 128:(n + 2) * 128, :], [], [bxtn], bxtn)
        P.act(junk[:], xt[:], AF.Square, [bxt], [bjunk, bst8], accum_out=st8[:, 0:1])
        P.ts("vector", st8[:, 1:2], st8[:, 0:1], 1.0 / D, 1e-6, ALU.mult, ALU.add, [bst8], [bst8])
        P.act(st8[:, 2:3], st8[:, 1:2], AF.Ln, [bst8], [bst8])
        P.act(st8[:, 3:4], st8[:, 2:3], AF.Exp, [bst8], [bst8], scale=-0.5)
        P.ts("vector", xn[:], xt[:], st8[:, 3:4], None, ALU.mult, None, [bxt, bst8], [bxn])
        for k in range(8):
            P.tr(pT[:, k, :], xn[:, k * 128:(k + 1) * 128], idb[:], [bxn, bidb], [bpT])
        P.cp("vector", hcur[:, :, 0:1], hprev[:, :, 128:129], [bhprev], [bhcur])
        for k in range(8):
            P.act(hcur[:, k, 1:129], pT[:, k, :], AF.Identity, [bpT, bmcol], [bhcur],
                  bias=B1c[:, k:k + 1], scale=A1c[:, k:k + 1])
        hx = lambda k: hcur[:, k, 1:129]
        hs = lambda k: hcur[:, k, 0:128]
        for cq in range(8):
            for k in range(8):
                P.mm(pQ[:, 0:128], wda[:, k, cq * 128:(cq + 1) * 128], hx(k), [bwda, bhcur], [bpQ],
                     start=(k == 0), stop=(k == 7))
            P.cp("scalar", qf[:], pQ[:, 0:128], [bpQ], [bqf])
            P.mm(pQ[:, 128:256], pmb[:], qf[:], [bpmb, bqf], [bpQ])
            P.cp("scalar", t2[:], pQ[:, 128:256], [bpQ], [bt2])
            P.tt("vector", t1[:], qf[:], ctab[:, tsl], ALU.mult, [bqf, bctab], [bt1])
            P.tt("gpsimd", qf2[:], t2[:], stab[:, tsl], ALU.mult, [bt2, bstab], [bqf2])
            P.tt("vector", qko[:, cq, :], t1[:], qf2[:], ALU.add, [bt1, bqf2], [bqko])
        P.dma("sync", qk_d.rearrange("(c p) t -> p c t", p=128)[:, :, tsl], qko[:], [bqko], [b_qk], bqko)
        for k in range(8):
            P.mm(pV[:], hx(k), wda[:, k, 1024:1536], [bhcur, bwda], [bpV], start=(k == 0), stop=(k == 7))
        P.cp("scalar", vo[:, :, 0:128], pV[:].rearrange("p (h d) -> p h d", h=4), [bpV], [bvo])
        P.dma("sync", v_d[tsl, :], vo[:].rearrange("p h d -> p (h d)"), [bvo], [b_v], bvo)
        if DBG_STOP < 2:
            continue
        for cc, (dst, bdst) in enumerate(((r_s, br), (k_s, bk), (v_s, bv))):
            pp, bpp = pV, bpV
            for k in range(8):
                P.mm(pp[:], hx(k), w1[:, k, cc * 512:(cc + 1) * 512], [bhcur, bw1], [bpp], start=(k == 0), stop=False)
            for k in range(8):
                P.mm(pp[:], hs(k), w2m[:, k, cc * 512:(cc + 1) * 512], [bhcur, bw2m], [bpp], start=False, stop=(k == 7))
            P.cp("scalar", dst[:], pp[:], [bpp], [bdst])
            if cc == 2:
                P.cp("gpsimd", v_b[:], v_s[:], [bv], [bvb])
        for lc in range(2):
            cs_ = slice(1536 + lc * 128, 1536 + (lc + 1) * 128)
            osl = pQ[:, 256:384]
            for k in range(8):
                P.mm(osl, w1[:, k, cs_], hx(k), [bw1, bhcur], [bpQ], start=(k == 0), stop=False)
            for k in range(8):
                P.mm(osl, w2m[:, k, cs_], hs(k), [bw2m, bhcur], [bpQ], start=False, stop=(k == 7))
            if lc == 0:
                P.act(lo0[0:64, :], pQ[0:64, 256:384], AF.Tanh, [bpQ], [blo0])
                P.cp("scalar", lo0[64:128, :], pQ[64:128, 256:384], [bpQ], [blo0])
            else:
                P.act(lo1[:], pQ[:, 256:384], AF.Sigmoid, [bpQ], [blo1])
        P.mm(pV[:], lo0[0:64, :], lw2[0:64, :], [blo0, blw2], [bpV])
        P.tt("vector", sig[:], pV[:], prm[:, 0, :], ALU.add, [bpV, bprm], [bsig])
        P.act(sig[:], sig[:], AF.Sigmoid, [bsig], [bsig])
        P.mm(pV[:], lo0[64:128, :], lw2[64:128, :], [blo0, blw2], [bpV])
        P.tt("vector", a_s[:], pV[:], prm[:, 1, :], ALU.add, [bpV, bprm], [ba])
        P.act(a_s[:], a_s[:], AF.Sigmoid, [ba], [ba])
        P.mm(pV[:], lo1[:], lg2[:], [blo1, blg2], [bpV])
        P.cp("scalar", g_s[:], pV[:], [bpV], [bg])
        P.tt(rr(), kk[:], k_s[:], prm[:, 2, :], ALU.mult, [bk, bprm], [bkk])
        P.tt(rr(), tm1[:], kk[:], kk[:], ALU.mult, [bkk], [btm1])
        P.op("vector", lambda e: e.tensor_reduce(out=st8[:, 0:8], in_=tm1[:].rearrange("p (h j) -> p h j", h=8),
                                                 axis=AX.X, op=ALU.add), [btm1], [bst8])
        P.ts("vector", st8[:, 0:8], st8[:, 0:8], 1e-24, None, ALU.max, None, [bst8], [bst8])
        P.act(st8[:, 0:8], st8[:, 0:8], AF.Ln, [bst8], [bst8])
        P.act(st8[:, 0:8], st8[:, 0:8], AF.Exp, [bst8], [bst8], scale=-0.5)
        P.tt("vector", kk[:].rearrange("p (h j) -> p h j", h=8), kk[:].rearrange("p (h j) -> p h j", h=8),
             st8[:, 0:8].unsqueeze(2).to_broadcast([128, 8, 64]), ALU.mult, [bkk, bst8], [bkk])
        P.stt(rr(), tm1[:], a_s[:], -1.0, prm[:, 3, :], ALU.add, ALU.mult, [ba, bprm], [btm1])
        P.stt(rr(), km[:], tm1[:], 1.0, k_s[:], ALU.add, ALU.mult, [btm1, bk], [bkm])
        P.tt(rr(), bb[:], kk[:], a_s[:], ALU.mult, [bkk, ba], [bbb])
        P.mm(pV[:], con[:, O_UI:O_UI + 128], sig[:], [bcon, bsig], [bpV], f32=True)
        P.act(e1[:], pV[:], AF.Exp, [bpV], [be1], scale=-C0)
        P.act(e2[:], pV[:], AF.Exp, [bpV], [be2], scale=C0)
        P.tt(rr(), Rt[:], r_s[:], e1[:], ALU.mult, [br, be1], [bRt])
        P.tt(rr(), Bt[:], bb[:], e2[:], ALU.mult, [bbb, be2], [bBt])
        P.tt(rr(), Kt[:], km[:], e2[:], ALU.mult, [bkm, be2], [bKt])
        P.mm(pV[:], con[:, O_SU:O_SU + 128], sig[:], [bcon, bsig], [bpV], f32=True)
        P.act(e1[:], pV[:], AF.Exp, [bpV], [be1], scale=-C0)
        P.stt(rr(), At[:], kk[:], -1.0, e1[:], ALU.mult, ALU.mult, [bkk, be1], [bAt])
        P.mm(pV[:], con[:, O_SL:O_SL + 128], sig[:], [bcon, bsig], [bpV], f32=True)
        P.act(e2[:], pV[:], AF.Exp, [bpV], [be2], scale=-C0)
        P.tt(rr(), Bh[:], bb[:], e2[:], ALU.mult, [bbb, be2], [bBh])
        P.tt(rr(), Kh[:], km[:], e2[:], ALU.mult, [bkm, be2], [bKh])
        for pr in range(4):
            P.mm(pQ[:, 384 + pr * 2:384 + pr * 2 + 2], sig[:, pr * 128:(pr + 1) * 128], con[:, O_IND:O_IND + 2],
                 [bsig, bcon], [bpQ], f32=True)
        P.act(pc[:].rearrange("p a c -> p (a c)"), pQ[:, 384:392], AF.Exp, [bpQ], [bpc], scale=-C0)
        P.tt(rr(), tm1[:], r_s[:], km[:], ALU.mult, [br, bkm], [btm1])
        P.tt(rr(), tm1[:], tm1[:], prm[:, 4, :], ALU.mult, [btm1, bprm], [btm1])
        P.op("vector", lambda e, bon=bon: e.tensor_reduce(out=bon[:, 0:8], in_=tm1[:].rearrange("p (h j) -> p h j", h=8),
                                                          axis=AX.X, op=ALU.add), [btm1], [bbon])
        if DBG_STOP < 3:
            continue
        for pr in range(4):
            psl = slice(pr * 128, (pr + 1) * 128)
            for ai, (arr, barr) in enumerate(((At, bAt), (Rt, bRt), (Bt, bBt), (Kt, bKt))):
                P.tr(pT[:, ai, :], arr[:, psl], idb[:], [barr, bidb], [bpT])
            P.cp("scalar", FM[:, pr, :, :], pT[:, 0:4, :], [bpT], [bFM[pr]])
            P.cp("vector", RP[:, pr, 0:64], FM[:, pr, 1, 0:64], [bFM[pr]], [bRP[pr]])
            P.cp("vector", RP[:, pr, 192:256], FM[:, pr, 1, 64:128], [bFM[pr]], [bRP[pr]])
        if DBG_STOP < 4:
            continue
        for h in range(8):
            pr = h // 2
            ph = (h % 2) * 64
            hp = h % 2
            Gt, bGt = Gs[hp]
            A_ = FM[ph:ph + 64, pr, 0, :]
            R_ = FM[ph:ph + 64, pr, 1, :]
            B_ = FM[ph:ph + 64, pr, 2, :]
            K_ = FM[ph:ph + 64, pr, 3, :]
            bF = bFM[pr]
            P.mm(pG0[:, 0:128], B_, A_, [bF], [bpG0])
            P.mm(pG0[:, 128:256], K_, A_, [bF], [bpG0])
            P.mm(pG0[:, 256:384], B_, R_, [bF], [bpG0])
            P.mm(pG0[:, 384:512], K_, R_, [bF], [bpG0])
            P.mm(pG1[:, 0:128], A_, B_, [bF], [bpG1])
            P.tt("vector", Gt[:, 0:256], pG0[:, 0:256], con[:, O_M5:O_M5 + 256], ALU.mult, [bpG0, bcon], [bGt])
            P.tt("vector", Gt[:, 384:640], pG0[:, 256:512], con[:, O_M5 + 384:O_M5 + 640], ALU.mult, [bpG0, bcon], [bGt])
            P.tt("vector", Gt[:, 256:384], pG1[:, 0:128], con[:, O_M5 + 256:O_M5 + 384], ALU.mult, [bpG1, bcon], [bGt])
            if DBG_X == 11:
                continue
            Ncur, bNcur = Gt[:, 0:128], bGt
            Lcur, bLcur = Gt[:, 256:384], bGt
            Tcur, bTcur = Tb[hp][0]
            P.tt(rr(), Tcur[:], Gt[:, 0:128], identf, ALU.add, [bGt, bcon], [bTcur])
            Tcur = Tcur[:]
            for kx in range(1, 6):
                if DBG_X in (13, 14) and kx > 1:
                    break
                if DBG_X == 15 and kx > 2:
                    break
                Ln_, bLn = Lb[hp][kx % 2]
                i0 = 128 + (kx % 3) * 128
                P.mm(pG1[:, i0:i0 + 128], Ncur, Lcur, [bNcur, bLcur], [bpG1])
                P.cp("vector", Ln_[:], pG1[:, i0:i0 + 128], [bpG1], [bLn])
                if DBG_X == 13:
                    break
                if kx <= 4:
                    Nn_, bNn = Nb[hp][kx % 2]
                    i1 = 128 + ((kx + 1) % 3) * 128
                    P.mm(pG1[:, i1:i1 + 128], Lcur, Ncur, [bNcur, bLcur], [bpG1])
                    P.cp("vector", Nn_[:], pG1[:, i1:i1 + 128], [bpG1], [bNn])
                Tn_, bTn = Tb[hp][kx % 2]
                i2 = 128 + ((kx + 2) % 3) * 128
                P.mm(pG1[:, i2:i2 + 128], Ln_[:], Tcur, [bLn, bTcur], [bpG1])
                P.tt("vector", Tn_[:], pG1[:, i2:i2 + 128], Tcur, ALU.add, [bpG1, bTcur], [bTn])
                Lcur, bLcur = Ln_[:], bLn
                if kx <= 4:
                    Ncur, bNcur = Nn_[:], bNn
                Tcur, bTcur = Tn_[:], bTn
            if DBG_X == 12:
                continue
            S0 = ST[ph:ph + 64, pr, :]
            S0b = STb[ph:ph + 64, pr, :]
            bS = bST[h]
            Zt, bZt = Zs[hp]
            Ut, bUt = Us[hp]
            hcol = slice(h * 64, (h + 1) * 64)
            for c in range(2):
                pv = c * 64
                pSb, sb0 = (pF, 0) if hp == 0 else (pF1, 0)
                zsl = pSb[:, sb0:sb0 + 64]
                usl = pSb[:, sb0 + 64:sb0 + 128]
                ssl = pSb[:, sb0 + 128:sb0 + 192]
                bz = bu = bs_ = (bpF if hp == 0 else bpF1)
                P.mm(zsl, A_, S0b, [bF, bS], [bz], start=True, stop=False, f32=True)
                P.mm(zsl, Gt[pv:pv + 64, 128:256], v_b[pv:pv + 64, hcol], [bGt, bvb], [bz], start=False, stop=True, f32=True)
                P.cp("vector", Zt[pv:pv + 64, :], zsl[pv:pv + 64, :], [bz], [bZt])
                P.mm(usl, Tcur[pv:pv + 64, :], Zt[pv:pv + 64, :], [bTcur, bZt], [bu], f32=True)
                P.cp("vector", Ut[pv:pv + 64, :], usl[pv:pv + 64, :], [bu], [bUt])
                P.mm(pY[:, hcol], RP[ph:ph + 64, pr, c * 128:(c + 1) * 128], S0b, [bRP[pr], bS], [bpY],
                     start=(c == 0), stop=False, f32=True)
                P.mm(pY[:, hcol], Gt[pv:pv + 64, 512:640], v_b[pv:pv + 64, hcol], [bGt, bvb], [bpY], start=False, stop=False, f32=True)
                P.mm(pY[:, hcol], Gt[pv:pv + 64, 384:512], Ut[pv:pv + 64, :], [bGt, bUt], [bpY], start=False, stop=(c == 1), f32=True)
                P.mm(ssl, Bh[pv:pv + 64, pr * 128:(pr + 1) * 128], Ut[pv:pv + 64, :], [bBh, bUt], [bs_], start=True, stop=False, f32=True)
                P.mm(ssl, Kh[pv:pv + 64, pr * 128:(pr + 1) * 128], v_b[pv:pv + 64, hcol], [bKh, bvb], [bs_], start=False, stop=True, f32=True)
                P.stt("vector", S0, S0, pc[ph:ph + 64, pr, c:c + 1], ssl[ph:ph + 64, :], ALU.mult, ALU.add,
                      [bS, bpc, bs_], [bS])
                P.cp("gpsimd", S0b, S0, [bS], [bS])
        if DBG_STOP < 5:
            continue
        v3 = lambda ap: ap.rearrange("p (h j) -> p h j", h=8)
        P.cp("scalar", ysb[:], pY[:], [bpY], [bysb])
        P.op("vector", lambda e, ysb=ysb: e.tensor_reduce(out=gn[:, 0:8], in_=ysb.rearrange("p (h j) -> p h j", h=8),
                                                          axis=AX.X, op=ALU.add), [bysb], [bgn])
        P.ts("vector", gn[:, 0:8], gn[:, 0:8], -1.0 / 64, None, ALU.mult, None, [bgn], [bgn])
        P.tt("vector", v3(ysb[:]), v3(ysb[:]), gn[:, 0:8].unsqueeze(2).to_broadcast([128, 8, 64]), ALU.add, [bysb, bgn], [bysb])
        P.tt(rr(), tm2[:], ysb[:], ysb[:], ALU.mult, [bysb], [btm2])
        P.op("vector", lambda e: e.tensor_reduce(out=gn[:, 8:16], in_=v3(tm2[:]), axis=AX.X, op=ALU.add), [btm2], [bgn])
        P.ts("vector", gn[:, 8:16], gn[:, 8:16], 1.0 / 64, 64e-5, ALU.mult, ALU.add, [bgn], [bgn])
        P.act(gn[:, 8:16], gn[:, 8:16], AF.Ln, [bgn], [bgn])
        P.act(gn[:, 8:16], gn[:, 8:16], AF.Exp, [bgn], [bgn], scale=-0.5)
        P.tt("vector", v3(ysb[:]), v3(ysb[:]), gn[:, 8:16].unsqueeze(2).to_broadcast([128, 8, 64]), ALU.mult, [bysb, bgn], [bysb])
        P.tt(rr(), ysb[:], ysb[:], prm[:, 5, :], ALU.mult, [bysb, bprm], [bysb])
        P.tt(rr(), ysb[:], ysb[:], prm[:, 6, :], ALU.add, [bysb, bprm], [bysb])
        P.tt("vector", v3(tm2[:]), v3(v_s[:]), bon[:, 0:8].unsqueeze(2).to_broadcast([128, 8, 64]), ALU.mult, [bv, bbon], [btm2])
        P.tt(rr(), ysb[:], ysb[:], tm2[:], ALU.add, [bysb, btm2], [bysb])
        P.tt("vector", yo[:], ysb[:], g_s[:], ALU.mult, [bysb, bg], [byo])
        P.dma("sync", yrw_d[tsl, :], yo[:], [byo], [b_yrw], byo)


def phase_attn(P, nc, G):
    P.noself = NOSELF_ATTN
    con_d, modp_d, lamv_d, subln_d, wout_d, rtw_d, rtb_d = (G[k] for k in
        ("con_d", "modp_d", "lamv_d", "subln_d", "wout_d", "rtw_d", "rtb_d"))
    x_d, qk_d, v_d, yrw_d, x1_d, h2T_d, gat_d = (G[k] for k in ("x_d", "qk_d", "v_d", "yrw_d", "x1_d", "h2T_d", "gat_d"))
    b_modp, b_qk, b_v, b_yrw, b_x1, b_h2T, b_gat = (G[k] for k in
        ("b_modp", "b_qk", "b_v", "b_yrw", "b_x1", "b_h2T", "b_gat"))
    rr = _rr(P)
    con, bcon = P.sb("con", [128, NCONST], F32)
    P.dma("sync", con[:], con_d, [], [bcon], bcon)
    identf = con[:, O_ID:O_ID + 128]
    idb, bidb = P.sb("idb", [128, 128], BF16)
    P.cp("vector", idb[:], identf, [bcon], [bidb])
    cmk, bcmk = P.sb("cmk", [128, 4, 512], BF16)
    P.cp("vector", cmk[:].rearrange("p a t -> p (a t)"), con[:, O_CM:O_CM + 2048], [bcon], [bcmk])
    rows, brows = P.sb("rows", [128, 3, D], F32)
    P.dma("sync", rows[:].rearrange("p a d -> p (a d)"),
          modp_d[2:5, :].rearrange("(o a) d -> o (a d)", o=1).partition_broadcast(128), [b_modp], [brows], brows)
    mcol, bmcol = P.sb("mcol", [128, 6, 8], F32)
    P.dma("sync", mcol[:], modp_d.rearrange("a (k p) -> p a k", p=128), [b_modp], [bmcol], bmcol,
          allow_slow_non_contiguous=True)
    lv, blv = P.sb("lv", [128, 4, 64], F32)
    P.dma("sync", lv[:].rearrange("p a d -> p (a d)"),
          lamv_d.rearrange("(o a) d -> o (a d)", o=1).partition_broadcast(128), [], [blv], blv)
    lam, blam = P.sb("lam", [128, 8], F32)
    lt, blt = P.sb("lt", [128, 2, 64], F32)
    P.tt("vector", lt[:, 0, :], lv[:, 0, :], lv[:, 1, :], ALU.mult, [blv], [blt])
    P.tt("vector", lt[:, 1, :], lv[:, 2, :], lv[:, 3, :], ALU.mult, [blv], [blt])
    P.op("vector", lambda e: e.tensor_reduce(out=lam[:, 0:2], in_=lt[:], axis=AX.X, op=ALU.add), [blt], [blam])
    P.act(lam[:, 2:4], lam[:, 0:2], AF.Exp, [blam], [blam])
    P.tt("vector", lam[:, 4:5], lam[:, 2:3], lam[:, 3:4], ALU.subtract, [blam], [blam])
    P.ts("vector", lam[:, 5:6], lam[:, 4:5], -1.0, -LAMBDA_INIT, ALU.mult, ALU.add, [blam], [blam])
    sub, bsub = P.sb("sub", [128, 128], F32)
    P.dma("sync", sub[:], subln_d.partition_broadcast(128), [], [bsub], bsub)
    P.ts("vector", sub[:], sub[:], 1.0 - LAMBDA_INIT, None, ALU.mult, None, [bsub], [bsub])
    wo, bwo = P.sb("wo", [128, 8, D], BF16)
    P.dma("gpsimd", wo[:], wout_d.rearrange("(k p) c -> p k c", p=128), [], [bwo], bwo)
    rw, brw = P.sb("rw", [128, 8, NE], BF16)
    P.dma("gpsimd", rw[:], rtw_d.rearrange("(k p) c -> p k c", p=128), [], [brw], brw)
    rb, brb = P.sb("rb", [128, NE], F32)
    P.dma("sync", rb[:], rtb_d.partition_broadcast(128), [], [brb], brb)
    kT, bkT = P.sb("kT", [128, 4, T], BF16)
    P.dma("sync", kT[:], qk_d[512:1024, :].rearrange("(c p) t -> p c t", p=128), [b_qk], [bkT], bkT)
    vv, bvv = P.sb("vv", [128, NT, 4 * 129], BF16)
    P.dma("sync", vv[:], v_d.rearrange("(n p) f -> p n f", p=128), [b_v], [bvv], bvv)
    qT = [P.sb(f"qT{i}", [128, 4, 512], BF16) for i in range(2)]
    pt = [P.sb(f"pt{i}", [128, 512], BF16) for i in range(3)]
    psc = [P.ps(f"psc{i}", [128, 512], F32) for i in range(2)]
    po = [P.ps(f"po{i}", [128, 4, 128], F32) for i in range(2)]
    pms, bpms_ = P.ps("pms", [128, 512], F32)
    pos_ = pms[:, 0:128].rearrange("p (a b c) -> p a b c", a=2, b=4)
    bpos_ = P.buf("possum")
    pw, bpw = P.ps("pw", [128, D], F32)
    ptr, bptr = P.ps("ptr", [128, 8, 128], BF16)
    prt = pms[:, 128:256]
    bprt = bpos_
    YC, bYC_ = P.sb("YC", [128, 4, D], BF16)
    bYC = [P.buf("YCs") for _ in range(4)]
    ycT, bycT = P.sb("ycT", [128, 8, 128], BF16)
    o0, bo0 = P.sb("o0", [128, 128], F32)
    o1, bo1 = P.sb("o1", [128, 128], F32)
    rs, brs = P.sb("rs", [128, 16], F32)
    XT4 = [P.sb(f"axt{i}", [128, D], F32) for i in range(4)]
    YR4 = [P.sb(f"ayr{i}", [128, 512], BF16) for i in range(4)]
    y1, by1 = P.sb("y1", [128, D], F32)
    junk, bjunk = P.sb("junk", [128, D], BF16)
    h2, bh2 = P.sb("h2", [128, D], BF16)
    h2T, bh2T = P.sb("h2T", [128, 8, 128], BF16)
    lg, blg = P.sb("lg", [128, NE], F32)
    gt, bgt = P.sb("gt", [128, NE], F32)
    t8, bt8 = P.sb("t8", [128, 16], F32)

    it = 0
    for qb in range(8):
        qcur, bqcur = qT[qb % 2]
        if qb == 0:
            P.dma("sync", qcur[:], qk_d[0:512, 0:512].rearrange("(c p) t -> p c t", p=128), [b_qk], [bqcur], bqcur)
        if qb + 1 < 8:
            qn_, bqn_ = qT[(qb + 1) % 2]
            P.dma("sync", qn_[:], qk_d[0:512, (qb + 1) * 512:(qb + 2) * 512].rearrange("(c p) t -> p c t", p=128),
                  [b_qk], [bqn_], bqn_)
        for s4 in range(4):
            n_ = qb * 4 + s4
            P.dma("sync", YR4[s4][0][:], yrw_d[n_ * 128:(n_ + 1) * 128, :], [b_yrw], [YR4[s4][1]], YR4[s4][1])
            P.dma("sync", XT4[s4][0][:], x_d[n_ * 128:(n_ + 1) * 128, :], [], [XT4[s4][1]], XT4[s4][1])
        ntk = (qb + 1) * 4
        for hd in range(4):
            for mp in range(2):
                m = hd * 2 + mp
                chn, pb = m // 2, (m % 2) * 64
                pot, bpot = po[mp]
                for tk in range(ntk):
                    ps_, bps_ = psc[it % 2]
                    ptile, bptile = pt[it % 3]
                    it += 1
                    P.mm(ps_[:], kT[pb:pb + 64, chn, tk * 128:(tk + 1) * 128], qcur[pb:pb + 64, chn, :],
                         [bkT, bqcur], [bps_])
                    P.act(ptile[:], ps_[:], AF.Exp, [bps_], [bptile], scale=0.125)
                    j = tk - qb * 4
                    if j >= 0:
                        P.tt("vector", ptile[:], ptile[:], cmk[:, j, :], ALU.mult, [bptile, bcmk], [bptile])
                    for s4 in range(4):
                        if j > s4:
                            continue
                        P.mm(pot[:, s4, :], ptile[:, s4 * 128:(s4 + 1) * 128], vv[:, tk, hd * 129:hd * 129 + 128],
                             [bptile, bvv], [bpot], start=(tk == 0 and s4 == 0), stop=(tk == ntk - 1 and s4 == 3))
                        P.mm(pos_[:, mp, s4, 0:1], ptile[:, s4 * 128:(s4 + 1) * 128], vv[:, tk, hd * 129 + 128:hd * 129 + 129],
                             [bptile, bvv], [bpos_], start=(mp == 0 and tk == 0 and s4 == 0),
                             stop=(mp == 1 and tk == ntk - 1 and s4 == 3))
            P.cp("vector", rs[:, 0:8].rearrange("p (a b) -> p a b", a=2), pos_[:, :, :, 0], [bpos_], [brs])
            P.op("vector", lambda e: e.reciprocal(out=rs[:, 8:16], in_=rs[:, 0:8]), [brs], [brs])
            P.ts("vector", rs[:, 12:16], rs[:, 12:16], lam[:, 5:6], None, ALU.mult, None, [brs, blam], [brs])
            for s4 in range(4):
                P.ts("vector", o0[:], po[0][0][:, s4, :], rs[:, 8 + s4:9 + s4], None, ALU.mult, None, [po[0][1], brs], [bo0])
                P.stt("vector", o0[:], po[1][0][:, s4, :], rs[:, 12 + s4:13 + s4], o0[:], ALU.mult, ALU.add,
                      [po[1][1], brs, bo0], [bo0])
                P.act(o1[:], o0[:], AF.Square, [bo0], [bo1, bt8], accum_out=t8[:, 0:1])
                P.ts("vector", t8[:, 1:2], t8[:, 0:1], 1.0 / 128, 1e-5, ALU.mult, ALU.add, [bt8], [bt8])
                P.act(t8[:, 2:3], t8[:, 1:2], AF.Ln, [bt8], [bt8])
                P.act(t8[:, 3:4], t8[:, 2:3], AF.Exp, [bt8], [bt8], scale=-0.5)
                P.stt("vector", YC[:, s4, hd * 128:(hd + 1) * 128], o0[:], t8[:, 3:4], sub[:], ALU.mult, ALU.mult,
                      [bo0, bt8, bsub], [bYC[s4]])
        for s4 in range(4):
            n = qb * 4 + s4
            tsl = slice(n * 128, (n + 1) * 128)
            yrt, byrt = YR4[s4]
            xt, bxt = XT4[s4]
            P.cp("vector", YC[:, s4, 512:1024], yrt[:], [byrt], [bYC[s4]])
            for k in range(8):
                P.tr(ptr[:, k, :], YC[:, s4, k * 128:(k + 1) * 128], idb[:], [bYC[s4], bidb], [bptr])
            P.cp("scalar", ycT[:].rearrange("p k t -> p (k t)"), ptr[:].rearrange("p k t -> p (k t)"), [bptr], [bycT])
            for hf in range(2):
                for k in range(8):
                    P.mm(pw[:, hf * 512:(hf + 1) * 512], ycT[:, k, :], wo[:, k, hf * 512:(hf + 1) * 512],
                         [bycT, bwo], [bpw], start=(k == 0), stop=(k == 7))
            P.cp("scalar", y1[:], pw[:], [bpw], [by1])
            P.act(junk[:], y1[:], AF.Square, [by1], [bjunk, bt8], accum_out=t8[:, 4:5])
            P.ts("vector", t8[:, 5:6], t8[:, 4:5], 1.0 / D, 1e-6, ALU.mult, ALU.add, [bt8], [bt8])
            P.act(t8[:, 6:7], t8[:, 5:6], AF.Ln, [bt8], [bt8])
            P.act(t8[:, 7:8], t8[:, 6:7], AF.Exp, [bt8], [bt8], scale=-0.5)
            P.stt("vector", y1[:], y1[:], t8[:, 7:8], rows[:, 0, :], ALU.mult, ALU.mult, [by1, bt8, brows], [by1])
            P.tt("gpsimd", xt[:], xt[:], y1[:], ALU.add, [bxt, by1], [bxt])
            P.dma("sync", x1_d[tsl, :], xt[:], [bxt], [b_x1], bxt)
            P.act(junk[:], xt[:], AF.Square, [bxt], [bjunk, bt8], accum_out=t8[:, 8:9])
            P.ts("vector", t8[:, 9:10], t8[:, 8:9], 1.0 / D, 1e-6, ALU.mult, ALU.add, [bt8], [bt8])
            P.act(t8[:, 10:11], t8[:, 9:10], AF.Ln, [bt8], [bt8])
            P.act(t8[:, 11:12], t8[:, 10:11], AF.Exp, [bt8], [bt8], scale=-0.5)
            P.ts("vector", h2[:], xt[:], t8[:, 11:12], None, ALU.mult, None, [bxt, bt8], [bh2])
            for k in range(8):
                P.tr(ptr[:, k, :], h2[:, k * 128:(k + 1) * 128], idb[:], [bh2, bidb], [bptr])
            for k in range(8):
                P.act(h2T[:, k, :], ptr[:, k, :], AF.Identity, [bptr, bmcol], [bh2T],
                      bias=mcol[:, 4, k:k + 1], scale=mcol[:, 3, k:k + 1])
            P.dma("sync", h2T_d.rearrange("(k p) t -> p k t", p=128)[:, :, tsl], h2T[:], [bh2T], [b_h2T], bh2T)
            for k in range(8):
                P.mm(prt[:, 0:NE], h2T[:, k, :], rw[:, k, :], [bh2T, brw], [bprt], start=(k == 0), stop=(k == 7))
            P.tt("vector", lg[:], prt[:, 0:NE], rb[:], ALU.add, [bprt, brb], [blg])
            P.op("vector", lambda e: e.max(out=t8[:, 0:8], in_=lg[:]), [blg], [bt8])
            P.ts("vector", gt[:], lg[:], t8[:, 3:4], None, ALU.is_ge, None, [blg, bt8], [bgt])
            P.ts("vector", t8[:, 12:13], t8[:, 0:1], -1.0, None, ALU.mult, None, [bt8], [bt8])
            P.act(lg[:], lg[:], AF.Exp, [blg, bt8], [blg], bias=t8[:, 12:13], scale=1.0)
            P.tt("vector", gt[:], gt[:], lg[:], ALU.mult, [bgt, blg], [bgt])
            P.op("vector", lambda e: e.tensor_reduce(out=t8[:, 13:14], in_=gt[:], axis=AX.X, op=ALU.add), [bgt], [bt8])
            P.op("vector", lambda e: e.reciprocal(out=t8[:, 14:15], in_=t8[:, 13:14]), [bt8], [bt8])
            P.ts("vector", gt[:], gt[:], t8[:, 14:15], None, ALU.mult, None, [bgt, bt8], [bgt])
            P.dma("sync", gat_d[tsl, :], gt[:], [bgt], [b_gat], bgt)


def phase_moe(P, nc, G):
    P.noself = NOSELF_MOE
    P.skip_keys.clear()
    modp_d, x1_d, h2T_d, gat_d, out_d = (G[k] for k in ("modp_d", "x1_d", "h2T_d", "gat_d", "out_d"))
    w1g_d, w1l_d, w2e_d, b1T_d, b2_d, con_d = (G[k] for k in ("w1g_d", "w1l_d", "w2e_d", "b1T_d", "b2_d", "con_d"))
    wbf_d, b_wbf = G["wbf_d"], G["b_wbf"]
    b_modp, b_x1, b_h2T, b_gat, b_out = (G[k] for k in ("b_modp", "b_x1", "b_h2T", "b_gat", "b_out"))
    idf, bidf = P.sb("idf", [128, 128], F32)
    P.dma("sync", idf[:], con_d[:, O_ID:O_ID + 128], [], [bidf], bidf)
    c2r, bc2r = P.sb("c2r", [128, D], F32)
    P.dma("sync", c2r[:], modp_d[5:6, :].partition_broadcast(128), [b_modp], [bc2r], bc2r)
    b1T, bb1T = P.sb("b1T", [128, NE, 16], F32)
    P.dma("sync", b1T[:].rearrange("p e c -> p (e c)"), b1T_d, [], [bb1T], bb1T)
    b2s, bb2s = P.sb("b2s", [NE, D], F32)
    P.dma("sync", b2s[:], b2_d, [], [bb2s], bb2s)
    W = [[P.sb(f"w{j}_{i}", [128, 8, D], BF16) for j in range(3)] for i in range(2)]
    hq, bhq = P.sb("hq", [128, 8, 1024], BF16)
    gq, bgq = P.sb("gq", [128, 8, NE], F32)
    gT, bgT = P.sb("gT", [NE, 128], F32)
    acc, bacc_ = P.sb("acc", [128, 8, D], F32)
    bacc = [P.buf("acct") for _ in range(8)]
    actT, bactT_ = P.sb("actT", [128, 2, 8, 512], BF16)
    bactT = [[P.buf("actc") for _ in range(8)] for _ in range(2)]
    GG = [P.sb(f"mg{i}", [128, 512], F32) for i in range(2)]
    SS = [P.sb(f"msg{i}", [128, 512], F32) for i in range(2)]
    LL = [P.sb(f"ml{i}", [128, 512], F32) for i in range(2)]
    b1p, bb1p = P.sb("b1p", [128, NE, 8], F32)
    P.ts("vector", b1p[:], b1T[:, :, 8:16], 1.0, None, ALU.add, None, [bb1T], [bb1p])
    xt, bxt = P.sb("mxt", [128, D], F32)
    junk, bjunk = P.sb("mjunk", [128, D], BF16)
    t8, bt8 = P.sb("mt8", [128, 8], F32)
    pg = [P.ps(f"pg{i}", [128, 512], F32) for i in range(2)]
    pl = [P.ps(f"pl{i}", [128, 512], F32) for i in range(2)]
    po = [P.ps(f"pmo{i}", [128, 512], F32) for i in range(2)]
    pm, bpm = P.ps("pmisc", [128, 512], F32)
    it = 0
    io = 0
    wi = 0
    stepi = 0
    for qt in range(DBG_NQ):
        q0 = qt * 1024
        P.dma("sync", hq[:], h2T_d.rearrange("(k p) t -> p k t", p=128)[:, :, q0:q0 + 1024], [b_h2T], [bhq], bhq)
        P.dma("sync", gq[:], gat_d[q0:q0 + 1024, :].rearrange("(n p) e -> p n e", p=128), [b_gat], [bgq], bgq)
        for n in range(8):
            P.tr(pm[0:NE, 0:128], gq[:, n, :], idf[:], [bgq, bidf], [bpm], f32=True)
            P.cp("vector", gT[:], pm[0:NE, 0:128], [bpm], [bgT])
            for hf in range(2):
                pp, bpp = po[io % 2]
                io += 1
                P.mm(pp[:], gT[:], b2s[:, hf * 512:(hf + 1) * 512], [bgT, bb2s], [bpp], f32=True)
                P.cp("scalar", acc[:, n, hf * 512:(hf + 1) * 512], pp[:], [bpp], [bacc[n]])
        def hid(e, blk, Wset, ab):
            nonlocal it
            (w1g, bw1g), (w1l, bw1l), (w2, bw2) = Wset
            bsl = slice(blk * 512, (blk + 1) * 512)
            for fc in range(8):
                pgt, bpgt = pg[it % 2]
                plt, bplt = pl[it % 2]
                it += 1
                for k in range(8):
                    P.mm(pgt[:], w1g[:, k, fc * 128:(fc + 1) * 128], hq[:, k, bsl], [bw1g, bhq], [bpgt],
                         start=(k == 0), stop=(k == 7))
                for k in range(8):
                    P.mm(plt[:], w1l[:, k, fc * 128:(fc + 1) * 128], hq[:, k, bsl], [bw1l, bhq], [bplt],
                         start=(k == 0), stop=(k == 7))
                gi, bgi = GG[it % 2]
                si, bsi = SS[it % 2]
                li, bli = LL[it % 2]
                P.ts("vector", gi[:], pgt[:], b1T[:, e, fc:fc + 1], 7.0, ALU.add, ALU.min, [bpgt, bb1T], [bgi])
                P.act(si[:], gi[:], AF.Sigmoid, [bgi], [bsi], scale=1.702)
                P.act(li[:], plt[:], AF.Identity, [bplt, bb1p], [bli], bias=b1p[:, e, fc:fc + 1], scale=1.0)
                P.ts("vector", li[:], li[:], -6.0, 8.0, ALU.max, ALU.min, [bli], [bli])
                P.tt("gpsimd", si[:], si[:], gi[:], ALU.mult, [bsi, bgi], [bsi])
                P.tt("vector", actT[:, ab, fc, :], si[:], li[:], ALU.mult, [bsi, bli], [bactT[ab][fc]])

        def second(e, blk, Wset, ab):
            nonlocal io
            (w1g, bw1g), (w1l, bw1l), (w2, bw2) = Wset
            for tt_ in range(4):
                n = blk * 4 + tt_
                for hf in range(2):
                    pp, bpp = po[io % 2]
                    io += 1
                    for fc in range(8):
                        P.mm(pp[:], actT[:, ab, fc, tt_ * 128:(tt_ + 1) * 128], w2[:, fc, hf * 512:(hf + 1) * 512],
                             [bactT[ab][fc], bw2], [bpp], start=(fc == 0), stop=(fc == 7))
                    P.stt("vector", acc[:, n, hf * 512:(hf + 1) * 512], pp[:], gq[:, n, e:e + 1],
                          acc[:, n, hf * 512:(hf + 1) * 512], ALU.mult, ALU.add, [bpp, bgq, bacc[n]], [bacc[n]])

        prev = None
        for e in range(DBG_NEXP):
            Wset = W[wi % 2]
            (w1g, bw1g), (w1l, bw1l), (w2, bw2) = Wset
            wi += 1
            P.dma("sync", w1g[:], wbf_d[e, 0].rearrange("(k p) f -> p k f", p=128), [b_wbf], [bw1g], bw1g)
            P.dma("sync", w1l[:], wbf_d[e, 1].rearrange("(k p) f -> p k f", p=128), [b_wbf], [bw1l], bw1l)
            P.dma("sync", w2[:], wbf_d[e, 2].rearrange("(k p) f -> p k f", p=128), [b_wbf], [bw2], bw2)
            for blk in range(2):
                ab = stepi % 2
                stepi += 1
                hid(e, blk, Wset, ab)
                if prev is not None:
                    second(*prev)
                prev = (e, blk, Wset, ab)
        second(*prev)
        for n in range(8):
            tsl = slice(q0 + n * 128, q0 + (n + 1) * 128)
            P.dma("gpsimd", xt[:], x1_d[tsl, :], [b_x1], [bxt], bxt)
            P.act(junk[:], acc[:, n, :], AF.Square, [bacc[n]], [bjunk, bt8], accum_out=t8[:, 0:1])
            P.ts("vector", t8[:, 1:2], t8[:, 0:1], 1.0 / D, 1e-6, ALU.mult, ALU.add, [bt8], [bt8])
            P.act(t8[:, 2:3], t8[:, 1:2], AF.Ln, [bt8], [bt8])
            P.act(t8[:, 3:4], t8[:, 2:3], AF.Exp, [bt8], [bt8], scale=-0.5)
            P.stt("vector", acc[:, n, :], acc[:, n, :], t8[:, 3:4], c2r[:], ALU.mult, ALU.mult, [bacc[n], bt8, bc2r], [bacc[n]])
            P.tt("vector", xt[:], xt[:], acc[:, n, :], ALU.add, [bxt, bacc[n]], [bxt])
            P.dma("gpsimd", out_d[tsl, :], xt[:], [bxt], [b_out], bxt)


def _consts():
    c = np.zeros((128, NCONST), np.float32)
    r = np.arange(128)[:, None]
    q = np.arange(128)[None, :]
    same = (r // 64) == (q // 64)
    su = (same & ((r % 64) < (q % 64))).astype(np.float32)
    sl = (same & ((r % 64) > (q % 64))).astype(np.float32)
    ui = (same & ((r % 64) <= (q % 64))).astype(np.float32)
    c[:, O_ID:O_ID + 128] = np.eye(128, dtype=np.float32)
    for i, m in enumerate((su, su, sl, ui, ui)):
        c[:, O_M5 + i * 128:O_M5 + (i + 1) * 128] = m
    c[:, O_ONES:O_ONES + 128] = same.astype(np.float32)
    c[:64, O_IND] = 1.0
    c[64:, O_IND + 1] = 1.0
    inv_freq = (500000.0 ** (-np.arange(0, 16, 2, dtype=np.float32) / 16)).astype(np.float32)
    for p in range(128):
        d = p % 64
        if d < 16:
            c[p, O_FREQ] = inv_freq[d % 8]
            c[p, O_SIGN] = -1.0 if d < 8 else 1.0
            pp = p + 8 if d < 8 else p - 8
            c[pp, O_PM + p] = 1.0
    tq = np.arange(512)[None, :]
    tk = np.arange(128)[:, None]
    for j in range(4):
        c[:, O_CM + j * 512:O_CM + (j + 1) * 512] = ((j * 128 + tk) <= tq).astype(np.float32)
    return c


_NC_CACHE = {}


def _in_maps(inp):
    f = lambda a: np.ascontiguousarray(np.asarray(a, dtype=np.float32))
    B = 8
    w1 = np.asarray(inp["moe_w1"])[0]
    w1g = np.ascontiguousarray(w1[:, :, 0::2])
    w1l = np.ascontiguousarray(w1[:, :, 1::2])
    b1 = np.asarray(inp["moe_b1"])[0]
    b1cat = np.concatenate([b1[:, 0::2].reshape(NE, 8, 128), b1[:, 1::2].reshape(NE, 8, 128)], axis=1)
    b1T = np.ascontiguousarray(b1cat.transpose(2, 0, 1).reshape(128, NE * 16)).astype(np.float32)
    shared = dict(
        ada_w=f(inp["ada_w"][0]), ada_b=f(inp["ada_b"]).reshape(1, 6 * D),
        norms=f(np.stack([inp["pre_mix_norm"][0], inp["post_mix_norm"][0], inp["pre_ffn_norm"][0], inp["post_ffn_norm"][0]])),
        w_in=f(inp["w_in"][0]), w_out=f(inp["w_out"][0]),
        lamv=f(np.stack([inp["da_lambda_q1"][0], inp["da_lambda_k1"][0], inp["da_lambda_q2"][0], inp["da_lambda_k2"][0]])),
        subln=f(inp["da_subln"]).reshape(1, 128), mu=f(inp["rw_mu"]).reshape(1, 1792),
        rwv=f(np.stack([inp["rw_w0"][0], inp["rw_a0"][0], inp["rw_k_k"][0], inp["rw_k_a"][0],
                        np.asarray(inp["rw_r_k"])[0].reshape(512), inp["rw_ln_w"][0], inp["rw_ln_b"][0]])),
        rw_w2=f(inp["rw_w2"][0]), rw_a2=f(inp["rw_a2"][0]), rw_g2=f(inp["rw_g2"][0]),
        router_w=f(inp["router_w"][0]), router_b=f(inp["router_b"]).reshape(1, NE),
        w1g=w1g, w1l=w1l, b1T=b1T, w2e=f(inp["moe_w2"][0]), b2=f(inp["moe_b2"][0]),
        consts=_consts(),
    )
    x = np.asarray(inp["x"], dtype=np.float32)
    c = np.asarray(inp["c"], dtype=np.float32)
    pos = np.asarray(inp["positions"]).astype(np.int32)
    in_maps = []
    for b in range(B):
        m = dict(shared)
        m["x"] = np.ascontiguousarray(x[b])
        m["cT"] = np.ascontiguousarray(c[b].reshape(8, 128).T)
        m["pos"] = np.ascontiguousarray(pos[b].reshape(1, T))
        in_maps.append(m)
    return in_maps


def kernel(**inp):
    B = 8
    if "nc" not in _NC_CACHE:
        _NC_CACHE["nc"] = build_program()
    nc = _NC_CACHE["nc"]
    in_maps = _in_maps(inp)
    res = run_bass_kernel_spmd(nc, in_maps, core_ids=list(range(B)))
    return np.stack([np.asarray(r["out"], dtype=np.float32) for r in res.results], axis=0)
```
